# Optimizing a Trainium2 kernel written in Bass

```python
import jax, jax.numpy as jnp
from jax import lax
import numpy as np

D_MODEL = 2048
BATCH = 2
SEQ = 4096
DEPTH = 4

GRID_W = 64
CTX_LEN = 256
CHUNK = 64

GLA_HEADS = 4
GLA_DK = 64
GLA_DV = 128
GLA_LR = 16
GLA_TAU = 16.0
MLSTM_HEADS = 6
MLSTM_DQK = 64
MLSTM_DV = 128
RWKV_HEADS = 12
RWKV_N = 64
RWKV_LR_W = 96
RWKV_LR_A = 96

GLA_W = GLA_HEADS * GLA_DV
MLSTM_W = MLSTM_HEADS * MLSTM_DV
RWKV_W = RWKV_HEADS * RWKV_N

GLA_COLS = 2 * GLA_HEADS * GLA_DK + 2 * GLA_W + 2 * GLA_LR
MLSTM_COLS = 2 * MLSTM_HEADS * MLSTM_DQK + 2 * MLSTM_W + 4 * MLSTM_HEADS
RWKV_COLS = 4 * RWKV_W + 2 * (RWKV_LR_W + RWKV_LR_A)
N_IN = GLA_COLS + MLSTM_COLS + RWKV_COLS

N_GROUPS = 4
EXPERTS_PER_GROUP = 8
N_EXPERTS = N_GROUPS * EXPERTS_PER_GROUP
TOP_K_IN_GROUP = 2
D_EXPERT = 512
MOE_BLOCK = 128

N_ADA = 6
NORM_EPS = 1e-6
RWKV_GN_EPS = 64e-5

kernel_name = "hybrid_gla_mlstm_rwkv7_hmoe_dit"


def _rmsnorm(x, g):
    xf = x.astype(jnp.float32)
    y = xf * lax.rsqrt(jnp.mean(xf * xf, -1, keepdims=True) + NORM_EPS)
    return (y * g.astype(jnp.float32)).astype(x.dtype)


def _head_norm(y, gain, eps, center):
    if center:
        y = y - jnp.mean(y, -1, keepdims=True)
    y = y * lax.rsqrt(jnp.mean(y * y, -1, keepdims=True) + eps)
    return y.reshape(y.shape[:2] + (-1,)) * gain


def _split(a, sizes):
    return jnp.split(a, [int(s) for s in np.cumsum(sizes)[:-1]], axis=-1)


def _heads(a, h):
    return a.reshape(a.shape[:2] + (h, -1))


def _to_chunks(a):
    b, n = a.shape[0], a.shape[1] // CHUNK
    a = a.reshape((b, n, CHUNK) + a.shape[2:])
    return jnp.swapaxes(jnp.moveaxis(a, 1, 0), 2, 3)


def _from_chunks(y):
    y = jnp.moveaxis(jnp.swapaxes(y, 2, 3), 0, 1)
    return y.reshape((y.shape[0], -1) + y.shape[3:])


def _flip(t):
    return tuple(jnp.flip(a, 1) for a in t)


def _two_way(scan_fn, ctx_f, lat_f, ctx_b, lat_b, s0):
    yc_f, sc_f = scan_fn(ctx_f, s0)
    yl_f, _ = scan_fn(lat_f, sc_f)
    yc_b, sc_b = scan_fn(_flip(ctx_b), s0)
    yl_b, _ = scan_fn(_flip(lat_b), sc_b)
    return yc_f + jnp.flip(yc_b, 1), yl_f + jnp.flip(yl_b, 1)


def _gla_scan(inp, s0):
    q, k, v, la = (_to_chunks(a) for a in inp)
    mask = jnp.tril(jnp.ones((CHUNK, CHUNK), dtype=bool))[:, :, None]

    def step(s, blk):
        qc, kc, vc, lac = blk
        b = jnp.cumsum(lac, axis=2)
        diff = b[:, :, :, None, :] - b[:, :, None, :, :]
        dec = jnp.where(mask, jnp.exp(jnp.where(mask, diff, 0.0)), 0.0)
        att = jnp.einsum('bhid,bhjd,bhijd->bhij', qc, kc, dec)
        o = (jnp.einsum('bhij,bhjv->bhiv', att, vc)
             + jnp.einsum('bhid,bhdv->bhiv', qc * jnp.exp(b), s))
        b_end = b[:, :, -1:, :]
        s_new = (jnp.exp(b_end[:, :, 0, :, None]) * s
                 + jnp.einsum('bhjd,bhjv->bhdv', kc * jnp.exp(b_end - b), vc))
        return s_new, o

    s_fin, ys = lax.scan(step, s0, (q, k, v, la))
    return _from_chunks(ys), s_fin


def _mlstm_scan(inp, state):
    q, k, v, ig, fg = (_to_chunks(a) for a in inp)
    mask = jnp.tril(jnp.ones((CHUNK, CHUNK), dtype=bool))

    def step(carry, blk):
        c_mat, n_vec, m = carry
        qc, kc, vc, ic, fc = blk
        b = jnp.cumsum(jax.nn.log_sigmoid(fc), axis=-1)
        log_d = jnp.where(mask, b[..., :, None] - b[..., None, :] + ic[..., None, :], -jnp.inf)
        log_inter = b + m[..., None]
        m_row = jnp.maximum(jnp.max(log_d, -1), log_inter)
        d = jnp.exp(log_d - m_row[..., None])
        inter = jnp.exp(log_inter - m_row)
        s = jnp.einsum('bhid,bhjd->bhij', qc, kc) * d
        num = (jnp.einsum('bhij,bhjv->bhiv', s, vc)
               + inter[..., None] * jnp.einsum('bhid,bhvd->bhiv', qc, c_mat))
        den = jnp.sum(s, -1) + inter * jnp.einsum('bhid,bhd->bhi', qc, n_vec)
        h = num / jnp.maximum(jnp.abs(den), jnp.exp(-m_row))[..., None]
        b_end = b[..., -1]
        log_w = b_end[..., None] - b + ic
        m_new = jnp.maximum(b_end + m, jnp.max(log_w, -1))
        w = jnp.exp(log_w - m_new[..., None])
        carry_decay = jnp.exp(b_end + m - m_new)
        c_new = carry_decay[..., None, None] * c_mat + jnp.einsum('bhj,bhjv,bhjd->bhvd', w, vc, kc)
        n_new = carry_decay[..., None] * n_vec + jnp.einsum('bhj,bhjd->bhd', w, kc)
        return (c_new, n_new, m_new), h

    fin, ys = lax.scan(step, state, (q, k, v, ig, fg))
    return _from_chunks(ys), fin


def _rwkv_scan(inp, s0):
    r, decay, k, v, kk, a = (jnp.moveaxis(t, 1, 0) for t in inp)

    def step(s, t):
        rt, wt, kt, vt, kkt, at = t
        sa = jnp.einsum('bhvk,bhk->bhv', s, -kkt)
        s = (s * wt[:, :, None, :] + sa[..., None] * (kkt * at)[:, :, None, :]
             + vt[..., None] * kt[:, :, None, :])
        return s, jnp.einsum('bhvk,bhk->bhv', s, rt)

    s_fin, ys = lax.scan(step, s0, (r, decay, k, v, kk, a))
    return jnp.moveaxis(ys, 0, 1), s_fin


def _conv3x3(a, rows, cols, w, bias):
    b, _, ch = a.shape
    img = a.reshape(b, rows, cols, ch)
    y = lax.conv_general_dilated(img, w.astype(a.dtype)[:, :, None, :], (1, 1), 'SAME',
                                 dimension_numbers=('NHWC', 'HWIO', 'NHWC'),
                                 feature_group_count=ch)
    return y.reshape(b, rows * cols, ch) + bias


def _token_shift(p, mu_prev, mu_next):
    prev = jnp.pad(p, ((0, 0), (1, 0), (0, 0)))[:, :-1]
    nxt = jnp.pad(p, ((0, 0), (0, 1), (0, 0)))[:, 1:]
    return p + mu_prev * (prev - p) + mu_next * (nxt - p)


def _gla_mixer(p_c, p_l, w_a2, b_a, g_norm):
    hk = GLA_HEADS * GLA_DK

    def prep(p):
        p = p.astype(jnp.float32)
        q, k, v, g, lr_f, lr_b = _split(p, [hk, hk, GLA_W, GLA_W, GLA_LR, GLA_LR])
        q = _heads(q, GLA_HEADS) * GLA_DK ** -0.5
        k, v = _heads(k, GLA_HEADS), _heads(v, GLA_HEADS)
        la = [_heads(jax.nn.log_sigmoid(lr @ w_a2[d] + b_a[d]) / GLA_TAU, GLA_HEADS)
              for d, lr in enumerate((lr_f, lr_b))]
        return (q, k, v, la[0]), (q, k, v, la[1]), g

    cf, cb, gc = prep(p_c)
    lf, lb, gl = prep(p_l)
    s0 = jnp.zeros((p_l.shape[0], GLA_HEADS, GLA_DK, GLA_DV), jnp.float32)
    yc, yl = _two_way(_gla_scan, cf, lf, cb, lb, s0)
    return (_head_norm(yc, g_norm, NORM_EPS, False) * jax.nn.silu(gc),
            _head_norm(yl, g_norm, NORM_EPS, False) * jax.nn.silu(gl))


def _mlstm_mixer(p_c, p_l, rows, conv_w, conv_b, gate_b, g_norm):
    hq, nh = MLSTM_HEADS * MLSTM_DQK, MLSTM_HEADS

    def prep(p, r, cols):
        p = p.astype(jnp.float32)
        qk, v, o, gates = _split(p, [2 * hq, MLSTM_W, MLSTM_W, 4 * nh])
        qk = jax.nn.silu(_conv3x3(qk, r, cols, conv_w, conv_b))
        q, k = jnp.split(qk, 2, axis=-1)
        q, k, v = _heads(q, nh), _heads(k, nh) * MLSTM_DQK ** -0.5, _heads(v, nh)
        gates = gates.reshape(gates.shape[:2] + (2, 2, nh)) + gate_b
        return ((q, k, v, gates[:, :, 0, 0], gates[:, :, 0, 1]),
                (q, k, v, gates[:, :, 1, 0], gates[:, :, 1, 1]), o)

    cf, cb, oc = prep(p_c, 1, p_c.shape[1])
    lf, lb, ol = prep(p_l, rows, GRID_W)
    bsz = p_l.shape[0]
    s0 = (jnp.zeros((bsz, nh, MLSTM_DV, MLSTM_DQK), jnp.float32),
          jnp.zeros((bsz, nh, MLSTM_DQK), jnp.float32),
          jnp.zeros((bsz, nh), jnp.float32))
    yc, yl = _two_way(_mlstm_scan, cf, lf, cb, lb, s0)
    return (_head_norm(yc, g_norm, NORM_EPS, True) * jax.nn.sigmoid(oc),
            _head_norm(yl, g_norm, NORM_EPS, True) * jax.nn.sigmoid(ol))


def _rwkv_mixer(p_c, p_l, mu, w2, w0, a2, a0, k_k, k_a, r_k, g_norm):
    nh = RWKV_HEADS

    def prep(p):
        p = _token_shift(p.astype(jnp.float32), mu[0], mu[1])
        r, k, v, g, w1f, w1b, a1f, a1b = _split(
            p, [RWKV_W] * 4 + [RWKV_LR_W] * 2 + [RWKV_LR_A] * 2)
        kk = _heads(k * k_k, nh)
        kk = kk / jnp.maximum(jnp.sqrt(jnp.sum(kk * kk, -1, keepdims=True)), 1e-12)
        dirs = []
        for d, (w1, a1) in enumerate(((w1f, a1f), (w1b, a1b))):
            w = -jax.nn.softplus(-(w0[d] + jnp.tanh(w1) @ w2[d])) - 0.5
            decay = jnp.exp(-jnp.exp(w))
            a = jax.nn.sigmoid(a0[d] + a1 @ a2[d])
            kd = k * (1.0 + (a - 1.0) * k_a)
            dirs.append((_heads(r, nh), _heads(decay, nh), _heads(kd, nh),
                         _heads(v, nh), kk, _heads(a, nh)))
        bonus = jnp.sum(_heads(r * k * r_k, nh), -1, keepdims=True) * _heads(v, nh)
        return dirs[0], dirs[1], g, bonus.reshape(bonus.shape[:2] + (-1,))

    cf, cb, gc, bc = prep(p_c)
    lf, lb, gl, bl = prep(p_l)
    s0 = jnp.zeros((p_l.shape[0], nh, RWKV_N, RWKV_N), jnp.float32)
    yc, yl = _two_way(_rwkv_scan, cf, lf, cb, lb, s0)
    return ((_head_norm(yc, g_norm, RWKV_GN_EPS, True) + bc) * jax.nn.sigmoid(gc),
            (_head_norm(yl, g_norm, RWKV_GN_EPS, True) + bl) * jax.nn.sigmoid(gl))


def _hier_moe(h, w_rg, b_rg, w_re, b_re, w_gate, w_up, w_down):
    t, d = h.shape
    pg = jax.nn.softmax((h @ w_rg + b_rg).astype(jnp.float32), -1)
    grp = jnp.argmax(pg, -1)
    p_grp = jnp.max(pg, -1)
    le = (h @ w_re + b_re).astype(jnp.float32).reshape(t, N_GROUPS, EXPERTS_PER_GROUP)
    le = jnp.take_along_axis(le, grp[:, None, None], axis=1)[:, 0]
    top_p, top_i = lax.top_k(jax.nn.softmax(le, -1), TOP_K_IN_GROUP)
    wts = (p_grp[:, None] * top_p / jnp.sum(top_p, -1, keepdims=True)).reshape(-1)
    eid = (grp[:, None] * EXPERTS_PER_GROUP + top_i).reshape(-1).astype(jnp.int32)
    tok = jnp.repeat(jnp.arange(t, dtype=jnp.int32), TOP_K_IN_GROUP)
    n_slots = t * TOP_K_IN_GROUP
    order = jnp.argsort(eid)
    e_s, t_s, w_s = eid[order], tok[order], wts[order]
    counts = jnp.zeros((N_EXPERTS,), jnp.int32).at[eid].add(1)
    padded = (counts + MOE_BLOCK - 1) // MOE_BLOCK * MOE_BLOCK
    start = jnp.cumsum(counts) - counts
    pend = jnp.cumsum(padded)
    pstart = pend - padded
    dest = pstart[e_s] + jnp.arange(n_slots, dtype=jnp.int32) - start[e_s]
    n_blocks = -(-(n_slots + N_EXPERTS * (MOE_BLOCK - 1)) // MOE_BLOCK)
    cap = n_blocks * MOE_BLOCK
    buf_tok = jnp.full((cap,), t, jnp.int32).at[dest].set(t_s)
    buf_w = jnp.zeros((cap,), jnp.float32).at[dest].set(w_s)
    blk_e = jnp.minimum(jnp.searchsorted(pend, jnp.arange(n_blocks, dtype=jnp.int32) * MOE_BLOCK,
                                         side='right'), N_EXPERTS - 1)
    h_pad = jnp.concatenate([h, jnp.zeros((1, d), h.dtype)], 0)

    def expert_block(args):
        idx, e = args
        xb = h_pad[idx]
        return (jax.nn.silu(xb @ w_gate[e]) * (xb @ w_up[e])) @ w_down[e]

    ys = lax.map(expert_block, (buf_tok.reshape(n_blocks, MOE_BLOCK), blk_e))
    ys = ys.reshape(cap, d) * buf_w[:, None].astype(h.dtype)
    return jax.ops.segment_sum(ys, buf_tok, num_segments=t + 1)[:t]


def setup_inputs(seed: int = 0) -> dict:
    key = jax.random.key(seed)
    ks = iter(jax.random.split(key, 48))
    D = D_MODEL

    def nrm(shape, scale):
        return jax.random.normal(next(ks), shape, jnp.float32) * scale

    ml_gate_b = jnp.concatenate([
        nrm((DEPTH, 2, 1, MLSTM_HEADS), 0.1) - 1.0,
        jnp.linspace(3.0, 6.0, MLSTM_HEADS, dtype=jnp.float32) + nrm((DEPTH, 2, 1, MLSTM_HEADS), 0.1)],
        axis=2)
    return {
        "x": nrm((BATCH, SEQ, D), 1.0),
        "c": nrm((BATCH, D), 1.0),
        "ctx": nrm((BATCH, CTX_LEN, D), 1.0),
        "c_ctx": nrm((D,), 1.0),
        "w_ada": nrm((DEPTH, D, N_ADA * D), 0.5 * D ** -0.5),
        "b_ada": nrm((DEPTH, N_ADA * D), 0.01),
        "g_norm1": 1.0 + nrm((DEPTH, D), 0.02),
        "g_norm2": 1.0 + nrm((DEPTH, D), 0.02),
        "w_in": nrm((DEPTH, D, N_IN), D ** -0.5),
        "gla_w_a2": nrm((DEPTH, 2, GLA_LR, GLA_HEADS * GLA_DK), GLA_LR ** -0.5),
        "gla_b_a": 0.5 + nrm((DEPTH, 2, GLA_HEADS * GLA_DK), 0.1),
        "gla_g_norm": 1.0 + nrm((DEPTH, GLA_W), 0.02),
        "ml_conv_w": nrm((DEPTH, 3, 3, 2 * MLSTM_HEADS * MLSTM_DQK), 1.0 / 3.0),
        "ml_conv_b": nrm((DEPTH, 2 * MLSTM_HEADS * MLSTM_DQK), 0.01),
        "ml_gate_b": ml_gate_b,
        "ml_g_norm": 1.0 + nrm((DEPTH, MLSTM_W), 0.02),
        "rw_mu": jax.random.uniform(next(ks), (DEPTH, 2, RWKV_COLS), jnp.float32, 0.0, 0.5),
        "rw_w2": nrm((DEPTH, 2, RWKV_LR_W, RWKV_W), 0.5 * RWKV_LR_W ** -0.5),
        "rw_w0": -2.0 + nrm((DEPTH, 2, RWKV_W), 0.5),
        "rw_a2": nrm((DEPTH, 2, RWKV_LR_A, RWKV_W), 0.5 * RWKV_LR_A ** -0.5),
        "rw_a0": nrm((DEPTH, 2, RWKV_W), 0.1),
        "rw_k_k": 0.85 + nrm((DEPTH, RWKV_W), 0.05),
        "rw_k_a": 1.0 + nrm((DEPTH, RWKV_W), 0.05),
        "rw_r_k": nrm((DEPTH, RWKV_W), 0.1),
        "rw_g_norm": 1.0 + nrm((DEPTH, RWKV_W), 0.02),
        "w_out": nrm((DEPTH, D, D), D ** -0.5),
        "moe_w_rg": nrm((DEPTH, D, N_GROUPS), D ** -0.5),
        "moe_b_rg": nrm((DEPTH, N_GROUPS), 0.01),
        "moe_w_re": nrm((DEPTH, D, N_EXPERTS), D ** -0.5),
        "moe_b_re": nrm((DEPTH, N_EXPERTS), 0.01),
        "moe_w_gate": nrm((DEPTH, N_EXPERTS, D, D_EXPERT), D ** -0.5),
        "moe_w_up": nrm((DEPTH, N_EXPERTS, D, D_EXPERT), D ** -0.5),
        "moe_w_down": nrm((DEPTH, N_EXPERTS, D_EXPERT, D), D_EXPERT ** -0.5),
        "g_final": 1.0 + nrm((D,), 0.02),
    }


def reference(x, c, ctx, c_ctx, w_ada, b_ada, g_norm1, g_norm2, w_in, gla_w_a2, gla_b_a,
              gla_g_norm, ml_conv_w, ml_conv_b, ml_gate_b, ml_g_norm, rw_mu, rw_w2, rw_w0,
              rw_a2, rw_a0, rw_k_k, rw_k_a, rw_r_k, rw_g_norm, w_out, moe_w_rg, moe_b_rg,
              moe_w_re, moe_b_re, moe_w_gate, moe_w_up, moe_w_down, g_final):
    bsz, seq, dm = x.shape
    lc = ctx.shape[1]
    rows = seq // GRID_W
    dt = x.dtype
    xl, xc = x, ctx
    s_c, s_cc = jax.nn.silu(c), jax.nn.silu(c_ctx)
    for l in range(DEPTH):
        last = l == DEPTH - 1
        sh1, sc1, gt1, sh2, sc2, gt2 = jnp.split(s_c @ w_ada[l] + b_ada[l], N_ADA, axis=-1)
        csh1, csc1, cgt1, csh2, csc2, cgt2 = jnp.split(s_cc @ w_ada[l] + b_ada[l], N_ADA, axis=-1)

        hl = _rmsnorm(xl, g_norm1[l]) * (1 + sc1[:, None]) + sh1[:, None]
        hc = _rmsnorm(xc, g_norm1[l]) * (1 + csc1) + csh1
        p = jnp.concatenate([hc, hl], axis=1) @ w_in[l]
        pa, pb, pr = _split(p, [GLA_COLS, MLSTM_COLS, RWKV_COLS])
        ya_c, ya_l = _gla_mixer(pa[:, :lc], pa[:, lc:], gla_w_a2[l], gla_b_a[l], gla_g_norm[l])
        yb_c, yb_l = _mlstm_mixer(pb[:, :lc], pb[:, lc:], rows, ml_conv_w[l], ml_conv_b[l],
                                  ml_gate_b[l], ml_g_norm[l])
        yr_c, yr_l = _rwkv_mixer(pr[:, :lc], pr[:, lc:], rw_mu[l], rw_w2[l], rw_w0[l], rw_a2[l],
                                 rw_a0[l], rw_k_k[l], rw_k_a[l], rw_r_k[l], rw_g_norm[l])
        y_l = jnp.concatenate([ya_l, yb_l, yr_l], -1).astype(dt)

        if last:
            xl = xl + gt1[:, None] * (y_l @ w_out[l])
            h2 = _rmsnorm(xl, g_norm2[l]) * (1 + sc2[:, None]) + sh2[:, None]
            f = _hier_moe(h2.reshape(-1, dm), moe_w_rg[l], moe_b_rg[l], moe_w_re[l], moe_b_re[l],
                          moe_w_gate[l], moe_w_up[l], moe_w_down[l]).reshape(bsz, seq, dm)
            xl = xl + gt2[:, None] * f
        else:
            y_c = jnp.concatenate([ya_c, yb_c, yr_c], -1).astype(dt)
            o = jnp.concatenate([y_c, y_l], axis=1) @ w_out[l]
            xc = xc + cgt1 * o[:, :lc]
            xl = xl + gt1[:, None] * o[:, lc:]
            h2 = jnp.concatenate([
                _rmsnorm(xc, g_norm2[l]) * (1 + csc2) + csh2,
                _rmsnorm(xl, g_norm2[l]) * (1 + sc2[:, None]) + sh2[:, None]], axis=1)
            f = _hier_moe(h2.reshape(-1, dm), moe_w_rg[l], moe_b_rg[l], moe_w_re[l], moe_b_re[l],
                          moe_w_gate[l], moe_w_up[l], moe_w_down[l]).reshape(bsz, lc + seq, dm)
            xc = xc + cgt2 * f[:, :lc]
            xl = xl + gt2[:, None] * f[:, lc:]
    return _rmsnorm(xl, g_final)
```

```python
import numpy as np
from contextlib import ExitStack
import concourse.bass as bass
import concourse.mybir as mybir
from concourse.bass_utils import run_bass_kernel_spmd

F32 = mybir.dt.float32
BF16 = mybir.dt.bfloat16
AF = mybir.ActivationFunctionType
ALU = mybir.AluOpType
AX = mybir.AxisListType

D = 2048
KC = 16
DEPTH = 4
GRID_W = 64
CH = 64
NEXP = 32
ENGS = ("sync", "scalar", "vector", "gpsimd", "tensor")
EPOCH = 6000
NDMA = 24
ARENA = 49000


class _Rec:
    def __getattr__(self, name):
        def f(*a, **k):
            self.call = (name, a, k)
            return self
        return f


class Prog:
    def __init__(self, nc):
        self.nc = nc
        self.es = ExitStack()
        self.streams = {e: [] for e in ENGS}
        self.count = {e: 0 for e in ENGS}
        self.esems = {e: [] for e in ENGS}
        self.dsems = [self.es.enter_context(nc.semaphore(f"dq{i}")) for i in range(NDMA)]
        self.dma_n = 0
        self.dma_tok = [None] * NDMA
        self.lastw = {}
        self.readers = {}
        self.seen = {e: {} for e in ENGS}
        self.final = []
        self.nbuf = 0
        self.last_tok = {}
        self.arena = self.sbuf([128, ARENA], F32, "arena")
        self.aptr = 0
        self.banks = [self.psum([128, 512], F32, f"bank{i}") for i in range(8)]

    def sbuf(self, shape, dtype=F32, name=None):
        self.nbuf += 1
        return self.es.enter_context(self.nc.sbuf_tensor(name or f"sb{self.nbuf}", list(shape), dtype))

    def psum(self, shape, dtype=F32, name=None):
        self.nbuf += 1
        return self.es.enter_context(self.nc.psum_tensor(name or f"ps{self.nbuf}", list(shape), dtype))

    def dram(self, name, shape, dtype=F32, kind="Internal"):
        return self.nc.dram_tensor(name, list(shape), dtype, kind=kind).ap()

    def alloc(self, n, dtype=F32):
        w = n if dtype == F32 else (n + 1) // 2
        assert self.aptr + w <= ARENA, ("arena overflow", self.aptr, w)
        ap = self.arena[:, self.aptr:self.aptr + w]
        self.aptr += w
        if dtype != F32:
            ap = ap.bitcast(dtype)[:, 0:n]
        return ap

    def _esem(self, eng, epoch):
        lst = self.esems[eng]
        while len(lst) <= epoch:
            lst.append(self.es.enter_context(self.nc.semaphore(f"p_{eng}_{len(lst)}")))
        return lst[epoch]

    def _need(self, eng, tok, waits):
        if tok is None:
            return
        sem, val, key = tok
        if self.seen[eng].get(key, 0) >= val:
            return
        self.seen[eng][key] = val
        waits.append((sem, val))

    def op(self, eng, fn, reads=(), writes=(), dma=False, out_final=False):
        rec = _Rec()
        fn(rec)
        fn = rec.call
        waits = []
        for k in reads:
            self._need(eng, self.lastw.get(k), waits)
        for k in writes:
            self._need(eng, self.lastw.get(k), waits)
            for t in self.readers.get(k, {}).values():
                self._need(eng, t, waits)
        if dma:
            slot = self.dma_n % NDMA
            val = 16 * (self.dma_n // NDMA + 1)
            self._need(eng, self.dma_tok[slot], waits)
            tok = (self.dsems[slot], val, ("d", slot))
            self.dma_tok[slot] = tok
            self.dma_n += 1
            inc = (self.dsems[slot], 16)
            if out_final:
                self.final.append(tok)
            rkey = ("d", self.dma_n)
        else:
            n = self.count[eng]
            epoch, idx = divmod(n, EPOCH)
            sem = self._esem(eng, epoch)
            tok = (sem, idx + 1, ("e", eng, epoch))
            self.count[eng] = n + 1
            inc = (sem, 1)
            self.last_tok[eng] = tok
            rkey = eng
        self.streams[eng].append((waits, fn, inc))
        for k in reads:
            self.readers.setdefault(k, {})[rkey] = tok
        for k in writes:
            self.lastw[k] = tok
            self.readers[k] = {}
        return tok

    def barrier(self):
        toks = [t for t in self.last_tok.values()] + [t for t in self.dma_tok if t is not None]
        for e in ENGS:
            waits = []
            for t in toks:
                self._need(e, t, waits)
            if waits:
                self.streams[e].append((waits, None, None))
        self.lastw = {}
        self.readers = {}

    def V(self, fn, r=(), w=()):
        return self.op("vector", fn, r, w)

    def S(self, fn, r=(), w=()):
        return self.op("scalar", fn, r, w)

    def G(self, fn, r=(), w=()):
        return self.op("gpsimd", fn, r, w)

    def T(self, fn, r=(), w=()):
        return self.op("tensor", fn, r, w)

    def dma(self, out, in_, r=(), w=(), eng="sync", final=False):
        return self.op(eng, lambda e: e.dma_start(out=out, in_=in_), r, w, dma=True, out_final=final)

    def emit(self):
        nc = self.nc
        fin = [(t[0], t[1]) for t in self.final]
        streams = self.streams
        with nc.Block() as block:
            def run(engname):
                def body(e):
                    for waits, fn, inc in streams[engname]:
                        for s, v in waits:
                            e.wait_ge(s, v)
                        if fn is not None:
                            getattr(e, fn[0])(*fn[1], **fn[2]).then_inc(inc[0], inc[1])
                    if engname == "sync":
                        for s, v in fin:
                            e.wait_ge(s, v)
                return body
            block.sync(run("sync"))
            block.scalar(run("scalar"))
            block.vector(run("vector"))
            block.gpsimd(run("gpsimd"))
            block.tensor(run("tensor"))

    def close(self):
        self.es.close()


def _run(nc, in_maps):
    res = run_bass_kernel_spmd(nc, in_maps, core_ids=list(range(len(in_maps))))
    return res.results


def pk(v):
    return np.ascontiguousarray(np.asarray(v, np.float32).reshape(KC, 128).T)


def build_A(L, NCOL):
    nc = bass.Bass("TRN2", target_bir_lowering=False)
    cT = nc.dram_tensor("cT", [128, KC, 3], F32, kind="ExternalInput").ap()
    w = nc.dram_tensor("w", [L, D, NCOL], F32, kind="ExternalInput").ap()
    b = nc.dram_tensor("b", [L, NCOL], F32, kind="ExternalInput").ap()
    out = nc.dram_tensor("out", [L, 3, NCOL], F32, kind="ExternalOutput").ap()
    P = Prog(nc)
    s = P.alloc(KC * 3).rearrange("p (k r) -> p k r", r=3)
    P.dma(s, cT, w=["s"])
    P.S(lambda e: e.activation(out=s, in_=s, func=AF.Silu), ["s"], ["s"])
    wb = [P.alloc(KC * 512).rearrange("p (k n) -> p k n", n=512) for _ in range(2)]
    bb = [P.alloc(512) for _ in range(2)]
    ob = [P.alloc(512) for _ in range(2)]
    it = 0
    for l in range(L):
        for c0 in range(0, NCOL, 512):
            n = min(512, NCOL - c0)
            i = it % 2
            it += 1
            P.dma(wb[i][:, :, 0:n], w[l, :, c0:c0 + n].rearrange("(k p) n -> p k n", p=128), w=[("wb", i)])
            P.dma(bb[i][0:3, 0:n], b[l:l + 1, c0:c0 + n].partition_broadcast(3), w=[("bb", i)], eng="gpsimd")
            ps = P.banks[i]
            for k in range(KC):
                P.T(lambda e, k=k, i=i, n=n, ps=ps: e.matmul(ps[0:3, 0:n], lhsT=s[:, k, :], rhs=wb[i][:, k, 0:n],
                                                              start=(k == 0), stop=(k == KC - 1)),
                    ["s", ("wb", i)], [("ps", i)])
            P.V(lambda e, i=i, n=n, ps=ps: e.tensor_tensor(out=ob[i][0:3, 0:n], in0=ps[0:3, 0:n], in1=bb[i][0:3, 0:n], op=ALU.add),
                [("ps", i), ("bb", i)], [("ob", i)])
            P.dma(out[l, :, c0:c0 + n], ob[i][0:3, 0:n], r=[("ob", i)], eng="gpsimd", final=True)
    P.emit()
    P.close()
    return nc


def rms_rstd(P, xt, n, ones, msbank, sq, rstd, keys_x, key_out, tag):
    P.S(lambda e: e.activation(out=sq[:, :, 0:n], in_=xt[:, :, 0:n], func=AF.Square), keys_x, [("sq", tag)])
    for k in range(KC):
        P.T(lambda e, k=k: e.matmul(msbank[:, 0:n], lhsT=ones, rhs=sq[:, k, 0:n], start=(k == 0), stop=(k == KC - 1)),
            [("sq", tag), "consts"], [("msb", tag)])
    P.S(lambda e: e.activation(out=rstd[:, 0:n], in_=msbank[:, 0:n], func=AF.Ln, scale=1.0 / D, bias=1e-6),
        [("msb", tag)], [key_out])
    P.S(lambda e: e.activation(out=rstd[:, 0:n], in_=rstd[:, 0:n], func=AF.Exp, scale=-0.5), [key_out], [key_out])


MSLOT = [(0, 1), (2, 3), (4, 4), (5, 5)]
FMA = (["GQ", "GK", "GLR", "GG"] + [f"{a}{s}" for s in range(2) for a in ("MQ", "MK", "MI", "MF", "MO")]
       + [f"{a}{j}" for j in range(3) for a in ("RR", "RK", "RV")] + ["RG01", "RG2", "W1F", "W1B", "A1F", "A1B"])
WBLK = ([("gq", 64), ("gk", 64), ("glrf", 16), ("glrb", 16), ("gg", 128)]
        + [(f"{a}{s}", n) for s in range(2) for a, n in (("mq", 64), ("mk", 64), ("mo", 128), ("mif", 64), ("mff", 64), ("mib", 64), ("mfb", 64))]
        + [(f"{a}{j}", 64) for j in range(3) for a in ("rr", "rk", "rv", "rg")]
        + [("w1f", 96), ("w1b", 96), ("a1f", 96), ("a1b", 96)])
WOFF = {}
_o = 0
for _n, _c in WBLK:
    WOFF[_n] = (_o, _c)
    _o += _c
NFMC = _o
JOBS = {"GQ": [(0, "gq", 0), (64, "gq", 1)], "GK": [(0, "gk", 0), (64, "gk", 1)],
        "GLR": [(0, "glrf", 0), (64, "glrb", 1)], "GG": [(0, "gg", 0)],
        "RG01": [(0, "rg0", 0), (64, "rg1", 0)], "RG2": [(0, "rg2", 0)],
        "W1F": [(0, "w1f", 0)], "W1B": [(0, "w1b", 1)], "A1F": [(0, "a1f", 0)], "A1B": [(0, "a1b", 1)]}
for _s in range(2):
    JOBS[f"MQ{_s}"] = [(0, f"mq{_s}", 0), (64, f"mq{_s}", 1)]
    JOBS[f"MK{_s}"] = [(0, f"mk{_s}", 0), (64, f"mk{_s}", 1)]
    JOBS[f"MI{_s}"] = [(0, f"mif{_s}", 0), (64, f"mib{_s}", 1)]
    JOBS[f"MF{_s}"] = [(0, f"mff{_s}", 0), (64, f"mfb{_s}", 1)]
    JOBS[f"MO{_s}"] = [(0, f"mo{_s}", 0)]
for _j in range(3):
    for _a in ("rr", "rk", "rv"):
        JOBS[f"{_a.upper()}{_j}"] = [(0, f"{_a}{_j}", 0), (64, f"{_a}{_j}", 1)]
NTMC = 384
PPN = {}
_o = 0
for _n, _c in ([("gba", 1), ("ggn", 1)]
               + [(f"{a}{s}", n) for s in range(2) for a, n in (("cwq", 9), ("cwk", 9), ("cbq", 1), ("cbk", 1), ("mbi", 1), ("mbf", 1), ("mgn", 1))]
               + [(f"{a}{j}", n) for j in range(3) for a, n in (("mur", 2), ("muk", 2), ("muv", 2), ("w0", 1), ("a0", 1), ("kk", 1), ("ka", 1), ("rk", 1), ("rgn", 1))]
               + [("mug01", 2), ("mug2", 2), ("muw1f", 2), ("muw1b", 2), ("mua1f", 2), ("mua1b", 2)]):
    PPN[_n] = (_o, _c)
    _o += _c
NPP = _o
CSTN = {}
_o = 0
for _n, _c in [("ident", 128), ("ones", 128), ("J", 128), ("mtri", 128), ("bones", 128), ("o128", 128),
               ("MS", 256), ("MI", 256), ("ML", 256), ("I2", 256), ("bm128", 256), ("bm129", 258), ("bm64", 128)]:
    CSTN[_n] = (_o, _c)
    _o += _c
NCST = _o


def make_consts():
    c = np.zeros((128, NCST), np.float32)

    def put(n, a):
        o, w = CSTN[n]
        c[:, o:o + w] = a
    idx = np.arange(128)
    put("ident", np.eye(128))
    put("ones", np.ones((128, 128)))
    put("J", np.eye(128)[::-1])
    put("mtri", ((idx[:, None] % 64) <= (idx[None, :] % 64)).astype(np.float32))
    same = (idx[:, None] // 64) == (idx[None, :] // 64)
    put("bones", same.astype(np.float32))
    put("o128", np.full((128, 128), 1.0 / 128))
    ms = (same & (idx[:, None] < idx[None, :])).astype(np.float32)
    mi = (same & (idx[:, None] <= idx[None, :])).astype(np.float32)
    put("MS", np.concatenate([ms, ms], 1))
    put("MI", np.concatenate([mi, mi], 1))
    put("ML", np.concatenate([ms.T, ms.T], 1))
    put("I2", np.concatenate([np.eye(128), np.eye(128)], 1))
    for n, dv in (("bm128", 128), ("bm129", 129), ("bm64", 64)):
        m = np.zeros((128, 2 * dv))
        m[:64, :dv] = 1
        m[64:, dv:] = 1
        put(n, m)
    return c


def build_B(CTX, SEQ):
    T = CTX + SEQ
    NT = T // 128
    NCHK = T // 64
    SEGS = [(0, CTX), (CTX, SEQ)]
    TW = 256
    NA = len(FMA)
    nc = bass.Bass("TRN2", target_bir_lowering=False)

    def din(name, shape):
        return nc.dram_tensor(name, list(shape), F32, kind="ExternalInput").ap()
    xT = din("xT", [D, T])
    modv_d = din("modv", [128, KC * 5])
    wfm = din("wfm", [D, NFMC])
    wtm = din("wtm", [D, NTMC])
    cst_d = din("cst", [128, NCST])
    pp_d = din("pp", [128, NPP])
    wa2_d = din("wa2", [128, 64])
    w2_d = din("w2", [96, 3 * 2 * 2 * 64])
    yT = nc.dram_tensor("yT", [576, T], F32, kind="ExternalOutput").ap()
    P = Prog(nc)
    FMS = P.dram("fms", [NA, 128, T])
    TMS = P.dram("tms", [2, T, NTMC])
    AI = {a: i for i, a in enumerate(FMA)}

    cst = P.sbuf([128, NCST], F32, "cst_sb")
    pp = P.sbuf([128, NPP], F32, "pp_sb")
    mv = P.sbuf([128, KC * 5], F32, "mv_sb")
    wa2 = P.sbuf([128, 64], F32, "wa2_sb")
    w2 = P.sbuf([96, 768], F32, "w2_sb")
    P.dma(cst[:], cst_d, w=["consts"])
    P.dma(pp[:], pp_d, w=["consts"])
    P.dma(mv[:], modv_d, w=["consts"])
    P.dma(wa2[:], wa2_d, w=["consts"])
    P.dma(w2[:], w2_d, w=["consts"])
    w2v = w2[:].rearrange("p (j w d c) -> p j w d c", j=3, w=2, d=2)
    mvv = mv[:].rearrange("p (k j) -> p k j", j=5)

    def C(n, r0=0, r1=128, c0=None, c1=None):
        o, w = CSTN[n]
        a = o if c0 is None else o + c0
        b = o + w if c1 is None else o + c1
        return cst[r0:r1, a:b]

    def PPc(n, j=0, w=1, r0=0, r1=128):
        o, _ = PPN[n]
        return pp[r0:r1, o + j:o + j + w]
    ident, ones, Jm = C("ident"), C("ones"), C("J")
    ones_b = cst[:, CSTN["ones"][0]:CSTN["ones"][0] + 1]

    gm = P.sbuf([128, KC * 2], F32, "gm_sb")
    gmv = gm[:].rearrange("p (k j) -> p k j", j=2)
    for j, col in enumerate((1, 3)):
        P.V(lambda e, j=j, col=col: e.tensor_scalar(out=gmv[:, :, j], in0=mvv[:, :, col], scalar1=1.0, scalar2=1.0, op0=ALU.add, op1=ALU.mult),
            ["consts"], ["gm"])
        P.V(lambda e, j=j: e.tensor_tensor(out=gmv[:, :, j], in0=gmv[:, :, j], in1=mvv[:, :, 0], op=ALU.mult), ["gm", "consts"], ["gm"])
    mark0 = P.aptr

    wsb = P.alloc(KC * NFMC, BF16).rearrange("p (k n) -> p k n", n=NFMC)
    wtsb = P.alloc(KC * NTMC, BF16).rearrange("p (k n) -> p k n", n=NTMC)
    for c0 in range(0, NFMC, 512):
        c1 = min(NFMC, c0 + 512)
        P.dma(wsb[:, :, c0:c1], wfm[:, c0:c1].rearrange("(k p) n -> p k n", p=128), w=["wsb"], eng="gpsimd")
    P.dma(wtsb[:, :, :], wtm.rearrange("(k p) n -> p k n", p=128), w=["wsb"], eng="gpsimd")
    xb = [P.alloc(KC * TW).rearrange("p (k n) -> p k n", n=TW) for _ in range(2)]
    sq = P.alloc(KC * TW).rearrange("p (k n) -> p k n", n=TW)
    rstd = P.alloc(TW)
    hT = [P.alloc(KC * TW, BF16).rearrange("p (k n) -> p k n", n=TW) for _ in range(2)]
    hR = [P.alloc(KC * TW, BF16).rearrange("p (k n) -> p k n", n=TW) for _ in range(2)]
    stg = [P.alloc(TW) for _ in range(4)]
    tstg = [P.alloc(NTMC) for _ in range(2)]
    tiles = [(s0, L, o, min(TW, L - o)) for (s0, L) in SEGS for o in range(0, L, TW)]
    cnt = 0
    tcn = 0
    for ti, (s0, L, o, n) in enumerate(tiles):
        b = ti % 2
        t0, tm = s0 + o, s0 + L - o - n
        sg = 0 if s0 == 0 else 1
        xt = xb[b]
        P.dma(xt[:, :, 0:n], xT[:, t0:t0 + n].rearrange("(k p) t -> p k t", p=128), w=[("xt", b)])
        rms_rstd(P, xt, n, ones, P.banks[0], sq, rstd, [("xt", b)], "rstd", 0)
        P.V(lambda e, xt=xt, n=n: e.tensor_tensor(out=sq[:, :, 0:n], in0=xt[:, :, 0:n],
                                                   in1=rstd[:, 0:n].unsqueeze(1).to_broadcast([128, KC, n]), op=ALU.mult),
            [("xt", b), "rstd", ("sq", 0)], [("sq", 0)])
        for k in range(KC):
            P.V(lambda e, k=k, b=b, n=n, sg=sg: e.tensor_scalar(out=hT[b][:, k, 0:n], in0=sq[:, k, 0:n], scalar1=gmv[:, k, sg:sg + 1],
                                                                 scalar2=mvv[:, k, 2 + 2 * sg:3 + 2 * sg], op0=ALU.mult, op1=ALU.add),
                [("sq", 0), "gm", "consts"], [("h", b, 0)])
        P.G(lambda e, b=b, n=n: e.tensor_copy(out=hR[b][:, :, 0:n][:, :, ::-1], in_=hT[b][:, :, 0:n]), [("h", b, 0)], [("h", b, 1)])
        hh = (hT[b], hR[b])
        for ai, a in enumerate(FMA):
            bi = 1 + (cnt % 4)
            cnt += 1
            bank = P.banks[bi]
            for (pb, blk, src) in JOBS[a]:
                c0, nr = WOFF[blk]
                for k in range(KC):
                    P.T(lambda e, k=k, pb=pb, nr=nr, c0=c0, src=src, bank=bank, n=n: e.matmul(
                        bank[pb:pb + nr, 0:n], lhsT=wsb[:, k, c0:c0 + nr], rhs=hh[src][:, k, 0:n], start=(k == 0), stop=(k == KC - 1)),
                        ["wsb", ("h", b, src)], [("bank", bi, pb)])
                si = cnt % 4 if False else (cnt + pb // 64) % 4
                st = stg[si]
                if (cnt + pb) % 2 == 0:
                    P.S(lambda e, st=st, pb=pb, nr=nr, bank=bank, n=n: e.copy(out=st[pb:pb + nr, 0:n], in_=bank[pb:pb + nr, 0:n]),
                        [("bank", bi, pb)], [("stg", si, pb)])
                else:
                    P.V(lambda e, st=st, pb=pb, nr=nr, bank=bank, n=n: e.tensor_copy(out=st[pb:pb + nr, 0:n], in_=bank[pb:pb + nr, 0:n]),
                        [("bank", bi, pb)], [("stg", si, pb)])
                col = t0 if src == 0 else tm
                P.dma(FMS[ai, pb:pb + nr, col:col + n], st[pb:pb + nr, 0:n], r=[("stg", si, pb)], eng="gpsimd")
        for j in range(n // 128):
            for src in (0, 1):
                bi = 5 + (tcn % 2)
                bank = P.banks[bi]
                st = tstg[tcn % 2]
                for k in range(KC):
                    P.T(lambda e, k=k, j=j, src=src, bank=bank: e.matmul(bank[:, 0:NTMC], lhsT=hh[src][:, k, j * 128:(j + 1) * 128],
                                                                         rhs=wtsb[:, k, :], start=(k == 0), stop=(k == KC - 1)),
                        ["wsb", ("h", b, src)], [("bank", bi)])
                P.S(lambda e, st=st, bank=bank: e.copy(out=st[:, 0:NTMC], in_=bank[:, 0:NTMC]), [("bank", bi)], [("tstg", tcn % 2)])
                row0 = (t0 if src == 0 else tm) + 128 * j
                P.dma(TMS[src, row0:row0 + 128, :], st[:, 0:NTMC], r=[("tstg", tcn % 2)], eng="gpsimd")
                tcn += 1
    P.barrier()

    def v3(ap, w=64):
        return ap.rearrange("p (c k) -> p c k", k=w)

    def chunk_local(EX, OFF):
        P.G(lambda e: e.memset(OFF[:, 0:1], 0.0), [], ["OFF"])
        P.V(lambda e: e.tensor_copy(out=OFF[:, 1:NCHK], in_=v3(EX)[:, 0:NCHK - 1, 63]), ["EX"], ["OFF"])
        P.V(lambda e: e.tensor_tensor(out=v3(EX), in0=v3(EX), in1=OFF[:, 0:NCHK].unsqueeze(2).to_broadcast([128, NCHK, 64]),
                                      op=ALU.subtract), ["EX", "OFF"], ["EX"])

    def headnorm(src, n, R, center, eps, gain, gate, bonus, Y, SQ, RS, bankm, tag):
        on = ones[0:R, 0:R]
        P.S(lambda e: e.copy(out=Y[0:R, 0:n], in_=src), [tag + "src"], [tag + "Y"])
        if center:
            P.T(lambda e: e.matmul(bankm[0:R, 0:n], lhsT=on, rhs=Y[0:R, 0:n], start=True, stop=True), [tag + "Y", "consts"], [tag + "bm"])
            P.V(lambda e: e.scalar_tensor_tensor(out=Y[0:R, 0:n], in0=bankm[0:R, 0:n], scalar=-1.0 / R, in1=Y[0:R, 0:n],
                                                 op0=ALU.mult, op1=ALU.add), [tag + "bm", tag + "Y"], [tag + "Y"])
        P.S(lambda e: e.activation(out=SQ[0:R, 0:n], in_=Y[0:R, 0:n], func=AF.Square), [tag + "Y"], [tag + "SQ"])
        P.T(lambda e: e.matmul(bankm[0:R, 0:n], lhsT=on, rhs=SQ[0:R, 0:n], start=True, stop=True), [tag + "SQ", "consts"], [tag + "bm"])
        P.S(lambda e: e.activation(out=RS[0:R, 0:n], in_=bankm[0:R, 0:n], func=AF.Ln, scale=1.0 / R, bias=eps), [tag + "bm"], [tag + "RS"])
        P.S(lambda e: e.activation(out=RS[0:R, 0:n], in_=RS[0:R, 0:n], func=AF.Exp, scale=-0.5), [tag + "RS"], [tag + "RS"])
        P.V(lambda e: e.tensor_tensor(out=Y[0:R, 0:n], in0=Y[0:R, 0:n], in1=RS[0:R, 0:n], op=ALU.mult), [tag + "Y", tag + "RS"], [tag + "Y"])
        if bonus is None:
            P.V(lambda e: e.scalar_tensor_tensor(out=Y[0:R, 0:n], in0=Y[0:R, 0:n], scalar=gain, in1=gate, op0=ALU.mult, op1=ALU.mult),
                [tag + "Y", tag + "gate", "consts"], [tag + "Y"])
        else:
            P.V(lambda e: e.scalar_tensor_tensor(out=Y[0:R, 0:n], in0=Y[0:R, 0:n], scalar=gain, in1=bonus, op0=ALU.mult, op1=ALU.add),
                [tag + "Y", tag + "bonus", "consts"], [tag + "Y"])
            P.V(lambda e: e.tensor_tensor(out=Y[0:R, 0:n], in0=Y[0:R, 0:n], in1=gate, op=ALU.mult), [tag + "Y", tag + "gate"], [tag + "Y"])

    def groups():
        for (s0, L) in SEGS:
            for g0 in range(0, L, 512):
                yield s0, L, g0, min(512, L - g0)

    PADW = 65
    for s in range(2):
        for arr, cwn, cbn in ((f"MQ{s}", f"cwq{s}", f"cbq{s}"), (f"MK{s}", f"cwk{s}", f"cbk{s}")):
            P.aptr = mark0
            oc, ol = PADW, PADW + CTX + PADW
            X0 = P.alloc(T + 3 * PADW)
            XA = P.alloc(SEQ + 2 * PADW)
            XB = P.alloc(SEQ + 2 * PADW)
            ACC = P.alloc(T)
            P.G(lambda e: e.memset(X0, 0.0), [], ["X0"])
            P.G(lambda e: e.memset(XA, 0.0), [], ["XA"])
            P.G(lambda e: e.memset(XB, 0.0), [], ["XB"])
            P.dma(X0[:, oc:oc + CTX], FMS[AI[arr], :, 0:CTX], w=["X0"])
            P.dma(X0[:, ol:ol + SEQ], FMS[AI[arr], :, CTX:T], w=["X0"])
            P.S(lambda e: e.copy(out=XA[:, PADW:PADW + SEQ], in_=X0[:, ol:ol + SEQ]), ["X0"], ["XA"])
            P.G(lambda e: e.tensor_copy(out=XB[:, PADW:PADW + SEQ], in_=X0[:, ol:ol + SEQ]), ["X0"], ["XB"])
            P.S(lambda e: e.memzero(v3(XA[:, PADW:PADW + SEQ])[:, :, 63]) if False else e.activation(
                out=v3(XA[:, PADW:PADW + SEQ])[:, :, 63], in_=v3(XA[:, PADW:PADW + SEQ])[:, :, 63], func=AF.Copy, scale=0.0), ["XA"], ["XA"])
            P.G(lambda e: e.memset(v3(XB[:, PADW:PADW + SEQ])[:, :, 0], 0.0), ["XB"], ["XB"])
            cw = lambda i, j: PPc(cwn, 3 * i + j)
            cb = PPc(cbn)
            P.V(lambda e: e.tensor_scalar(out=ACC[:, 0:CTX], in0=X0[:, oc:oc + CTX], scalar1=cw(1, 1), scalar2=cb, op0=ALU.mult, op1=ALU.add),
                ["X0", "consts"], ["ACC"])
            for j in (0, 2):
                P.V(lambda e, j=j: e.scalar_tensor_tensor(out=ACC[:, 0:CTX], in0=X0[:, oc + j - 1:oc + j - 1 + CTX], scalar=cw(1, j),
                                                          in1=ACC[:, 0:CTX], op0=ALU.mult, op1=ALU.add), ["X0", "ACC", "consts"], ["ACC"])
            P.V(lambda e: e.tensor_scalar(out=ACC[:, CTX:T], in0=X0[:, ol:ol + SEQ], scalar1=cw(1, 1), scalar2=cb, op0=ALU.mult, op1=ALU.add),
                ["X0", "consts"], ["ACC"])
            for i in range(3):
                for j in range(3):
                    if i == 1 and j == 1:
                        continue
                    off = 64 * (i - 1) + (j - 1)
                    if j == 1:
                        srcv = X0[:, ol + off:ol + off + SEQ]
                    elif j == 0:
                        srcv = XA[:, PADW + off:PADW + off + SEQ]
                    else:
                        srcv = XB[:, PADW + off:PADW + off + SEQ]
                    P.V(lambda e, srcv=srcv, i=i, j=j: e.scalar_tensor_tensor(out=ACC[:, CTX:T], in0=srcv, scalar=cw(i, j), in1=ACC[:, CTX:T],
                                                                              op0=ALU.mult, op1=ALU.add), ["X0", "XA", "XB", "ACC", "consts"], ["ACC"])
            P.S(lambda e: e.activation(out=ACC, in_=ACC, func=AF.Silu), ["ACC"], ["ACC"])
            P.dma(FMS[AI[arr], :, :], ACC, r=["ACC"], eng="gpsimd")
            P.barrier()

    def glalike(kind, s):
        P.aptr = mark0
        dv = 128 if kind == "g" else 129
        QS, KS, LA, EX = P.alloc(T), P.alloc(T), P.alloc(T), P.alloc(T)
        IG = P.alloc(T) if kind == "m" else None
        V2f = P.alloc(NT * 2 * dv)
        V2 = V2f.rearrange("p (i d v) -> p i d v", d=2, v=dv)
        Of = P.alloc(NT * 256)
        O = Of.rearrange("p (i d v) -> p i d v", d=2, v=128)
        OFF, EB = P.alloc(NCHK), P.alloc(NCHK)
        Sb, tmpS = P.alloc(2 * dv), P.alloc(2 * dv)
        nb, rec = P.alloc(1), P.alloc(2)
        att = [P.alloc(128) for _ in range(2)]
        kws = [P.alloc(128) for _ in range(2)]
        Yb, SQb, RSb = P.alloc(512), P.alloc(512), P.alloc(512)
        bm = C("bm128") if kind == "g" else C("bm129")
        if kind == "g":
            P.dma(QS, FMS[AI["GQ"]], w=["QS"])
            P.dma(KS, FMS[AI["GK"]], w=["KS"])
            P.dma(EX, FMS[AI["GLR"]], w=["EX"])
            P.V(lambda e: e.tensor_scalar(out=nb, in0=PPc("gba"), scalar1=-1.0, scalar2=0.0, op0=ALU.mult, op1=ALU.add), ["consts"], ["nb"])
            for t0 in range(0, T, 512):
                n = min(512, T - t0)
                bk = P.banks[(t0 // 512) % 2]
                for d in range(2):
                    P.T(lambda e, d=d, t0=t0, n=n, bk=bk: e.matmul(bk[64 * d:64 * d + 64, 0:n], lhsT=wa2[64 * d:64 * d + 16, :],
                                                                  rhs=EX[64 * d:64 * d + 16, t0:t0 + n], start=True, stop=True),
                        ["EX", "consts"], [("bk", (t0 // 512) % 2)])
                P.S(lambda e, t0=t0, n=n, bk=bk: e.activation(out=LA[:, t0:t0 + n], in_=bk[:, 0:n], func=AF.Exp, scale=-1.0, bias=nb),
                    [("bk", (t0 // 512) % 2), "nb"], ["LA"])
            sc = 1.0 / 16.0
            gname, gfn, gain, row0, center = "GG", AF.Silu, PPc("ggn"), 0, False
        else:
            P.dma(QS, FMS[AI[f"MQ{s}"]], w=["QS"])
            P.dma(KS, FMS[AI[f"MK{s}"]], w=["KS"])
            P.dma(LA, FMS[AI[f"MF{s}"]], w=["LA"])
            P.dma(IG, FMS[AI[f"MI{s}"]], w=["IG"])
            P.V(lambda e: e.tensor_scalar(out=nb, in0=PPc(f"mbf{s}"), scalar1=-1.0, scalar2=0.0, op0=ALU.mult, op1=ALU.add), ["consts"], ["nb"])
            P.S(lambda e: e.activation(out=LA, in_=LA, func=AF.Exp, scale=-1.0, bias=nb), ["LA", "nb"], ["LA"])
            sc = 1.0
            gname, gfn, gain, row0, center = f"MO{s}", AF.Sigmoid, PPc(f"mgn{s}"), 128 + 128 * s, True
        P.S(lambda e: e.activation(out=LA, in_=LA, func=AF.Ln, bias=1.0), ["LA"], ["LA"])
        P.V(lambda e: e.tensor_tensor_scan(out=EX, data0=ones_b.to_broadcast([128, T]), data1=LA, initial=0.0, op0=ALU.mult, op1=ALU.add),
            ["LA", "consts", "EX"], ["EX"])
        chunk_local(EX, OFF)
        P.S(lambda e: e.activation(out=LA, in_=EX, func=AF.Exp, scale=-sc), ["EX"], ["LA"])
        P.V(lambda e: e.tensor_copy(out=EB[:, 0:NCHK], in_=v3(LA)[:, :, 63]), ["LA"], ["EB"])
        P.V(lambda e: e.scalar_tensor_tensor(out=QS, in0=QS, scalar=0.125, in1=LA, op0=ALU.mult, op1=ALU.mult), ["QS", "LA"], ["QS"])
        if kind == "g":
            P.S(lambda e: e.activation(out=EX, in_=EX, func=AF.Exp, scale=sc), ["EX"], ["EX"])
        else:
            P.V(lambda e: e.scalar_tensor_tensor(out=EX, in0=EX, scalar=sc, in1=IG, op0=ALU.mult, op1=ALU.add), ["EX", "IG"], ["EX"])
            P.S(lambda e: e.activation(out=EX, in_=EX, func=AF.Exp, bias=PPc(f"mbi{s}")), ["EX", "consts"], ["EX"])
        P.V(lambda e: e.tensor_tensor(out=KS, in0=KS, in1=EX, op=ALU.mult), ["KS", "EX"], ["KS"])
        tcol = {"g": 0, "m": 128 + 128 * s}[kind]
        for d in range(2):
            P.dma(V2[:, :, d, 0:128], TMS[d, :, tcol:tcol + 128].rearrange("(i p) c -> p i c", p=128), w=["V2"])
        if kind == "m":
            P.G(lambda e: e.memset(V2[:, :, :, 128:129], 1.0), ["V2"], ["V2"])
        P.G(lambda e: e.memset(Sb, 0.0), [], ["S"])
        for c in range(NCHK):
            i, h = divmod(c, 2)
            pb = 64 * h
            cs = slice(c * 64, c * 64 + 64)
            z = c % 2
            ab, ob, kb, sbk = P.banks[z], P.banks[2 + z], P.banks[4 + z], P.banks[6 + z]
            for d in range(2):
                P.T(lambda e, d=d, ab=ab, pb=pb, cs=cs: e.matmul(ab[pb:pb + 64, d * 64:(d + 1) * 64], lhsT=KS[64 * d:64 * d + 64, cs],
                                                                 rhs=QS[64 * d:64 * d + 64, cs], start=True, stop=True), ["KS", "QS"], [("ab", z)])
            P.V(lambda e, ab=ab, pb=pb, z=z: e.tensor_tensor(out=att[z][pb:pb + 64, :], in0=ab[pb:pb + 64, 0:128], in1=C("mtri", pb, pb + 64),
                                                             op=ALU.mult), [("ab", z), "consts"], [("att", z)])
            P.T(lambda e, ob=ob, pb=pb, cs=cs: e.matmul(ob[pb:pb + 64, 0:2 * dv], lhsT=QS[:, cs], rhs=Sb[:, 0:2 * dv], start=True, stop=False),
                ["QS", "S"], [("ob", z)])
            for d in range(2):
                P.T(lambda e, d=d, ob=ob, pb=pb, i=i, z=z: e.matmul(ob[pb:pb + 64, d * dv:(d + 1) * dv], lhsT=att[z][pb:pb + 64, d * 64:(d + 1) * 64],
                                                                    rhs=V2[pb:pb + 64, i, d, :], start=False, stop=(d == 1)),
                    [("att", z), "V2"], [("ob", z)])
            if kind == "g":
                P.S(lambda e, ob=ob, pb=pb, i=i: e.copy(out=O[pb:pb + 64, i, :, :], in_=ob[pb:pb + 64, 0:256].rearrange("p (d v) -> p d v", d=2)),
                    [("ob", z)], ["O"])
            else:
                P.S(lambda e, ob=ob, pb=pb: e.activation(out=rec[pb:pb + 64, 0:2], in_=ob[pb:pb + 64, 0:258].rearrange("p (d v) -> p d v", d=2)[:, :, 128],
                                                         func=AF.Abs), [("ob", z)], ["rec"])
                P.V(lambda e, pb=pb: e.tensor_scalar(out=rec[pb:pb + 64, 0:2], in0=rec[pb:pb + 64, 0:2], scalar1=1.0, scalar2=1.0, op0=ALU.max, op1=ALU.mult),
                    ["rec"], ["rec"])
                P.V(lambda e, pb=pb: e.reciprocal(out=rec[pb:pb + 64, 0:2], in_=rec[pb:pb + 64, 0:2]), ["rec"], ["rec"])
                for d in range(2):
                    P.V(lambda e, d=d, ob=ob, pb=pb, i=i: e.tensor_scalar_mul(out=O[pb:pb + 64, i, d, :], in0=ob[pb:pb + 64, d * dv:d * dv + 128],
                                                                             scalar1=rec[pb:pb + 64, d:d + 1]), [("ob", z), "rec"], ["O"])
            for d in range(2):
                P.T(lambda e, d=d, kb=kb, pb=pb, cs=cs: e.matmul(kb[pb:pb + 64, d * 64:(d + 1) * 64], lhsT=KS[64 * d:64 * d + 64, cs],
                                                                 rhs=ident[64 * d:64 * d + 64, 64 * d:64 * d + 64], start=True, stop=True),
                    ["KS", "consts"], [("kb", z)])
            P.S(lambda e, kb=kb, pb=pb, z=z: e.copy(out=kws[z][pb:pb + 64, :], in_=kb[pb:pb + 64, 0:128]), [("kb", z)], [("kws", z)])
            P.T(lambda e, sbk=sbk, pb=pb, i=i, z=z: e.matmul(sbk[:, 0:2 * dv], lhsT=kws[z][pb:pb + 64, :],
                                                              rhs=V2[pb:pb + 64, i, :, :].rearrange("p d v -> p (d v)"), start=True, stop=True),
                [("kws", z), "V2"], [("sbk", z)])
            P.V(lambda e, sbk=sbk: e.tensor_tensor(out=tmpS, in0=Sb, in1=sbk[:, 0:2 * dv], op=ALU.add), ["S", ("sbk", z)], ["tmpS"])
            P.V(lambda e, c=c: e.scalar_tensor_tensor(out=Sb, in0=tmpS, scalar=EB[:, c:c + 1], in1=bm, op0=ALU.mult, op1=ALU.mult),
                ["tmpS", "EB", "consts"], ["S"])
        GT = QS
        P.dma(GT, FMS[AI[gname]], w=["QS", "gate"])
        P.S(lambda e: e.activation(out=GT, in_=GT, func=gfn), ["gate"], ["pgate"])
        for gi, (s0, L, g0, n) in enumerate(groups()):
            yb = P.banks[gi % 2]
            for jj in range(n // 128):
                i = (s0 + g0) // 128 + jj
                im = s0 // 128 + (L // 128 - 1 - (i - s0 // 128))
                P.T(lambda e, yb=yb, jj=jj, i=i: e.matmul(yb[:, jj * 128:(jj + 1) * 128], lhsT=O[:, i, 0, :], rhs=ident, start=True, stop=False),
                    ["O", "consts"], ["psrc"])
                P.T(lambda e, yb=yb, jj=jj, im=im: e.matmul(yb[:, jj * 128:(jj + 1) * 128], lhsT=O[:, im, 1, :], rhs=Jm, start=False, stop=True),
                    ["O", "consts"], ["psrc"])
            t0 = s0 + g0
            headnorm(yb[:, 0:n], n, 128, center, 1e-6, gain, GT[:, t0:t0 + n], None, Yb, SQb, RSb, P.banks[2 + gi % 2], "p")
            P.dma(yT[row0:row0 + 128, t0:t0 + n], Yb[:, 0:n], r=["pY"], eng="gpsimd", final=True)
        P.barrier()

    glalike("g", 0)
    glalike("m", 0)
    glalike("m", 1)

    def shift(X, Y, rows, mun):
        cp, cn = PPc(mun, 0, 1, 0, rows), PPc(mun, 1, 1, 0, rows)
        c0 = P.alloc(1)
        P.V(lambda e: e.tensor_tensor(out=c0[0:rows], in0=cp, in1=cn, op=ALU.add), ["consts"], ["c0"])
        P.V(lambda e: e.tensor_scalar(out=c0[0:rows], in0=c0[0:rows], scalar1=-1.0, scalar2=1.0, op0=ALU.mult, op1=ALU.add), ["c0"], ["c0"])
        kx, ky = ("sh", id(X)), ("sh", id(Y))
        P.V(lambda e: e.tensor_scalar_mul(out=Y[0:rows], in0=X[0:rows], scalar1=c0[0:rows]), [kx, "c0"], [ky])
        for (s0, L) in SEGS:
            P.V(lambda e, s0=s0, L=L: e.scalar_tensor_tensor(out=Y[0:rows, s0 + 1:s0 + L], in0=X[0:rows, s0:s0 + L - 1], scalar=cp,
                                                             in1=Y[0:rows, s0 + 1:s0 + L], op0=ALU.mult, op1=ALU.add), [kx, ky, "consts"], [ky])
            P.V(lambda e, s0=s0, L=L: e.scalar_tensor_tensor(out=Y[0:rows, s0:s0 + L - 1], in0=X[0:rows, s0 + 1:s0 + L], scalar=cn,
                                                             in1=Y[0:rows, s0:s0 + L - 1], op0=ALU.mult, op1=ALU.add), [kx, ky, "consts"], [ky])
        return kx, ky

    for arr, mun, rows, fn in (("W1F", "muw1f", 96, AF.Tanh), ("W1B", "muw1b", 96, AF.Tanh), ("A1F", "mua1f", 96, None),
                               ("A1B", "mua1b", 96, None), ("RG01", "mug01", 128, AF.Sigmoid), ("RG2", "mug2", 64, AF.Sigmoid)):
        P.aptr = mark0
        X, Y = P.alloc(T), P.alloc(T)
        kx, ky = ("sh", id(X)), ("sh", id(Y))
        P.dma(X[0:rows], FMS[AI[arr], 0:rows, :], w=[kx])
        shift(X, Y, rows, mun)
        if fn is not None:
            P.S(lambda e, fn=fn: e.activation(out=Y[0:rows], in_=Y[0:rows], func=fn), [ky], [ky])
        P.dma(FMS[AI[arr], 0:rows, :], Y[0:rows], r=[ky], eng="gpsimd")
        P.barrier()

    def rwkv_head(j):
        P.aptr = mark0
        U = [P.alloc(T) for _ in range(9)]
        K_ = lambda i: ("U", i)
        Of = U[2]
        O = Of.rearrange("p (i c) -> p i c", c=128)
        OFF, EW = P.alloc(NCHK), P.alloc(NCHK)
        omka = P.alloc(1)
        SR = P.alloc(5120)
        lrt = [SR[:, z_ * 2048:(z_ + 1) * 2048].rearrange("p (a n) -> p a n", a=4) for z_ in range(2)]
        bst = [SR[:, 4096 + z_ * 512:4096 + (z_ + 1) * 512] for z_ in range(2)]
        for i_, a in enumerate(("RR", "RK", "RV")):
            P.dma(U[i_], FMS[AI[f"{a}{j}"]], w=[("sh", id(U[i_]))])
        shift(U[0], U[3], 128, f"mur{j}")
        shift(U[1], U[4], 128, f"muk{j}")
        shift(U[2], U[0], 128, f"muv{j}")
        R, Kk, Vv = U[3], U[4], U[0]
        kR, kK, kV = ("sh", id(U[3])), ("sh", id(U[4])), ("sh", id(U[0]))
        P.V(lambda e: e.scalar_tensor_tensor(out=U[1], in0=R, scalar=PPc(f"rk{j}"), in1=Kk, op0=ALU.mult, op1=ALU.mult),
            [kR, kK, ("sh", id(U[1])), "consts"], [K_(1)])
        for ti, t0 in enumerate(range(0, T, 512)):
            n = min(512, T - t0)
            z = ti % 2
            bk = P.banks[z]
            P.T(lambda e, bk=bk, t0=t0, n=n: e.matmul(bk[:, 0:n], lhsT=C("bones"), rhs=U[1][:, t0:t0 + n], start=True, stop=True),
                [K_(1), "consts"], [("bk", z)])
            P.V(lambda e, bk=bk, t0=t0, n=n, z=z: e.tensor_tensor(out=bst[z][:, 0:n], in0=bk[:, 0:n], in1=Vv[:, t0:t0 + n], op=ALU.mult),
                [("bk", z), kV], [("bst", z)])
            P.dma(FMS[AI[f"RR{j}"], :, t0:t0 + n], bst[z][:, 0:n], r=[("bst", z)], eng="gpsimd")
        for ti, t0 in enumerate(range(0, T, 512)):
            n = min(512, T - t0)
            z = ti % 2
            for ai_, a in enumerate(("W1F", "W1B", "A1F", "A1B")):
                P.dma(lrt[z][0:96, ai_, 0:n], FMS[AI[a], 0:96, t0:t0 + n], w=[("lrt", z)])
            for w_, (dst, bn, srcs) in enumerate(((U[6], f"w0{j}", (0, 1)), (U[5], f"a0{j}", (2, 3)))):
                bk = P.banks[2 + 2 * w_ + z]
                for d in range(2):
                    P.T(lambda e, bk=bk, d=d, w_=w_, srcs=srcs, z=z, n=n: e.matmul(bk[64 * d:64 * d + 64, 0:n], lhsT=w2v[:, j, w_, d, :],
                                                                                  rhs=lrt[z][0:96, srcs[d], 0:n], start=True, stop=True),
                        [("lrt", z), "consts"], [("bk2", w_, z)])
                P.S(lambda e, bk=bk, dst=dst, bn=bn, t0=t0, n=n: e.activation(out=dst[:, t0:t0 + n], in_=bk[:, 0:n], func=AF.Sigmoid, bias=PPc(bn)),
                    [("bk2", w_, z), "consts"], [K_(5 + (1 - w_))])
        P.V(lambda e: e.tensor_scalar_mul(out=U[1], in0=Kk, scalar1=PPc(f"kk{j}")), [kK, K_(1), "consts"], [K_(1)])
        P.S(lambda e: e.activation(out=U[8], in_=U[1], func=AF.Square), [K_(1)], [K_(8)])
        for ti, t0 in enumerate(range(0, T, 512)):
            n = min(512, T - t0)
            z = ti % 2
            bk = P.banks[z]
            P.T(lambda e, bk=bk, t0=t0, n=n: e.matmul(bk[:, 0:n], lhsT=C("bones"), rhs=U[8][:, t0:t0 + n], start=True, stop=True),
                [K_(8), "consts"], [("bk", z)])
            P.S(lambda e, bk=bk, t0=t0, n=n: e.activation(out=U[7][:, t0:t0 + n], in_=bk[:, 0:n], func=AF.Ln, bias=1e-24), [("bk", z)], [K_(7)])
        P.S(lambda e: e.activation(out=U[7], in_=U[7], func=AF.Exp, scale=-0.5), [K_(7)], [K_(7)])
        P.V(lambda e: e.tensor_tensor(out=U[1], in0=U[1], in1=U[7], op=ALU.mult), [K_(1), K_(7)], [K_(1)])
        P.V(lambda e: e.tensor_scalar(out=omka, in0=PPc(f"ka{j}"), scalar1=-1.0, scalar2=1.0, op0=ALU.mult, op1=ALU.add), ["consts"], ["omka"])
        P.V(lambda e: e.tensor_scalar(out=U[7], in0=U[5], scalar1=PPc(f"ka{j}"), scalar2=omka, op0=ALU.mult, op1=ALU.add),
            [K_(5), K_(7), "omka", "consts"], [K_(7)])
        P.V(lambda e: e.tensor_tensor(out=Kk, in0=Kk, in1=U[7], op=ALU.mult), [kK, K_(7), K_(1)], [kK])
        P.V(lambda e: e.tensor_scalar(out=U[6], in0=U[6], scalar1=-float(np.exp(-0.5)), scalar2=0.0, op0=ALU.mult, op1=ALU.add), [K_(6)], [K_(6)])
        P.V(lambda e: e.tensor_tensor_scan(out=U[7], data0=ones_b.to_broadcast([128, T]), data1=U[6], initial=0.0, op0=ALU.mult, op1=ALU.add),
            [K_(6), K_(7), kK, "consts"], ["EX"])
        chunk_local(U[7], OFF)
        P.S(lambda e: e.activation(out=U[8], in_=U[7], func=AF.Exp), ["EX", K_(8)], [K_(8)])
        P.V(lambda e: e.tensor_tensor(out=R, in0=R, in1=U[8], op=ALU.mult), [kR, K_(8), K_(1)], [kR])
        P.S(lambda e: e.activation(out=U[6], in_=U[6], func=AF.Exp, scale=-1.0), [K_(6), "EX"], [K_(6)])
        P.V(lambda e: e.tensor_tensor(out=U[6], in0=U[6], in1=U[8], op=ALU.mult), [K_(6), K_(8)], [K_(6)])
        P.V(lambda e: e.scalar_tensor_tensor(out=U[6], in0=U[6], scalar=-1.0, in1=U[1], op0=ALU.mult, op1=ALU.mult), [K_(6), K_(1)], [K_(6)])
        P.S(lambda e: e.activation(out=U[8], in_=U[7], func=AF.Exp, scale=-1.0), ["EX", K_(8), kR, K_(6)], [K_(8)])
        P.V(lambda e: e.tensor_tensor(out=U[1], in0=U[1], in1=U[5], op=ALU.mult), [K_(1), K_(5), K_(6)], [K_(1)])
        P.V(lambda e: e.tensor_tensor(out=U[1], in0=U[1], in1=U[8], op=ALU.mult), [K_(1), K_(8)], [K_(1)])
        P.V(lambda e: e.tensor_tensor(out=Kk, in0=Kk, in1=U[8], op=ALU.mult), [kK, K_(8)], [kK])
        P.S(lambda e: e.activation(out=EW[:, 0:NCHK], in_=v3(U[7])[:, :, 63], func=AF.Exp), ["EX"], ["EW"])
        P.V(lambda e: e.tensor_tensor(out=v3(U[5]), in0=v3(U[1]), in1=EW[:, 0:NCHK].unsqueeze(2).to_broadcast([128, NCHK, 64]), op=ALU.mult),
            [K_(1), K_(5), "EW"], [K_(5)])
        P.V(lambda e: e.tensor_tensor(out=v3(U[7]), in0=v3(Kk), in1=EW[:, 0:NCHK].unsqueeze(2).to_broadcast([128, NCHK, 64]), op=ALU.mult),
            [kK, "EX", "EW", K_(8)], ["KW"])
        AH, BH, KH, RH, BW, KW = U[6], U[1], Kk, R, U[5], U[7]
        kAH, kBH, kKH, kRH, kBW, kKW = K_(6), K_(1), kK, kR, K_(5), "KW"
        P.barrier()
        pool = [[U[8], 0, T], [SR, 0, 5120]]

        def carve(n):
            for pl in pool:
                if pl[1] + n <= pl[2]:
                    a_ = pl[0][:, pl[1]:pl[1] + n]
                    pl[1] += n
                    return a_
            return P.alloc(n)
        prod = {nm: [carve(256) for _ in range(2)] for nm in ("AAK", "ARK", "NT", "ARB", "NN")}
        tmb = {nm: [carve(128) for _ in range(2)] for nm in ("V", "BW", "KW")}
        Yi = [carve(256) for _ in range(2)]
        Zi = [carve(256) for _ in range(2)]
        Qb = [carve(256) for _ in range(2)]
        rhs_sb, u_sb = carve(128), carve(128)
        Sb, tmpS = carve(128), carve(128)
        Yb, SQb, RSb, Gt, Bt = carve(512), carve(512), carve(512), carve(512), carve(512)
        P.G(lambda e: e.memset(Sb, 0.0), [], ["S"])
        pc = 0
        for i in range(NT):
            ts = slice(128 * i, 128 * i + 128)
            z = i % 2
            specs = (("AAK", KH, AH, kKH, kAH, "MS"), ("ARK", KH, RH, kKH, kRH, "MI"), ("NT", BH, AH, kBH, kAH, "MS"),
                     ("ARB", BH, RH, kBH, kRH, "MI"), ("NN", AH, BH, kAH, kBH, "ML"))
            for nm, Lh, Rh, kl, kr, mk in specs:
                bi = pc % 2
                pc += 1
                bk = P.banks[bi]
                for d in range(2):
                    P.T(lambda e, bk=bk, d=d, Lh=Lh, Rh=Rh, ts=ts: e.matmul(bk[:, d * 128:(d + 1) * 128], lhsT=Lh[64 * d:64 * d + 64, ts],
                                                                           rhs=Rh[64 * d:64 * d + 64, ts], start=True, stop=True), [kl, kr], [("pb", bi)])
                P.V(lambda e, bk=bk, nm=nm, mk=mk, z=z: e.tensor_tensor(out=prod[nm][z], in0=bk[:, 0:256], in1=C(mk), op=ALU.mult),
                    [("pb", bi), "consts"], [(nm, z)])
            for nm, Xs, kx_ in (("V", Vv, kV), ("BW", BW, kBW), ("KW", KW, kKW)):
                bi = pc % 2
                pc += 1
                bk = P.banks[bi]
                P.T(lambda e, bk=bk, Xs=Xs, ts=ts: e.transpose(bk[:, 0:128], Xs[:, ts], ident), [kx_, "consts"], [("pb", bi)])
                P.S(lambda e, bk=bk, nm=nm, z=z: e.copy(out=tmb[nm][z], in_=bk[:, 0:128]), [("pb", bi)], [("tm" + nm, z)])
            Yc, Zc, Q = prod["NT"][z], prod["NN"][z], Qb[z]
            kY, kZ = ("NT", z), ("NN", z)
            P.V(lambda e, Q=Q, Yc=Yc: e.tensor_tensor(out=Q, in0=Yc, in1=C("I2"), op=ALU.add), [kY, "consts"], [("Q", z)])
            for it in range(5):
                Yn, Zn = Yi[it % 2], Zi[it % 2]
                for d in range(2):
                    ds_ = slice(d * 128, (d + 1) * 128)
                    P.T(lambda e, ds_=ds_, Yc=Yc, Zc=Zc: e.matmul(P.banks[2][:, ds_], lhsT=Zc[:, ds_], rhs=Yc[:, ds_], start=True, stop=True),
                        [kY, kZ], ["bY"])
                    P.T(lambda e, ds_=ds_, Yc=Yc, Zc=Zc: e.matmul(P.banks[3][:, ds_], lhsT=Yc[:, ds_], rhs=Zc[:, ds_], start=True, stop=True),
                        [kY, kZ], ["bZ"])
                P.S(lambda e, Yn=Yn: e.copy(out=Yn, in_=P.banks[2][:, 0:256]), ["bY"], [("Yi", it % 2)])
                P.V(lambda e, Zn=Zn: e.tensor_copy(out=Zn, in_=P.banks[3][:, 0:256]), ["bZ"], [("Zi", it % 2)])
                Yc, Zc, kY, kZ = Yn, Zn, ("Yi", it % 2), ("Zi", it % 2)
                for d in range(2):
                    ds_ = slice(d * 128, (d + 1) * 128)
                    P.T(lambda e, ds_=ds_, Zc=Zc, Q=Q: e.matmul(P.banks[4][:, ds_], lhsT=Zc[:, ds_], rhs=Q[:, ds_], start=True, stop=True),
                        [kZ, ("Q", z)], ["bQ"])
                P.V(lambda e, Q=Q: e.tensor_tensor(out=Q, in0=Q, in1=P.banks[4][:, 0:256], op=ALU.add), ["bQ", ("Q", z)], [("Q", z)])
            AAK, ARK, ARB = prod["AAK"][z], prod["ARK"][z], prod["ARB"][z]
            Vt, BWt, KWt = tmb["V"][z], tmb["BW"][z], tmb["KW"][z]
            for h in range(2):
                c = 2 * i + h
                pb = 64 * h
                cs = slice(c * 64, c * 64 + 64)
                bR, bU = P.banks[5][pb:pb + 64, 0:128], P.banks[5][pb:pb + 64, 128:256]
                bY2, bS = P.banks[6][pb:pb + 64, 0:128], P.banks[7][:, 0:128]

                def blk(M_, d, pb=pb):
                    return M_[pb:pb + 64, d * 128 + pb:d * 128 + pb + 64]
                P.T(lambda e, bR=bR, cs=cs: e.matmul(bR, lhsT=AH[:, cs], rhs=Sb, start=True, stop=False), [kAH, "S"], ["bR"])
                for d in range(2):
                    P.T(lambda e, d=d, bR=bR, pb=pb: e.matmul(bR[:, d * 64:(d + 1) * 64], lhsT=blk(AAK, d), rhs=Vt[pb:pb + 64, d * 64:(d + 1) * 64],
                                                              start=False, stop=(d == 1)), [("AAK", z), ("tmV", z)], ["bR"])
                P.S(lambda e, bR=bR, pb=pb: e.copy(out=rhs_sb[pb:pb + 64, :], in_=bR), ["bR"], ["rhs_sb"])
                for d in range(2):
                    P.T(lambda e, d=d, bU=bU, pb=pb: e.matmul(bU[:, d * 64:(d + 1) * 64], lhsT=blk(Q, d), rhs=rhs_sb[pb:pb + 64, d * 64:(d + 1) * 64],
                                                              start=True, stop=True), [("Q", z), "rhs_sb"], ["bU"])
                P.V(lambda e, bU=bU, pb=pb: e.tensor_copy(out=u_sb[pb:pb + 64, :], in_=bU), ["bU"], ["u_sb"])
                P.T(lambda e, bY2=bY2, cs=cs: e.matmul(bY2, lhsT=RH[:, cs], rhs=Sb, start=True, stop=False), [kRH, "S"], ["bY2"])
                for d in range(2):
                    P.T(lambda e, d=d, bY2=bY2, pb=pb: e.matmul(bY2[:, d * 64:(d + 1) * 64], lhsT=blk(ARB, d), rhs=u_sb[pb:pb + 64, d * 64:(d + 1) * 64],
                                                                start=False, stop=False), [("ARB", z), "u_sb"], ["bY2"])
                    P.T(lambda e, d=d, bY2=bY2, pb=pb: e.matmul(bY2[:, d * 64:(d + 1) * 64], lhsT=blk(ARK, d), rhs=Vt[pb:pb + 64, d * 64:(d + 1) * 64],
                                                                start=False, stop=(d == 1)), [("ARK", z), ("tmV", z)], ["bY2"])
                P.S(lambda e, bY2=bY2, pb=pb, i=i: e.copy(out=O[pb:pb + 64, i, :], in_=bY2), ["bY2", K_(2), ("sh", id(U[2]))], ["O"])
                P.T(lambda e, bS=bS, pb=pb: e.matmul(bS, lhsT=BWt[pb:pb + 64, :], rhs=u_sb[pb:pb + 64, :], start=True, stop=False),
                    [("tmBW", z), "u_sb"], ["bS"])
                P.T(lambda e, bS=bS, pb=pb: e.matmul(bS, lhsT=KWt[pb:pb + 64, :], rhs=Vt[pb:pb + 64, :], start=False, stop=True),
                    [("tmKW", z), ("tmV", z)], ["bS"])
                P.V(lambda e, bS=bS, c=c: e.scalar_tensor_tensor(out=tmpS, in0=Sb, scalar=EW[:, c:c + 1], in1=bS, op0=ALU.mult, op1=ALU.add),
                    ["S", "EW", "bS"], ["tmpS"])
                P.V(lambda e: e.tensor_tensor(out=Sb, in0=tmpS, in1=C("bm64"), op=ALU.mult), ["tmpS", "consts"], ["S"])
        garr, gr0 = (("RG01", 0), ("RG01", 64), ("RG2", 0))[j]
        for gi, (s0, L, g0, n) in enumerate(groups()):
            yb = P.banks[gi % 2]
            t0 = s0 + g0
            for jj in range(n // 128):
                i = t0 // 128 + jj
                im = s0 // 128 + (L // 128 - 1 - (i - s0 // 128))
                P.T(lambda e, yb=yb, jj=jj, i=i: e.matmul(yb[0:64, jj * 128:(jj + 1) * 128], lhsT=O[:, i, 0:64], rhs=ident, start=True, stop=False),
                    ["O", "consts"], ["rsrc"])
                P.T(lambda e, yb=yb, jj=jj, im=im: e.matmul(yb[0:64, jj * 128:(jj + 1) * 128], lhsT=O[:, im, 64:128], rhs=Jm, start=False, stop=True),
                    ["O", "consts"], ["rsrc"])
            P.dma(Gt[0:64, 0:n], FMS[AI[garr], gr0:gr0 + 64, t0:t0 + n], w=["rgate"])
            P.dma(Bt[0:64, 0:n], FMS[AI[f"RR{j}"], 0:64, t0:t0 + n], w=["rbonus"])
            headnorm(yb[0:64, 0:n], n, 64, True, 64e-5, PPc(f"rgn{j}", 0, 1, 0, 64), Gt[0:64, 0:n], Bt[0:64, 0:n], Yb, SQb, RSb, P.banks[2 + gi % 2], "r")
            P.dma(yT[384 + 64 * j:448 + 64 * j, t0:t0 + n], Yb[0:64, 0:n], r=["rY"], eng="gpsimd", final=True)
        P.barrier()

    for j in range(3):
        rwkv_head(j)
    P.emit()
    P.close()
    return nc


GLA_BASE, ML_BASE, RW_BASE = 0, 1568, 3896


def _wcols(g):
    ar = np.arange
    cols = {"gq": g * 64 + ar(64), "gk": 256 + g * 64 + ar(64), "glrf": 1536 + ar(16), "glrb": 1552 + ar(16),
            "gg": 1024 + g * 128 + ar(128), "gv": 512 + g * 128 + ar(128)}
    for s, h in enumerate(MSLOT[g]):
        cols[f"mq{s}"] = ML_BASE + h * 64 + ar(64)
        cols[f"mk{s}"] = ML_BASE + 384 + h * 64 + ar(64)
        cols[f"mv{s}"] = ML_BASE + 768 + h * 128 + ar(128)
        cols[f"mo{s}"] = ML_BASE + 1536 + h * 128 + ar(128)
        for nm, d, w in (("mif", 0, 0), ("mff", 0, 1), ("mib", 1, 0), ("mfb", 1, 1)):
            cols[f"{nm}{s}"] = np.full(64, ML_BASE + 2304 + d * 12 + w * 6 + h)
    for j in range(3):
        hh = 3 * g + j
        for k_, nm in enumerate(("rr", "rk", "rv", "rg")):
            cols[f"{nm}{j}"] = RW_BASE + 768 * k_ + hh * 64 + ar(64)
    for k_, nm in enumerate(("w1f", "w1b", "a1f", "a1b")):
        cols[nm] = RW_BASE + 3072 + 96 * k_ + ar(96)
    return cols


def prep_B(l, b, g, inp, mod, xc, xl):
    f32 = np.float32
    cols = _wcols(g)
    w_in = inp["w_in"][l]
    wfm = np.concatenate([w_in[:, cols[n]] for n, _ in WBLK], axis=1)
    wtm = np.concatenate([w_in[:, cols[n]] for n in ("gv", "mv0", "mv1")], axis=1)
    xT = np.ascontiguousarray(np.concatenate([xc[b], xl[b]], 0).T)
    sh1, sc1 = mod[l][:, 0:D], mod[l][:, D:2 * D]
    modv = np.stack([pk(inp["g_norm1"][l]), pk(sc1[2]), pk(sh1[2]), pk(sc1[b]), pk(sh1[b])], axis=2).reshape(128, KC * 5)
    pp = np.zeros((128, NPP), f32)

    def put(n, a, r0=0):
        o, w = PPN[n]
        a = np.asarray(a, f32).reshape(-1, w)
        pp[r0:r0 + a.shape[0], o:o + w] = a
    put("gba", inp["gla_b_a"][l][0, g * 64:(g + 1) * 64])
    put("gba", inp["gla_b_a"][l][1, g * 64:(g + 1) * 64], 64)
    put("ggn", inp["gla_g_norm"][l][g * 128:(g + 1) * 128])
    cwt = inp["ml_conv_w"][l].reshape(9, 768)
    for s, h in enumerate(MSLOT[g]):
        for nm, c0 in (("q", h * 64), ("k", 384 + h * 64)):
            wv = cwt[:, c0:c0 + 64].T
            put(f"cw{nm}{s}", wv)
            put(f"cw{nm}{s}", wv[:, ::-1], 64)
            put(f"cb{nm}{s}", inp["ml_conv_b"][l][c0:c0 + 64])
            put(f"cb{nm}{s}", inp["ml_conv_b"][l][c0:c0 + 64], 64)
        for d in range(2):
            put(f"mbi{s}", np.full(64, inp["ml_gate_b"][l][d, 0, h]), 64 * d)
            put(f"mbf{s}", np.full(64, inp["ml_gate_b"][l][d, 1, h]), 64 * d)
        put(f"mgn{s}", inp["ml_g_norm"][l][h * 128:(h + 1) * 128])
    mu = inp["rw_mu"][l]

    def mupair(c, swap):
        m = np.stack([mu[0][c], mu[1][c]], 1)
        return m[:, ::-1] if swap else m
    for j in range(3):
        hh = 3 * g + j
        hc = hh * 64 + np.arange(64)
        for nm, off in (("mur", 0), ("muk", 768), ("muv", 1536)):
            put(f"{nm}{j}", mupair(off + hc, False))
            put(f"{nm}{j}", mupair(off + hc, True), 64)
        for d in range(2):
            put(f"w0{j}", inp["rw_w0"][l][d, hc], 64 * d)
            put(f"a0{j}", inp["rw_a0"][l][d, hc], 64 * d)
            put(f"kk{j}", inp["rw_k_k"][l][hc], 64 * d)
            put(f"ka{j}", inp["rw_k_a"][l][hc], 64 * d)
            put(f"rk{j}", inp["rw_r_k"][l][hc], 64 * d)
        put(f"rgn{j}", inp["rw_g_norm"][l][hc])
    put("mug01", mupair(2304 + (3 * g) * 64 + np.arange(64), False))
    put("mug01", mupair(2304 + (3 * g + 1) * 64 + np.arange(64), False), 64)
    put("mug2", mupair(2304 + (3 * g + 2) * 64 + np.arange(64), False))
    put("muw1f", mupair(3072 + np.arange(96), False))
    put("muw1b", mupair(3168 + np.arange(96), True))
    put("mua1f", mupair(3264 + np.arange(96), False))
    put("mua1b", mupair(3360 + np.arange(96), True))
    wa2 = np.zeros((128, 64), f32)
    wa2[0:16] = inp["gla_w_a2"][l][0][:, g * 64:(g + 1) * 64]
    wa2[64:80] = inp["gla_w_a2"][l][1][:, g * 64:(g + 1) * 64]
    w2 = np.zeros((96, 3, 2, 2, 64), f32)
    for j in range(3):
        hc = (3 * g + j) * 64 + np.arange(64)
        for d in range(2):
            w2[:, j, 0, d] = inp["rw_w2"][l][d][:, hc]
            w2[:, j, 1, d] = inp["rw_a2"][l][d][:, hc]
    return {"xT": xT, "modv": np.ascontiguousarray(modv, f32), "wfm": np.ascontiguousarray(wfm), "wtm": np.ascontiguousarray(wtm),
            "cst": make_consts(), "pp": pp, "wa2": wa2, "w2": w2.reshape(96, 768)}


def assemble_yT(res, B, T):
    yT = np.zeros((B, D, T), np.float32)
    for b in range(B):
        for g in range(4):
            r = res[b * 4 + g]["yT"]
            yT[b, g * 128:(g + 1) * 128] = r[0:128]
            for s, h in enumerate(MSLOT[g]):
                yT[b, 512 + h * 128:512 + (h + 1) * 128] = r[128 + 128 * s:256 + 128 * s]
            for j in range(3):
                hh = 3 * g + j
                yT[b, 1280 + hh * 64:1280 + (hh + 1) * 64] = r[384 + 64 * j:448 + 64 * j]
    return yT


def build_C1(NCc, NLl):
    NTK = NCc + NLl
    TW = 256
    BIG = 1.0e30
    nc = bass.Bass("TRN2", target_bir_lowering=False)

    def din(name, shape, dt=F32):
        return nc.dram_tensor(name, list(shape), dt, kind="ExternalInput").ap()
    yT, xT = din("yT", [D, NTK]), din("xT", [D, NTK])
    wo_d, modv_d, wr_d, br_d, cst_d = din("wo", [D, D]), din("modv", [128, KC * 7]), din("wr", [D, 36]), din("br", [128, 36]), din("cst", [128, NCST])
    x1T = nc.dram_tensor("x1T", [D, NTK], F32, kind="ExternalOutput").ap()
    h2T = nc.dram_tensor("h2T", [D, NTK], BF16, kind="ExternalOutput").ap()
    gates = nc.dram_tensor("gates", [NTK, 32], F32, kind="ExternalOutput").ap()
    P = Prog(nc)
    cst = P.sbuf([128, NCST], F32, "cst_sb")
    mv = P.sbuf([128, KC * 7], F32, "mv_sb")
    wr = P.sbuf([128, KC * 36], F32, "wr_sb")
    br = P.sbuf([128, 36], F32, "br_sb")
    gm = P.sbuf([128, KC * 2], F32, "gm_sb")
    P.dma(cst[:], cst_d, w=["consts"])
    P.dma(mv[:], modv_d, w=["consts"])
    P.dma(br[:], br_d, w=["consts"])
    wrv = wr[:].rearrange("p (k n) -> p k n", n=36)
    P.dma(wrv, wr_d.rearrange("(k p) n -> p k n", p=128), w=["consts"])
    mvv = mv[:].rearrange("p (k j) -> p k j", j=7)
    gmv = gm[:].rearrange("p (k j) -> p k j", j=2)
    ones = cst[:, CSTN["ones"][0]:CSTN["ones"][0] + 128]
    for j, col in enumerate((2, 5)):
        P.V(lambda e: e.tensor_scalar(out=gmv[:, :, j], in0=mvv[:, :, col], scalar1=1.0, scalar2=1.0, op0=ALU.add, op1=ALU.mult), ["consts"], ["gm"])
        P.V(lambda e: e.tensor_tensor(out=gmv[:, :, j], in0=gmv[:, :, j], in1=mvv[:, :, 0], op=ALU.mult), ["gm", "consts"], ["gm"])
    wo = P.alloc(KC * D, BF16).rearrange("p (k n) -> p k n", n=D)
    for c0 in range(0, D, 512):
        P.dma(wo[:, :, c0:c0 + 512], wo_d[:, c0:c0 + 512].rearrange("(k p) n -> p k n", p=128), w=["wo"], eng="gpsimd")
    yb = [P.alloc(KC * TW, BF16).rearrange("p (k n) -> p k n", n=TW) for _ in range(2)]
    xb = [P.alloc(KC * TW).rearrange("p (k n) -> p k n", n=TW) for _ in range(2)]
    sq = P.alloc(KC * TW).rearrange("p (k n) -> p k n", n=TW)
    hb = P.alloc(KC * TW, BF16).rearrange("p (k n) -> p k n", n=TW)
    rstd = P.alloc(TW)
    Lg, LM, I1, I2_, G1 = P.alloc(36), P.alloc(32), P.alloc(32), P.alloc(32), P.alloc(32)
    sm = P.alloc(16)
    tiles = [(s0, o, min(TW, L - o)) for (s0, L) in ((0, NCc), (NCc, NLl)) for o in range(0, L, TW)]
    for ti, (s0, o, n) in enumerate(tiles):
        b = ti % 2
        t0 = s0 + o
        sg = 0 if s0 == 0 else 1
        yt, xt = yb[b], xb[b]
        P.dma(yt[:, :, 0:n], yT[:, t0:t0 + n].rearrange("(k p) t -> p k t", p=128), w=[("yt", b)], eng="gpsimd")
        P.dma(xt[:, :, 0:n], xT[:, t0:t0 + n].rearrange("(k p) t -> p k t", p=128), w=[("xt", b)])
        for fo in range(KC):
            bi = fo % 4
            bank = P.banks[1 + bi]
            for k in range(KC):
                P.T(lambda e: e.matmul(bank[:, 0:n], lhsT=wo[:, k, fo * 128:(fo + 1) * 128], rhs=yt[:, k, 0:n], start=(k == 0), stop=(k == KC - 1)),
                    ["wo", ("yt", b)], [("bank", bi)])
            P.V(lambda e: e.scalar_tensor_tensor(out=xt[:, fo, 0:n], in0=bank[:, 0:n], scalar=mvv[:, fo, 1 + 3 * sg:2 + 3 * sg], in1=xt[:, fo, 0:n],
                                                 op0=ALU.mult, op1=ALU.add), [("bank", bi), ("xt", b), "consts"], [("xt", b)])
        P.dma(x1T[:, t0:t0 + n].rearrange("(k p) t -> p k t", p=128), xt[:, :, 0:n], r=[("xt", b)], eng="gpsimd", final=True)
        rms_rstd(P, xt, n, ones, P.banks[0], sq, rstd, [("xt", b)], "rstd", 0)
        P.V(lambda e: e.tensor_tensor(out=sq[:, :, 0:n], in0=xt[:, :, 0:n], in1=rstd[:, 0:n].unsqueeze(1).to_broadcast([128, KC, n]), op=ALU.mult),
            [("xt", b), "rstd", ("sq", 0)], [("sq", 0)])
        for k in range(KC):
            P.V(lambda e: e.tensor_scalar(out=sq[:, k, 0:n], in0=sq[:, k, 0:n], scalar1=gmv[:, k, sg:sg + 1],
                                          scalar2=mvv[:, k, 3 + 3 * sg:4 + 3 * sg], op0=ALU.mult, op1=ALU.add), [("sq", 0), "gm", "consts"], [("sq", 0)])
        P.G(lambda e: e.tensor_copy(out=hb[:, :, 0:n], in_=sq[:, :, 0:n]), [("sq", 0)], ["hb"])
        P.dma(h2T[:, t0:t0 + n].rearrange("(k p) t -> p k t", p=128), hb[:, :, 0:n], r=["hb"], eng="gpsimd", final=True)
        for m0 in range(0, n, 128):
            m = min(128, n - m0)
            bank = P.banks[5]
            for k in range(KC):
                P.T(lambda e: e.matmul(bank[0:m, 0:36], lhsT=sq[:, k, m0:m0 + m], rhs=wrv[:, k, :], start=(k == 0), stop=(k == KC - 1)),
                    [("sq", 0), "consts"], ["rb"])
            A = lambda t_, c0=0, c1=None: t_[0:m, c0:(c1 if c1 is not None else t_.shape[1])]
            P.V(lambda e: e.tensor_tensor(out=A(Lg), in0=bank[0:m, 0:36], in1=br[0:m, :], op=ALU.add), ["rb", "consts"], ["Lg"])
            P.V(lambda e: e.reduce_max(out=sm[0:m, 0:1], in_=Lg[0:m, 0:4], axis=AX.X), ["Lg"], ["sm"])
            P.V(lambda e: e.tensor_scalar(out=sm[0:m, 1:2], in0=sm[0:m, 0:1], scalar1=-1.0, scalar2=0.0, op0=ALU.mult, op1=ALU.add), ["sm"], ["sm"])
            P.S(lambda e: e.activation(out=sm[0:m, 8:12], in_=Lg[0:m, 0:4], func=AF.Exp, bias=sm[0:m, 1:2], accum_out=sm[0:m, 2:3]), ["Lg", "sm"], ["sm"])
            P.V(lambda e: e.reciprocal(out=sm[0:m, 3:4], in_=sm[0:m, 2:3]), ["sm"], ["sm"])
            P.V(lambda e: e.tensor_scalar(out=sm[0:m, 12:16], in0=Lg[0:m, 0:4], scalar1=sm[0:m, 0:1], scalar2=BIG, op0=ALU.is_ge, op1=ALU.mult),
                ["Lg", "sm"], ["sm"])
            P.V(lambda e: e.tensor_scalar(out=sm[0:m, 12:16], in0=sm[0:m, 12:16], scalar1=-BIG, scalar2=1.0, op0=ALU.add, op1=ALU.mult), ["sm"], ["sm"])
            P.V(lambda e: e.tensor_tensor(out=LM[0:m, :].rearrange("p (g x) -> p g x", g=4), in0=Lg[0:m, 4:36].rearrange("p (g x) -> p g x", g=4),
                                          in1=sm[0:m, 12:16].unsqueeze(2).to_broadcast([m, 4, 8]), op=ALU.add), ["Lg", "sm"], ["LM"])
            P.V(lambda e: e.reduce_max(out=sm[0:m, 4:5], in_=LM[0:m, :], axis=AX.X), ["LM"], ["sm"])
            P.V(lambda e: e.tensor_scalar(out=I1[0:m, :], in0=LM[0:m, :], scalar1=sm[0:m, 4:5], scalar2=1.0, op0=ALU.is_ge, op1=ALU.mult), ["LM", "sm"], ["I1"])
            P.V(lambda e: e.scalar_tensor_tensor(out=LM[0:m, :], in0=I1[0:m, :], scalar=-BIG, in1=LM[0:m, :], op0=ALU.mult, op1=ALU.add), ["I1", "LM"], ["LM"])
            P.V(lambda e: e.reduce_max(out=sm[0:m, 5:6], in_=LM[0:m, :], axis=AX.X), ["LM"], ["sm"])
            P.V(lambda e: e.tensor_scalar(out=I2_[0:m, :], in0=LM[0:m, :], scalar1=sm[0:m, 5:6], scalar2=1.0, op0=ALU.is_ge, op1=ALU.mult), ["LM", "sm"], ["I2"])
            P.V(lambda e: e.tensor_tensor(out=sm[0:m, 6:7], in0=sm[0:m, 4:5], in1=sm[0:m, 5:6], op=ALU.subtract), ["sm"], ["sm"])
            P.S(lambda e: e.activation(out=sm[0:m, 6:7], in_=sm[0:m, 6:7], func=AF.Sigmoid), ["sm"], ["sm"])
            P.V(lambda e: e.tensor_tensor(out=sm[0:m, 6:7], in0=sm[0:m, 6:7], in1=sm[0:m, 3:4], op=ALU.mult), ["sm"], ["sm"])
            P.V(lambda e: e.tensor_tensor(out=sm[0:m, 7:8], in0=sm[0:m, 3:4], in1=sm[0:m, 6:7], op=ALU.subtract), ["sm"], ["sm"])
            P.V(lambda e: e.tensor_scalar_mul(out=G1[0:m, :], in0=I1[0:m, :], scalar1=sm[0:m, 6:7]), ["I1", "sm"], ["G1"])
            P.V(lambda e: e.scalar_tensor_tensor(out=G1[0:m, :], in0=I2_[0:m, :], scalar=sm[0:m, 7:8], in1=G1[0:m, :], op0=ALU.mult, op1=ALU.add),
                ["I2", "sm", "G1"], ["G1"])
            P.dma(gates[t0 + m0:t0 + m0 + m, :], G1[0:m, :], r=["G1"], eng="gpsimd", final=True)
    P.emit()
    P.close()
    return nc


def build_C2(TT, DE):
    NK = DE // 128
    TW = 512
    nc = bass.Bass("TRN2", target_bir_lowering=False)
    h2T = nc.dram_tensor("h2T", [D, TT], BF16, kind="ExternalInput").ap()
    gT = nc.dram_tensor("gT", [4, TT], F32, kind="ExternalInput").ap()
    wg_d = nc.dram_tensor("wg", [4, D, DE], F32, kind="ExternalInput").ap()
    wu_d = nc.dram_tensor("wu", [4, D, DE], F32, kind="ExternalInput").ap()
    wd_d = nc.dram_tensor("wd", [4, DE, D], F32, kind="ExternalInput").ap()
    fT = nc.dram_tensor("fT", [D, TT], F32, kind="ExternalOutput").ap()
    P = Prog(nc)
    wg = [P.alloc(KC * DE, BF16).rearrange("p (k n) -> p k n", n=DE) for _ in range(2)]
    wu = [P.alloc(KC * DE, BF16).rearrange("p (k n) -> p k n", n=DE) for _ in range(2)]
    wd = [P.alloc(NK * D, BF16).rearrange("p (k n) -> p k n", n=D) for _ in range(2)]
    hb = [P.alloc(KC * TW, BF16).rearrange("p (k n) -> p k n", n=TW) for _ in range(2)]
    Ab = [[[P.alloc(TW, BF16) for _ in range(NK)] for _ in range(2)] for _ in range(2)]
    gb = [[P.alloc(TW) for _ in range(2)] for _ in range(2)]
    sgb = [P.alloc(TW) for _ in range(2)]
    ost = [P.alloc(TW) for _ in range(3)]
    prv = [P.alloc(TW) for _ in range(2)]
    tiles = [(t0, min(TW, TT - t0)) for t0 in range(0, TT, TW)]
    for p in range(2):
        for e_ in range(2):
            ex = 2 * p + e_
            P.dma(wg[e_][:, :, :], wg_d[ex].rearrange("(k p) n -> p k n", p=128), w=[("W", e_)], eng="gpsimd")
            P.dma(wu[e_][:, :, :], wu_d[ex].rearrange("(k p) n -> p k n", p=128), w=[("W", e_)], eng="gpsimd")
            for c0 in range(0, D, 512):
                P.dma(wd[e_][:, :, c0:c0 + 512], wd_d[ex, :, c0:c0 + 512].rearrange("(k p) n -> p k n", p=128), w=[("W", e_)], eng="gpsimd")
        gc = 0
        for ti, (t0, n) in enumerate(tiles):
            b = ti % 2
            h = hb[b]
            P.dma(h[:, :, 0:n], h2T[:, t0:t0 + n].rearrange("(k p) t -> p k t", p=128), w=[("h", b)])
            for e_ in range(2):
                P.dma(gb[b][e_][:, 0:n], gT[2 * p + e_:2 * p + e_ + 1, t0:t0 + n].partition_broadcast(128), w=[("g", b, e_)])
            for e_ in range(2):
                for kc in range(NK):
                    z = gc % 2
                    gc += 1
                    bG, bU = P.banks[z], P.banks[2 + z]
                    for k in range(KC):
                        P.T(lambda e: e.matmul(bG[:, 0:n], lhsT=wg[e_][:, k, kc * 128:(kc + 1) * 128], rhs=h[:, k, 0:n], start=(k == 0), stop=(k == KC - 1)),
                            [("W", e_), ("h", b)], [("bG", z)])
                    for k in range(KC):
                        P.T(lambda e: e.matmul(bU[:, 0:n], lhsT=wu[e_][:, k, kc * 128:(kc + 1) * 128], rhs=h[:, k, 0:n], start=(k == 0), stop=(k == KC - 1)),
                            [("W", e_), ("h", b)], [("bU", z)])
                    P.S(lambda e: e.activation(out=sgb[z][:, 0:n], in_=bG[:, 0:n], func=AF.Silu), [("bG", z)], [("sg", z)])
                    P.V(lambda e: e.tensor_tensor(out=sgb[z][:, 0:n], in0=sgb[z][:, 0:n], in1=bU[:, 0:n], op=ALU.mult), [("sg", z), ("bU", z)], [("sg", z)])
                    P.G(lambda e: e.tensor_tensor(out=Ab[b][e_][kc][:, 0:n], in0=sgb[z][:, 0:n], in1=gb[b][e_][:, 0:n], op=ALU.mult),
                        [("sg", z), ("g", b, e_)], [("A", b)])
            for fo in range(KC):
                z = fo % 2
                bF = P.banks[4 + z]
                o_ = ost[fo % 3]
                first = True
                for e_ in range(2):
                    for kc in range(NK):
                        last = (e_ == 1 and kc == NK - 1)
                        P.T(lambda e: e.matmul(bF[:, 0:n], lhsT=wd[e_][:, kc, fo * 128:(fo + 1) * 128], rhs=Ab[b][e_][kc][:, 0:n], start=first, stop=last),
                            [("W", e_), ("A", b)], [("bF", z)])
                        first = False
                if p == 0:
                    P.S(lambda e: e.copy(out=o_[:, 0:n], in_=bF[:, 0:n]), [("bF", z)], [("ost", fo % 3)])
                else:
                    pv = prv[fo % 2]
                    P.dma(pv[:, 0:n], fT[fo * 128:(fo + 1) * 128, t0:t0 + n], w=[("prv", fo % 2)])
                    P.V(lambda e: e.tensor_tensor(out=o_[:, 0:n], in0=bF[:, 0:n], in1=pv[:, 0:n], op=ALU.add), [("bF", z), ("prv", fo % 2)], [("ost", fo % 3)])
                P.dma(fT[fo * 128:(fo + 1) * 128, t0:t0 + n], o_[:, 0:n], r=[("ost", fo % 3)], eng="gpsimd", final=(p == 1))
        P.barrier()
    P.emit()
    P.close()
    return nc


def build_C3(NCc, NLl, NP):
    NTK = NCc + NLl
    TW = 256
    nc = bass.Bass("TRN2", target_bir_lowering=False)
    fTp = nc.dram_tensor("fTp", [NP, D, NTK], F32, kind="ExternalInput").ap()
    x1T = nc.dram_tensor("x1T", [D, NTK], F32, kind="ExternalInput").ap()
    modv_d = nc.dram_tensor("modv", [128, KC * 3], F32, kind="ExternalInput").ap()
    cst_d = nc.dram_tensor("cst", [128, NCST], F32, kind="ExternalInput").ap()
    x2T = nc.dram_tensor("x2T", [D, NTK], F32, kind="ExternalOutput").ap()
    onT = nc.dram_tensor("onT", [D, NTK], F32, kind="ExternalOutput").ap()
    P = Prog(nc)
    cst = P.sbuf([128, NCST], F32, "cst_sb")
    mv = P.sbuf([128, KC * 3], F32, "mv_sb")
    P.dma(cst[:], cst_d, w=["consts"])
    P.dma(mv[:], modv_d, w=["consts"])
    mvv = mv[:].rearrange("p (k j) -> p k j", j=3)
    ones = cst[:, CSTN["ones"][0]:CSTN["ones"][0] + 128]
    pb_ = [P.alloc(KC * TW).rearrange("p (k n) -> p k n", n=TW) for _ in range(3)]
    acc = [P.alloc(KC * TW).rearrange("p (k n) -> p k n", n=TW) for _ in range(2)]
    xb = [P.alloc(KC * TW).rearrange("p (k n) -> p k n", n=TW) for _ in range(2)]
    sq = P.alloc(KC * TW).rearrange("p (k n) -> p k n", n=TW)
    rstd = P.alloc(TW)
    tiles = [(s0, o, min(TW, L - o)) for (s0, L) in ((0, NCc), (NCc, NLl)) for o in range(0, L, TW)]
    pc = 0
    for ti, (s0, o, n) in enumerate(tiles):
        b = ti % 2
        t0 = s0 + o
        sg = 0 if s0 == 0 else 1
        a_, xt = acc[b], xb[b]
        P.dma(xt[:, :, 0:n], x1T[:, t0:t0 + n].rearrange("(k p) t -> p k t", p=128), w=[("xt", b)])
        P.dma(a_[:, :, 0:n], fTp[0, :, t0:t0 + n].rearrange("(k p) t -> p k t", p=128), w=[("acc", b)])
        for c in range(1, NP):
            z = pc % 3
            pc += 1
            P.dma(pb_[z][:, :, 0:n], fTp[c, :, t0:t0 + n].rearrange("(k p) t -> p k t", p=128), w=[("pb", z)])
            P.V(lambda e: e.tensor_tensor(out=a_[:, :, 0:n], in0=a_[:, :, 0:n], in1=pb_[z][:, :, 0:n], op=ALU.add), [("acc", b), ("pb", z)], [("acc", b)])
        P.V(lambda e: e.tensor_tensor(out=a_[:, :, 0:n], in0=a_[:, :, 0:n], in1=mvv[:, :, sg:sg + 1].to_broadcast([128, KC, n]), op=ALU.mult),
            [("acc", b), "consts"], [("acc", b)])
        P.V(lambda e: e.tensor_tensor(out=a_[:, :, 0:n], in0=a_[:, :, 0:n], in1=xt[:, :, 0:n], op=ALU.add), [("acc", b), ("xt", b)], [("acc", b)])
        P.dma(x2T[:, t0:t0 + n].rearrange("(k p) t -> p k t", p=128), a_[:, :, 0:n], r=[("acc", b)], eng="gpsimd", final=True)
        rms_rstd(P, a_, n, ones, P.banks[0], sq, rstd, [("acc", b)], "rstd", 0)
        P.V(lambda e: e.tensor_tensor(out=sq[:, :, 0:n], in0=a_[:, :, 0:n], in1=rstd[:, 0:n].unsqueeze(1).to_broadcast([128, KC, n]), op=ALU.mult),
            [("acc", b), "rstd", ("sq", 0)], [("sq", 0)])
        P.V(lambda e: e.tensor_tensor(out=sq[:, :, 0:n], in0=sq[:, :, 0:n], in1=mvv[:, :, 2:3].to_broadcast([128, KC, n]), op=ALU.mult),
            [("sq", 0), "consts"], [("sq", 0)])
        P.dma(onT[:, t0:t0 + n].rearrange("(k p) t -> p k t", p=128), sq[:, :, 0:n], r=[("sq", 0)], eng="gpsimd", final=True)
    P.emit()
    P.close()
    return nc


_NC_CACHE = {}


def _get(key, fn):
    if key not in _NC_CACHE:
        _NC_CACHE[key] = fn()
    return _NC_CACHE[key]


def kernel(x, c, ctx, c_ctx, w_ada, b_ada, g_norm1, g_norm2, w_in, gla_w_a2, gla_b_a,
           gla_g_norm, ml_conv_w, ml_conv_b, ml_gate_b, ml_g_norm, rw_mu, rw_w2, rw_w0,
           rw_a2, rw_a0, rw_k_k, rw_k_a, rw_r_k, rw_g_norm, w_out, moe_w_rg, moe_b_rg,
           moe_w_re, moe_b_re, moe_w_gate, moe_w_up, moe_w_down, g_final):
    f32 = np.float32
    inp = dict(w_in=w_in, g_norm1=g_norm1, gla_w_a2=gla_w_a2, gla_b_a=gla_b_a, gla_g_norm=gla_g_norm, ml_conv_w=ml_conv_w,
               ml_conv_b=ml_conv_b, ml_gate_b=ml_gate_b, ml_g_norm=ml_g_norm, rw_mu=rw_mu, rw_w2=rw_w2, rw_w0=rw_w0, rw_a2=rw_a2,
               rw_a0=rw_a0, rw_k_k=rw_k_k, rw_k_a=rw_k_a, rw_r_k=rw_r_k, rw_g_norm=rw_g_norm)
    inp = {k: np.asarray(v, f32) for k, v in inp.items()}
    x, ctx = np.asarray(x, f32), np.asarray(ctx, f32)
    B, SEQ, _ = x.shape
    CTX = ctx.shape[1]
    L = w_ada.shape[0]
    DE = moe_w_gate.shape[-1]
    T = CTX + SEQ
    TT = B * T
    NCORE = 8
    NCc, NLl = CTX // 4, SEQ // 4
    NTK = NCc + NLl
    assert B == 2 and CTX % 512 == 0 or True
    cstv = make_consts()
    NCOL = 6 * D // NCORE
    ncA = _get(("A", L, NCOL), lambda: build_A(L, NCOL))
    cv = np.concatenate([np.asarray(c, f32), np.asarray(c_ctx, f32)[None]], 0)
    cT = np.ascontiguousarray(cv.T.reshape(KC, 128, 3).transpose(1, 0, 2))
    w_ada, b_ada = np.asarray(w_ada, f32), np.asarray(b_ada, f32)
    resA = _run(ncA, [{"cT": cT, "w": np.ascontiguousarray(w_ada[:, :, i * NCOL:(i + 1) * NCOL]),
                       "b": np.ascontiguousarray(b_ada[:, i * NCOL:(i + 1) * NCOL])} for i in range(NCORE)])
    modall = np.concatenate([r["out"] for r in resA], axis=2)
    mod = [modall[l] for l in range(L)]
    ncB = _get(("B", CTX, SEQ), lambda: build_B(CTX, SEQ))
    ncC1 = _get(("C1", NCc, NLl), lambda: build_C1(NCc, NLl))
    ncC2 = _get(("C2", TT, DE), lambda: build_C2(TT, DE))
    ncC3 = _get(("C3", NCc, NLl), lambda: build_C3(NCc, NLl, NCORE))
    xc, xl = ctx.copy(), x.copy()
    tok_idx = [np.concatenate([q * NCc + np.arange(NCc), CTX + q * NLl + np.arange(NLl)]) for q in range(4)]
    out = None
    for l in range(L):
        m = mod[l]
        sh1, sc1, gt1, sh2, sc2, gt2 = [m[:, i * D:(i + 1) * D] for i in range(6)]
        resB = _run(ncB, [prep_B(l, b, g, inp, mod, xc, xl) for b in range(B) for g in range(4)])
        yT = assemble_yT(resB, B, T)
        XT = [np.ascontiguousarray(np.concatenate([xc[b], xl[b]], 0).T) for b in range(B)]
        wr = np.ascontiguousarray(np.concatenate([np.asarray(moe_w_rg[l], f32), np.asarray(moe_w_re[l], f32)], 1))
        br = np.ascontiguousarray(np.broadcast_to(np.concatenate([np.asarray(moe_b_rg[l], f32), np.asarray(moe_b_re[l], f32)])[None], (128, 36)))
        wo = np.ascontiguousarray(np.asarray(w_out[l], f32))
        g2 = pk(np.asarray(g_norm2[l], f32))
        mapsC1 = []
        for b in range(B):
            for q in range(4):
                modv = np.stack([g2, pk(gt1[2]), pk(sc2[2]), pk(sh2[2]), pk(gt1[b]), pk(sc2[b]), pk(sh2[b])], axis=2).reshape(128, KC * 7)
                mapsC1.append({"yT": np.ascontiguousarray(yT[b][:, tok_idx[q]]), "xT": np.ascontiguousarray(XT[b][:, tok_idx[q]]),
                               "wo": wo, "modv": np.ascontiguousarray(modv), "wr": wr, "br": br, "cst": cstv})
        resC1 = _run(ncC1, mapsC1)
        h2all = np.ascontiguousarray(np.concatenate([r["h2T"] for r in resC1], axis=1))
        gall = np.concatenate([r["gates"] for r in resC1], axis=0)
        mapsC2 = [{"h2T": h2all, "gT": np.ascontiguousarray(gall[:, 4 * i:4 * i + 4].T),
                   "wg": np.ascontiguousarray(np.asarray(moe_w_gate[l][4 * i:4 * i + 4], f32)),
                   "wu": np.ascontiguousarray(np.asarray(moe_w_up[l][4 * i:4 * i + 4], f32)),
                   "wd": np.ascontiguousarray(np.asarray(moe_w_down[l][4 * i:4 * i + 4], f32))} for i in range(NCORE)]
        resC2 = _run(ncC2, mapsC2)
        gf = pk(np.asarray(g_final, f32))
        mapsC3 = []
        for b in range(B):
            for q in range(4):
                ci = b * 4 + q
                fTp = np.ascontiguousarray(np.stack([resC2[i]["fT"][:, ci * NTK:(ci + 1) * NTK] for i in range(NCORE)], 0))
                modv = np.stack([pk(gt2[2]), pk(gt2[b]), gf], axis=2).reshape(128, KC * 3)
                mapsC3.append({"fTp": fTp, "x1T": resC1[ci]["x1T"], "modv": np.ascontiguousarray(modv), "cst": cstv})
        resC3 = _run(ncC3, mapsC3)
        for b in range(B):
            X = XT[b]
            for q in range(4):
                X[:, tok_idx[q]] = resC3[b * 4 + q]["x2T"]
            xc[b] = X[:, :CTX].T
            xl[b] = X[:, CTX:].T
        if l == L - 1:
            out = np.zeros((B, SEQ, D), f32)
            for b in range(B):
                for q in range(4):
                    out[b, q * NLl:(q + 1) * NLl] = resC3[b * 4 + q]["onT"][:, NCc:].T
    return out
```

```python
import numpy as np
from contextlib import ExitStack
import concourse.bass as bass
import concourse.mybir as mybir
from concourse.bass_utils import run_bass_kernel_spmd

F32 = mybir.dt.float32
BF16 = mybir.dt.bfloat16
AF = mybir.ActivationFunctionType
ALU = mybir.AluOpType
AX = mybir.AxisListType

D = 2048
KC = 16
DEPTH = 4
GRID_W = 64
CH = 64
NEXP = 32
ENGS = ("sync", "scalar", "vector", "gpsimd", "tensor")
EPOCH = 6000
NDMA = 24
ARENA = 49000
FM_NOINC = False
RMS_NOINC = True


class _Rec:
    def __getattr__(self, name):
        def f(*a, **k):
            self.call = (name, a, k)
            return self
        return f


class Prog:
    def __init__(self, nc):
        self.nc = nc
        self.es = ExitStack()
        self.streams = {e: [] for e in ENGS}
        self.count = {e: 0 for e in ENGS}
        self.esems = {e: [] for e in ENGS}
        self.dsems = [self.es.enter_context(nc.semaphore(f"dq{i}")) for i in range(NDMA)]
        self.dma_n = 0
        self.dma_tok = [None] * NDMA
        self.lastw = {}
        self.readers = {}
        self.seen = {e: {} for e in ENGS}
        self.final = []
        self.nbuf = 0
        self.last_tok = {}
        self.arena = self.sbuf([128, ARENA], F32, "arena")
        self.aptr = 0
        self.banks = [self.psum([128, 512], F32, f"bank{i}") for i in range(8)]

    def sbuf(self, shape, dtype=F32, name=None):
        self.nbuf += 1
        return self.es.enter_context(self.nc.sbuf_tensor(name or f"sb{self.nbuf}", list(shape), dtype))

    def psum(self, shape, dtype=F32, name=None):
        self.nbuf += 1
        return self.es.enter_context(self.nc.psum_tensor(name or f"ps{self.nbuf}", list(shape), dtype))

    def dram(self, name, shape, dtype=F32, kind="Internal"):
        return self.nc.dram_tensor(name, list(shape), dtype, kind=kind).ap()

    def alloc(self, n, dtype=F32):
        w = n if dtype == F32 else (n + 1) // 2
        assert self.aptr + w <= ARENA, ("arena overflow", self.aptr, w)
        ap = self.arena[:, self.aptr:self.aptr + w]
        self.aptr += w
        if dtype != F32:
            ap = ap.bitcast(dtype)[:, 0:n]
        return ap

    def _esem(self, eng, epoch):
        lst = self.esems[eng]
        while len(lst) <= epoch:
            lst.append(self.es.enter_context(self.nc.semaphore(f"p_{eng}_{len(lst)}")))
        return lst[epoch]

    def _need(self, eng, tok, waits):
        if tok is None:
            return
        sem, val, key = tok
        if self.seen[eng].get(key, 0) >= val:
            return
        if key[0] == "e" and key[1] == eng and key[2] * EPOCH + val > self.count[eng]:
            return
        self.seen[eng][key] = val
        waits.append((sem, val))

    def op(self, eng, fn, reads=(), writes=(), dma=False, out_final=False, inc=True):
        rec = _Rec()
        fn(rec)
        fn = rec.call
        waits = []
        for k in reads:
            self._need(eng, self.lastw.get(k), waits)
        for k in writes:
            self._need(eng, self.lastw.get(k), waits)
            for t in self.readers.get(k, {}).values():
                self._need(eng, t, waits)
        if dma:
            slot = self.dma_n % NDMA
            val = 16 * (self.dma_n // NDMA + 1)
            self._need(eng, self.dma_tok[slot], waits)
            tok = (self.dsems[slot], val, ("d", slot))
            self.dma_tok[slot] = tok
            self.dma_n += 1
            inc = (self.dsems[slot], 16)
            if out_final:
                self.final.append(tok)
            rkey = ("d", self.dma_n)
        else:
            n = self.count[eng]
            epoch, idx = divmod(n, EPOCH)
            sem = self._esem(eng, epoch)
            tok = (sem, idx + 1, ("e", eng, epoch))
            if inc:
                self.count[eng] = n + 1
                inc = (sem, 1)
                self.last_tok[eng] = tok
            else:
                inc = None
            rkey = eng
        self.streams[eng].append((waits, fn, inc))
        for k in reads:
            self.readers.setdefault(k, {})[rkey] = tok
        for k in writes:
            self.lastw[k] = tok
            self.readers[k] = {}
        return tok

    def barrier(self):
        toks = [t for t in self.last_tok.values()] + [t for t in self.dma_tok if t is not None]
        for e in ENGS:
            waits = []
            for t in toks:
                self._need(e, t, waits)
            if waits:
                self.streams[e].append((waits, None, None))
        self.lastw = {}
        self.readers = {}

    def V(self, fn, r=(), w=()):
        return self.op("vector", fn, r, w)

    def S(self, fn, r=(), w=()):
        return self.op("scalar", fn, r, w)

    def G(self, fn, r=(), w=()):
        return self.op("gpsimd", fn, r, w)

    def T(self, fn, r=(), w=(), inc=True):
        return self.op("tensor", fn, r, w, inc=inc)

    def dma(self, out, in_, r=(), w=(), eng="sync", final=False):
        return self.op(eng, lambda e: e.dma_start(out=out, in_=in_), r, w, dma=True, out_final=final)

    def emit(self):
        nc = self.nc
        fin = [(t[0], t[1]) for t in self.final]
        streams = self.streams
        with nc.Block() as block:
            def run(engname):
                def body(e):
                    for waits, fn, inc in streams[engname]:
                        for s, v in waits:
                            e.wait_ge(s, v)
                        if fn is not None:
                            ins = getattr(e, fn[0])(*fn[1], **fn[2])
                            if inc is not None:
                                ins.then_inc(inc[0], inc[1])
                    if engname == "sync":
                        for s, v in fin:
                            e.wait_ge(s, v)
                return body
            block.sync(run("sync"))
            block.scalar(run("scalar"))
            block.vector(run("vector"))
            block.gpsimd(run("gpsimd"))
            block.tensor(run("tensor"))

    def close(self):
        self.es.close()


def _run(nc, in_maps):
    res = run_bass_kernel_spmd(nc, in_maps, core_ids=list(range(len(in_maps))))
    return res.results


def pk(v):
    return np.ascontiguousarray(np.asarray(v, np.float32).reshape(KC, 128).T)


def build_A(L, NCOL):
    nc = bass.Bass("TRN2", target_bir_lowering=False)
    cT = nc.dram_tensor("cT", [128, KC, 3], F32, kind="ExternalInput").ap()
    w = nc.dram_tensor("w", [L, D, NCOL], F32, kind="ExternalInput").ap()
    b = nc.dram_tensor("b", [L, NCOL], F32, kind="ExternalInput").ap()
    out = nc.dram_tensor("out", [L, 3, NCOL], F32, kind="ExternalOutput").ap()
    P = Prog(nc)
    s = P.alloc(KC * 3).rearrange("p (k r) -> p k r", r=3)
    P.dma(s, cT, w=["s"])
    P.S(lambda e: e.activation(out=s, in_=s, func=AF.Silu), ["s"], ["s"])
    wb = [P.alloc(KC * 512).rearrange("p (k n) -> p k n", n=512) for _ in range(2)]
    bb = [P.alloc(512) for _ in range(2)]
    ob = [P.alloc(512) for _ in range(2)]
    it = 0
    for l in range(L):
        for c0 in range(0, NCOL, 512):
            n = min(512, NCOL - c0)
            i = it % 2
            it += 1
            P.dma(wb[i][:, :, 0:n], w[l, :, c0:c0 + n].rearrange("(k p) n -> p k n", p=128), w=[("wb", i)])
            P.dma(bb[i][0:3, 0:n], b[l:l + 1, c0:c0 + n].partition_broadcast(3), w=[("bb", i)], eng="gpsimd")
            ps = P.banks[i]
            for k in range(KC):
                P.T(lambda e, k=k, i=i, n=n, ps=ps: e.matmul(ps[0:3, 0:n], lhsT=s[:, k, :], rhs=wb[i][:, k, 0:n],
                                                              start=(k == 0), stop=(k == KC - 1)),
                    ["s", ("wb", i)], [("ps", i)], inc=(k == KC - 1))
            P.V(lambda e, i=i, n=n, ps=ps: e.tensor_tensor(out=ob[i][0:3, 0:n], in0=ps[0:3, 0:n], in1=bb[i][0:3, 0:n], op=ALU.add),
                [("ps", i), ("bb", i)], [("ob", i)])
            P.dma(out[l, :, c0:c0 + n], ob[i][0:3, 0:n], r=[("ob", i)], eng="gpsimd", final=True)
    P.emit()
    P.close()
    return nc


def rms_rstd(P, xt, n, ones, msbank, sq, rstd, keys_x, key_out, tag):
    P.S(lambda e: e.activation(out=sq[:, :, 0:n], in_=xt[:, :, 0:n], func=AF.Square), keys_x, [("sq", tag)])
    for k in range(KC):
        P.T(lambda e, k=k: e.matmul(msbank[:, 0:n], lhsT=ones, rhs=sq[:, k, 0:n], start=(k == 0), stop=(k == KC - 1)),
            [("sq", tag), "consts"], [("msb", tag)], inc=(k == KC - 1) or not RMS_NOINC)
    P.S(lambda e: e.activation(out=rstd[:, 0:n], in_=msbank[:, 0:n], func=AF.Ln, scale=1.0 / D, bias=1e-6),
        [("msb", tag)], [key_out])
    P.S(lambda e: e.activation(out=rstd[:, 0:n], in_=rstd[:, 0:n], func=AF.Exp, scale=-0.5), [key_out], [key_out])


MSLOT = [(0, 1), (2, 3), (4, 4), (5, 5)]
FMA = (["GQ", "GK", "GLR", "GG"] + [f"{a}{s}" for s in range(2) for a in ("MQ", "MK", "MI", "MF", "MO")]
       + [f"{a}{j}" for j in range(3) for a in ("RR", "RK", "RV")] + ["RG01", "RG2", "W1F", "W1B", "A1F", "A1B"])
WBLK = ([("gq", 64), ("gk", 64), ("glrf", 16), ("glrb", 16), ("gg", 128)]
        + [(f"{a}{s}", n) for s in range(2) for a, n in (("mq", 64), ("mk", 64), ("mo", 128), ("mif", 64), ("mff", 64), ("mib", 64), ("mfb", 64))]
        + [(f"{a}{j}", 64) for j in range(3) for a in ("rr", "rk", "rv", "rg")]
        + [("w1f", 96), ("w1b", 96), ("a1f", 96), ("a1b", 96)])
WOFF = {}
_o = 0
for _n, _c in WBLK:
    WOFF[_n] = (_o, _c)
    _o += _c
NFMC = _o
JOBS = {"GQ": [(0, "gq", 0), (64, "gq", 1)], "GK": [(0, "gk", 0), (64, "gk", 1)],
        "GLR": [(0, "glrf", 0), (64, "glrb", 1)], "GG": [(0, "gg", 0)],
        "RG01": [(0, "rg0", 0), (64, "rg1", 0)], "RG2": [(0, "rg2", 0)],
        "W1F": [(0, "w1f", 0)], "W1B": [(0, "w1b", 1)], "A1F": [(0, "a1f", 0)], "A1B": [(0, "a1b", 1)]}
for _s in range(2):
    JOBS[f"MQ{_s}"] = [(0, f"mq{_s}", 0), (64, f"mq{_s}", 1)]
    JOBS[f"MK{_s}"] = [(0, f"mk{_s}", 0), (64, f"mk{_s}", 1)]
    JOBS[f"MI{_s}"] = [(0, f"mif{_s}", 0), (64, f"mib{_s}", 1)]
    JOBS[f"MF{_s}"] = [(0, f"mff{_s}", 0), (64, f"mfb{_s}", 1)]
    JOBS[f"MO{_s}"] = [(0, f"mo{_s}", 0)]
for _j in range(3):
    for _a in ("rr", "rk", "rv"):
        JOBS[f"{_a.upper()}{_j}"] = [(0, f"{_a}{_j}", 0), (64, f"{_a}{_j}", 1)]
NTMC = 384
PPN = {}
_o = 0
for _n, _c in ([("gba", 1), ("ggn", 1)]
               + [(f"{a}{s}", n) for s in range(2) for a, n in (("cwq", 9), ("cwk", 9), ("cbq", 1), ("cbk", 1), ("mbi", 1), ("mbf", 1), ("mgn", 1))]
               + [(f"{a}{j}", n) for j in range(3) for a, n in (("mur", 2), ("muk", 2), ("muv", 2), ("w0", 1), ("a0", 1), ("kk", 1), ("ka", 1), ("rk", 1), ("rgn", 1))]
               + [("mug01", 2), ("mug2", 2), ("muw1f", 2), ("muw1b", 2), ("mua1f", 2), ("mua1b", 2)]):
    PPN[_n] = (_o, _c)
    _o += _c
NPP = _o
CSTN = {}
_o = 0
for _n, _c in [("ident", 128), ("ones", 128), ("J", 128), ("mtri", 128), ("bones", 128), ("o128", 128),
               ("MS", 256), ("MI", 256), ("ML", 256), ("I2", 256), ("bm128", 256), ("bm129", 258), ("bm64", 128)]:
    CSTN[_n] = (_o, _c)
    _o += _c
NCST = _o


def make_consts():
    c = np.zeros((128, NCST), np.float32)

    def put(n, a):
        o, w = CSTN[n]
        c[:, o:o + w] = a
    idx = np.arange(128)
    put("ident", np.eye(128))
    put("ones", np.ones((128, 128)))
    put("J", np.eye(128)[::-1])
    put("mtri", ((idx[:, None] % 64) <= (idx[None, :] % 64)).astype(np.float32))
    same = (idx[:, None] // 64) == (idx[None, :] // 64)
    put("bones", same.astype(np.float32))
    put("o128", np.full((128, 128), 1.0 / 128))
    ms = (same & (idx[:, None] < idx[None, :])).astype(np.float32)
    mi = (same & (idx[:, None] <= idx[None, :])).astype(np.float32)
    put("MS", np.concatenate([ms, ms], 1))
    put("MI", np.concatenate([mi, mi], 1))
    put("ML", np.concatenate([ms.T, ms.T], 1))
    put("I2", np.concatenate([np.eye(128), np.eye(128)], 1))
    for n, dv in (("bm128", 128), ("bm129", 129), ("bm64", 64)):
        m = np.zeros((128, 2 * dv))
        m[:64, :dv] = 1
        m[64:, dv:] = 1
        put(n, m)
    return c


def build_B(CTX, SEQ):
    T = CTX + SEQ
    NT = T // 128
    NCHK = T // 64
    SEGS = [(0, CTX), (CTX, SEQ)]
    TW = 256
    NA = len(FMA)
    nc = bass.Bass("TRN2", target_bir_lowering=False)

    def din(name, shape):
        return nc.dram_tensor(name, list(shape), F32, kind="ExternalInput").ap()
    xT = din("xT", [D, T])
    modv_d = din("modv", [128, KC * 5])
    wfm = din("wfm", [D, NFMC])
    wtm = din("wtm", [D, NTMC])
    cst_d = din("cst", [128, NCST])
    pp_d = din("pp", [128, NPP])
    wa2_d = din("wa2", [128, 64])
    w2_d = din("w2", [96, 3 * 2 * 2 * 64])
    yT = nc.dram_tensor("yT", [576, T], F32, kind="ExternalOutput").ap()
    P = Prog(nc)
    FMS = P.dram("fms", [NA, 128, T])
    TMS = P.dram("tms", [2, T, NTMC])
    AI = {a: i for i, a in enumerate(FMA)}

    cst = P.sbuf([128, NCST], F32, "cst_sb")
    pp = P.sbuf([128, NPP], F32, "pp_sb")
    mv = P.sbuf([128, KC * 5], F32, "mv_sb")
    wa2 = P.sbuf([128, 64], F32, "wa2_sb")
    w2 = P.sbuf([96, 768], F32, "w2_sb")
    P.dma(cst[:], cst_d, w=["consts"])
    P.dma(pp[:], pp_d, w=["consts"])
    P.dma(mv[:], modv_d, w=["consts"])
    P.dma(wa2[:], wa2_d, w=["consts"])
    P.dma(w2[:], w2_d, w=["consts"])
    w2v = w2[:].rearrange("p (j w d c) -> p j w d c", j=3, w=2, d=2)
    mvv = mv[:].rearrange("p (k j) -> p k j", j=5)

    def C(n, r0=0, r1=128, c0=None, c1=None):
        o, w = CSTN[n]
        a = o if c0 is None else o + c0
        b = o + w if c1 is None else o + c1
        return cst[r0:r1, a:b]

    def PPc(n, j=0, w=1, r0=0, r1=128):
        o, _ = PPN[n]
        return pp[r0:r1, o + j:o + j + w]
    ident, ones, Jm = C("ident"), C("ones"), C("J")
    ones_b = cst[:, CSTN["ones"][0]:CSTN["ones"][0] + 1]

    gm = P.sbuf([128, KC * 2], F32, "gm_sb")
    gmv = gm[:].rearrange("p (k j) -> p k j", j=2)
    for j, col in enumerate((1, 3)):
        P.V(lambda e, j=j, col=col: e.tensor_scalar(out=gmv[:, :, j], in0=mvv[:, :, col], scalar1=1.0, scalar2=1.0, op0=ALU.add, op1=ALU.mult),
            ["consts"], ["gm"])
        P.V(lambda e, j=j: e.tensor_tensor(out=gmv[:, :, j], in0=gmv[:, :, j], in1=mvv[:, :, 0], op=ALU.mult), ["gm", "consts"], ["gm"])
    mark0 = P.aptr

    wsb = P.alloc(KC * NFMC, BF16).rearrange("p (k n) -> p k n", n=NFMC)
    wtsb = P.alloc(KC * NTMC, BF16).rearrange("p (k n) -> p k n", n=NTMC)
    for c0 in range(0, NFMC, 512):
        c1 = min(NFMC, c0 + 512)
        P.dma(wsb[:, :, c0:c1], wfm[:, c0:c1].rearrange("(k p) n -> p k n", p=128), w=["wsb"], eng="gpsimd")
    P.dma(wtsb[:, :, :], wtm.rearrange("(k p) n -> p k n", p=128), w=["wsb"], eng="gpsimd")
    xb = [P.alloc(KC * TW).rearrange("p (k n) -> p k n", n=TW) for _ in range(2)]
    sq = P.alloc(KC * TW).rearrange("p (k n) -> p k n", n=TW)
    rstd = P.alloc(TW)
    hT = [P.alloc(KC * TW, BF16).rearrange("p (k n) -> p k n", n=TW) for _ in range(2)]
    hR = [P.alloc(KC * TW, BF16).rearrange("p (k n) -> p k n", n=TW) for _ in range(2)]
    stg = [P.alloc(TW) for _ in range(4)]
    tstg = [P.alloc(NTMC) for _ in range(2)]
    tiles = [(s0, L, o, min(TW, L - o)) for (s0, L) in SEGS for o in range(0, L, TW)]
    cnt = 0
    tcn = 0
    for ti, (s0, L, o, n) in enumerate(tiles):
        b = ti % 2
        t0, tm = s0 + o, s0 + L - o - n
        sg = 0 if s0 == 0 else 1
        xt = xb[b]
        P.dma(xt[:, :, 0:n], xT[:, t0:t0 + n].rearrange("(k p) t -> p k t", p=128), w=[("xt", b)])
        rms_rstd(P, xt, n, ones, P.banks[0], sq, rstd, [("xt", b)], "rstd", 0)
        P.V(lambda e, xt=xt, n=n: e.tensor_tensor(out=sq[:, :, 0:n], in0=xt[:, :, 0:n],
                                                   in1=rstd[:, 0:n].unsqueeze(1).to_broadcast([128, KC, n]), op=ALU.mult),
            [("xt", b), "rstd", ("sq", 0)], [("sq", 0)])
        for k in range(KC):
            P.V(lambda e, k=k, b=b, n=n, sg=sg: e.tensor_scalar(out=hT[b][:, k, 0:n], in0=sq[:, k, 0:n], scalar1=gmv[:, k, sg:sg + 1],
                                                                 scalar2=mvv[:, k, 2 + 2 * sg:3 + 2 * sg], op0=ALU.mult, op1=ALU.add),
                [("sq", 0), "gm", "consts"], [("h", b, 0)])
        P.V(lambda e, b=b, n=n: e.tensor_copy(out=hR[b][:, :, 0:n][:, :, ::-1], in_=hT[b][:, :, 0:n]), [("h", b, 0)], [("h", b, 1)])
        hh = (hT[b], hR[b])
        for ai, a in enumerate(FMA):
            bi = 1 + (cnt % 4)
            cnt += 1
            bank = P.banks[bi]
            for (pb, blk, src) in JOBS[a]:
                c0, nr = WOFF[blk]
                for k in range(KC):
                    P.T(lambda e, k=k, pb=pb, nr=nr, c0=c0, src=src, bank=bank, n=n: e.matmul(
                        bank[pb:pb + nr, 0:n], lhsT=wsb[:, k, c0:c0 + nr], rhs=hh[src][:, k, 0:n], start=(k == 0), stop=(k == KC - 1)),
                        ["wsb", ("h", b, src)], [("bank", bi, pb)], inc=(k == KC - 1) or not FM_NOINC)
                si = cnt % 4 if False else (cnt + pb // 64) % 4
                st = stg[si]
                if (cnt + pb) % 2 == 0:
                    P.S(lambda e, st=st, pb=pb, nr=nr, bank=bank, n=n: e.copy(out=st[pb:pb + nr, 0:n], in_=bank[pb:pb + nr, 0:n]),
                        [("bank", bi, pb)], [("stg", si, pb)])
                else:
                    P.V(lambda e, st=st, pb=pb, nr=nr, bank=bank, n=n: e.tensor_copy(out=st[pb:pb + nr, 0:n], in_=bank[pb:pb + nr, 0:n]),
                        [("bank", bi, pb)], [("stg", si, pb)])
                col = t0 if src == 0 else tm
                P.dma(FMS[ai, pb:pb + nr, col:col + n], st[pb:pb + nr, 0:n], r=[("stg", si, pb)], eng="gpsimd")
        for j in range(n // 128):
            for src in (0, 1):
                bi = 5 + (tcn % 2)
                bank = P.banks[bi]
                st = tstg[tcn % 2]
                for k in range(KC):
                    P.T(lambda e, k=k, j=j, src=src, bank=bank: e.matmul(bank[:, 0:NTMC], lhsT=hh[src][:, k, j * 128:(j + 1) * 128],
                                                                         rhs=wtsb[:, k, :], start=(k == 0), stop=(k == KC - 1)),
                        ["wsb", ("h", b, src)], [("bank", bi)])
                P.S(lambda e, st=st, bank=bank: e.copy(out=st[:, 0:NTMC], in_=bank[:, 0:NTMC]), [("bank", bi)], [("tstg", tcn % 2)])
                row0 = (t0 if src == 0 else tm) + 128 * j
                P.dma(TMS[src, row0:row0 + 128, :], st[:, 0:NTMC], r=[("tstg", tcn % 2)], eng="gpsimd")
                tcn += 1
    P.barrier()

    def v3(ap, w=64):
        return ap.rearrange("p (c k) -> p c k", k=w)

    def chunk_local(EX, OFF):
        P.G(lambda e: e.memset(OFF[:, 0:1], 0.0), [], ["OFF"])
        P.V(lambda e: e.tensor_copy(out=OFF[:, 1:NCHK], in_=v3(EX)[:, 0:NCHK - 1, 63]), ["EX"], ["OFF"])
        P.V(lambda e: e.tensor_tensor(out=v3(EX), in0=v3(EX), in1=OFF[:, 0:NCHK].unsqueeze(2).to_broadcast([128, NCHK, 64]),
                                      op=ALU.subtract), ["EX", "OFF"], ["EX"])

    def headnorm(src, n, R, center, eps, gain, gate, bonus, Y, SQ, RS, bankm, tag):
        on = ones[0:R, 0:R]
        P.S(lambda e: e.copy(out=Y[0:R, 0:n], in_=src), [tag + "src"], [tag + "Y"])
        if center:
            P.T(lambda e: e.matmul(bankm[0:R, 0:n], lhsT=on, rhs=Y[0:R, 0:n], start=True, stop=True), [tag + "Y", "consts"], [tag + "bm"])
            P.V(lambda e: e.scalar_tensor_tensor(out=Y[0:R, 0:n], in0=bankm[0:R, 0:n], scalar=-1.0 / R, in1=Y[0:R, 0:n],
                                                 op0=ALU.mult, op1=ALU.add), [tag + "bm", tag + "Y"], [tag + "Y"])
        P.S(lambda e: e.activation(out=SQ[0:R, 0:n], in_=Y[0:R, 0:n], func=AF.Square), [tag + "Y"], [tag + "SQ"])
        P.T(lambda e: e.matmul(bankm[0:R, 0:n], lhsT=on, rhs=SQ[0:R, 0:n], start=True, stop=True), [tag + "SQ", "consts"], [tag + "bm"])
        P.S(lambda e: e.activation(out=RS[0:R, 0:n], in_=bankm[0:R, 0:n], func=AF.Ln, scale=1.0 / R, bias=eps), [tag + "bm"], [tag + "RS"])
        P.S(lambda e: e.activation(out=RS[0:R, 0:n], in_=RS[0:R, 0:n], func=AF.Exp, scale=-0.5), [tag + "RS"], [tag + "RS"])
        P.V(lambda e: e.tensor_tensor(out=Y[0:R, 0:n], in0=Y[0:R, 0:n], in1=RS[0:R, 0:n], op=ALU.mult), [tag + "Y", tag + "RS"], [tag + "Y"])
        if bonus is None:
            P.V(lambda e: e.scalar_tensor_tensor(out=Y[0:R, 0:n], in0=Y[0:R, 0:n], scalar=gain, in1=gate, op0=ALU.mult, op1=ALU.mult),
                [tag + "Y", tag + "gate", "consts"], [tag + "Y"])
        else:
            P.V(lambda e: e.scalar_tensor_tensor(out=Y[0:R, 0:n], in0=Y[0:R, 0:n], scalar=gain, in1=bonus, op0=ALU.mult, op1=ALU.add),
                [tag + "Y", tag + "bonus", "consts"], [tag + "Y"])
            P.V(lambda e: e.tensor_tensor(out=Y[0:R, 0:n], in0=Y[0:R, 0:n], in1=gate, op=ALU.mult), [tag + "Y", tag + "gate"], [tag + "Y"])

    def groups():
        for (s0, L) in SEGS:
            for g0 in range(0, L, 512):
                yield s0, L, g0, min(512, L - g0)

    PADW = 65
    for s in range(2):
        for arr, cwn, cbn in ((f"MQ{s}", f"cwq{s}", f"cbq{s}"), (f"MK{s}", f"cwk{s}", f"cbk{s}")):
            P.aptr = mark0
            oc, ol = PADW, PADW + CTX + PADW
            X0 = P.alloc(T + 3 * PADW)
            XA = P.alloc(SEQ + 2 * PADW)
            XB = P.alloc(SEQ + 2 * PADW)
            ACC = P.alloc(T)
            P.G(lambda e: e.memset(X0, 0.0), [], ["X0"])
            P.G(lambda e: e.memset(XA, 0.0), [], ["XA"])
            P.G(lambda e: e.memset(XB, 0.0), [], ["XB"])
            P.dma(X0[:, oc:oc + CTX], FMS[AI[arr], :, 0:CTX], w=["X0"])
            P.dma(X0[:, ol:ol + SEQ], FMS[AI[arr], :, CTX:T], w=["X0"])
            P.S(lambda e: e.copy(out=XA[:, PADW:PADW + SEQ], in_=X0[:, ol:ol + SEQ]), ["X0"], ["XA"])
            P.G(lambda e: e.tensor_copy(out=XB[:, PADW:PADW + SEQ], in_=X0[:, ol:ol + SEQ]), ["X0"], ["XB"])
            P.S(lambda e: e.memzero(v3(XA[:, PADW:PADW + SEQ])[:, :, 63]) if False else e.activation(
                out=v3(XA[:, PADW:PADW + SEQ])[:, :, 63], in_=v3(XA[:, PADW:PADW + SEQ])[:, :, 63], func=AF.Copy, scale=0.0), ["XA"], ["XA"])
            P.G(lambda e: e.memset(v3(XB[:, PADW:PADW + SEQ])[:, :, 0], 0.0), ["XB"], ["XB"])
            cw = lambda i, j: PPc(cwn, 3 * i + j)
            cb = PPc(cbn)
            P.V(lambda e: e.tensor_scalar(out=ACC[:, 0:CTX], in0=X0[:, oc:oc + CTX], scalar1=cw(1, 1), scalar2=cb, op0=ALU.mult, op1=ALU.add),
                ["X0", "consts"], ["ACC"])
            for j in (0, 2):
                P.V(lambda e, j=j: e.scalar_tensor_tensor(out=ACC[:, 0:CTX], in0=X0[:, oc + j - 1:oc + j - 1 + CTX], scalar=cw(1, j),
                                                          in1=ACC[:, 0:CTX], op0=ALU.mult, op1=ALU.add), ["X0", "ACC", "consts"], ["ACC"])
            P.V(lambda e: e.tensor_scalar(out=ACC[:, CTX:T], in0=X0[:, ol:ol + SEQ], scalar1=cw(1, 1), scalar2=cb, op0=ALU.mult, op1=ALU.add),
                ["X0", "consts"], ["ACC"])
            for i in range(3):
                for j in range(3):
                    if i == 1 and j == 1:
                        continue
                    off = 64 * (i - 1) + (j - 1)
                    if j == 1:
                        srcv = X0[:, ol + off:ol + off + SEQ]
                    elif j == 0:
                        srcv = XA[:, PADW + off:PADW + off + SEQ]
                    else:
                        srcv = XB[:, PADW + off:PADW + off + SEQ]
                    P.V(lambda e, srcv=srcv, i=i, j=j: e.scalar_tensor_tensor(out=ACC[:, CTX:T], in0=srcv, scalar=cw(i, j), in1=ACC[:, CTX:T],
                                                                              op0=ALU.mult, op1=ALU.add), ["X0", "XA", "XB", "ACC", "consts"], ["ACC"])
            P.S(lambda e: e.activation(out=ACC, in_=ACC, func=AF.Silu), ["ACC"], ["ACC"])
            P.dma(FMS[AI[arr], :, :], ACC, r=["ACC"], eng="gpsimd")
            P.barrier()

    def glalike(kind, s):
        P.aptr = mark0
        dv = 128 if kind == "g" else 129
        QS, KS, LA, EX = P.alloc(T), P.alloc(T), P.alloc(T), P.alloc(T)
        IG = P.alloc(T) if kind == "m" else None
        V2f = P.alloc(NT * 2 * dv)
        V2 = V2f.rearrange("p (i d v) -> p i d v", d=2, v=dv)
        Of = P.alloc(NT * 256)
        O = Of.rearrange("p (i d v) -> p i d v", d=2, v=128)
        OFF, EB = P.alloc(NCHK), P.alloc(NCHK)
        Sb, tmpS = P.alloc(2 * dv), P.alloc(2 * dv)
        nb, rec = P.alloc(1), P.alloc(2)
        att = [P.alloc(128) for _ in range(2)]
        kws = [P.alloc(128) for _ in range(2)]
        Yb, SQb, RSb = P.alloc(512), P.alloc(512), P.alloc(512)
        bm = C("bm128") if kind == "g" else C("bm129")
        if kind == "g":
            P.dma(QS, FMS[AI["GQ"]], w=["QS"])
            P.dma(KS, FMS[AI["GK"]], w=["KS"])
            P.dma(EX, FMS[AI["GLR"]], w=["EX"])
            P.V(lambda e: e.tensor_scalar(out=nb, in0=PPc("gba"), scalar1=-1.0, scalar2=0.0, op0=ALU.mult, op1=ALU.add), ["consts"], ["nb"])
            for t0 in range(0, T, 512):
                n = min(512, T - t0)
                bk = P.banks[(t0 // 512) % 2]
                for d in range(2):
                    P.T(lambda e, d=d, t0=t0, n=n, bk=bk: e.matmul(bk[64 * d:64 * d + 64, 0:n], lhsT=wa2[64 * d:64 * d + 16, :],
                                                                  rhs=EX[64 * d:64 * d + 16, t0:t0 + n], start=True, stop=True),
                        ["EX", "consts"], [("bk", (t0 // 512) % 2)])
                P.S(lambda e, t0=t0, n=n, bk=bk: e.activation(out=LA[:, t0:t0 + n], in_=bk[:, 0:n], func=AF.Exp, scale=-1.0, bias=nb),
                    [("bk", (t0 // 512) % 2), "nb"], ["LA"])
            sc = 1.0 / 16.0
            gname, gfn, gain, row0, center = "GG", AF.Silu, PPc("ggn"), 0, False
        else:
            P.dma(QS, FMS[AI[f"MQ{s}"]], w=["QS"])
            P.dma(KS, FMS[AI[f"MK{s}"]], w=["KS"])
            P.dma(LA, FMS[AI[f"MF{s}"]], w=["LA"])
            P.dma(IG, FMS[AI[f"MI{s}"]], w=["IG"])
            P.V(lambda e: e.tensor_scalar(out=nb, in0=PPc(f"mbf{s}"), scalar1=-1.0, scalar2=0.0, op0=ALU.mult, op1=ALU.add), ["consts"], ["nb"])
            P.S(lambda e: e.activation(out=LA, in_=LA, func=AF.Exp, scale=-1.0, bias=nb), ["LA", "nb"], ["LA"])
            sc = 1.0
            gname, gfn, gain, row0, center = f"MO{s}", AF.Sigmoid, PPc(f"mgn{s}"), 128 + 128 * s, True
        P.S(lambda e: e.activation(out=LA, in_=LA, func=AF.Ln, bias=1.0), ["LA"], ["LA"])
        P.V(lambda e: e.tensor_tensor_scan(out=EX, data0=ones_b.to_broadcast([128, T]), data1=LA, initial=0.0, op0=ALU.mult, op1=ALU.add),
            ["LA", "consts", "EX"], ["EX"])
        chunk_local(EX, OFF)
        P.S(lambda e: e.activation(out=LA, in_=EX, func=AF.Exp, scale=-sc), ["EX"], ["LA"])
        P.V(lambda e: e.tensor_copy(out=EB[:, 0:NCHK], in_=v3(LA)[:, :, 63]), ["LA"], ["EB"])
        P.V(lambda e: e.scalar_tensor_tensor(out=QS, in0=QS, scalar=0.125, in1=LA, op0=ALU.mult, op1=ALU.mult), ["QS", "LA"], ["QS"])
        if kind == "g":
            P.S(lambda e: e.activation(out=EX, in_=EX, func=AF.Exp, scale=sc), ["EX"], ["EX"])
        else:
            P.V(lambda e: e.scalar_tensor_tensor(out=EX, in0=EX, scalar=sc, in1=IG, op0=ALU.mult, op1=ALU.add), ["EX", "IG"], ["EX"])
            P.S(lambda e: e.activation(out=EX, in_=EX, func=AF.Exp, bias=PPc(f"mbi{s}")), ["EX", "consts"], ["EX"])
        P.V(lambda e: e.tensor_tensor(out=KS, in0=KS, in1=EX, op=ALU.mult), ["KS", "EX"], ["KS"])
        tcol = {"g": 0, "m": 128 + 128 * s}[kind]
        for d in range(2):
            P.dma(V2[:, :, d, 0:128], TMS[d, :, tcol:tcol + 128].rearrange("(i p) c -> p i c", p=128), w=["V2"])
        if kind == "m":
            P.G(lambda e: e.memset(V2[:, :, :, 128:129], 1.0), ["V2"], ["V2"])
        P.G(lambda e: e.memset(Sb, 0.0), [], ["S"])
        for c in range(NCHK):
            i, h = divmod(c, 2)
            pb = 64 * h
            cs = slice(c * 64, c * 64 + 64)
            z = c % 2
            ab, ob, kb, sbk = P.banks[z], P.banks[2 + z], P.banks[4 + z], P.banks[6 + z]
            for d in range(2):
                P.T(lambda e, d=d, ab=ab, pb=pb, cs=cs: e.matmul(ab[pb:pb + 64, d * 64:(d + 1) * 64], lhsT=KS[64 * d:64 * d + 64, cs],
                                                                 rhs=QS[64 * d:64 * d + 64, cs], start=True, stop=True), ["KS", "QS"], [("ab", z)])
            P.V(lambda e, ab=ab, pb=pb, z=z: e.tensor_tensor(out=att[z][pb:pb + 64, :], in0=ab[pb:pb + 64, 0:128], in1=C("mtri", pb, pb + 64),
                                                             op=ALU.mult), [("ab", z), "consts"], [("att", z)])
            P.T(lambda e, ob=ob, pb=pb, cs=cs: e.matmul(ob[pb:pb + 64, 0:2 * dv], lhsT=QS[:, cs], rhs=Sb[:, 0:2 * dv], start=True, stop=False),
                ["QS", "S"], [("ob", z)])
            for d in range(2):
                P.T(lambda e, d=d, ob=ob, pb=pb, i=i, z=z: e.matmul(ob[pb:pb + 64, d * dv:(d + 1) * dv], lhsT=att[z][pb:pb + 64, d * 64:(d + 1) * 64],
                                                                    rhs=V2[pb:pb + 64, i, d, :], start=False, stop=(d == 1)),
                    [("att", z), "V2"], [("ob", z)])
            if kind == "g":
                P.S(lambda e, ob=ob, pb=pb, i=i: e.copy(out=O[pb:pb + 64, i, :, :], in_=ob[pb:pb + 64, 0:256].rearrange("p (d v) -> p d v", d=2)),
                    [("ob", z)], ["O"])
            else:
                P.S(lambda e, ob=ob, pb=pb: e.activation(out=rec[pb:pb + 64, 0:2], in_=ob[pb:pb + 64, 0:258].rearrange("p (d v) -> p d v", d=2)[:, :, 128],
                                                         func=AF.Abs), [("ob", z)], ["rec"])
                P.V(lambda e, pb=pb: e.tensor_scalar(out=rec[pb:pb + 64, 0:2], in0=rec[pb:pb + 64, 0:2], scalar1=1.0, scalar2=1.0, op0=ALU.max, op1=ALU.mult),
                    ["rec"], ["rec"])
                P.V(lambda e, pb=pb: e.reciprocal(out=rec[pb:pb + 64, 0:2], in_=rec[pb:pb + 64, 0:2]), ["rec"], ["rec"])
                for d in range(2):
                    P.V(lambda e, d=d, ob=ob, pb=pb, i=i: e.tensor_scalar_mul(out=O[pb:pb + 64, i, d, :], in0=ob[pb:pb + 64, d * dv:d * dv + 128],
                                                                             scalar1=rec[pb:pb + 64, d:d + 1]), [("ob", z), "rec"], ["O"])
            for d in range(2):
                P.T(lambda e, d=d, kb=kb, pb=pb, cs=cs: e.matmul(kb[pb:pb + 64, d * 64:(d + 1) * 64], lhsT=KS[64 * d:64 * d + 64, cs],
                                                                 rhs=ident[64 * d:64 * d + 64, 64 * d:64 * d + 64], start=True, stop=True),
                    ["KS", "consts"], [("kb", z)])
            P.S(lambda e, kb=kb, pb=pb, z=z: e.copy(out=kws[z][pb:pb + 64, :], in_=kb[pb:pb + 64, 0:128]), [("kb", z)], [("kws", z)])
            P.T(lambda e, sbk=sbk, pb=pb, i=i, z=z: e.matmul(sbk[:, 0:2 * dv], lhsT=kws[z][pb:pb + 64, :],
                                                              rhs=V2[pb:pb + 64, i, :, :].rearrange("p d v -> p (d v)"), start=True, stop=True),
                [("kws", z), "V2"], [("sbk", z)])
            P.V(lambda e, sbk=sbk: e.tensor_tensor(out=tmpS, in0=Sb, in1=sbk[:, 0:2 * dv], op=ALU.add), ["S", ("sbk", z)], ["tmpS"])
            P.V(lambda e, c=c: e.scalar_tensor_tensor(out=Sb, in0=tmpS, scalar=EB[:, c:c + 1], in1=bm, op0=ALU.mult, op1=ALU.mult),
                ["tmpS", "EB", "consts"], ["S"])
        GT = QS
        P.dma(GT, FMS[AI[gname]], w=["QS", "gate"])
        P.S(lambda e: e.activation(out=GT, in_=GT, func=gfn), ["gate"], ["pgate"])
        for gi, (s0, L, g0, n) in enumerate(groups()):
            yb = P.banks[gi % 2]
            for jj in range(n // 128):
                i = (s0 + g0) // 128 + jj
                im = s0 // 128 + (L // 128 - 1 - (i - s0 // 128))
                P.T(lambda e, yb=yb, jj=jj, i=i: e.matmul(yb[:, jj * 128:(jj + 1) * 128], lhsT=O[:, i, 0, :], rhs=ident, start=True, stop=False),
                    ["O", "consts"], ["psrc"])
                P.T(lambda e, yb=yb, jj=jj, im=im: e.matmul(yb[:, jj * 128:(jj + 1) * 128], lhsT=O[:, im, 1, :], rhs=Jm, start=False, stop=True),
                    ["O", "consts"], ["psrc"])
            t0 = s0 + g0
            headnorm(yb[:, 0:n], n, 128, center, 1e-6, gain, GT[:, t0:t0 + n], None, Yb, SQb, RSb, P.banks[2 + gi % 2], "p")
            P.dma(yT[row0:row0 + 128, t0:t0 + n], Yb[:, 0:n], r=["pY"], eng="gpsimd", final=True)
        P.barrier()

    glalike("g", 0)
    glalike("m", 0)
    glalike("m", 1)

    def shift(X, Y, rows, mun):
        cp, cn = PPc(mun, 0, 1, 0, rows), PPc(mun, 1, 1, 0, rows)
        c0 = P.alloc(1)
        P.V(lambda e: e.tensor_tensor(out=c0[0:rows], in0=cp, in1=cn, op=ALU.add), ["consts"], ["c0"])
        P.V(lambda e: e.tensor_scalar(out=c0[0:rows], in0=c0[0:rows], scalar1=-1.0, scalar2=1.0, op0=ALU.mult, op1=ALU.add), ["c0"], ["c0"])
        kx, ky = ("sh", id(X)), ("sh", id(Y))
        P.V(lambda e: e.tensor_scalar_mul(out=Y[0:rows], in0=X[0:rows], scalar1=c0[0:rows]), [kx, "c0"], [ky])
        for (s0, L) in SEGS:
            P.V(lambda e, s0=s0, L=L: e.scalar_tensor_tensor(out=Y[0:rows, s0 + 1:s0 + L], in0=X[0:rows, s0:s0 + L - 1], scalar=cp,
                                                             in1=Y[0:rows, s0 + 1:s0 + L], op0=ALU.mult, op1=ALU.add), [kx, ky, "consts"], [ky])
            P.V(lambda e, s0=s0, L=L: e.scalar_tensor_tensor(out=Y[0:rows, s0:s0 + L - 1], in0=X[0:rows, s0 + 1:s0 + L], scalar=cn,
                                                             in1=Y[0:rows, s0:s0 + L - 1], op0=ALU.mult, op1=ALU.add), [kx, ky, "consts"], [ky])
        return kx, ky

    for arr, mun, rows, fn in (("W1F", "muw1f", 96, AF.Tanh), ("W1B", "muw1b", 96, AF.Tanh), ("A1F", "mua1f", 96, None),
                               ("A1B", "mua1b", 96, None), ("RG01", "mug01", 128, AF.Sigmoid), ("RG2", "mug2", 64, AF.Sigmoid)):
        P.aptr = mark0
        X, Y = P.alloc(T), P.alloc(T)
        kx, ky = ("sh", id(X)), ("sh", id(Y))
        P.dma(X[0:rows], FMS[AI[arr], 0:rows, :], w=[kx])
        shift(X, Y, rows, mun)
        if fn is not None:
            P.S(lambda e, fn=fn: e.activation(out=Y[0:rows], in_=Y[0:rows], func=fn), [ky], [ky])
        P.dma(FMS[AI[arr], 0:rows, :], Y[0:rows], r=[ky], eng="gpsimd")
        P.barrier()

    def rwkv_head(j):
        P.aptr = mark0
        U = [P.alloc(T) for _ in range(9)]
        K_ = lambda i: ("U", i)
        Of = U[2]
        O = Of.rearrange("p (i c) -> p i c", c=128)
        OFF, EW = P.alloc(NCHK), P.alloc(NCHK)
        omka = P.alloc(1)
        SR = P.alloc(5120)
        lrt = [SR[:, z_ * 2048:(z_ + 1) * 2048].rearrange("p (a n) -> p a n", a=4) for z_ in range(2)]
        bst = [SR[:, 4096 + z_ * 512:4096 + (z_ + 1) * 512] for z_ in range(2)]
        for i_, a in enumerate(("RR", "RK", "RV")):
            P.dma(U[i_], FMS[AI[f"{a}{j}"]], w=[("sh", id(U[i_]))])
        shift(U[0], U[3], 128, f"mur{j}")
        shift(U[1], U[4], 128, f"muk{j}")
        shift(U[2], U[0], 128, f"muv{j}")
        R, Kk, Vv = U[3], U[4], U[0]
        kR, kK, kV = ("sh", id(U[3])), ("sh", id(U[4])), ("sh", id(U[0]))
        P.V(lambda e: e.scalar_tensor_tensor(out=U[1], in0=R, scalar=PPc(f"rk{j}"), in1=Kk, op0=ALU.mult, op1=ALU.mult),
            [kR, kK, ("sh", id(U[1])), "consts"], [K_(1)])
        for ti, t0 in enumerate(range(0, T, 512)):
            n = min(512, T - t0)
            z = ti % 2
            bk = P.banks[z]
            P.T(lambda e, bk=bk, t0=t0, n=n: e.matmul(bk[:, 0:n], lhsT=C("bones"), rhs=U[1][:, t0:t0 + n], start=True, stop=True),
                [K_(1), "consts"], [("bk", z)])
            P.V(lambda e, bk=bk, t0=t0, n=n, z=z: e.tensor_tensor(out=bst[z][:, 0:n], in0=bk[:, 0:n], in1=Vv[:, t0:t0 + n], op=ALU.mult),
                [("bk", z), kV], [("bst", z)])
            P.dma(FMS[AI[f"RR{j}"], :, t0:t0 + n], bst[z][:, 0:n], r=[("bst", z)], eng="gpsimd")
        for ti, t0 in enumerate(range(0, T, 512)):
            n = min(512, T - t0)
            z = ti % 2
            for ai_, a in enumerate(("W1F", "W1B", "A1F", "A1B")):
                P.dma(lrt[z][0:96, ai_, 0:n], FMS[AI[a], 0:96, t0:t0 + n], w=[("lrt", z)])
            for w_, (dst, bn, srcs) in enumerate(((U[6], f"w0{j}", (0, 1)), (U[5], f"a0{j}", (2, 3)))):
                bk = P.banks[2 + 2 * w_ + z]
                for d in range(2):
                    P.T(lambda e, bk=bk, d=d, w_=w_, srcs=srcs, z=z, n=n: e.matmul(bk[64 * d:64 * d + 64, 0:n], lhsT=w2v[:, j, w_, d, :],
                                                                                  rhs=lrt[z][0:96, srcs[d], 0:n], start=True, stop=True),
                        [("lrt", z), "consts"], [("bk2", w_, z)])
                P.S(lambda e, bk=bk, dst=dst, bn=bn, t0=t0, n=n: e.activation(out=dst[:, t0:t0 + n], in_=bk[:, 0:n], func=AF.Sigmoid, bias=PPc(bn)),
                    [("bk2", w_, z), "consts"], [K_(5 + (1 - w_))])
        P.V(lambda e: e.tensor_scalar_mul(out=U[1], in0=Kk, scalar1=PPc(f"kk{j}")), [kK, K_(1), "consts"], [K_(1)])
        P.S(lambda e: e.activation(out=U[8], in_=U[1], func=AF.Square), [K_(1)], [K_(8)])
        for ti, t0 in enumerate(range(0, T, 512)):
            n = min(512, T - t0)
            z = ti % 2
            bk = P.banks[z]
            P.T(lambda e, bk=bk, t0=t0, n=n: e.matmul(bk[:, 0:n], lhsT=C("bones"), rhs=U[8][:, t0:t0 + n], start=True, stop=True),
                [K_(8), "consts"], [("bk", z)])
            P.S(lambda e, bk=bk, t0=t0, n=n: e.activation(out=U[7][:, t0:t0 + n], in_=bk[:, 0:n], func=AF.Ln, bias=1e-24), [("bk", z)], [K_(7)])
        P.S(lambda e: e.activation(out=U[7], in_=U[7], func=AF.Exp, scale=-0.5), [K_(7)], [K_(7)])
        P.V(lambda e: e.tensor_tensor(out=U[1], in0=U[1], in1=U[7], op=ALU.mult), [K_(1), K_(7)], [K_(1)])
        P.V(lambda e: e.tensor_scalar(out=omka, in0=PPc(f"ka{j}"), scalar1=-1.0, scalar2=1.0, op0=ALU.mult, op1=ALU.add), ["consts"], ["omka"])
        P.V(lambda e: e.tensor_scalar(out=U[7], in0=U[5], scalar1=PPc(f"ka{j}"), scalar2=omka, op0=ALU.mult, op1=ALU.add),
            [K_(5), K_(7), "omka", "consts"], [K_(7)])
        P.V(lambda e: e.tensor_tensor(out=Kk, in0=Kk, in1=U[7], op=ALU.mult), [kK, K_(7), K_(1)], [kK])
        P.V(lambda e: e.tensor_scalar(out=U[6], in0=U[6], scalar1=-float(np.exp(-0.5)), scalar2=0.0, op0=ALU.mult, op1=ALU.add), [K_(6)], [K_(6)])
        P.V(lambda e: e.tensor_tensor_scan(out=U[7], data0=ones_b.to_broadcast([128, T]), data1=U[6], initial=0.0, op0=ALU.mult, op1=ALU.add),
            [K_(6), K_(7), kK, "consts"], ["EX"])
        chunk_local(U[7], OFF)
        P.S(lambda e: e.activation(out=U[8], in_=U[7], func=AF.Exp), ["EX", K_(8)], [K_(8)])
        P.V(lambda e: e.tensor_tensor(out=R, in0=R, in1=U[8], op=ALU.mult), [kR, K_(8), K_(1)], [kR])
        P.S(lambda e: e.activation(out=U[6], in_=U[6], func=AF.Exp, scale=-1.0), [K_(6), "EX"], [K_(6)])
        P.V(lambda e: e.tensor_tensor(out=U[6], in0=U[6], in1=U[8], op=ALU.mult), [K_(6), K_(8)], [K_(6)])
        P.V(lambda e: e.scalar_tensor_tensor(out=U[6], in0=U[6], scalar=-1.0, in1=U[1], op0=ALU.mult, op1=ALU.mult), [K_(6), K_(1)], [K_(6)])
        P.S(lambda e: e.activation(out=U[8], in_=U[7], func=AF.Exp, scale=-1.0), ["EX", K_(8), kR, K_(6)], [K_(8)])
        P.V(lambda e: e.tensor_tensor(out=U[1], in0=U[1], in1=U[5], op=ALU.mult), [K_(1), K_(5), K_(6)], [K_(1)])
        P.V(lambda e: e.tensor_tensor(out=U[1], in0=U[1], in1=U[8], op=ALU.mult), [K_(1), K_(8)], [K_(1)])
        P.V(lambda e: e.tensor_tensor(out=Kk, in0=Kk, in1=U[8], op=ALU.mult), [kK, K_(8)], [kK])
        P.S(lambda e: e.activation(out=EW[:, 0:NCHK], in_=v3(U[7])[:, :, 63], func=AF.Exp), ["EX"], ["EW"])
        P.V(lambda e: e.tensor_tensor(out=v3(U[5]), in0=v3(U[1]), in1=EW[:, 0:NCHK].unsqueeze(2).to_broadcast([128, NCHK, 64]), op=ALU.mult),
            [K_(1), K_(5), "EW"], [K_(5)])
        P.V(lambda e: e.tensor_tensor(out=v3(U[7]), in0=v3(Kk), in1=EW[:, 0:NCHK].unsqueeze(2).to_broadcast([128, NCHK, 64]), op=ALU.mult),
            [kK, "EX", "EW", K_(8)], ["KW"])
        AH, BH, KH, RH, BW, KW = U[6], U[1], Kk, R, U[5], U[7]
        kAH, kBH, kKH, kRH, kBW, kKW = K_(6), K_(1), kK, kR, K_(5), "KW"
        P.barrier()
        pool = [[U[8], 0, T], [SR, 0, 5120]]

        def carve(n):
            for pl in pool:
                if pl[1] + n <= pl[2]:
                    a_ = pl[0][:, pl[1]:pl[1] + n]
                    pl[1] += n
                    return a_
            return P.alloc(n)
        prod = {nm: [carve(256) for _ in range(2)] for nm in ("AAK", "ARK", "NT", "ARB", "NN")}
        tmb = {nm: [carve(128) for _ in range(2)] for nm in ("V", "BW", "KW")}
        Yi = [carve(256) for _ in range(2)]
        Zi = [carve(256) for _ in range(2)]
        Qb = [carve(256) for _ in range(2)]
        rhs_sb, u_sb = carve(128), carve(128)
        Sb, tmpS = carve(128), carve(128)
        Yb, SQb, RSb, Gt, Bt = carve(512), carve(512), carve(512), carve(512), carve(512)
        P.G(lambda e: e.memset(Sb, 0.0), [], ["S"])
        pc = 0
        for i in range(NT):
            ts = slice(128 * i, 128 * i + 128)
            z = i % 2
            specs = (("AAK", KH, AH, kKH, kAH, "MS"), ("ARK", KH, RH, kKH, kRH, "MI"), ("NT", BH, AH, kBH, kAH, "MS"),
                     ("ARB", BH, RH, kBH, kRH, "MI"), ("NN", AH, BH, kAH, kBH, "ML"))
            for nm, Lh, Rh, kl, kr, mk in specs:
                bi = pc % 2
                pc += 1
                bk = P.banks[bi]
                for d in range(2):
                    P.T(lambda e, bk=bk, d=d, Lh=Lh, Rh=Rh, ts=ts: e.matmul(bk[:, d * 128:(d + 1) * 128], lhsT=Lh[64 * d:64 * d + 64, ts],
                                                                           rhs=Rh[64 * d:64 * d + 64, ts], start=True, stop=True), [kl, kr], [("pb", bi)])
                P.V(lambda e, bk=bk, nm=nm, mk=mk, z=z: e.tensor_tensor(out=prod[nm][z], in0=bk[:, 0:256], in1=C(mk), op=ALU.mult),
                    [("pb", bi), "consts"], [(nm, z)])
            for nm, Xs, kx_ in (("V", Vv, kV), ("BW", BW, kBW), ("KW", KW, kKW)):
                bi = pc % 2
                pc += 1
                bk = P.banks[bi]
                P.T(lambda e, bk=bk, Xs=Xs, ts=ts: e.transpose(bk[:, 0:128], Xs[:, ts], ident), [kx_, "consts"], [("pb", bi)])
                P.S(lambda e, bk=bk, nm=nm, z=z: e.copy(out=tmb[nm][z], in_=bk[:, 0:128]), [("pb", bi)], [("tm" + nm, z)])
            Yc, Zc, Q = prod["NT"][z], prod["NN"][z], Qb[z]
            kY, kZ = ("NT", z), ("NN", z)
            P.V(lambda e, Q=Q, Yc=Yc: e.tensor_tensor(out=Q, in0=Yc, in1=C("I2"), op=ALU.add), [kY, "consts"], [("Q", z)])
            for it in range(5):
                Yn, Zn = Yi[it % 2], Zi[it % 2]
                for d in range(2):
                    ds_ = slice(d * 128, (d + 1) * 128)
                    P.T(lambda e, ds_=ds_, Yc=Yc, Zc=Zc: e.matmul(P.banks[2][:, ds_], lhsT=Zc[:, ds_], rhs=Yc[:, ds_], start=True, stop=True),
                        [kY, kZ], ["bY"])
                    P.T(lambda e, ds_=ds_, Yc=Yc, Zc=Zc: e.matmul(P.banks[3][:, ds_], lhsT=Yc[:, ds_], rhs=Zc[:, ds_], start=True, stop=True),
                        [kY, kZ], ["bZ"])
                P.S(lambda e, Yn=Yn: e.copy(out=Yn, in_=P.banks[2][:, 0:256]), ["bY"], [("Yi", it % 2)])
                P.V(lambda e, Zn=Zn: e.tensor_copy(out=Zn, in_=P.banks[3][:, 0:256]), ["bZ"], [("Zi", it % 2)])
                Yc, Zc, kY, kZ = Yn, Zn, ("Yi", it % 2), ("Zi", it % 2)
                for d in range(2):
                    ds_ = slice(d * 128, (d + 1) * 128)
                    P.T(lambda e, ds_=ds_, Zc=Zc, Q=Q: e.matmul(P.banks[4][:, ds_], lhsT=Zc[:, ds_], rhs=Q[:, ds_], start=True, stop=True),
                        [kZ, ("Q", z)], ["bQ"])
                P.V(lambda e, Q=Q: e.tensor_tensor(out=Q, in0=Q, in1=P.banks[4][:, 0:256], op=ALU.add), ["bQ", ("Q", z)], [("Q", z)])
            AAK, ARK, ARB = prod["AAK"][z], prod["ARK"][z], prod["ARB"][z]
            Vt, BWt, KWt = tmb["V"][z], tmb["BW"][z], tmb["KW"][z]
            for h in range(2):
                c = 2 * i + h
                pb = 64 * h
                cs = slice(c * 64, c * 64 + 64)
                bR, bU = P.banks[5][pb:pb + 64, 0:128], P.banks[5][pb:pb + 64, 128:256]
                bY2, bS = P.banks[6][pb:pb + 64, 0:128], P.banks[7][:, 0:128]

                def blk(M_, d, pb=pb):
                    return M_[pb:pb + 64, d * 128 + pb:d * 128 + pb + 64]
                P.T(lambda e, bR=bR, cs=cs: e.matmul(bR, lhsT=AH[:, cs], rhs=Sb, start=True, stop=False), [kAH, "S"], ["bR"])
                for d in range(2):
                    P.T(lambda e, d=d, bR=bR, pb=pb: e.matmul(bR[:, d * 64:(d + 1) * 64], lhsT=blk(AAK, d), rhs=Vt[pb:pb + 64, d * 64:(d + 1) * 64],
                                                              start=False, stop=(d == 1)), [("AAK", z), ("tmV", z)], ["bR"])
                P.S(lambda e, bR=bR, pb=pb: e.copy(out=rhs_sb[pb:pb + 64, :], in_=bR), ["bR"], ["rhs_sb"])
                for d in range(2):
                    P.T(lambda e, d=d, bU=bU, pb=pb: e.matmul(bU[:, d * 64:(d + 1) * 64], lhsT=blk(Q, d), rhs=rhs_sb[pb:pb + 64, d * 64:(d + 1) * 64],
                                                              start=True, stop=True), [("Q", z), "rhs_sb"], ["bU"])
                P.V(lambda e, bU=bU, pb=pb: e.tensor_copy(out=u_sb[pb:pb + 64, :], in_=bU), ["bU"], ["u_sb"])
                P.T(lambda e, bY2=bY2, cs=cs: e.matmul(bY2, lhsT=RH[:, cs], rhs=Sb, start=True, stop=False), [kRH, "S"], ["bY2"])
                for d in range(2):
                    P.T(lambda e, d=d, bY2=bY2, pb=pb: e.matmul(bY2[:, d * 64:(d + 1) * 64], lhsT=blk(ARB, d), rhs=u_sb[pb:pb + 64, d * 64:(d + 1) * 64],
                                                                start=False, stop=False), [("ARB", z), "u_sb"], ["bY2"])
                    P.T(lambda e, d=d, bY2=bY2, pb=pb: e.matmul(bY2[:, d * 64:(d + 1) * 64], lhsT=blk(ARK, d), rhs=Vt[pb:pb + 64, d * 64:(d + 1) * 64],
                                                                start=False, stop=(d == 1)), [("ARK", z), ("tmV", z)], ["bY2"])
                P.S(lambda e, bY2=bY2, pb=pb, i=i: e.copy(out=O[pb:pb + 64, i, :], in_=bY2), ["bY2", K_(2), ("sh", id(U[2]))], ["O"])
                P.T(lambda e, bS=bS, pb=pb: e.matmul(bS, lhsT=BWt[pb:pb + 64, :], rhs=u_sb[pb:pb + 64, :], start=True, stop=False),
                    [("tmBW", z), "u_sb"], ["bS"])
                P.T(lambda e, bS=bS, pb=pb: e.matmul(bS, lhsT=KWt[pb:pb + 64, :], rhs=Vt[pb:pb + 64, :], start=False, stop=True),
                    [("tmKW", z), ("tmV", z)], ["bS"])
                P.V(lambda e, bS=bS, c=c: e.scalar_tensor_tensor(out=tmpS, in0=Sb, scalar=EW[:, c:c + 1], in1=bS, op0=ALU.mult, op1=ALU.add),
                    ["S", "EW", "bS"], ["tmpS"])
                P.V(lambda e: e.tensor_tensor(out=Sb, in0=tmpS, in1=C("bm64"), op=ALU.mult), ["tmpS", "consts"], ["S"])
        garr, gr0 = (("RG01", 0), ("RG01", 64), ("RG2", 0))[j]
        for gi, (s0, L, g0, n) in enumerate(groups()):
            yb = P.banks[gi % 2]
            t0 = s0 + g0
            for jj in range(n // 128):
                i = t0 // 128 + jj
                im = s0 // 128 + (L // 128 - 1 - (i - s0 // 128))
                P.T(lambda e, yb=yb, jj=jj, i=i: e.matmul(yb[0:64, jj * 128:(jj + 1) * 128], lhsT=O[:, i, 0:64], rhs=ident, start=True, stop=False),
                    ["O", "consts"], ["rsrc"])
                P.T(lambda e, yb=yb, jj=jj, im=im: e.matmul(yb[0:64, jj * 128:(jj + 1) * 128], lhsT=O[:, im, 64:128], rhs=Jm, start=False, stop=True),
                    ["O", "consts"], ["rsrc"])
            P.dma(Gt[0:64, 0:n], FMS[AI[garr], gr0:gr0 + 64, t0:t0 + n], w=["rgate"])
            P.dma(Bt[0:64, 0:n], FMS[AI[f"RR{j}"], 0:64, t0:t0 + n], w=["rbonus"])
            headnorm(yb[0:64, 0:n], n, 64, True, 64e-5, PPc(f"rgn{j}", 0, 1, 0, 64), Gt[0:64, 0:n], Bt[0:64, 0:n], Yb, SQb, RSb, P.banks[2 + gi % 2], "r")
            P.dma(yT[384 + 64 * j:448 + 64 * j, t0:t0 + n], Yb[0:64, 0:n], r=["rY"], eng="gpsimd", final=True)
        P.barrier()

    for j in range(3):
        rwkv_head(j)
    P.emit()
    P.close()
    return nc


GLA_BASE, ML_BASE, RW_BASE = 0, 1568, 3896


def _wcols(g):
    ar = np.arange
    cols = {"gq": g * 64 + ar(64), "gk": 256 + g * 64 + ar(64), "glrf": 1536 + ar(16), "glrb": 1552 + ar(16),
            "gg": 1024 + g * 128 + ar(128), "gv": 512 + g * 128 + ar(128)}
    for s, h in enumerate(MSLOT[g]):
        cols[f"mq{s}"] = ML_BASE + h * 64 + ar(64)
        cols[f"mk{s}"] = ML_BASE + 384 + h * 64 + ar(64)
        cols[f"mv{s}"] = ML_BASE + 768 + h * 128 + ar(128)
        cols[f"mo{s}"] = ML_BASE + 1536 + h * 128 + ar(128)
        for nm, d, w in (("mif", 0, 0), ("mff", 0, 1), ("mib", 1, 0), ("mfb", 1, 1)):
            cols[f"{nm}{s}"] = np.full(64, ML_BASE + 2304 + d * 12 + w * 6 + h)
    for j in range(3):
        hh = 3 * g + j
        for k_, nm in enumerate(("rr", "rk", "rv", "rg")):
            cols[f"{nm}{j}"] = RW_BASE + 768 * k_ + hh * 64 + ar(64)
    for k_, nm in enumerate(("w1f", "w1b", "a1f", "a1b")):
        cols[nm] = RW_BASE + 3072 + 96 * k_ + ar(96)
    return cols


def prep_B(l, b, g, inp, mod, xc, xl):
    f32 = np.float32
    cols = _wcols(g)
    w_in = inp["w_in"][l]
    wfm = np.concatenate([w_in[:, cols[n]] for n, _ in WBLK], axis=1)
    wtm = np.concatenate([w_in[:, cols[n]] for n in ("gv", "mv0", "mv1")], axis=1)
    xT = np.ascontiguousarray(np.concatenate([xc[b], xl[b]], 0).T)
    sh1, sc1 = mod[l][:, 0:D], mod[l][:, D:2 * D]
    modv = np.stack([pk(inp["g_norm1"][l]), pk(sc1[2]), pk(sh1[2]), pk(sc1[b]), pk(sh1[b])], axis=2).reshape(128, KC * 5)
    pp = np.zeros((128, NPP), f32)

    def put(n, a, r0=0):
        o, w = PPN[n]
        a = np.asarray(a, f32).reshape(-1, w)
        pp[r0:r0 + a.shape[0], o:o + w] = a
    put("gba", inp["gla_b_a"][l][0, g * 64:(g + 1) * 64])
    put("gba", inp["gla_b_a"][l][1, g * 64:(g + 1) * 64], 64)
    put("ggn", inp["gla_g_norm"][l][g * 128:(g + 1) * 128])
    cwt = inp["ml_conv_w"][l].reshape(9, 768)
    for s, h in enumerate(MSLOT[g]):
        for nm, c0 in (("q", h * 64), ("k", 384 + h * 64)):
            wv = cwt[:, c0:c0 + 64].T
            put(f"cw{nm}{s}", wv)
            put(f"cw{nm}{s}", wv[:, ::-1], 64)
            put(f"cb{nm}{s}", inp["ml_conv_b"][l][c0:c0 + 64])
            put(f"cb{nm}{s}", inp["ml_conv_b"][l][c0:c0 + 64], 64)
        for d in range(2):
            put(f"mbi{s}", np.full(64, inp["ml_gate_b"][l][d, 0, h]), 64 * d)
            put(f"mbf{s}", np.full(64, inp["ml_gate_b"][l][d, 1, h]), 64 * d)
        put(f"mgn{s}", inp["ml_g_norm"][l][h * 128:(h + 1) * 128])
    mu = inp["rw_mu"][l]

    def mupair(c, swap):
        m = np.stack([mu[0][c], mu[1][c]], 1)
        return m[:, ::-1] if swap else m
    for j in range(3):
        hh = 3 * g + j
        hc = hh * 64 + np.arange(64)
        for nm, off in (("mur", 0), ("muk", 768), ("muv", 1536)):
            put(f"{nm}{j}", mupair(off + hc, False))
            put(f"{nm}{j}", mupair(off + hc, True), 64)
        for d in range(2):
            put(f"w0{j}", inp["rw_w0"][l][d, hc], 64 * d)
            put(f"a0{j}", inp["rw_a0"][l][d, hc], 64 * d)
            put(f"kk{j}", inp["rw_k_k"][l][hc], 64 * d)
            put(f"ka{j}", inp["rw_k_a"][l][hc], 64 * d)
            put(f"rk{j}", inp["rw_r_k"][l][hc], 64 * d)
        put(f"rgn{j}", inp["rw_g_norm"][l][hc])
    put("mug01", mupair(2304 + (3 * g) * 64 + np.arange(64), False))
    put("mug01", mupair(2304 + (3 * g + 1) * 64 + np.arange(64), False), 64)
    put("mug2", mupair(2304 + (3 * g + 2) * 64 + np.arange(64), False))
    put("muw1f", mupair(3072 + np.arange(96), False))
    put("muw1b", mupair(3168 + np.arange(96), True))
    put("mua1f", mupair(3264 + np.arange(96), False))
    put("mua1b", mupair(3360 + np.arange(96), True))
    wa2 = np.zeros((128, 64), f32)
    wa2[0:16] = inp["gla_w_a2"][l][0][:, g * 64:(g + 1) * 64]
    wa2[64:80] = inp["gla_w_a2"][l][1][:, g * 64:(g + 1) * 64]
    w2 = np.zeros((96, 3, 2, 2, 64), f32)
    for j in range(3):
        hc = (3 * g + j) * 64 + np.arange(64)
        for d in range(2):
            w2[:, j, 0, d] = inp["rw_w2"][l][d][:, hc]
            w2[:, j, 1, d] = inp["rw_a2"][l][d][:, hc]
    return {"xT": xT, "modv": np.ascontiguousarray(modv, f32), "wfm": np.ascontiguousarray(wfm), "wtm": np.ascontiguousarray(wtm),
            "cst": make_consts(), "pp": pp, "wa2": wa2, "w2": w2.reshape(96, 768)}


def assemble_yT(res, B, T):
    yT = np.zeros((B, D, T), np.float32)
    for b in range(B):
        for g in range(4):
            r = res[b * 4 + g]["yT"]
            yT[b, g * 128:(g + 1) * 128] = r[0:128]
            for s, h in enumerate(MSLOT[g]):
                yT[b, 512 + h * 128:512 + (h + 1) * 128] = r[128 + 128 * s:256 + 128 * s]
            for j in range(3):
                hh = 3 * g + j
                yT[b, 1280 + hh * 64:1280 + (hh + 1) * 64] = r[384 + 64 * j:448 + 64 * j]
    return yT


def build_C1(NCc, NLl):
    NTK = NCc + NLl
    TW = 256
    BIG = 1.0e30
    nc = bass.Bass("TRN2", target_bir_lowering=False)

    def din(name, shape, dt=F32):
        return nc.dram_tensor(name, list(shape), dt, kind="ExternalInput").ap()
    yT, xT = din("yT", [D, NTK]), din("xT", [D, NTK])
    wo_d, modv_d, wr_d, br_d, cst_d = din("wo", [D, D]), din("modv", [128, KC * 7]), din("wr", [D, 36]), din("br", [128, 36]), din("cst", [128, NCST])
    x1T = nc.dram_tensor("x1T", [D, NTK], F32, kind="ExternalOutput").ap()
    h2T = nc.dram_tensor("h2T", [D, NTK], BF16, kind="ExternalOutput").ap()
    gates = nc.dram_tensor("gates", [NTK, 32], F32, kind="ExternalOutput").ap()
    P = Prog(nc)
    cst = P.sbuf([128, NCST], F32, "cst_sb")
    mv = P.sbuf([128, KC * 7], F32, "mv_sb")
    wr = P.sbuf([128, KC * 36], F32, "wr_sb")
    br = P.sbuf([128, 36], F32, "br_sb")
    gm = P.sbuf([128, KC * 2], F32, "gm_sb")
    P.dma(cst[:], cst_d, w=["consts"])
    P.dma(mv[:], modv_d, w=["consts"])
    P.dma(br[:], br_d, w=["consts"])
    wrv = wr[:].rearrange("p (k n) -> p k n", n=36)
    P.dma(wrv, wr_d.rearrange("(k p) n -> p k n", p=128), w=["consts"])
    mvv = mv[:].rearrange("p (k j) -> p k j", j=7)
    gmv = gm[:].rearrange("p (k j) -> p k j", j=2)
    ones = cst[:, CSTN["ones"][0]:CSTN["ones"][0] + 128]
    for j, col in enumerate((2, 5)):
        P.V(lambda e: e.tensor_scalar(out=gmv[:, :, j], in0=mvv[:, :, col], scalar1=1.0, scalar2=1.0, op0=ALU.add, op1=ALU.mult), ["consts"], ["gm"])
        P.V(lambda e: e.tensor_tensor(out=gmv[:, :, j], in0=gmv[:, :, j], in1=mvv[:, :, 0], op=ALU.mult), ["gm", "consts"], ["gm"])
    wo = P.alloc(KC * D, BF16).rearrange("p (k n) -> p k n", n=D)
    for c0 in range(0, D, 512):
        P.dma(wo[:, :, c0:c0 + 512], wo_d[:, c0:c0 + 512].rearrange("(k p) n -> p k n", p=128), w=["wo"], eng="gpsimd")
    yb = [P.alloc(KC * TW, BF16).rearrange("p (k n) -> p k n", n=TW) for _ in range(2)]
    xb = [P.alloc(KC * TW).rearrange("p (k n) -> p k n", n=TW) for _ in range(2)]
    sq = P.alloc(KC * TW).rearrange("p (k n) -> p k n", n=TW)
    hb = P.alloc(KC * TW, BF16).rearrange("p (k n) -> p k n", n=TW)
    rstd = P.alloc(TW)
    Lg, LM, I1, I2_, G1 = P.alloc(36), P.alloc(32), P.alloc(32), P.alloc(32), P.alloc(32)
    sm = P.alloc(16)
    tiles = [(s0, o, min(TW, L - o)) for (s0, L) in ((0, NCc), (NCc, NLl)) for o in range(0, L, TW)]
    for ti, (s0, o, n) in enumerate(tiles):
        b = ti % 2
        t0 = s0 + o
        sg = 0 if s0 == 0 else 1
        yt, xt = yb[b], xb[b]
        P.dma(yt[:, :, 0:n], yT[:, t0:t0 + n].rearrange("(k p) t -> p k t", p=128), w=[("yt", b)], eng="gpsimd")
        P.dma(xt[:, :, 0:n], xT[:, t0:t0 + n].rearrange("(k p) t -> p k t", p=128), w=[("xt", b)])
        for fo in range(KC):
            bi = fo % 4
            bank = P.banks[1 + bi]
            for k in range(KC):
                P.T(lambda e: e.matmul(bank[:, 0:n], lhsT=wo[:, k, fo * 128:(fo + 1) * 128], rhs=yt[:, k, 0:n], start=(k == 0), stop=(k == KC - 1)),
                    ["wo", ("yt", b)], [("bank", bi)], inc=(k == KC - 1))
            P.V(lambda e: e.scalar_tensor_tensor(out=xt[:, fo, 0:n], in0=bank[:, 0:n], scalar=mvv[:, fo, 1 + 3 * sg:2 + 3 * sg], in1=xt[:, fo, 0:n],
                                                 op0=ALU.mult, op1=ALU.add), [("bank", bi), ("xt", b), "consts"], [("xt", b)])
        P.dma(x1T[:, t0:t0 + n].rearrange("(k p) t -> p k t", p=128), xt[:, :, 0:n], r=[("xt", b)], eng="gpsimd", final=True)
        rms_rstd(P, xt, n, ones, P.banks[0], sq, rstd, [("xt", b)], "rstd", 0)
        P.V(lambda e: e.tensor_tensor(out=sq[:, :, 0:n], in0=xt[:, :, 0:n], in1=rstd[:, 0:n].unsqueeze(1).to_broadcast([128, KC, n]), op=ALU.mult),
            [("xt", b), "rstd", ("sq", 0)], [("sq", 0)])
        for k in range(KC):
            P.V(lambda e: e.tensor_scalar(out=sq[:, k, 0:n], in0=sq[:, k, 0:n], scalar1=gmv[:, k, sg:sg + 1],
                                          scalar2=mvv[:, k, 3 + 3 * sg:4 + 3 * sg], op0=ALU.mult, op1=ALU.add), [("sq", 0), "gm", "consts"], [("sq", 0)])
        P.G(lambda e: e.tensor_copy(out=hb[:, :, 0:n], in_=sq[:, :, 0:n]), [("sq", 0)], ["hb"])
        P.dma(h2T[:, t0:t0 + n].rearrange("(k p) t -> p k t", p=128), hb[:, :, 0:n], r=["hb"], eng="gpsimd", final=True)
        for m0 in range(0, n, 128):
            m = min(128, n - m0)
            bank = P.banks[5]
            for k in range(KC):
                P.T(lambda e: e.matmul(bank[0:m, 0:36], lhsT=sq[:, k, m0:m0 + m], rhs=wrv[:, k, :], start=(k == 0), stop=(k == KC - 1)),
                    [("sq", 0), "consts"], ["rb"], inc=(k == KC - 1))
            A = lambda t_, c0=0, c1=None: t_[0:m, c0:(c1 if c1 is not None else t_.shape[1])]
            P.V(lambda e: e.tensor_tensor(out=A(Lg), in0=bank[0:m, 0:36], in1=br[0:m, :], op=ALU.add), ["rb", "consts"], ["Lg"])
            P.V(lambda e: e.reduce_max(out=sm[0:m, 0:1], in_=Lg[0:m, 0:4], axis=AX.X), ["Lg"], ["sm"])
            P.V(lambda e: e.tensor_scalar(out=sm[0:m, 1:2], in0=sm[0:m, 0:1], scalar1=-1.0, scalar2=0.0, op0=ALU.mult, op1=ALU.add), ["sm"], ["sm"])
            P.S(lambda e: e.activation(out=sm[0:m, 8:12], in_=Lg[0:m, 0:4], func=AF.Exp, bias=sm[0:m, 1:2], accum_out=sm[0:m, 2:3]), ["Lg", "sm"], ["sm"])
            P.V(lambda e: e.reciprocal(out=sm[0:m, 3:4], in_=sm[0:m, 2:3]), ["sm"], ["sm"])
            P.V(lambda e: e.tensor_scalar(out=sm[0:m, 12:16], in0=Lg[0:m, 0:4], scalar1=sm[0:m, 0:1], scalar2=BIG, op0=ALU.is_ge, op1=ALU.mult),
                ["Lg", "sm"], ["sm"])
            P.V(lambda e: e.tensor_scalar(out=sm[0:m, 12:16], in0=sm[0:m, 12:16], scalar1=-BIG, scalar2=1.0, op0=ALU.add, op1=ALU.mult), ["sm"], ["sm"])
            P.V(lambda e: e.tensor_tensor(out=LM[0:m, :].rearrange("p (g x) -> p g x", g=4), in0=Lg[0:m, 4:36].rearrange("p (g x) -> p g x", g=4),
                                          in1=sm[0:m, 12:16].unsqueeze(2).to_broadcast([m, 4, 8]), op=ALU.add), ["Lg", "sm"], ["LM"])
            P.V(lambda e: e.reduce_max(out=sm[0:m, 4:5], in_=LM[0:m, :], axis=AX.X), ["LM"], ["sm"])
            P.V(lambda e: e.tensor_scalar(out=I1[0:m, :], in0=LM[0:m, :], scalar1=sm[0:m, 4:5], scalar2=1.0, op0=ALU.is_ge, op1=ALU.mult), ["LM", "sm"], ["I1"])
            P.V(lambda e: e.scalar_tensor_tensor(out=LM[0:m, :], in0=I1[0:m, :], scalar=-BIG, in1=LM[0:m, :], op0=ALU.mult, op1=ALU.add), ["I1", "LM"], ["LM"])
            P.V(lambda e: e.reduce_max(out=sm[0:m, 5:6], in_=LM[0:m, :], axis=AX.X), ["LM"], ["sm"])
            P.V(lambda e: e.tensor_scalar(out=I2_[0:m, :], in0=LM[0:m, :], scalar1=sm[0:m, 5:6], scalar2=1.0, op0=ALU.is_ge, op1=ALU.mult), ["LM", "sm"], ["I2"])
            P.V(lambda e: e.tensor_tensor(out=sm[0:m, 6:7], in0=sm[0:m, 4:5], in1=sm[0:m, 5:6], op=ALU.subtract), ["sm"], ["sm"])
            P.S(lambda e: e.activation(out=sm[0:m, 6:7], in_=sm[0:m, 6:7], func=AF.Sigmoid), ["sm"], ["sm"])
            P.V(lambda e: e.tensor_tensor(out=sm[0:m, 6:7], in0=sm[0:m, 6:7], in1=sm[0:m, 3:4], op=ALU.mult), ["sm"], ["sm"])
            P.V(lambda e: e.tensor_tensor(out=sm[0:m, 7:8], in0=sm[0:m, 3:4], in1=sm[0:m, 6:7], op=ALU.subtract), ["sm"], ["sm"])
            P.V(lambda e: e.tensor_scalar_mul(out=G1[0:m, :], in0=I1[0:m, :], scalar1=sm[0:m, 6:7]), ["I1", "sm"], ["G1"])
            P.V(lambda e: e.scalar_tensor_tensor(out=G1[0:m, :], in0=I2_[0:m, :], scalar=sm[0:m, 7:8], in1=G1[0:m, :], op0=ALU.mult, op1=ALU.add),
                ["I2", "sm", "G1"], ["G1"])
            P.dma(gates[t0 + m0:t0 + m0 + m, :], G1[0:m, :], r=["G1"], eng="gpsimd", final=True)
    P.emit()
    P.close()
    return nc


def build_C2(TT, DE):
    NK = DE // 128
    TW = 512
    nc = bass.Bass("TRN2", target_bir_lowering=False)
    h2T = nc.dram_tensor("h2T", [D, TT], BF16, kind="ExternalInput").ap()
    gT = nc.dram_tensor("gT", [4, TT], F32, kind="ExternalInput").ap()
    wg_d = nc.dram_tensor("wg", [4, D, DE], F32, kind="ExternalInput").ap()
    wu_d = nc.dram_tensor("wu", [4, D, DE], F32, kind="ExternalInput").ap()
    wd_d = nc.dram_tensor("wd", [4, DE, D], F32, kind="ExternalInput").ap()
    fT = nc.dram_tensor("fT", [D, TT], F32, kind="ExternalOutput").ap()
    P = Prog(nc)
    wg = [P.alloc(KC * DE, BF16).rearrange("p (k n) -> p k n", n=DE) for _ in range(2)]
    wu = [P.alloc(KC * DE, BF16).rearrange("p (k n) -> p k n", n=DE) for _ in range(2)]
    wd = [P.alloc(NK * D, BF16).rearrange("p (k n) -> p k n", n=D) for _ in range(2)]
    hb = [P.alloc(KC * TW, BF16).rearrange("p (k n) -> p k n", n=TW) for _ in range(2)]
    Ab = [[[P.alloc(TW, BF16) for _ in range(NK)] for _ in range(2)] for _ in range(2)]
    gb = [[P.alloc(TW) for _ in range(2)] for _ in range(2)]
    sgb = [P.alloc(TW) for _ in range(2)]
    ost = [P.alloc(TW) for _ in range(3)]
    prv = [P.alloc(TW) for _ in range(2)]
    tiles = [(t0, min(TW, TT - t0)) for t0 in range(0, TT, TW)]
    for p in range(2):
        for e_ in range(2):
            ex = 2 * p + e_
            P.dma(wg[e_][:, :, :], wg_d[ex].rearrange("(k p) n -> p k n", p=128), w=[("W", e_)], eng="gpsimd")
            P.dma(wu[e_][:, :, :], wu_d[ex].rearrange("(k p) n -> p k n", p=128), w=[("W", e_)], eng="gpsimd")
            for c0 in range(0, D, 512):
                P.dma(wd[e_][:, :, c0:c0 + 512], wd_d[ex, :, c0:c0 + 512].rearrange("(k p) n -> p k n", p=128), w=[("W", e_)], eng="gpsimd")
        gc = 0
        for ti, (t0, n) in enumerate(tiles):
            b = ti % 2
            h = hb[b]
            P.dma(h[:, :, 0:n], h2T[:, t0:t0 + n].rearrange("(k p) t -> p k t", p=128), w=[("h", b)])
            for e_ in range(2):
                P.dma(gb[b][e_][:, 0:n], gT[2 * p + e_:2 * p + e_ + 1, t0:t0 + n].partition_broadcast(128), w=[("g", b, e_)])
            for e_ in range(2):
                for kc in range(NK):
                    z = gc % 2
                    gc += 1
                    bG, bU = P.banks[z], P.banks[2 + z]
                    for k in range(KC):
                        P.T(lambda e: e.matmul(bG[:, 0:n], lhsT=wg[e_][:, k, kc * 128:(kc + 1) * 128], rhs=h[:, k, 0:n], start=(k == 0), stop=(k == KC - 1)),
                            [("W", e_), ("h", b)], [("bG", z)], inc=(k == KC - 1))
                    for k in range(KC):
                        P.T(lambda e: e.matmul(bU[:, 0:n], lhsT=wu[e_][:, k, kc * 128:(kc + 1) * 128], rhs=h[:, k, 0:n], start=(k == 0), stop=(k == KC - 1)),
                            [("W", e_), ("h", b)], [("bU", z)], inc=(k == KC - 1))
                    P.S(lambda e: e.activation(out=sgb[z][:, 0:n], in_=bG[:, 0:n], func=AF.Silu), [("bG", z)], [("sg", z)])
                    P.V(lambda e: e.tensor_tensor(out=sgb[z][:, 0:n], in0=sgb[z][:, 0:n], in1=bU[:, 0:n], op=ALU.mult), [("sg", z), ("bU", z)], [("sg", z)])
                    P.G(lambda e: e.tensor_tensor(out=Ab[b][e_][kc][:, 0:n], in0=sgb[z][:, 0:n], in1=gb[b][e_][:, 0:n], op=ALU.mult),
                        [("sg", z), ("g", b, e_)], [("A", b)])
            for fo in range(KC):
                z = fo % 2
                bF = P.banks[4 + z]
                o_ = ost[fo % 3]
                first = True
                for e_ in range(2):
                    for kc in range(NK):
                        last = (e_ == 1 and kc == NK - 1)
                        P.T(lambda e: e.matmul(bF[:, 0:n], lhsT=wd[e_][:, kc, fo * 128:(fo + 1) * 128], rhs=Ab[b][e_][kc][:, 0:n], start=first, stop=last),
                            [("W", e_), ("A", b)], [("bF", z)], inc=last)
                        first = False
                if p == 0:
                    P.S(lambda e: e.copy(out=o_[:, 0:n], in_=bF[:, 0:n]), [("bF", z)], [("ost", fo % 3)])
                else:
                    pv = prv[fo % 2]
                    P.dma(pv[:, 0:n], fT[fo * 128:(fo + 1) * 128, t0:t0 + n], w=[("prv", fo % 2)])
                    P.V(lambda e: e.tensor_tensor(out=o_[:, 0:n], in0=bF[:, 0:n], in1=pv[:, 0:n], op=ALU.add), [("bF", z), ("prv", fo % 2)], [("ost", fo % 3)])
                P.dma(fT[fo * 128:(fo + 1) * 128, t0:t0 + n], o_[:, 0:n], r=[("ost", fo % 3)], eng="gpsimd", final=(p == 1))
        P.barrier()
    P.emit()
    P.close()
    return nc


def build_C3(NCc, NLl, NP):
    NTK = NCc + NLl
    TW = 256
    nc = bass.Bass("TRN2", target_bir_lowering=False)
    fTp = nc.dram_tensor("fTp", [NP, D, NTK], F32, kind="ExternalInput").ap()
    x1T = nc.dram_tensor("x1T", [D, NTK], F32, kind="ExternalInput").ap()
    modv_d = nc.dram_tensor("modv", [128, KC * 3], F32, kind="ExternalInput").ap()
    cst_d = nc.dram_tensor("cst", [128, NCST], F32, kind="ExternalInput").ap()
    x2T = nc.dram_tensor("x2T", [D, NTK], F32, kind="ExternalOutput").ap()
    onT = nc.dram_tensor("onT", [D, NTK], F32, kind="ExternalOutput").ap()
    P = Prog(nc)
    cst = P.sbuf([128, NCST], F32, "cst_sb")
    mv = P.sbuf([128, KC * 3], F32, "mv_sb")
    P.dma(cst[:], cst_d, w=["consts"])
    P.dma(mv[:], modv_d, w=["consts"])
    mvv = mv[:].rearrange("p (k j) -> p k j", j=3)
    ones = cst[:, CSTN["ones"][0]:CSTN["ones"][0] + 128]
    pb_ = [P.alloc(KC * TW).rearrange("p (k n) -> p k n", n=TW) for _ in range(3)]
    acc = [P.alloc(KC * TW).rearrange("p (k n) -> p k n", n=TW) for _ in range(2)]
    xb = [P.alloc(KC * TW).rearrange("p (k n) -> p k n", n=TW) for _ in range(2)]
    sq = P.alloc(KC * TW).rearrange("p (k n) -> p k n", n=TW)
    rstd = P.alloc(TW)
    tiles = [(s0, o, min(TW, L - o)) for (s0, L) in ((0, NCc), (NCc, NLl)) for o in range(0, L, TW)]
    pc = 0
    for ti, (s0, o, n) in enumerate(tiles):
        b = ti % 2
        t0 = s0 + o
        sg = 0 if s0 == 0 else 1
        a_, xt = acc[b], xb[b]
        P.dma(xt[:, :, 0:n], x1T[:, t0:t0 + n].rearrange("(k p) t -> p k t", p=128), w=[("xt", b)])
        P.dma(a_[:, :, 0:n], fTp[0, :, t0:t0 + n].rearrange("(k p) t -> p k t", p=128), w=[("acc", b)])
        for c in range(1, NP):
            z = pc % 3
            pc += 1
            P.dma(pb_[z][:, :, 0:n], fTp[c, :, t0:t0 + n].rearrange("(k p) t -> p k t", p=128), w=[("pb", z)])
            P.V(lambda e: e.tensor_tensor(out=a_[:, :, 0:n], in0=a_[:, :, 0:n], in1=pb_[z][:, :, 0:n], op=ALU.add), [("acc", b), ("pb", z)], [("acc", b)])
        P.V(lambda e: e.tensor_tensor(out=a_[:, :, 0:n], in0=a_[:, :, 0:n], in1=mvv[:, :, sg:sg + 1].to_broadcast([128, KC, n]), op=ALU.mult),
            [("acc", b), "consts"], [("acc", b)])
        P.V(lambda e: e.tensor_tensor(out=a_[:, :, 0:n], in0=a_[:, :, 0:n], in1=xt[:, :, 0:n], op=ALU.add), [("acc", b), ("xt", b)], [("acc", b)])
        P.dma(x2T[:, t0:t0 + n].rearrange("(k p) t -> p k t", p=128), a_[:, :, 0:n], r=[("acc", b)], eng="gpsimd", final=True)
        rms_rstd(P, a_, n, ones, P.banks[0], sq, rstd, [("acc", b)], "rstd", 0)
        P.V(lambda e: e.tensor_tensor(out=sq[:, :, 0:n], in0=a_[:, :, 0:n], in1=rstd[:, 0:n].unsqueeze(1).to_broadcast([128, KC, n]), op=ALU.mult),
            [("acc", b), "rstd", ("sq", 0)], [("sq", 0)])
        P.V(lambda e: e.tensor_tensor(out=sq[:, :, 0:n], in0=sq[:, :, 0:n], in1=mvv[:, :, 2:3].to_broadcast([128, KC, n]), op=ALU.mult),
            [("sq", 0), "consts"], [("sq", 0)])
        P.dma(onT[:, t0:t0 + n].rearrange("(k p) t -> p k t", p=128), sq[:, :, 0:n], r=[("sq", 0)], eng="gpsimd", final=True)
    P.emit()
    P.close()
    return nc


_NC_CACHE = {}


def _get(key, fn):
    if key not in _NC_CACHE:
        _NC_CACHE[key] = fn()
    return _NC_CACHE[key]


def kernel(x, c, ctx, c_ctx, w_ada, b_ada, g_norm1, g_norm2, w_in, gla_w_a2, gla_b_a,
           gla_g_norm, ml_conv_w, ml_conv_b, ml_gate_b, ml_g_norm, rw_mu, rw_w2, rw_w0,
           rw_a2, rw_a0, rw_k_k, rw_k_a, rw_r_k, rw_g_norm, w_out, moe_w_rg, moe_b_rg,
           moe_w_re, moe_b_re, moe_w_gate, moe_w_up, moe_w_down, g_final):
    f32 = np.float32
    inp = dict(w_in=w_in, g_norm1=g_norm1, gla_w_a2=gla_w_a2, gla_b_a=gla_b_a, gla_g_norm=gla_g_norm, ml_conv_w=ml_conv_w,
               ml_conv_b=ml_conv_b, ml_gate_b=ml_gate_b, ml_g_norm=ml_g_norm, rw_mu=rw_mu, rw_w2=rw_w2, rw_w0=rw_w0, rw_a2=rw_a2,
               rw_a0=rw_a0, rw_k_k=rw_k_k, rw_k_a=rw_k_a, rw_r_k=rw_r_k, rw_g_norm=rw_g_norm)
    inp = {k: np.asarray(v, f32) for k, v in inp.items()}
    x, ctx = np.asarray(x, f32), np.asarray(ctx, f32)
    B, SEQ, _ = x.shape
    CTX = ctx.shape[1]
    L = w_ada.shape[0]
    DE = moe_w_gate.shape[-1]
    T = CTX + SEQ
    TT = B * T
    NCORE = 8
    NCc, NLl = CTX // 4, SEQ // 4
    NTK = NCc + NLl
    assert B == 2 and CTX % 512 == 0 or True
    cstv = make_consts()
    NCOL = 6 * D // NCORE
    ncA = _get(("A", L, NCOL), lambda: build_A(L, NCOL))
    cv = np.concatenate([np.asarray(c, f32), np.asarray(c_ctx, f32)[None]], 0)
    cT = np.ascontiguousarray(cv.T.reshape(KC, 128, 3).transpose(1, 0, 2))
    w_ada, b_ada = np.asarray(w_ada, f32), np.asarray(b_ada, f32)
    resA = _run(ncA, [{"cT": cT, "w": np.ascontiguousarray(w_ada[:, :, i * NCOL:(i + 1) * NCOL]),
                       "b": np.ascontiguousarray(b_ada[:, i * NCOL:(i + 1) * NCOL])} for i in range(NCORE)])
    modall = np.concatenate([r["out"] for r in resA], axis=2)
    mod = [modall[l] for l in range(L)]
    ncB = _get(("B", CTX, SEQ), lambda: build_B(CTX, SEQ))
    ncC1 = _get(("C1", NCc, NLl), lambda: build_C1(NCc, NLl))
    ncC2 = _get(("C2", TT, DE), lambda: build_C2(TT, DE))
    ncC3 = _get(("C3", NCc, NLl), lambda: build_C3(NCc, NLl, NCORE))
    xc, xl = ctx.copy(), x.copy()
    tok_idx = [np.concatenate([q * NCc + np.arange(NCc), CTX + q * NLl + np.arange(NLl)]) for q in range(4)]
    out = None
    for l in range(L):
        m = mod[l]
        sh1, sc1, gt1, sh2, sc2, gt2 = [m[:, i * D:(i + 1) * D] for i in range(6)]
        resB = _run(ncB, [prep_B(l, b, g, inp, mod, xc, xl) for b in range(B) for g in range(4)])
        yT = assemble_yT(resB, B, T)
        XT = [np.ascontiguousarray(np.concatenate([xc[b], xl[b]], 0).T) for b in range(B)]
        wr = np.ascontiguousarray(np.concatenate([np.asarray(moe_w_rg[l], f32), np.asarray(moe_w_re[l], f32)], 1))
        br = np.ascontiguousarray(np.broadcast_to(np.concatenate([np.asarray(moe_b_rg[l], f32), np.asarray(moe_b_re[l], f32)])[None], (128, 36)))
        wo = np.ascontiguousarray(np.asarray(w_out[l], f32))
        g2 = pk(np.asarray(g_norm2[l], f32))
        mapsC1 = []
        for b in range(B):
            for q in range(4):
                modv = np.stack([g2, pk(gt1[2]), pk(sc2[2]), pk(sh2[2]), pk(gt1[b]), pk(sc2[b]), pk(sh2[b])], axis=2).reshape(128, KC * 7)
                mapsC1.append({"yT": np.ascontiguousarray(yT[b][:, tok_idx[q]]), "xT": np.ascontiguousarray(XT[b][:, tok_idx[q]]),
                               "wo": wo, "modv": np.ascontiguousarray(modv), "wr": wr, "br": br, "cst": cstv})
        resC1 = _run(ncC1, mapsC1)
        h2all = np.ascontiguousarray(np.concatenate([r["h2T"] for r in resC1], axis=1))
        gall = np.concatenate([r["gates"] for r in resC1], axis=0)
        mapsC2 = [{"h2T": h2all, "gT": np.ascontiguousarray(gall[:, 4 * i:4 * i + 4].T),
                   "wg": np.ascontiguousarray(np.asarray(moe_w_gate[l][4 * i:4 * i + 4], f32)),
                   "wu": np.ascontiguousarray(np.asarray(moe_w_up[l][4 * i:4 * i + 4], f32)),
                   "wd": np.ascontiguousarray(np.asarray(moe_w_down[l][4 * i:4 * i + 4], f32))} for i in range(NCORE)]
        resC2 = _run(ncC2, mapsC2)
        gf = pk(np.asarray(g_final, f32))
        mapsC3 = []
        for b in range(B):
            for q in range(4):
                ci = b * 4 + q
                fTp = np.ascontiguousarray(np.stack([resC2[i]["fT"][:, ci * NTK:(ci + 1) * NTK] for i in range(NCORE)], 0))
                modv = np.stack([pk(gt2[2]), pk(gt2[b]), gf], axis=2).reshape(128, KC * 3)
                mapsC3.append({"fTp": fTp, "x1T": resC1[ci]["x1T"], "modv": np.ascontiguousarray(modv), "cst": cstv})
        resC3 = _run(ncC3, mapsC3)
        for b in range(B):
            X = XT[b]
            for q in range(4):
                X[:, tok_idx[q]] = resC3[b * 4 + q]["x2T"]
            xc[b] = X[:, :CTX].T
            xl[b] = X[:, CTX:].T
        if l == L - 1:
            out = np.zeros((B, SEQ, D), f32)
            for b in range(B):
                for q in range(4):
                    out[b, q * NLl:(q + 1) * NLl] = resC3[b * 4 + q]["onT"][:, NCc:].T
    return out
```

```python
import numpy as np
from contextlib import ExitStack
import concourse.bass as bass
import concourse.mybir as mybir
from concourse.bass_utils import run_bass_kernel_spmd

F32 = mybir.dt.float32
BF16 = mybir.dt.bfloat16
AF = mybir.ActivationFunctionType
ALU = mybir.AluOpType
AX = mybir.AxisListType

D = 2048
KC = 16
DEPTH = 4
GRID_W = 64
CH = 64
NEXP = 32
ENGS = ("sync", "scalar", "vector", "gpsimd", "tensor")
EPOCH = 6000
NDMA = 24
ARENA = 49000
FM_NOINC = True
RMS_NOINC = True


class _Rec:
    def __getattr__(self, name):
        def f(*a, **k):
            self.call = (name, a, k)
            return self
        return f


class Prog:
    def __init__(self, nc):
        self.nc = nc
        self.es = ExitStack()
        self.streams = {e: [] for e in ENGS}
        self.count = {e: 0 for e in ENGS}
        self.esems = {e: [] for e in ENGS}
        self.dsems = [self.es.enter_context(nc.semaphore(f"dq{i}")) for i in range(NDMA)]
        self.dma_n = 0
        self.dma_tok = [None] * NDMA
        self.lastw = {}
        self.readers = {}
        self.seen = {e: {} for e in ENGS}
        self.final = []
        self.nbuf = 0
        self.last_tok = {}
        self.arena = self.sbuf([128, ARENA], F32, "arena")
        self.aptr = 0
        self.banks = [self.psum([128, 512], F32, f"bank{i}") for i in range(8)]

    def sbuf(self, shape, dtype=F32, name=None):
        self.nbuf += 1
        return self.es.enter_context(self.nc.sbuf_tensor(name or f"sb{self.nbuf}", list(shape), dtype))

    def psum(self, shape, dtype=F32, name=None):
        self.nbuf += 1
        return self.es.enter_context(self.nc.psum_tensor(name or f"ps{self.nbuf}", list(shape), dtype))

    def dram(self, name, shape, dtype=F32, kind="Internal"):
        return self.nc.dram_tensor(name, list(shape), dtype, kind=kind).ap()

    def alloc(self, n, dtype=F32):
        w = n if dtype == F32 else (n + 1) // 2
        assert self.aptr + w <= ARENA, ("arena overflow", self.aptr, w)
        ap = self.arena[:, self.aptr:self.aptr + w]
        self.aptr += w
        if dtype != F32:
            ap = ap.bitcast(dtype)[:, 0:n]
        return ap

    def _esem(self, eng, epoch):
        lst = self.esems[eng]
        while len(lst) <= epoch:
            lst.append(self.es.enter_context(self.nc.semaphore(f"p_{eng}_{len(lst)}")))
        return lst[epoch]

    def _need(self, eng, tok, waits):
        if tok is None:
            return
        sem, val, key = tok
        if self.seen[eng].get(key, 0) >= val:
            return
        if key[0] == "e" and key[1] == eng and key[2] * EPOCH + val > self.count[eng]:
            return
        self.seen[eng][key] = val
        waits.append((sem, val))

    def op(self, eng, fn, reads=(), writes=(), dma=False, out_final=False, inc=True):
        rec = _Rec()
        fn(rec)
        fn = rec.call
        waits = []
        for k in reads:
            self._need(eng, self.lastw.get(k), waits)
        for k in writes:
            self._need(eng, self.lastw.get(k), waits)
            for t in self.readers.get(k, {}).values():
                self._need(eng, t, waits)
        if dma:
            slot = self.dma_n % NDMA
            val = 16 * (self.dma_n // NDMA + 1)
            self._need(eng, self.dma_tok[slot], waits)
            tok = (self.dsems[slot], val, ("d", slot))
            self.dma_tok[slot] = tok
            self.dma_n += 1
            inc = (self.dsems[slot], 16)
            if out_final:
                self.final.append(tok)
            rkey = ("d", self.dma_n)
        else:
            n = self.count[eng]
            epoch, idx = divmod(n, EPOCH)
            sem = self._esem(eng, epoch)
            tok = (sem, idx + 1, ("e", eng, epoch))
            if inc:
                self.count[eng] = n + 1
                inc = (sem, 1)
                self.last_tok[eng] = tok
            else:
                inc = None
            rkey = eng
        self.streams[eng].append((waits, fn, inc))
        for k in reads:
            self.readers.setdefault(k, {})[rkey] = tok
        for k in writes:
            self.lastw[k] = tok
            self.readers[k] = {}
        return tok

    def barrier(self):
        toks = [t for t in self.last_tok.values()] + [t for t in self.dma_tok if t is not None]
        for e in ENGS:
            waits = []
            for t in toks:
                self._need(e, t, waits)
            if waits:
                self.streams[e].append((waits, None, None))
        self.lastw = {}
        self.readers = {}

    def V(self, fn, r=(), w=()):
        return self.op("vector", fn, r, w)

    def S(self, fn, r=(), w=()):
        return self.op("scalar", fn, r, w)

    def G(self, fn, r=(), w=()):
        return self.op("gpsimd", fn, r, w)

    def T(self, fn, r=(), w=(), inc=True):
        return self.op("tensor", fn, r, w, inc=inc)

    def dma(self, out, in_, r=(), w=(), eng="sync", final=False):
        return self.op(eng, lambda e: e.dma_start(out=out, in_=in_), r, w, dma=True, out_final=final)

    def emit(self):
        nc = self.nc
        fin = [(t[0], t[1]) for t in self.final]
        streams = self.streams
        with nc.Block() as block:
            def run(engname):
                def body(e):
                    for waits, fn, inc in streams[engname]:
                        for s, v in waits:
                            e.wait_ge(s, v)
                        if fn is not None:
                            ins = getattr(e, fn[0])(*fn[1], **fn[2])
                            if inc is not None:
                                ins.then_inc(inc[0], inc[1])
                    if engname == "sync":
                        for s, v in fin:
                            e.wait_ge(s, v)
                return body
            block.sync(run("sync"))
            block.scalar(run("scalar"))
            block.vector(run("vector"))
            block.gpsimd(run("gpsimd"))
            block.tensor(run("tensor"))

    def close(self):
        self.es.close()


def _run(nc, in_maps):
    res = run_bass_kernel_spmd(nc, in_maps, core_ids=list(range(len(in_maps))))
    return res.results


def pk(v):
    return np.ascontiguousarray(np.asarray(v, np.float32).reshape(KC, 128).T)


def build_A(L, NCOL):
    nc = bass.Bass("TRN2", target_bir_lowering=False)
    cT = nc.dram_tensor("cT", [128, KC, 3], F32, kind="ExternalInput").ap()
    w = nc.dram_tensor("w", [L, D, NCOL], F32, kind="ExternalInput").ap()
    b = nc.dram_tensor("b", [L, NCOL], F32, kind="ExternalInput").ap()
    out = nc.dram_tensor("out", [L, 3, NCOL], F32, kind="ExternalOutput").ap()
    P = Prog(nc)
    s = P.alloc(KC * 3).rearrange("p (k r) -> p k r", r=3)
    P.dma(s, cT, w=["s"])
    P.S(lambda e: e.activation(out=s, in_=s, func=AF.Silu), ["s"], ["s"])
    wb = [P.alloc(KC * 512).rearrange("p (k n) -> p k n", n=512) for _ in range(2)]
    bb = [P.alloc(512) for _ in range(2)]
    ob = [P.alloc(512) for _ in range(2)]
    it = 0
    for l in range(L):
        for c0 in range(0, NCOL, 512):
            n = min(512, NCOL - c0)
            i = it % 2
            it += 1
            P.dma(wb[i][:, :, 0:n], w[l, :, c0:c0 + n].rearrange("(k p) n -> p k n", p=128), w=[("wb", i)])
            P.dma(bb[i][0:3, 0:n], b[l:l + 1, c0:c0 + n].partition_broadcast(3), w=[("bb", i)], eng="gpsimd")
            ps = P.banks[i]
            for k in range(KC):
                P.T(lambda e, k=k, i=i, n=n, ps=ps: e.matmul(ps[0:3, 0:n], lhsT=s[:, k, :], rhs=wb[i][:, k, 0:n],
                                                              start=(k == 0), stop=(k == KC - 1)),
                    ["s", ("wb", i)], [("ps", i)], inc=(k == KC - 1))
            P.V(lambda e, i=i, n=n, ps=ps: e.tensor_tensor(out=ob[i][0:3, 0:n], in0=ps[0:3, 0:n], in1=bb[i][0:3, 0:n], op=ALU.add),
                [("ps", i), ("bb", i)], [("ob", i)])
            P.dma(out[l, :, c0:c0 + n], ob[i][0:3, 0:n], r=[("ob", i)], eng="gpsimd", final=True)
    P.emit()
    P.close()
    return nc


def rms_rstd(P, xt, n, ones, msbank, sq, rstd, keys_x, key_out, tag):
    P.S(lambda e: e.activation(out=sq[:, :, 0:n], in_=xt[:, :, 0:n], func=AF.Square), keys_x, [("sq", tag)])
    for k in range(KC):
        P.T(lambda e, k=k: e.matmul(msbank[:, 0:n], lhsT=ones, rhs=sq[:, k, 0:n], start=(k == 0), stop=(k == KC - 1)),
            [("sq", tag), "consts"], [("msb", tag)], inc=(k == KC - 1) or not RMS_NOINC)
    P.S(lambda e: e.activation(out=rstd[:, 0:n], in_=msbank[:, 0:n], func=AF.Ln, scale=1.0 / D, bias=1e-6),
        [("msb", tag)], [key_out])
    P.S(lambda e: e.activation(out=rstd[:, 0:n], in_=rstd[:, 0:n], func=AF.Exp, scale=-0.5), [key_out], [key_out])


MSLOT = [(0, 1), (2, 3), (4, 4), (5, 5)]
FMA = (["GQ", "GK", "GLR", "GG"] + [f"{a}{s}" for s in range(2) for a in ("MQ", "MK", "MI", "MF", "MO")]
       + [f"{a}{j}" for j in range(3) for a in ("RR", "RK", "RV")] + ["RG01", "RG2", "W1F", "W1B", "A1F", "A1B"])
WBLK = ([("gq", 64), ("gk", 64), ("glrf", 16), ("glrb", 16), ("gg", 128)]
        + [(f"{a}{s}", n) for s in range(2) for a, n in (("mq", 64), ("mk", 64), ("mo", 128), ("mif", 64), ("mff", 64), ("mib", 64), ("mfb", 64))]
        + [(f"{a}{j}", 64) for j in range(3) for a in ("rr", "rk", "rv", "rg")]
        + [("w1f", 96), ("w1b", 96), ("a1f", 96), ("a1b", 96)])
WOFF = {}
_o = 0
for _n, _c in WBLK:
    WOFF[_n] = (_o, _c)
    _o += _c
NFMC = _o
JOBS = {"GQ": [(0, "gq", 0), (64, "gq", 1)], "GK": [(0, "gk", 0), (64, "gk", 1)],
        "GLR": [(0, "glrf", 0), (64, "glrb", 1)], "GG": [(0, "gg", 0)],
        "RG01": [(0, "rg0", 0), (64, "rg1", 0)], "RG2": [(0, "rg2", 0)],
        "W1F": [(0, "w1f", 0)], "W1B": [(0, "w1b", 1)], "A1F": [(0, "a1f", 0)], "A1B": [(0, "a1b", 1)]}
for _s in range(2):
    JOBS[f"MQ{_s}"] = [(0, f"mq{_s}", 0), (64, f"mq{_s}", 1)]
    JOBS[f"MK{_s}"] = [(0, f"mk{_s}", 0), (64, f"mk{_s}", 1)]
    JOBS[f"MI{_s}"] = [(0, f"mif{_s}", 0), (64, f"mib{_s}", 1)]
    JOBS[f"MF{_s}"] = [(0, f"mff{_s}", 0), (64, f"mfb{_s}", 1)]
    JOBS[f"MO{_s}"] = [(0, f"mo{_s}", 0)]
for _j in range(3):
    for _a in ("rr", "rk", "rv"):
        JOBS[f"{_a.upper()}{_j}"] = [(0, f"{_a}{_j}", 0), (64, f"{_a}{_j}", 1)]
NTMC = 384


def _mjobs():
    J = []

    def pair(a, b, A, B, src, keepB=True):
        c0 = WOFF[a][0]
        n = WOFF[a][1] + WOFF[b][1]
        assert WOFF[b][0] == c0 + WOFF[a][1]
        d = [(0, 64, A, 64 * src)]
        if keepB:
            d.append((64, 64, B[0], B[1]))
        J.append((c0, n if keepB else 64, src, d))
    for src in (0, 1):
        pair("gq", "gk", "GQ", ("GK", 64 * src), src)
    J.append((WOFF["glrf"][0], 16, 0, [(0, 16, "GLR", 0)]))
    J.append((WOFF["glrb"][0], 16, 1, [(0, 16, "GLR", 64)]))
    J.append((WOFF["gg"][0], 128, 0, [(0, 128, "GG", 0)]))
    for s in range(2):
        for src in (0, 1):
            pair(f"mq{s}", f"mk{s}", f"MQ{s}", (f"MK{s}", 64 * src), src)
        pair(f"mif{s}", f"mff{s}", f"MI{s}", (f"MF{s}", 0), 0)
        pair(f"mib{s}", f"mfb{s}", f"MI{s}", (f"MF{s}", 64), 1)
        J.append((WOFF[f"mo{s}"][0], 128, 0, [(0, 128, f"MO{s}", 0)]))
    for j in range(3):
        for src in (0, 1):
            pair(f"rr{j}", f"rk{j}", f"RR{j}", (f"RK{j}", 64 * src), src)
        garr, gr0 = (("RG01", 0), ("RG01", 64), ("RG2", 0))[j]
        pair(f"rv{j}", f"rg{j}", f"RV{j}", (garr, gr0), 0)
        pair(f"rv{j}", f"rg{j}", f"RV{j}", None, 1, keepB=False)
    for a, A, src in (("w1f", "W1F", 0), ("w1b", "W1B", 1), ("a1f", "A1F", 0), ("a1b", "A1B", 1)):
        J.append((WOFF[a][0], 96, src, [(0, 96, A, 0)]))
    return J


MJOBS = _mjobs()
PPN = {}
_o = 0
for _n, _c in ([("gba", 1), ("ggn", 1)]
               + [(f"{a}{s}", n) for s in range(2) for a, n in (("cwq", 9), ("cwk", 9), ("cbq", 1), ("cbk", 1), ("mbi", 1), ("mbf", 1), ("mgn", 1))]
               + [(f"{a}{j}", n) for j in range(3) for a, n in (("mur", 2), ("muk", 2), ("muv", 2), ("w0", 1), ("a0", 1), ("kk", 1), ("ka", 1), ("rk", 1), ("rgn", 1))]
               + [("mug01", 2), ("mug2", 2), ("muw1f", 2), ("muw1b", 2), ("mua1f", 2), ("mua1b", 2)]):
    PPN[_n] = (_o, _c)
    _o += _c
NPP = _o
CSTN = {}
_o = 0
for _n, _c in [("ident", 128), ("ones", 128), ("J", 128), ("mtri", 128), ("bones", 128), ("o128", 128),
               ("MS", 256), ("MI", 256), ("ML", 256), ("I2", 256), ("bm128", 256), ("bm129", 258), ("bm64", 128)]:
    CSTN[_n] = (_o, _c)
    _o += _c
NCST = _o


def make_consts():
    c = np.zeros((128, NCST), np.float32)

    def put(n, a):
        o, w = CSTN[n]
        c[:, o:o + w] = a
    idx = np.arange(128)
    put("ident", np.eye(128))
    put("ones", np.ones((128, 128)))
    put("J", np.eye(128)[::-1])
    put("mtri", ((idx[:, None] % 64) <= (idx[None, :] % 64)).astype(np.float32))
    same = (idx[:, None] // 64) == (idx[None, :] // 64)
    put("bones", same.astype(np.float32))
    put("o128", np.full((128, 128), 1.0 / 128))
    ms = (same & (idx[:, None] < idx[None, :])).astype(np.float32)
    mi = (same & (idx[:, None] <= idx[None, :])).astype(np.float32)
    put("MS", np.concatenate([ms, ms], 1))
    put("MI", np.concatenate([mi, mi], 1))
    put("ML", np.concatenate([ms.T, ms.T], 1))
    put("I2", np.concatenate([np.eye(128), np.eye(128)], 1))
    for n, dv in (("bm128", 128), ("bm129", 129), ("bm64", 64)):
        m = np.zeros((128, 2 * dv))
        m[:64, :dv] = 1
        m[64:, dv:] = 1
        put(n, m)
    return c


def build_B(CTX, SEQ):
    T = CTX + SEQ
    NT = T // 128
    NCHK = T // 64
    SEGS = [(0, CTX), (CTX, SEQ)]
    TW = 256
    NA = len(FMA)
    nc = bass.Bass("TRN2", target_bir_lowering=False)

    def din(name, shape):
        return nc.dram_tensor(name, list(shape), F32, kind="ExternalInput").ap()
    xT = din("xT", [D, T])
    modv_d = din("modv", [128, KC * 5])
    wfm = din("wfm", [D, NFMC])
    wtm = din("wtm", [D, NTMC])
    cst_d = din("cst", [128, NCST])
    pp_d = din("pp", [128, NPP])
    wa2_d = din("wa2", [128, 64])
    w2_d = din("w2", [96, 3 * 2 * 2 * 64])
    yT = nc.dram_tensor("yT", [576, T], F32, kind="ExternalOutput").ap()
    P = Prog(nc)
    FMS = P.dram("fms", [NA, 128, T])
    TMS = P.dram("tms", [2, T, NTMC])
    AI = {a: i for i, a in enumerate(FMA)}

    cst = P.sbuf([128, NCST], F32, "cst_sb")
    pp = P.sbuf([128, NPP], F32, "pp_sb")
    mv = P.sbuf([128, KC * 5], F32, "mv_sb")
    wa2 = P.sbuf([128, 64], F32, "wa2_sb")
    w2 = P.sbuf([96, 768], F32, "w2_sb")
    P.dma(cst[:], cst_d, w=["consts"])
    P.dma(pp[:], pp_d, w=["consts"])
    P.dma(mv[:], modv_d, w=["consts"])
    P.dma(wa2[:], wa2_d, w=["consts"])
    P.dma(w2[:], w2_d, w=["consts"])
    w2v = w2[:].rearrange("p (j w d c) -> p j w d c", j=3, w=2, d=2)
    mvv = mv[:].rearrange("p (k j) -> p k j", j=5)

    def C(n, r0=0, r1=128, c0=None, c1=None):
        o, w = CSTN[n]
        a = o if c0 is None else o + c0
        b = o + w if c1 is None else o + c1
        return cst[r0:r1, a:b]

    def PPc(n, j=0, w=1, r0=0, r1=128):
        o, _ = PPN[n]
        return pp[r0:r1, o + j:o + j + w]
    ident, ones, Jm = C("ident"), C("ones"), C("J")
    ones_b = cst[:, CSTN["ones"][0]:CSTN["ones"][0] + 1]

    gm = P.sbuf([128, KC * 2], F32, "gm_sb")
    gmv = gm[:].rearrange("p (k j) -> p k j", j=2)
    for j, col in enumerate((1, 3)):
        P.V(lambda e, j=j, col=col: e.tensor_scalar(out=gmv[:, :, j], in0=mvv[:, :, col], scalar1=1.0, scalar2=1.0, op0=ALU.add, op1=ALU.mult),
            ["consts"], ["gm"])
        P.V(lambda e, j=j: e.tensor_tensor(out=gmv[:, :, j], in0=gmv[:, :, j], in1=mvv[:, :, 0], op=ALU.mult), ["gm", "consts"], ["gm"])
    mark0 = P.aptr

    wsb = P.alloc(KC * NFMC, BF16).rearrange("p (k n) -> p k n", n=NFMC)
    wtsb = P.alloc(KC * NTMC, BF16).rearrange("p (k n) -> p k n", n=NTMC)
    for c0 in range(0, NFMC, 512):
        c1 = min(NFMC, c0 + 512)
        P.dma(wsb[:, :, c0:c1], wfm[:, c0:c1].rearrange("(k p) n -> p k n", p=128), w=["wsb"], eng="gpsimd")
    P.dma(wtsb[:, :, :], wtm.rearrange("(k p) n -> p k n", p=128), w=["wsb"], eng="gpsimd")
    xb = [P.alloc(KC * TW).rearrange("p (k n) -> p k n", n=TW) for _ in range(2)]
    sq = P.alloc(KC * TW).rearrange("p (k n) -> p k n", n=TW)
    rstd = P.alloc(TW)
    hT = [P.alloc(KC * TW, BF16).rearrange("p (k n) -> p k n", n=TW) for _ in range(2)]
    hR = [P.alloc(KC * TW, BF16).rearrange("p (k n) -> p k n", n=TW) for _ in range(2)]
    stg = [P.alloc(TW) for _ in range(4)]
    tstg = [P.alloc(NTMC) for _ in range(2)]
    tiles = [(s0, L, o, min(TW, L - o)) for (s0, L) in SEGS for o in range(0, L, TW)]
    cnt = 0
    tcn = 0
    for ti, (s0, L, o, n) in enumerate(tiles):
        b = ti % 2
        t0, tm = s0 + o, s0 + L - o - n
        sg = 0 if s0 == 0 else 1
        xt = xb[b]
        P.dma(xt[:, :, 0:n], xT[:, t0:t0 + n].rearrange("(k p) t -> p k t", p=128), w=[("xt", b)])
        rms_rstd(P, xt, n, ones, P.banks[0], sq, rstd, [("xt", b)], "rstd", 0)
        P.V(lambda e, xt=xt, n=n: e.tensor_tensor(out=sq[:, :, 0:n], in0=xt[:, :, 0:n],
                                                   in1=rstd[:, 0:n].unsqueeze(1).to_broadcast([128, KC, n]), op=ALU.mult),
            [("xt", b), "rstd", ("sq", 0)], [("sq", 0)])
        for k in range(KC):
            P.V(lambda e, k=k, b=b, n=n, sg=sg: e.tensor_scalar(out=hT[b][:, k, 0:n], in0=sq[:, k, 0:n], scalar1=gmv[:, k, sg:sg + 1],
                                                                 scalar2=mvv[:, k, 2 + 2 * sg:3 + 2 * sg], op0=ALU.mult, op1=ALU.add),
                [("sq", 0), "gm", "consts"], [("h", b, 0)])
        P.V(lambda e, b=b, n=n: e.tensor_copy(out=hR[b][:, :, 0:n][:, :, ::-1], in_=hT[b][:, :, 0:n]), [("h", b, 0)], [("h", b, 1)])
        hh = (hT[b], hR[b])
        for (c0, ncol, src, dests) in MJOBS:
            bi = 1 + (cnt % 4)
            si = cnt % 4
            cnt += 1
            bank = P.banks[bi]
            st = stg[si]
            for k in range(KC):
                P.T(lambda e: e.matmul(bank[0:ncol, 0:n], lhsT=wsb[:, k, c0:c0 + ncol], rhs=hh[src][:, k, 0:n], start=(k == 0), stop=(k == KC - 1)),
                    ["wsb", ("h", b, src)], [("bank", bi)], inc=(k == KC - 1) or not FM_NOINC)
            if cnt % 2 == 0:
                P.S(lambda e: e.copy(out=st[0:ncol, 0:n], in_=bank[0:ncol, 0:n]), [("bank", bi)], [("stg", si)])
            else:
                P.V(lambda e: e.tensor_copy(out=st[0:ncol, 0:n], in_=bank[0:ncol, 0:n]), [("bank", bi)], [("stg", si)])
            col = t0 if src == 0 else tm
            for (pr0, nr, arr, ar0) in dests:
                P.dma(FMS[AI[arr], ar0:ar0 + nr, col:col + n], st[pr0:pr0 + nr, 0:n], r=[("stg", si)], eng="gpsimd")
        for j in range(n // 128):
            for src in (0, 1):
                bi = 5 + (tcn % 2)
                bank = P.banks[bi]
                st = tstg[tcn % 2]
                for k in range(KC):
                    P.T(lambda e, k=k, j=j, src=src, bank=bank: e.matmul(bank[:, 0:NTMC], lhsT=hh[src][:, k, j * 128:(j + 1) * 128],
                                                                         rhs=wtsb[:, k, :], start=(k == 0), stop=(k == KC - 1)),
                        ["wsb", ("h", b, src)], [("bank", bi)])
                P.S(lambda e, st=st, bank=bank: e.copy(out=st[:, 0:NTMC], in_=bank[:, 0:NTMC]), [("bank", bi)], [("tstg", tcn % 2)])
                row0 = (t0 if src == 0 else tm) + 128 * j
                P.dma(TMS[src, row0:row0 + 128, :], st[:, 0:NTMC], r=[("tstg", tcn % 2)], eng="gpsimd")
                tcn += 1
    P.barrier()

    def v3(ap, w=64):
        return ap.rearrange("p (c k) -> p c k", k=w)

    def chunk_local(EX, OFF):
        P.G(lambda e: e.memset(OFF[:, 0:1], 0.0), [], ["OFF"])
        P.V(lambda e: e.tensor_copy(out=OFF[:, 1:NCHK], in_=v3(EX)[:, 0:NCHK - 1, 63]), ["EX"], ["OFF"])
        P.V(lambda e: e.tensor_tensor(out=v3(EX), in0=v3(EX), in1=OFF[:, 0:NCHK].unsqueeze(2).to_broadcast([128, NCHK, 64]),
                                      op=ALU.subtract), ["EX", "OFF"], ["EX"])

    def headnorm(src, n, R, center, eps, gain, gate, bonus, Y, SQ, RS, bankm, tag):
        on = ones[0:R, 0:R]
        P.S(lambda e: e.copy(out=Y[0:R, 0:n], in_=src), [tag + "src"], [tag + "Y"])
        if center:
            P.T(lambda e: e.matmul(bankm[0:R, 0:n], lhsT=on, rhs=Y[0:R, 0:n], start=True, stop=True), [tag + "Y", "consts"], [tag + "bm"])
            P.V(lambda e: e.scalar_tensor_tensor(out=Y[0:R, 0:n], in0=bankm[0:R, 0:n], scalar=-1.0 / R, in1=Y[0:R, 0:n],
                                                 op0=ALU.mult, op1=ALU.add), [tag + "bm", tag + "Y"], [tag + "Y"])
        P.S(lambda e: e.activation(out=SQ[0:R, 0:n], in_=Y[0:R, 0:n], func=AF.Square), [tag + "Y"], [tag + "SQ"])
        P.T(lambda e: e.matmul(bankm[0:R, 0:n], lhsT=on, rhs=SQ[0:R, 0:n], start=True, stop=True), [tag + "SQ", "consts"], [tag + "bm"])
        P.S(lambda e: e.activation(out=RS[0:R, 0:n], in_=bankm[0:R, 0:n], func=AF.Ln, scale=1.0 / R, bias=eps), [tag + "bm"], [tag + "RS"])
        P.S(lambda e: e.activation(out=RS[0:R, 0:n], in_=RS[0:R, 0:n], func=AF.Exp, scale=-0.5), [tag + "RS"], [tag + "RS"])
        P.V(lambda e: e.tensor_tensor(out=Y[0:R, 0:n], in0=Y[0:R, 0:n], in1=RS[0:R, 0:n], op=ALU.mult), [tag + "Y", tag + "RS"], [tag + "Y"])
        if bonus is None:
            P.V(lambda e: e.scalar_tensor_tensor(out=Y[0:R, 0:n], in0=Y[0:R, 0:n], scalar=gain, in1=gate, op0=ALU.mult, op1=ALU.mult),
                [tag + "Y", tag + "gate", "consts"], [tag + "Y"])
        else:
            P.V(lambda e: e.scalar_tensor_tensor(out=Y[0:R, 0:n], in0=Y[0:R, 0:n], scalar=gain, in1=bonus, op0=ALU.mult, op1=ALU.add),
                [tag + "Y", tag + "bonus", "consts"], [tag + "Y"])
            P.V(lambda e: e.tensor_tensor(out=Y[0:R, 0:n], in0=Y[0:R, 0:n], in1=gate, op=ALU.mult), [tag + "Y", tag + "gate"], [tag + "Y"])

    def groups():
        for (s0, L) in SEGS:
            for g0 in range(0, L, 512):
                yield s0, L, g0, min(512, L - g0)

    PADW = 65
    for s in range(2):
        for arr, cwn, cbn in ((f"MQ{s}", f"cwq{s}", f"cbq{s}"), (f"MK{s}", f"cwk{s}", f"cbk{s}")):
            P.aptr = mark0
            oc, ol = PADW, PADW + CTX + PADW
            X0 = P.alloc(T + 3 * PADW)
            XA = P.alloc(SEQ + 2 * PADW)
            XB = P.alloc(SEQ + 2 * PADW)
            ACC = P.alloc(T)
            P.G(lambda e: e.memset(X0, 0.0), [], ["X0"])
            P.G(lambda e: e.memset(XA, 0.0), [], ["XA"])
            P.G(lambda e: e.memset(XB, 0.0), [], ["XB"])
            P.dma(X0[:, oc:oc + CTX], FMS[AI[arr], :, 0:CTX], w=["X0"])
            P.dma(X0[:, ol:ol + SEQ], FMS[AI[arr], :, CTX:T], w=["X0"])
            P.S(lambda e: e.copy(out=XA[:, PADW:PADW + SEQ], in_=X0[:, ol:ol + SEQ]), ["X0"], ["XA"])
            P.G(lambda e: e.tensor_copy(out=XB[:, PADW:PADW + SEQ], in_=X0[:, ol:ol + SEQ]), ["X0"], ["XB"])
            P.S(lambda e: e.memzero(v3(XA[:, PADW:PADW + SEQ])[:, :, 63]) if False else e.activation(
                out=v3(XA[:, PADW:PADW + SEQ])[:, :, 63], in_=v3(XA[:, PADW:PADW + SEQ])[:, :, 63], func=AF.Copy, scale=0.0), ["XA"], ["XA"])
            P.G(lambda e: e.memset(v3(XB[:, PADW:PADW + SEQ])[:, :, 0], 0.0), ["XB"], ["XB"])
            cw = lambda i, j: PPc(cwn, 3 * i + j)
            cb = PPc(cbn)
            P.V(lambda e: e.tensor_scalar(out=ACC[:, 0:CTX], in0=X0[:, oc:oc + CTX], scalar1=cw(1, 1), scalar2=cb, op0=ALU.mult, op1=ALU.add),
                ["X0", "consts"], ["ACC"])
            for j in (0, 2):
                P.V(lambda e, j=j: e.scalar_tensor_tensor(out=ACC[:, 0:CTX], in0=X0[:, oc + j - 1:oc + j - 1 + CTX], scalar=cw(1, j),
                                                          in1=ACC[:, 0:CTX], op0=ALU.mult, op1=ALU.add), ["X0", "ACC", "consts"], ["ACC"])
            P.V(lambda e: e.tensor_scalar(out=ACC[:, CTX:T], in0=X0[:, ol:ol + SEQ], scalar1=cw(1, 1), scalar2=cb, op0=ALU.mult, op1=ALU.add),
                ["X0", "consts"], ["ACC"])
            for i in range(3):
                for j in range(3):
                    if i == 1 and j == 1:
                        continue
                    off = 64 * (i - 1) + (j - 1)
                    if j == 1:
                        srcv = X0[:, ol + off:ol + off + SEQ]
                    elif j == 0:
                        srcv = XA[:, PADW + off:PADW + off + SEQ]
                    else:
                        srcv = XB[:, PADW + off:PADW + off + SEQ]
                    P.V(lambda e, srcv=srcv, i=i, j=j: e.scalar_tensor_tensor(out=ACC[:, CTX:T], in0=srcv, scalar=cw(i, j), in1=ACC[:, CTX:T],
                                                                              op0=ALU.mult, op1=ALU.add), ["X0", "XA", "XB", "ACC", "consts"], ["ACC"])
            P.S(lambda e: e.activation(out=ACC, in_=ACC, func=AF.Silu), ["ACC"], ["ACC"])
            P.dma(FMS[AI[arr], :, :], ACC, r=["ACC"], eng="gpsimd")
            P.barrier()

    def glalike(kind, s):
        P.aptr = mark0
        dv = 128 if kind == "g" else 129
        QS, KS, LA, EX = P.alloc(T), P.alloc(T), P.alloc(T), P.alloc(T)
        IG = P.alloc(T) if kind == "m" else None
        V2f = P.alloc(NT * 2 * dv)
        V2 = V2f.rearrange("p (i d v) -> p i d v", d=2, v=dv)
        Of = P.alloc(NT * 256)
        O = Of.rearrange("p (i d v) -> p i d v", d=2, v=128)
        OFF, EB = P.alloc(NCHK), P.alloc(NCHK)
        Sb, tmpS = P.alloc(2 * dv), P.alloc(2 * dv)
        nb, rec = P.alloc(1), P.alloc(2)
        att = [P.alloc(128) for _ in range(2)]
        kws = [P.alloc(128) for _ in range(2)]
        Yb, SQb, RSb = P.alloc(512), P.alloc(512), P.alloc(512)
        bm = C("bm128") if kind == "g" else C("bm129")
        if kind == "g":
            P.dma(QS, FMS[AI["GQ"]], w=["QS"])
            P.dma(KS, FMS[AI["GK"]], w=["KS"])
            P.dma(EX, FMS[AI["GLR"]], w=["EX"])
            P.V(lambda e: e.tensor_scalar(out=nb, in0=PPc("gba"), scalar1=-1.0, scalar2=0.0, op0=ALU.mult, op1=ALU.add), ["consts"], ["nb"])
            for t0 in range(0, T, 512):
                n = min(512, T - t0)
                bk = P.banks[(t0 // 512) % 2]
                for d in range(2):
                    P.T(lambda e, d=d, t0=t0, n=n, bk=bk: e.matmul(bk[64 * d:64 * d + 64, 0:n], lhsT=wa2[64 * d:64 * d + 16, :],
                                                                  rhs=EX[64 * d:64 * d + 16, t0:t0 + n], start=True, stop=True),
                        ["EX", "consts"], [("bk", (t0 // 512) % 2)])
                P.S(lambda e, t0=t0, n=n, bk=bk: e.activation(out=LA[:, t0:t0 + n], in_=bk[:, 0:n], func=AF.Exp, scale=-1.0, bias=nb),
                    [("bk", (t0 // 512) % 2), "nb"], ["LA"])
            sc = 1.0 / 16.0
            gname, gfn, gain, row0, center = "GG", AF.Silu, PPc("ggn"), 0, False
        else:
            P.dma(QS, FMS[AI[f"MQ{s}"]], w=["QS"])
            P.dma(KS, FMS[AI[f"MK{s}"]], w=["KS"])
            P.dma(LA, FMS[AI[f"MF{s}"]], w=["LA"])
            P.dma(IG, FMS[AI[f"MI{s}"]], w=["IG"])
            P.V(lambda e: e.tensor_scalar(out=nb, in0=PPc(f"mbf{s}"), scalar1=-1.0, scalar2=0.0, op0=ALU.mult, op1=ALU.add), ["consts"], ["nb"])
            P.S(lambda e: e.activation(out=LA, in_=LA, func=AF.Exp, scale=-1.0, bias=nb), ["LA", "nb"], ["LA"])
            sc = 1.0
            gname, gfn, gain, row0, center = f"MO{s}", AF.Sigmoid, PPc(f"mgn{s}"), 128 + 128 * s, True
        P.S(lambda e: e.activation(out=LA, in_=LA, func=AF.Ln, bias=1.0), ["LA"], ["LA"])
        P.V(lambda e: e.tensor_tensor_scan(out=EX, data0=ones_b.to_broadcast([128, T]), data1=LA, initial=0.0, op0=ALU.mult, op1=ALU.add),
            ["LA", "consts", "EX"], ["EX"])
        chunk_local(EX, OFF)
        P.S(lambda e: e.activation(out=LA, in_=EX, func=AF.Exp, scale=-sc), ["EX"], ["LA"])
        P.V(lambda e: e.tensor_copy(out=EB[:, 0:NCHK], in_=v3(LA)[:, :, 63]), ["LA"], ["EB"])
        P.V(lambda e: e.scalar_tensor_tensor(out=QS, in0=QS, scalar=0.125, in1=LA, op0=ALU.mult, op1=ALU.mult), ["QS", "LA"], ["QS"])
        if kind == "g":
            P.S(lambda e: e.activation(out=EX, in_=EX, func=AF.Exp, scale=sc), ["EX"], ["EX"])
        else:
            P.V(lambda e: e.scalar_tensor_tensor(out=EX, in0=EX, scalar=sc, in1=IG, op0=ALU.mult, op1=ALU.add), ["EX", "IG"], ["EX"])
            P.S(lambda e: e.activation(out=EX, in_=EX, func=AF.Exp, bias=PPc(f"mbi{s}")), ["EX", "consts"], ["EX"])
        P.V(lambda e: e.tensor_tensor(out=KS, in0=KS, in1=EX, op=ALU.mult), ["KS", "EX"], ["KS"])
        tcol = {"g": 0, "m": 128 + 128 * s}[kind]
        for d in range(2):
            P.dma(V2[:, :, d, 0:128], TMS[d, :, tcol:tcol + 128].rearrange("(i p) c -> p i c", p=128), w=["V2"])
        if kind == "m":
            P.G(lambda e: e.memset(V2[:, :, :, 128:129], 1.0), ["V2"], ["V2"])
        P.G(lambda e: e.memset(Sb, 0.0), [], ["S"])
        ATTa = LA.rearrange("p (i c) -> p i c", c=128)
        KWa = EX.rearrange("p (i c) -> p i c", c=128)
        P.barrier()
        for c in range(NCHK):
            i, h = divmod(c, 2)
            pb = 64 * h
            cs = slice(c * 64, c * 64 + 64)
            z = c % 2
            ab, kb = P.banks[z], P.banks[4 + z]
            for d in range(2):
                P.T(lambda e: e.matmul(ab[pb:pb + 64, d * 64:(d + 1) * 64], lhsT=KS[64 * d:64 * d + 64, cs],
                                       rhs=QS[64 * d:64 * d + 64, cs], start=True, stop=True), ["KS", "QS"], [("ab", z)])
            P.V(lambda e: e.tensor_tensor(out=ATTa[pb:pb + 64, i, :], in0=ab[pb:pb + 64, 0:128], in1=C("mtri", pb, pb + 64), op=ALU.mult),
                [("ab", z), "consts"], [("att", c)])
            for d in range(2):
                P.T(lambda e: e.matmul(kb[pb:pb + 64, d * 64:(d + 1) * 64], lhsT=KS[64 * d:64 * d + 64, cs],
                                       rhs=ident[64 * d:64 * d + 64, 64 * d:64 * d + 64], start=True, stop=True), ["KS", "consts"], [("kb", z)])
            P.S(lambda e: e.copy(out=KWa[pb:pb + 64, i, :], in_=kb[pb:pb + 64, 0:128]), [("kb", z)], [("kwa", c)])
        for c in range(NCHK):
            i, h = divmod(c, 2)
            pb = 64 * h
            cs = slice(c * 64, c * 64 + 64)
            z = c % 2
            ob, sbk = P.banks[2 + z], P.banks[6 + z]
            P.T(lambda e: e.matmul(sbk[:, 0:2 * dv], lhsT=KWa[pb:pb + 64, i, :], rhs=V2[pb:pb + 64, i, :, :].rearrange("p d v -> p (d v)"),
                                   start=True, stop=True), [("kwa", c), "V2"], [("sbk", z)])
            P.T(lambda e: e.matmul(ob[pb:pb + 64, 0:2 * dv], lhsT=QS[:, cs], rhs=Sb[:, 0:2 * dv], start=True, stop=False),
                ["QS", "S"], [("ob", z)])
            for d in range(2):
                P.T(lambda e: e.matmul(ob[pb:pb + 64, d * dv:(d + 1) * dv], lhsT=ATTa[pb:pb + 64, i, d * 64:(d + 1) * 64],
                                       rhs=V2[pb:pb + 64, i, d, :], start=False, stop=(d == 1)), [("att", c), "V2"], [("ob", z)])
            if kind == "g":
                P.S(lambda e: e.copy(out=O[pb:pb + 64, i, :, :], in_=ob[pb:pb + 64, 0:256].rearrange("p (d v) -> p d v", d=2)),
                    [("ob", z)], ["O"])
            else:
                P.S(lambda e: e.activation(out=rec[pb:pb + 64, 0:2], in_=ob[pb:pb + 64, 0:258].rearrange("p (d v) -> p d v", d=2)[:, :, 128],
                                           func=AF.Abs), [("ob", z)], ["rec"])
                P.V(lambda e: e.tensor_scalar(out=rec[pb:pb + 64, 0:2], in0=rec[pb:pb + 64, 0:2], scalar1=1.0, scalar2=1.0, op0=ALU.max, op1=ALU.mult),
                    ["rec"], ["rec"])
                P.V(lambda e: e.reciprocal(out=rec[pb:pb + 64, 0:2], in_=rec[pb:pb + 64, 0:2]), ["rec"], ["rec"])
                for d in range(2):
                    P.V(lambda e: e.tensor_scalar_mul(out=O[pb:pb + 64, i, d, :], in0=ob[pb:pb + 64, d * dv:d * dv + 128],
                                                      scalar1=rec[pb:pb + 64, d:d + 1]), [("ob", z), "rec"], ["O"])
            P.V(lambda e: e.tensor_tensor(out=tmpS, in0=Sb, in1=sbk[:, 0:2 * dv], op=ALU.add), ["S", ("sbk", z)], ["tmpS"])
            P.V(lambda e: e.scalar_tensor_tensor(out=Sb, in0=tmpS, scalar=EB[:, c:c + 1], in1=bm, op0=ALU.mult, op1=ALU.mult),
                ["tmpS", "EB", "consts"], ["S"])
        GT = QS
        P.dma(GT, FMS[AI[gname]], w=["QS", "gate"])
        P.S(lambda e: e.activation(out=GT, in_=GT, func=gfn), ["gate"], ["pgate"])
        for gi, (s0, L, g0, n) in enumerate(groups()):
            yb = P.banks[gi % 2]
            for jj in range(n // 128):
                i = (s0 + g0) // 128 + jj
                im = s0 // 128 + (L // 128 - 1 - (i - s0 // 128))
                P.T(lambda e, yb=yb, jj=jj, i=i: e.matmul(yb[:, jj * 128:(jj + 1) * 128], lhsT=O[:, i, 0, :], rhs=ident, start=True, stop=False),
                    ["O", "consts"], ["psrc"])
                P.T(lambda e, yb=yb, jj=jj, im=im: e.matmul(yb[:, jj * 128:(jj + 1) * 128], lhsT=O[:, im, 1, :], rhs=Jm, start=False, stop=True),
                    ["O", "consts"], ["psrc"])
            t0 = s0 + g0
            headnorm(yb[:, 0:n], n, 128, center, 1e-6, gain, GT[:, t0:t0 + n], None, Yb, SQb, RSb, P.banks[2 + gi % 2], "p")
            P.dma(yT[row0:row0 + 128, t0:t0 + n], Yb[:, 0:n], r=["pY"], eng="gpsimd", final=True)
        P.barrier()

    glalike("g", 0)
    glalike("m", 0)
    glalike("m", 1)

    def shift(X, Y, rows, mun):
        cp, cn = PPc(mun, 0, 1, 0, rows), PPc(mun, 1, 1, 0, rows)
        c0 = P.alloc(1)
        P.V(lambda e: e.tensor_tensor(out=c0[0:rows], in0=cp, in1=cn, op=ALU.add), ["consts"], ["c0"])
        P.V(lambda e: e.tensor_scalar(out=c0[0:rows], in0=c0[0:rows], scalar1=-1.0, scalar2=1.0, op0=ALU.mult, op1=ALU.add), ["c0"], ["c0"])
        kx, ky = ("sh", id(X)), ("sh", id(Y))
        P.V(lambda e: e.tensor_scalar_mul(out=Y[0:rows], in0=X[0:rows], scalar1=c0[0:rows]), [kx, "c0"], [ky])
        for (s0, L) in SEGS:
            P.V(lambda e, s0=s0, L=L: e.scalar_tensor_tensor(out=Y[0:rows, s0 + 1:s0 + L], in0=X[0:rows, s0:s0 + L - 1], scalar=cp,
                                                             in1=Y[0:rows, s0 + 1:s0 + L], op0=ALU.mult, op1=ALU.add), [kx, ky, "consts"], [ky])
            P.V(lambda e, s0=s0, L=L: e.scalar_tensor_tensor(out=Y[0:rows, s0:s0 + L - 1], in0=X[0:rows, s0 + 1:s0 + L], scalar=cn,
                                                             in1=Y[0:rows, s0:s0 + L - 1], op0=ALU.mult, op1=ALU.add), [kx, ky, "consts"], [ky])
        return kx, ky

    for arr, mun, rows, fn in (("W1F", "muw1f", 96, AF.Tanh), ("W1B", "muw1b", 96, AF.Tanh), ("A1F", "mua1f", 96, None),
                               ("A1B", "mua1b", 96, None), ("RG01", "mug01", 128, AF.Sigmoid), ("RG2", "mug2", 64, AF.Sigmoid)):
        P.aptr = mark0
        X, Y = P.alloc(T), P.alloc(T)
        kx, ky = ("sh", id(X)), ("sh", id(Y))
        P.dma(X[0:rows], FMS[AI[arr], 0:rows, :], w=[kx])
        shift(X, Y, rows, mun)
        if fn is not None:
            P.S(lambda e, fn=fn: e.activation(out=Y[0:rows], in_=Y[0:rows], func=fn), [ky], [ky])
        P.dma(FMS[AI[arr], 0:rows, :], Y[0:rows], r=[ky], eng="gpsimd")
        P.barrier()

    def rwkv_head(j):
        P.aptr = mark0
        U = [P.alloc(T) for _ in range(9)]
        K_ = lambda i: ("U", i)
        Of = U[2]
        O = Of.rearrange("p (i c) -> p i c", c=128)
        OFF, EW = P.alloc(NCHK), P.alloc(NCHK)
        omka = P.alloc(1)
        SR = P.alloc(5120)
        lrt = [SR[:, z_ * 2048:(z_ + 1) * 2048].rearrange("p (a n) -> p a n", a=4) for z_ in range(2)]
        bst = [SR[:, 4096 + z_ * 512:4096 + (z_ + 1) * 512] for z_ in range(2)]
        for i_, a in enumerate(("RR", "RK", "RV")):
            P.dma(U[i_], FMS[AI[f"{a}{j}"]], w=[("sh", id(U[i_]))])
        shift(U[0], U[3], 128, f"mur{j}")
        shift(U[1], U[4], 128, f"muk{j}")
        shift(U[2], U[0], 128, f"muv{j}")
        R, Kk, Vv = U[3], U[4], U[0]
        kR, kK, kV = ("sh", id(U[3])), ("sh", id(U[4])), ("sh", id(U[0]))
        P.V(lambda e: e.scalar_tensor_tensor(out=U[1], in0=R, scalar=PPc(f"rk{j}"), in1=Kk, op0=ALU.mult, op1=ALU.mult),
            [kR, kK, ("sh", id(U[1])), "consts"], [K_(1)])
        for ti, t0 in enumerate(range(0, T, 512)):
            n = min(512, T - t0)
            z = ti % 2
            bk = P.banks[z]
            P.T(lambda e, bk=bk, t0=t0, n=n: e.matmul(bk[:, 0:n], lhsT=C("bones"), rhs=U[1][:, t0:t0 + n], start=True, stop=True),
                [K_(1), "consts"], [("bk", z)])
            P.V(lambda e, bk=bk, t0=t0, n=n, z=z: e.tensor_tensor(out=bst[z][:, 0:n], in0=bk[:, 0:n], in1=Vv[:, t0:t0 + n], op=ALU.mult),
                [("bk", z), kV], [("bst", z)])
            P.dma(FMS[AI[f"RR{j}"], :, t0:t0 + n], bst[z][:, 0:n], r=[("bst", z)], eng="gpsimd")
        for ti, t0 in enumerate(range(0, T, 512)):
            n = min(512, T - t0)
            z = ti % 2
            for ai_, a in enumerate(("W1F", "W1B", "A1F", "A1B")):
                P.dma(lrt[z][0:96, ai_, 0:n], FMS[AI[a], 0:96, t0:t0 + n], w=[("lrt", z)])
            for w_, (dst, bn, srcs) in enumerate(((U[6], f"w0{j}", (0, 1)), (U[5], f"a0{j}", (2, 3)))):
                bk = P.banks[2 + 2 * w_ + z]
                for d in range(2):
                    P.T(lambda e, bk=bk, d=d, w_=w_, srcs=srcs, z=z, n=n: e.matmul(bk[64 * d:64 * d + 64, 0:n], lhsT=w2v[:, j, w_, d, :],
                                                                                  rhs=lrt[z][0:96, srcs[d], 0:n], start=True, stop=True),
                        [("lrt", z), "consts"], [("bk2", w_, z)])
                P.S(lambda e, bk=bk, dst=dst, bn=bn, t0=t0, n=n: e.activation(out=dst[:, t0:t0 + n], in_=bk[:, 0:n], func=AF.Sigmoid, bias=PPc(bn)),
                    [("bk2", w_, z), "consts"], [K_(5 + (1 - w_))])
        P.V(lambda e: e.tensor_scalar_mul(out=U[1], in0=Kk, scalar1=PPc(f"kk{j}")), [kK, K_(1), "consts"], [K_(1)])
        P.S(lambda e: e.activation(out=U[8], in_=U[1], func=AF.Square), [K_(1)], [K_(8)])
        for ti, t0 in enumerate(range(0, T, 512)):
            n = min(512, T - t0)
            z = ti % 2
            bk = P.banks[z]
            P.T(lambda e, bk=bk, t0=t0, n=n: e.matmul(bk[:, 0:n], lhsT=C("bones"), rhs=U[8][:, t0:t0 + n], start=True, stop=True),
                [K_(8), "consts"], [("bk", z)])
            P.S(lambda e, bk=bk, t0=t0, n=n: e.activation(out=U[7][:, t0:t0 + n], in_=bk[:, 0:n], func=AF.Ln, bias=1e-24), [("bk", z)], [K_(7)])
        P.S(lambda e: e.activation(out=U[7], in_=U[7], func=AF.Exp, scale=-0.5), [K_(7)], [K_(7)])
        P.V(lambda e: e.tensor_tensor(out=U[1], in0=U[1], in1=U[7], op=ALU.mult), [K_(1), K_(7)], [K_(1)])
        P.V(lambda e: e.tensor_scalar(out=omka, in0=PPc(f"ka{j}"), scalar1=-1.0, scalar2=1.0, op0=ALU.mult, op1=ALU.add), ["consts"], ["omka"])
        P.V(lambda e: e.tensor_scalar(out=U[7], in0=U[5], scalar1=PPc(f"ka{j}"), scalar2=omka, op0=ALU.mult, op1=ALU.add),
            [K_(5), K_(7), "omka", "consts"], [K_(7)])
        P.V(lambda e: e.tensor_tensor(out=Kk, in0=Kk, in1=U[7], op=ALU.mult), [kK, K_(7), K_(1)], [kK])
        P.V(lambda e: e.tensor_scalar(out=U[6], in0=U[6], scalar1=-float(np.exp(-0.5)), scalar2=0.0, op0=ALU.mult, op1=ALU.add), [K_(6)], [K_(6)])
        P.V(lambda e: e.tensor_tensor_scan(out=U[7], data0=ones_b.to_broadcast([128, T]), data1=U[6], initial=0.0, op0=ALU.mult, op1=ALU.add),
            [K_(6), K_(7), kK, "consts"], ["EX"])
        chunk_local(U[7], OFF)
        P.S(lambda e: e.activation(out=U[8], in_=U[7], func=AF.Exp), ["EX", K_(8)], [K_(8)])
        P.V(lambda e: e.tensor_tensor(out=R, in0=R, in1=U[8], op=ALU.mult), [kR, K_(8), K_(1)], [kR])
        P.S(lambda e: e.activation(out=U[6], in_=U[6], func=AF.Exp, scale=-1.0), [K_(6), "EX"], [K_(6)])
        P.V(lambda e: e.tensor_tensor(out=U[6], in0=U[6], in1=U[8], op=ALU.mult), [K_(6), K_(8)], [K_(6)])
        P.V(lambda e: e.scalar_tensor_tensor(out=U[6], in0=U[6], scalar=-1.0, in1=U[1], op0=ALU.mult, op1=ALU.mult), [K_(6), K_(1)], [K_(6)])
        P.S(lambda e: e.activation(out=U[8], in_=U[7], func=AF.Exp, scale=-1.0), ["EX", K_(8), kR, K_(6)], [K_(8)])
        P.V(lambda e: e.tensor_tensor(out=U[1], in0=U[1], in1=U[5], op=ALU.mult), [K_(1), K_(5), K_(6)], [K_(1)])
        P.V(lambda e: e.tensor_tensor(out=U[1], in0=U[1], in1=U[8], op=ALU.mult), [K_(1), K_(8)], [K_(1)])
        P.V(lambda e: e.tensor_tensor(out=Kk, in0=Kk, in1=U[8], op=ALU.mult), [kK, K_(8)], [kK])
        P.S(lambda e: e.activation(out=EW[:, 0:NCHK], in_=v3(U[7])[:, :, 63], func=AF.Exp), ["EX"], ["EW"])
        P.V(lambda e: e.tensor_tensor(out=v3(U[5]), in0=v3(U[1]), in1=EW[:, 0:NCHK].unsqueeze(2).to_broadcast([128, NCHK, 64]), op=ALU.mult),
            [K_(1), K_(5), "EW"], [K_(5)])
        P.V(lambda e: e.tensor_tensor(out=v3(U[7]), in0=v3(Kk), in1=EW[:, 0:NCHK].unsqueeze(2).to_broadcast([128, NCHK, 64]), op=ALU.mult),
            [kK, "EX", "EW", K_(8)], ["KW"])
        AH, BH, KH, RH, BW, KW = U[6], U[1], Kk, R, U[5], U[7]
        kAH, kBH, kKH, kRH, kBW, kKW = K_(6), K_(1), kK, kR, K_(5), "KW"
        P.barrier()
        pool = [[U[8], 0, T], [SR, 0, 5120]]

        def carve(n):
            for pl in pool:
                if pl[1] + n <= pl[2]:
                    a_ = pl[0][:, pl[1]:pl[1] + n]
                    pl[1] += n
                    return a_
            return P.alloc(n)
        prod = {nm: [carve(256) for _ in range(2)] for nm in ("AAK", "ARK", "NT", "ARB", "NN")}
        tmb = {nm: [carve(128) for _ in range(2)] for nm in ("V", "BW", "KW")}
        Yi = [carve(256) for _ in range(2)]
        Zi = [carve(256) for _ in range(2)]
        Qb = [carve(256) for _ in range(2)]
        rhs_sb, u_sb = carve(128), carve(128)
        Sb, tmpS = carve(128), carve(128)
        Yb, SQb, RSb, Gt, Bt = carve(512), carve(512), carve(512), carve(512), carve(512)
        P.G(lambda e: e.memset(Sb, 0.0), [], ["S"])
        pc = 0
        for i in range(NT):
            ts = slice(128 * i, 128 * i + 128)
            z = i % 2
            specs = (("AAK", KH, AH, kKH, kAH, "MS"), ("ARK", KH, RH, kKH, kRH, "MI"), ("NT", BH, AH, kBH, kAH, "MS"),
                     ("ARB", BH, RH, kBH, kRH, "MI"), ("NN", AH, BH, kAH, kBH, "ML"))
            for nm, Lh, Rh, kl, kr, mk in specs:
                bi = pc % 2
                pc += 1
                bk = P.banks[bi]
                for d in range(2):
                    P.T(lambda e, bk=bk, d=d, Lh=Lh, Rh=Rh, ts=ts: e.matmul(bk[:, d * 128:(d + 1) * 128], lhsT=Lh[64 * d:64 * d + 64, ts],
                                                                           rhs=Rh[64 * d:64 * d + 64, ts], start=True, stop=True), [kl, kr], [("pb", bi)])
                P.V(lambda e, bk=bk, nm=nm, mk=mk, z=z: e.tensor_tensor(out=prod[nm][z], in0=bk[:, 0:256], in1=C(mk), op=ALU.mult),
                    [("pb", bi), "consts"], [(nm, z)])
            for nm, Xs, kx_ in (("V", Vv, kV), ("BW", BW, kBW), ("KW", KW, kKW)):
                bi = pc % 2
                pc += 1
                bk = P.banks[bi]
                P.T(lambda e, bk=bk, Xs=Xs, ts=ts: e.transpose(bk[:, 0:128], Xs[:, ts], ident), [kx_, "consts"], [("pb", bi)])
                P.S(lambda e, bk=bk, nm=nm, z=z: e.copy(out=tmb[nm][z], in_=bk[:, 0:128]), [("pb", bi)], [("tm" + nm, z)])
            Yc, Zc, Q = prod["NT"][z], prod["NN"][z], Qb[z]
            kY, kZ = ("NT", z), ("NN", z)
            P.V(lambda e, Q=Q, Yc=Yc: e.tensor_tensor(out=Q, in0=Yc, in1=C("I2"), op=ALU.add), [kY, "consts"], [("Q", z)])
            for it in range(5):
                Yn, Zn = Yi[it % 2], Zi[it % 2]
                for d in range(2):
                    ds_ = slice(d * 128, (d + 1) * 128)
                    P.T(lambda e, ds_=ds_, Yc=Yc, Zc=Zc: e.matmul(P.banks[2][:, ds_], lhsT=Zc[:, ds_], rhs=Yc[:, ds_], start=True, stop=True),
                        [kY, kZ], ["bY"])
                    P.T(lambda e, ds_=ds_, Yc=Yc, Zc=Zc: e.matmul(P.banks[3][:, ds_], lhsT=Yc[:, ds_], rhs=Zc[:, ds_], start=True, stop=True),
                        [kY, kZ], ["bZ"])
                P.S(lambda e, Yn=Yn: e.copy(out=Yn, in_=P.banks[2][:, 0:256]), ["bY"], [("Yi", it % 2)])
                P.V(lambda e, Zn=Zn: e.tensor_copy(out=Zn, in_=P.banks[3][:, 0:256]), ["bZ"], [("Zi", it % 2)])
                Yc, Zc, kY, kZ = Yn, Zn, ("Yi", it % 2), ("Zi", it % 2)
                for d in range(2):
                    ds_ = slice(d * 128, (d + 1) * 128)
                    P.T(lambda e, ds_=ds_, Zc=Zc, Q=Q: e.matmul(P.banks[4][:, ds_], lhsT=Zc[:, ds_], rhs=Q[:, ds_], start=True, stop=True),
                        [kZ, ("Q", z)], ["bQ"])
                P.V(lambda e, Q=Q: e.tensor_tensor(out=Q, in0=Q, in1=P.banks[4][:, 0:256], op=ALU.add), ["bQ", ("Q", z)], [("Q", z)])
            AAK, ARK, ARB = prod["AAK"][z], prod["ARK"][z], prod["ARB"][z]
            Vt, BWt, KWt = tmb["V"][z], tmb["BW"][z], tmb["KW"][z]
            for h in range(2):
                c = 2 * i + h
                pb = 64 * h
                cs = slice(c * 64, c * 64 + 64)
                bR, bU = P.banks[5][pb:pb + 64, 0:128], P.banks[5][pb:pb + 64, 128:256]
                bY2, bS = P.banks[6][pb:pb + 64, 0:128], P.banks[7][:, 0:128]

                def blk(M_, d, pb=pb):
                    return M_[pb:pb + 64, d * 128 + pb:d * 128 + pb + 64]
                P.T(lambda e, bR=bR, cs=cs: e.matmul(bR, lhsT=AH[:, cs], rhs=Sb, start=True, stop=False), [kAH, "S"], ["bR"])
                for d in range(2):
                    P.T(lambda e, d=d, bR=bR, pb=pb: e.matmul(bR[:, d * 64:(d + 1) * 64], lhsT=blk(AAK, d), rhs=Vt[pb:pb + 64, d * 64:(d + 1) * 64],
                                                              start=False, stop=(d == 1)), [("AAK", z), ("tmV", z)], ["bR"])
                P.S(lambda e, bR=bR, pb=pb: e.copy(out=rhs_sb[pb:pb + 64, :], in_=bR), ["bR"], ["rhs_sb"])
                for d in range(2):
                    P.T(lambda e, d=d, bU=bU, pb=pb: e.matmul(bU[:, d * 64:(d + 1) * 64], lhsT=blk(Q, d), rhs=rhs_sb[pb:pb + 64, d * 64:(d + 1) * 64],
                                                              start=True, stop=True), [("Q", z), "rhs_sb"], ["bU"])
                P.V(lambda e, bU=bU, pb=pb: e.tensor_copy(out=u_sb[pb:pb + 64, :], in_=bU), ["bU"], ["u_sb"])
                P.T(lambda e, bY2=bY2, cs=cs: e.matmul(bY2, lhsT=RH[:, cs], rhs=Sb, start=True, stop=False), [kRH, "S"], ["bY2"])
                for d in range(2):
                    P.T(lambda e, d=d, bY2=bY2, pb=pb: e.matmul(bY2[:, d * 64:(d + 1) * 64], lhsT=blk(ARB, d), rhs=u_sb[pb:pb + 64, d * 64:(d + 1) * 64],
                                                                start=False, stop=False), [("ARB", z), "u_sb"], ["bY2"])
                    P.T(lambda e, d=d, bY2=bY2, pb=pb: e.matmul(bY2[:, d * 64:(d + 1) * 64], lhsT=blk(ARK, d), rhs=Vt[pb:pb + 64, d * 64:(d + 1) * 64],
                                                                start=False, stop=(d == 1)), [("ARK", z), ("tmV", z)], ["bY2"])
                P.S(lambda e, bY2=bY2, pb=pb, i=i: e.copy(out=O[pb:pb + 64, i, :], in_=bY2), ["bY2", K_(2), ("sh", id(U[2]))], ["O"])
                P.T(lambda e, bS=bS, pb=pb: e.matmul(bS, lhsT=BWt[pb:pb + 64, :], rhs=u_sb[pb:pb + 64, :], start=True, stop=False),
                    [("tmBW", z), "u_sb"], ["bS"])
                P.T(lambda e, bS=bS, pb=pb: e.matmul(bS, lhsT=KWt[pb:pb + 64, :], rhs=Vt[pb:pb + 64, :], start=False, stop=True),
                    [("tmKW", z), ("tmV", z)], ["bS"])
                P.V(lambda e, bS=bS, c=c: e.scalar_tensor_tensor(out=tmpS, in0=Sb, scalar=EW[:, c:c + 1], in1=bS, op0=ALU.mult, op1=ALU.add),
                    ["S", "EW", "bS"], ["tmpS"])
                P.V(lambda e: e.tensor_tensor(out=Sb, in0=tmpS, in1=C("bm64"), op=ALU.mult), ["tmpS", "consts"], ["S"])
        garr, gr0 = (("RG01", 0), ("RG01", 64), ("RG2", 0))[j]
        for gi, (s0, L, g0, n) in enumerate(groups()):
            yb = P.banks[gi % 2]
            t0 = s0 + g0
            for jj in range(n // 128):
                i = t0 // 128 + jj
                im = s0 // 128 + (L // 128 - 1 - (i - s0 // 128))
                P.T(lambda e, yb=yb, jj=jj, i=i: e.matmul(yb[0:64, jj * 128:(jj + 1) * 128], lhsT=O[:, i, 0:64], rhs=ident, start=True, stop=False),
                    ["O", "consts"], ["rsrc"])
                P.T(lambda e, yb=yb, jj=jj, im=im: e.matmul(yb[0:64, jj * 128:(jj + 1) * 128], lhsT=O[:, im, 64:128], rhs=Jm, start=False, stop=True),
                    ["O", "consts"], ["rsrc"])
            P.dma(Gt[0:64, 0:n], FMS[AI[garr], gr0:gr0 + 64, t0:t0 + n], w=["rgate"])
            P.dma(Bt[0:64, 0:n], FMS[AI[f"RR{j}"], 0:64, t0:t0 + n], w=["rbonus"])
            headnorm(yb[0:64, 0:n], n, 64, True, 64e-5, PPc(f"rgn{j}", 0, 1, 0, 64), Gt[0:64, 0:n], Bt[0:64, 0:n], Yb, SQb, RSb, P.banks[2 + gi % 2], "r")
            P.dma(yT[384 + 64 * j:448 + 64 * j, t0:t0 + n], Yb[0:64, 0:n], r=["rY"], eng="gpsimd", final=True)
        P.barrier()

    for j in range(3):
        rwkv_head(j)
    P.emit()
    P.close()
    return nc


GLA_BASE, ML_BASE, RW_BASE = 0, 1568, 3896


def _wcols(g):
    ar = np.arange
    cols = {"gq": g * 64 + ar(64), "gk": 256 + g * 64 + ar(64), "glrf": 1536 + ar(16), "glrb": 1552 + ar(16),
            "gg": 1024 + g * 128 + ar(128), "gv": 512 + g * 128 + ar(128)}
    for s, h in enumerate(MSLOT[g]):
        cols[f"mq{s}"] = ML_BASE + h * 64 + ar(64)
        cols[f"mk{s}"] = ML_BASE + 384 + h * 64 + ar(64)
        cols[f"mv{s}"] = ML_BASE + 768 + h * 128 + ar(128)
        cols[f"mo{s}"] = ML_BASE + 1536 + h * 128 + ar(128)
        for nm, d, w in (("mif", 0, 0), ("mff", 0, 1), ("mib", 1, 0), ("mfb", 1, 1)):
            cols[f"{nm}{s}"] = np.full(64, ML_BASE + 2304 + d * 12 + w * 6 + h)
    for j in range(3):
        hh = 3 * g + j
        for k_, nm in enumerate(("rr", "rk", "rv", "rg")):
            cols[f"{nm}{j}"] = RW_BASE + 768 * k_ + hh * 64 + ar(64)
    for k_, nm in enumerate(("w1f", "w1b", "a1f", "a1b")):
        cols[nm] = RW_BASE + 3072 + 96 * k_ + ar(96)
    return cols


def prep_B(l, b, g, inp, mod, xc, xl):
    f32 = np.float32
    cols = _wcols(g)
    w_in = inp["w_in"][l]
    wfm = np.concatenate([w_in[:, cols[n]] for n, _ in WBLK], axis=1)
    wtm = np.concatenate([w_in[:, cols[n]] for n in ("gv", "mv0", "mv1")], axis=1)
    xT = np.ascontiguousarray(np.concatenate([xc[b], xl[b]], 0).T)
    sh1, sc1 = mod[l][:, 0:D], mod[l][:, D:2 * D]
    modv = np.stack([pk(inp["g_norm1"][l]), pk(sc1[2]), pk(sh1[2]), pk(sc1[b]), pk(sh1[b])], axis=2).reshape(128, KC * 5)
    pp = np.zeros((128, NPP), f32)

    def put(n, a, r0=0):
        o, w = PPN[n]
        a = np.asarray(a, f32).reshape(-1, w)
        pp[r0:r0 + a.shape[0], o:o + w] = a
    put("gba", inp["gla_b_a"][l][0, g * 64:(g + 1) * 64])
    put("gba", inp["gla_b_a"][l][1, g * 64:(g + 1) * 64], 64)
    put("ggn", inp["gla_g_norm"][l][g * 128:(g + 1) * 128])
    cwt = inp["ml_conv_w"][l].reshape(9, 768)
    for s, h in enumerate(MSLOT[g]):
        for nm, c0 in (("q", h * 64), ("k", 384 + h * 64)):
            wv = cwt[:, c0:c0 + 64].T
            put(f"cw{nm}{s}", wv)
            put(f"cw{nm}{s}", wv[:, ::-1], 64)
            put(f"cb{nm}{s}", inp["ml_conv_b"][l][c0:c0 + 64])
            put(f"cb{nm}{s}", inp["ml_conv_b"][l][c0:c0 + 64], 64)
        for d in range(2):
            put(f"mbi{s}", np.full(64, inp["ml_gate_b"][l][d, 0, h]), 64 * d)
            put(f"mbf{s}", np.full(64, inp["ml_gate_b"][l][d, 1, h]), 64 * d)
        put(f"mgn{s}", inp["ml_g_norm"][l][h * 128:(h + 1) * 128])
    mu = inp["rw_mu"][l]

    def mupair(c, swap):
        m = np.stack([mu[0][c], mu[1][c]], 1)
        return m[:, ::-1] if swap else m
    for j in range(3):
        hh = 3 * g + j
        hc = hh * 64 + np.arange(64)
        for nm, off in (("mur", 0), ("muk", 768), ("muv", 1536)):
            put(f"{nm}{j}", mupair(off + hc, False))
            put(f"{nm}{j}", mupair(off + hc, True), 64)
        for d in range(2):
            put(f"w0{j}", inp["rw_w0"][l][d, hc], 64 * d)
            put(f"a0{j}", inp["rw_a0"][l][d, hc], 64 * d)
            put(f"kk{j}", inp["rw_k_k"][l][hc], 64 * d)
            put(f"ka{j}", inp["rw_k_a"][l][hc], 64 * d)
            put(f"rk{j}", inp["rw_r_k"][l][hc], 64 * d)
        put(f"rgn{j}", inp["rw_g_norm"][l][hc])
    put("mug01", mupair(2304 + (3 * g) * 64 + np.arange(64), False))
    put("mug01", mupair(2304 + (3 * g + 1) * 64 + np.arange(64), False), 64)
    put("mug2", mupair(2304 + (3 * g + 2) * 64 + np.arange(64), False))
    put("muw1f", mupair(3072 + np.arange(96), False))
    put("muw1b", mupair(3168 + np.arange(96), True))
    put("mua1f", mupair(3264 + np.arange(96), False))
    put("mua1b", mupair(3360 + np.arange(96), True))
    wa2 = np.zeros((128, 64), f32)
    wa2[0:16] = inp["gla_w_a2"][l][0][:, g * 64:(g + 1) * 64]
    wa2[64:80] = inp["gla_w_a2"][l][1][:, g * 64:(g + 1) * 64]
    w2 = np.zeros((96, 3, 2, 2, 64), f32)
    for j in range(3):
        hc = (3 * g + j) * 64 + np.arange(64)
        for d in range(2):
            w2[:, j, 0, d] = inp["rw_w2"][l][d][:, hc]
            w2[:, j, 1, d] = inp["rw_a2"][l][d][:, hc]
    return {"xT": xT, "modv": np.ascontiguousarray(modv, f32), "wfm": np.ascontiguousarray(wfm), "wtm": np.ascontiguousarray(wtm),
            "cst": make_consts(), "pp": pp, "wa2": wa2, "w2": w2.reshape(96, 768)}


def assemble_yT(res, B, T):
    yT = np.zeros((B, D, T), np.float32)
    for b in range(B):
        for g in range(4):
            r = res[b * 4 + g]["yT"]
            yT[b, g * 128:(g + 1) * 128] = r[0:128]
            for s, h in enumerate(MSLOT[g]):
                yT[b, 512 + h * 128:512 + (h + 1) * 128] = r[128 + 128 * s:256 + 128 * s]
            for j in range(3):
                hh = 3 * g + j
                yT[b, 1280 + hh * 64:1280 + (hh + 1) * 64] = r[384 + 64 * j:448 + 64 * j]
    return yT


def build_C1(NCc, NLl):
    NTK = NCc + NLl
    TW = 256
    BIG = 1.0e30
    nc = bass.Bass("TRN2", target_bir_lowering=False)

    def din(name, shape, dt=F32):
        return nc.dram_tensor(name, list(shape), dt, kind="ExternalInput").ap()
    yT, xT = din("yT", [D, NTK]), din("xT", [D, NTK])
    wo_d, modv_d, wr_d, br_d, cst_d = din("wo", [D, D]), din("modv", [128, KC * 7]), din("wr", [D, 36]), din("br", [128, 36]), din("cst", [128, NCST])
    x1T = nc.dram_tensor("x1T", [D, NTK], F32, kind="ExternalOutput").ap()
    h2T = nc.dram_tensor("h2T", [D, NTK], BF16, kind="ExternalOutput").ap()
    gates = nc.dram_tensor("gates", [NTK, 32], F32, kind="ExternalOutput").ap()
    P = Prog(nc)
    cst = P.sbuf([128, NCST], F32, "cst_sb")
    mv = P.sbuf([128, KC * 7], F32, "mv_sb")
    wr = P.sbuf([128, KC * 36], F32, "wr_sb")
    br = P.sbuf([128, 36], F32, "br_sb")
    gm = P.sbuf([128, KC * 2], F32, "gm_sb")
    P.dma(cst[:], cst_d, w=["consts"])
    P.dma(mv[:], modv_d, w=["consts"])
    P.dma(br[:], br_d, w=["consts"])
    wrv = wr[:].rearrange("p (k n) -> p k n", n=36)
    P.dma(wrv, wr_d.rearrange("(k p) n -> p k n", p=128), w=["consts"])
    mvv = mv[:].rearrange("p (k j) -> p k j", j=7)
    gmv = gm[:].rearrange("p (k j) -> p k j", j=2)
    ones = cst[:, CSTN["ones"][0]:CSTN["ones"][0] + 128]
    for j, col in enumerate((2, 5)):
        P.V(lambda e: e.tensor_scalar(out=gmv[:, :, j], in0=mvv[:, :, col], scalar1=1.0, scalar2=1.0, op0=ALU.add, op1=ALU.mult), ["consts"], ["gm"])
        P.V(lambda e: e.tensor_tensor(out=gmv[:, :, j], in0=gmv[:, :, j], in1=mvv[:, :, 0], op=ALU.mult), ["gm", "consts"], ["gm"])
    wo = P.alloc(KC * D, BF16).rearrange("p (k n) -> p k n", n=D)
    for c0 in range(0, D, 512):
        P.dma(wo[:, :, c0:c0 + 512], wo_d[:, c0:c0 + 512].rearrange("(k p) n -> p k n", p=128), w=["wo"], eng="gpsimd")
    yb = [P.alloc(KC * TW, BF16).rearrange("p (k n) -> p k n", n=TW) for _ in range(2)]
    xb = [P.alloc(KC * TW).rearrange("p (k n) -> p k n", n=TW) for _ in range(2)]
    sq = P.alloc(KC * TW).rearrange("p (k n) -> p k n", n=TW)
    hb = P.alloc(KC * TW, BF16).rearrange("p (k n) -> p k n", n=TW)
    rstd = P.alloc(TW)
    Lg, LM, I1, I2_, G1 = P.alloc(36), P.alloc(32), P.alloc(32), P.alloc(32), P.alloc(32)
    sm = P.alloc(16)
    tiles = [(s0, o, min(TW, L - o)) for (s0, L) in ((0, NCc), (NCc, NLl)) for o in range(0, L, TW)]
    for ti, (s0, o, n) in enumerate(tiles):
        b = ti % 2
        t0 = s0 + o
        sg = 0 if s0 == 0 else 1
        yt, xt = yb[b], xb[b]
        P.dma(yt[:, :, 0:n], yT[:, t0:t0 + n].rearrange("(k p) t -> p k t", p=128), w=[("yt", b)], eng="gpsimd")
        P.dma(xt[:, :, 0:n], xT[:, t0:t0 + n].rearrange("(k p) t -> p k t", p=128), w=[("xt", b)])
        for fo in range(KC):
            bi = fo % 4
            bank = P.banks[1 + bi]
            for k in range(KC):
                P.T(lambda e: e.matmul(bank[:, 0:n], lhsT=wo[:, k, fo * 128:(fo + 1) * 128], rhs=yt[:, k, 0:n], start=(k == 0), stop=(k == KC - 1)),
                    ["wo", ("yt", b)], [("bank", bi)], inc=(k == KC - 1))
            P.V(lambda e: e.scalar_tensor_tensor(out=xt[:, fo, 0:n], in0=bank[:, 0:n], scalar=mvv[:, fo, 1 + 3 * sg:2 + 3 * sg], in1=xt[:, fo, 0:n],
                                                 op0=ALU.mult, op1=ALU.add), [("bank", bi), ("xt", b), "consts"], [("xt", b)])
        P.dma(x1T[:, t0:t0 + n].rearrange("(k p) t -> p k t", p=128), xt[:, :, 0:n], r=[("xt", b)], eng="gpsimd", final=True)
        rms_rstd(P, xt, n, ones, P.banks[0], sq, rstd, [("xt", b)], "rstd", 0)
        P.V(lambda e: e.tensor_tensor(out=sq[:, :, 0:n], in0=xt[:, :, 0:n], in1=rstd[:, 0:n].unsqueeze(1).to_broadcast([128, KC, n]), op=ALU.mult),
            [("xt", b), "rstd", ("sq", 0)], [("sq", 0)])
        for k in range(KC):
            P.V(lambda e: e.tensor_scalar(out=sq[:, k, 0:n], in0=sq[:, k, 0:n], scalar1=gmv[:, k, sg:sg + 1],
                                          scalar2=mvv[:, k, 3 + 3 * sg:4 + 3 * sg], op0=ALU.mult, op1=ALU.add), [("sq", 0), "gm", "consts"], [("sq", 0)])
        P.G(lambda e: e.tensor_copy(out=hb[:, :, 0:n], in_=sq[:, :, 0:n]), [("sq", 0)], ["hb"])
        P.dma(h2T[:, t0:t0 + n].rearrange("(k p) t -> p k t", p=128), hb[:, :, 0:n], r=["hb"], eng="gpsimd", final=True)
        for m0 in range(0, n, 128):
            m = min(128, n - m0)
            bank = P.banks[5]
            for k in range(KC):
                P.T(lambda e: e.matmul(bank[0:m, 0:36], lhsT=sq[:, k, m0:m0 + m], rhs=wrv[:, k, :], start=(k == 0), stop=(k == KC - 1)),
                    [("sq", 0), "consts"], ["rb"], inc=(k == KC - 1))
            A = lambda t_, c0=0, c1=None: t_[0:m, c0:(c1 if c1 is not None else t_.shape[1])]
            P.V(lambda e: e.tensor_tensor(out=A(Lg), in0=bank[0:m, 0:36], in1=br[0:m, :], op=ALU.add), ["rb", "consts"], ["Lg"])
            P.V(lambda e: e.reduce_max(out=sm[0:m, 0:1], in_=Lg[0:m, 0:4], axis=AX.X), ["Lg"], ["sm"])
            P.V(lambda e: e.tensor_scalar(out=sm[0:m, 1:2], in0=sm[0:m, 0:1], scalar1=-1.0, scalar2=0.0, op0=ALU.mult, op1=ALU.add), ["sm"], ["sm"])
            P.S(lambda e: e.activation(out=sm[0:m, 8:12], in_=Lg[0:m, 0:4], func=AF.Exp, bias=sm[0:m, 1:2], accum_out=sm[0:m, 2:3]), ["Lg", "sm"], ["sm"])
            P.V(lambda e: e.reciprocal(out=sm[0:m, 3:4], in_=sm[0:m, 2:3]), ["sm"], ["sm"])
            P.V(lambda e: e.tensor_scalar(out=sm[0:m, 12:16], in0=Lg[0:m, 0:4], scalar1=sm[0:m, 0:1], scalar2=BIG, op0=ALU.is_ge, op1=ALU.mult),
                ["Lg", "sm"], ["sm"])
            P.V(lambda e: e.tensor_scalar(out=sm[0:m, 12:16], in0=sm[0:m, 12:16], scalar1=-BIG, scalar2=1.0, op0=ALU.add, op1=ALU.mult), ["sm"], ["sm"])
            P.V(lambda e: e.tensor_tensor(out=LM[0:m, :].rearrange("p (g x) -> p g x", g=4), in0=Lg[0:m, 4:36].rearrange("p (g x) -> p g x", g=4),
                                          in1=sm[0:m, 12:16].unsqueeze(2).to_broadcast([m, 4, 8]), op=ALU.add), ["Lg", "sm"], ["LM"])
            P.V(lambda e: e.reduce_max(out=sm[0:m, 4:5], in_=LM[0:m, :], axis=AX.X), ["LM"], ["sm"])
            P.V(lambda e: e.tensor_scalar(out=I1[0:m, :], in0=LM[0:m, :], scalar1=sm[0:m, 4:5], scalar2=1.0, op0=ALU.is_ge, op1=ALU.mult), ["LM", "sm"], ["I1"])
            P.V(lambda e: e.scalar_tensor_tensor(out=LM[0:m, :], in0=I1[0:m, :], scalar=-BIG, in1=LM[0:m, :], op0=ALU.mult, op1=ALU.add), ["I1", "LM"], ["LM"])
            P.V(lambda e: e.reduce_max(out=sm[0:m, 5:6], in_=LM[0:m, :], axis=AX.X), ["LM"], ["sm"])
            P.V(lambda e: e.tensor_scalar(out=I2_[0:m, :], in0=LM[0:m, :], scalar1=sm[0:m, 5:6], scalar2=1.0, op0=ALU.is_ge, op1=ALU.mult), ["LM", "sm"], ["I2"])
            P.V(lambda e: e.tensor_tensor(out=sm[0:m, 6:7], in0=sm[0:m, 4:5], in1=sm[0:m, 5:6], op=ALU.subtract), ["sm"], ["sm"])
            P.S(lambda e: e.activation(out=sm[0:m, 6:7], in_=sm[0:m, 6:7], func=AF.Sigmoid), ["sm"], ["sm"])
            P.V(lambda e: e.tensor_tensor(out=sm[0:m, 6:7], in0=sm[0:m, 6:7], in1=sm[0:m, 3:4], op=ALU.mult), ["sm"], ["sm"])
            P.V(lambda e: e.tensor_tensor(out=sm[0:m, 7:8], in0=sm[0:m, 3:4], in1=sm[0:m, 6:7], op=ALU.subtract), ["sm"], ["sm"])
            P.V(lambda e: e.tensor_scalar_mul(out=G1[0:m, :], in0=I1[0:m, :], scalar1=sm[0:m, 6:7]), ["I1", "sm"], ["G1"])
            P.V(lambda e: e.scalar_tensor_tensor(out=G1[0:m, :], in0=I2_[0:m, :], scalar=sm[0:m, 7:8], in1=G1[0:m, :], op0=ALU.mult, op1=ALU.add),
                ["I2", "sm", "G1"], ["G1"])
            P.dma(gates[t0 + m0:t0 + m0 + m, :], G1[0:m, :], r=["G1"], eng="gpsimd", final=True)
    P.emit()
    P.close()
    return nc


def build_C2(TT, DE):
    NK = DE // 128
    TW = 512
    nc = bass.Bass("TRN2", target_bir_lowering=False)
    h2T = nc.dram_tensor("h2T", [D, TT], BF16, kind="ExternalInput").ap()
    gT = nc.dram_tensor("gT", [4, TT], F32, kind="ExternalInput").ap()
    wg_d = nc.dram_tensor("wg", [4, D, DE], F32, kind="ExternalInput").ap()
    wu_d = nc.dram_tensor("wu", [4, D, DE], F32, kind="ExternalInput").ap()
    wd_d = nc.dram_tensor("wd", [4, DE, D], F32, kind="ExternalInput").ap()
    fT = nc.dram_tensor("fT", [D, TT], F32, kind="ExternalOutput").ap()
    P = Prog(nc)
    wg = [P.alloc(KC * DE, BF16).rearrange("p (k n) -> p k n", n=DE) for _ in range(2)]
    wu = [P.alloc(KC * DE, BF16).rearrange("p (k n) -> p k n", n=DE) for _ in range(2)]
    wd = [P.alloc(NK * D, BF16).rearrange("p (k n) -> p k n", n=D) for _ in range(2)]
    hb = [P.alloc(KC * TW, BF16).rearrange("p (k n) -> p k n", n=TW) for _ in range(2)]
    Ab = [[[P.alloc(TW, BF16) for _ in range(NK)] for _ in range(2)] for _ in range(2)]
    gb = [[P.alloc(TW) for _ in range(2)] for _ in range(2)]
    sgb = [P.alloc(TW) for _ in range(2)]
    ost = [P.alloc(TW) for _ in range(3)]
    prv = [P.alloc(TW) for _ in range(2)]
    tiles = [(t0, min(TW, TT - t0)) for t0 in range(0, TT, TW)]
    for p in range(2):
        for e_ in range(2):
            ex = 2 * p + e_
            P.dma(wg[e_][:, :, :], wg_d[ex].rearrange("(k p) n -> p k n", p=128), w=[("W", e_)], eng="gpsimd")
            P.dma(wu[e_][:, :, :], wu_d[ex].rearrange("(k p) n -> p k n", p=128), w=[("W", e_)], eng="gpsimd")
            for c0 in range(0, D, 512):
                P.dma(wd[e_][:, :, c0:c0 + 512], wd_d[ex, :, c0:c0 + 512].rearrange("(k p) n -> p k n", p=128), w=[("W", e_)], eng="gpsimd")
        gc = 0
        for ti, (t0, n) in enumerate(tiles):
            b = ti % 2
            h = hb[b]
            P.dma(h[:, :, 0:n], h2T[:, t0:t0 + n].rearrange("(k p) t -> p k t", p=128), w=[("h", b)])
            for e_ in range(2):
                P.dma(gb[b][e_][:, 0:n], gT[2 * p + e_:2 * p + e_ + 1, t0:t0 + n].partition_broadcast(128), w=[("g", b, e_)])
            for e_ in range(2):
                for kc in range(NK):
                    z = gc % 2
                    gc += 1
                    bG, bU = P.banks[z], P.banks[2 + z]
                    for k in range(KC):
                        P.T(lambda e: e.matmul(bG[:, 0:n], lhsT=wg[e_][:, k, kc * 128:(kc + 1) * 128], rhs=h[:, k, 0:n], start=(k == 0), stop=(k == KC - 1)),
                            [("W", e_), ("h", b)], [("bG", z)], inc=(k == KC - 1))
                    for k in range(KC):
                        P.T(lambda e: e.matmul(bU[:, 0:n], lhsT=wu[e_][:, k, kc * 128:(kc + 1) * 128], rhs=h[:, k, 0:n], start=(k == 0), stop=(k == KC - 1)),
                            [("W", e_), ("h", b)], [("bU", z)], inc=(k == KC - 1))
                    P.S(lambda e: e.activation(out=sgb[z][:, 0:n], in_=bG[:, 0:n], func=AF.Silu), [("bG", z)], [("sg", z)])
                    P.V(lambda e: e.tensor_tensor(out=sgb[z][:, 0:n], in0=sgb[z][:, 0:n], in1=bU[:, 0:n], op=ALU.mult), [("sg", z), ("bU", z)], [("sg", z)])
                    P.G(lambda e: e.tensor_tensor(out=Ab[b][e_][kc][:, 0:n], in0=sgb[z][:, 0:n], in1=gb[b][e_][:, 0:n], op=ALU.mult),
                        [("sg", z), ("g", b, e_)], [("A", b)])
            for fo in range(KC):
                z = fo % 2
                bF = P.banks[4 + z]
                o_ = ost[fo % 3]
                first = True
                for e_ in range(2):
                    for kc in range(NK):
                        last = (e_ == 1 and kc == NK - 1)
                        P.T(lambda e: e.matmul(bF[:, 0:n], lhsT=wd[e_][:, kc, fo * 128:(fo + 1) * 128], rhs=Ab[b][e_][kc][:, 0:n], start=first, stop=last),
                            [("W", e_), ("A", b)], [("bF", z)], inc=last)
                        first = False
                if p == 0:
                    P.S(lambda e: e.copy(out=o_[:, 0:n], in_=bF[:, 0:n]), [("bF", z)], [("ost", fo % 3)])
                else:
                    pv = prv[fo % 2]
                    P.dma(pv[:, 0:n], fT[fo * 128:(fo + 1) * 128, t0:t0 + n], w=[("prv", fo % 2)])
                    P.V(lambda e: e.tensor_tensor(out=o_[:, 0:n], in0=bF[:, 0:n], in1=pv[:, 0:n], op=ALU.add), [("bF", z), ("prv", fo % 2)], [("ost", fo % 3)])
                P.dma(fT[fo * 128:(fo + 1) * 128, t0:t0 + n], o_[:, 0:n], r=[("ost", fo % 3)], eng="gpsimd", final=(p == 1))
        P.barrier()
    P.emit()
    P.close()
    return nc


def build_C3(NCc, NLl, NP):
    NTK = NCc + NLl
    TW = 256
    nc = bass.Bass("TRN2", target_bir_lowering=False)
    fTp = nc.dram_tensor("fTp", [NP, D, NTK], F32, kind="ExternalInput").ap()
    x1T = nc.dram_tensor("x1T", [D, NTK], F32, kind="ExternalInput").ap()
    modv_d = nc.dram_tensor("modv", [128, KC * 3], F32, kind="ExternalInput").ap()
    cst_d = nc.dram_tensor("cst", [128, NCST], F32, kind="ExternalInput").ap()
    x2T = nc.dram_tensor("x2T", [D, NTK], F32, kind="ExternalOutput").ap()
    onT = nc.dram_tensor("onT", [D, NTK], F32, kind="ExternalOutput").ap()
    P = Prog(nc)
    cst = P.sbuf([128, NCST], F32, "cst_sb")
    mv = P.sbuf([128, KC * 3], F32, "mv_sb")
    P.dma(cst[:], cst_d, w=["consts"])
    P.dma(mv[:], modv_d, w=["consts"])
    mvv = mv[:].rearrange("p (k j) -> p k j", j=3)
    ones = cst[:, CSTN["ones"][0]:CSTN["ones"][0] + 128]
    pb_ = [P.alloc(KC * TW).rearrange("p (k n) -> p k n", n=TW) for _ in range(3)]
    acc = [P.alloc(KC * TW).rearrange("p (k n) -> p k n", n=TW) for _ in range(2)]
    xb = [P.alloc(KC * TW).rearrange("p (k n) -> p k n", n=TW) for _ in range(2)]
    sq = P.alloc(KC * TW).rearrange("p (k n) -> p k n", n=TW)
    rstd = P.alloc(TW)
    tiles = [(s0, o, min(TW, L - o)) for (s0, L) in ((0, NCc), (NCc, NLl)) for o in range(0, L, TW)]
    pc = 0
    for ti, (s0, o, n) in enumerate(tiles):
        b = ti % 2
        t0 = s0 + o
        sg = 0 if s0 == 0 else 1
        a_, xt = acc[b], xb[b]
        P.dma(xt[:, :, 0:n], x1T[:, t0:t0 + n].rearrange("(k p) t -> p k t", p=128), w=[("xt", b)])
        P.dma(a_[:, :, 0:n], fTp[0, :, t0:t0 + n].rearrange("(k p) t -> p k t", p=128), w=[("acc", b)])
        for c in range(1, NP):
            z = pc % 3
            pc += 1
            P.dma(pb_[z][:, :, 0:n], fTp[c, :, t0:t0 + n].rearrange("(k p) t -> p k t", p=128), w=[("pb", z)])
            P.V(lambda e: e.tensor_tensor(out=a_[:, :, 0:n], in0=a_[:, :, 0:n], in1=pb_[z][:, :, 0:n], op=ALU.add), [("acc", b), ("pb", z)], [("acc", b)])
        P.V(lambda e: e.tensor_tensor(out=a_[:, :, 0:n], in0=a_[:, :, 0:n], in1=mvv[:, :, sg:sg + 1].to_broadcast([128, KC, n]), op=ALU.mult),
            [("acc", b), "consts"], [("acc", b)])
        P.V(lambda e: e.tensor_tensor(out=a_[:, :, 0:n], in0=a_[:, :, 0:n], in1=xt[:, :, 0:n], op=ALU.add), [("acc", b), ("xt", b)], [("acc", b)])
        P.dma(x2T[:, t0:t0 + n].rearrange("(k p) t -> p k t", p=128), a_[:, :, 0:n], r=[("acc", b)], eng="gpsimd", final=True)
        rms_rstd(P, a_, n, ones, P.banks[0], sq, rstd, [("acc", b)], "rstd", 0)
        P.V(lambda e: e.tensor_tensor(out=sq[:, :, 0:n], in0=a_[:, :, 0:n], in1=rstd[:, 0:n].unsqueeze(1).to_broadcast([128, KC, n]), op=ALU.mult),
            [("acc", b), "rstd", ("sq", 0)], [("sq", 0)])
        P.V(lambda e: e.tensor_tensor(out=sq[:, :, 0:n], in0=sq[:, :, 0:n], in1=mvv[:, :, 2:3].to_broadcast([128, KC, n]), op=ALU.mult),
            [("sq", 0), "consts"], [("sq", 0)])
        P.dma(onT[:, t0:t0 + n].rearrange("(k p) t -> p k t", p=128), sq[:, :, 0:n], r=[("sq", 0)], eng="gpsimd", final=True)
    P.emit()
    P.close()
    return nc


_NC_CACHE = {}


def _get(key, fn):
    if key not in _NC_CACHE:
        _NC_CACHE[key] = fn()
    return _NC_CACHE[key]


def kernel(x, c, ctx, c_ctx, w_ada, b_ada, g_norm1, g_norm2, w_in, gla_w_a2, gla_b_a,
           gla_g_norm, ml_conv_w, ml_conv_b, ml_gate_b, ml_g_norm, rw_mu, rw_w2, rw_w0,
           rw_a2, rw_a0, rw_k_k, rw_k_a, rw_r_k, rw_g_norm, w_out, moe_w_rg, moe_b_rg,
           moe_w_re, moe_b_re, moe_w_gate, moe_w_up, moe_w_down, g_final):
    f32 = np.float32
    inp = dict(w_in=w_in, g_norm1=g_norm1, gla_w_a2=gla_w_a2, gla_b_a=gla_b_a, gla_g_norm=gla_g_norm, ml_conv_w=ml_conv_w,
               ml_conv_b=ml_conv_b, ml_gate_b=ml_gate_b, ml_g_norm=ml_g_norm, rw_mu=rw_mu, rw_w2=rw_w2, rw_w0=rw_w0, rw_a2=rw_a2,
               rw_a0=rw_a0, rw_k_k=rw_k_k, rw_k_a=rw_k_a, rw_r_k=rw_r_k, rw_g_norm=rw_g_norm)
    inp = {k: np.asarray(v, f32) for k, v in inp.items()}
    x, ctx = np.asarray(x, f32), np.asarray(ctx, f32)
    B, SEQ, _ = x.shape
    CTX = ctx.shape[1]
    L = w_ada.shape[0]
    DE = moe_w_gate.shape[-1]
    T = CTX + SEQ
    TT = B * T
    NCORE = 8
    NCc, NLl = CTX // 4, SEQ // 4
    NTK = NCc + NLl
    assert B == 2 and CTX % 512 == 0 or True
    cstv = make_consts()
    NCOL = 6 * D // NCORE
    ncA = _get(("A", L, NCOL), lambda: build_A(L, NCOL))
    cv = np.concatenate([np.asarray(c, f32), np.asarray(c_ctx, f32)[None]], 0)
    cT = np.ascontiguousarray(cv.T.reshape(KC, 128, 3).transpose(1, 0, 2))
    w_ada, b_ada = np.asarray(w_ada, f32), np.asarray(b_ada, f32)
    resA = _run(ncA, [{"cT": cT, "w": np.ascontiguousarray(w_ada[:, :, i * NCOL:(i + 1) * NCOL]),
                       "b": np.ascontiguousarray(b_ada[:, i * NCOL:(i + 1) * NCOL])} for i in range(NCORE)])
    modall = np.concatenate([r["out"] for r in resA], axis=2)
    mod = [modall[l] for l in range(L)]
    ncB = _get(("B", CTX, SEQ), lambda: build_B(CTX, SEQ))
    ncC1 = _get(("C1", NCc, NLl), lambda: build_C1(NCc, NLl))
    ncC2 = _get(("C2", TT, DE), lambda: build_C2(TT, DE))
    ncC3 = _get(("C3", NCc, NLl), lambda: build_C3(NCc, NLl, NCORE))
    xc, xl = ctx.copy(), x.copy()
    tok_idx = [np.concatenate([q * NCc + np.arange(NCc), CTX + q * NLl + np.arange(NLl)]) for q in range(4)]
    out = None
    for l in range(L):
        m = mod[l]
        sh1, sc1, gt1, sh2, sc2, gt2 = [m[:, i * D:(i + 1) * D] for i in range(6)]
        resB = _run(ncB, [prep_B(l, b, g, inp, mod, xc, xl) for b in range(B) for g in range(4)])
        yT = assemble_yT(resB, B, T)
        XT = [np.ascontiguousarray(np.concatenate([xc[b], xl[b]], 0).T) for b in range(B)]
        wr = np.ascontiguousarray(np.concatenate([np.asarray(moe_w_rg[l], f32), np.asarray(moe_w_re[l], f32)], 1))
        br = np.ascontiguousarray(np.broadcast_to(np.concatenate([np.asarray(moe_b_rg[l], f32), np.asarray(moe_b_re[l], f32)])[None], (128, 36)))
        wo = np.ascontiguousarray(np.asarray(w_out[l], f32))
        g2 = pk(np.asarray(g_norm2[l], f32))
        mapsC1 = []
        for b in range(B):
            for q in range(4):
                modv = np.stack([g2, pk(gt1[2]), pk(sc2[2]), pk(sh2[2]), pk(gt1[b]), pk(sc2[b]), pk(sh2[b])], axis=2).reshape(128, KC * 7)
                mapsC1.append({"yT": np.ascontiguousarray(yT[b][:, tok_idx[q]]), "xT": np.ascontiguousarray(XT[b][:, tok_idx[q]]),
                               "wo": wo, "modv": np.ascontiguousarray(modv), "wr": wr, "br": br, "cst": cstv})
        resC1 = _run(ncC1, mapsC1)
        h2all = np.ascontiguousarray(np.concatenate([r["h2T"] for r in resC1], axis=1))
        gall = np.concatenate([r["gates"] for r in resC1], axis=0)
        mapsC2 = [{"h2T": h2all, "gT": np.ascontiguousarray(gall[:, 4 * i:4 * i + 4].T),
                   "wg": np.ascontiguousarray(np.asarray(moe_w_gate[l][4 * i:4 * i + 4], f32)),
                   "wu": np.ascontiguousarray(np.asarray(moe_w_up[l][4 * i:4 * i + 4], f32)),
                   "wd": np.ascontiguousarray(np.asarray(moe_w_down[l][4 * i:4 * i + 4], f32))} for i in range(NCORE)]
        resC2 = _run(ncC2, mapsC2)
        gf = pk(np.asarray(g_final, f32))
        mapsC3 = []
        for b in range(B):
            for q in range(4):
                ci = b * 4 + q
                fTp = np.ascontiguousarray(np.stack([resC2[i]["fT"][:, ci * NTK:(ci + 1) * NTK] for i in range(NCORE)], 0))
                modv = np.stack([pk(gt2[2]), pk(gt2[b]), gf], axis=2).reshape(128, KC * 3)
                mapsC3.append({"fTp": fTp, "x1T": resC1[ci]["x1T"], "modv": np.ascontiguousarray(modv), "cst": cstv})
        resC3 = _run(ncC3, mapsC3)
        for b in range(B):
            X = XT[b]
            for q in range(4):
                X[:, tok_idx[q]] = resC3[b * 4 + q]["x2T"]
            xc[b] = X[:, :CTX].T
            xl[b] = X[:, CTX:].T
        if l == L - 1:
            out = np.zeros((B, SEQ, D), f32)
            for b in range(B):
                for q in range(4):
                    out[b, q * NLl:(q + 1) * NLl] = resC3[b * 4 + q]["onT"][:, NCc:].T
    return out
```

```python
import numpy as np
from contextlib import ExitStack
import concourse.bass as bass
import concourse.mybir as mybir
from concourse.bass_utils import run_bass_kernel_spmd

F32 = mybir.dt.float32
BF16 = mybir.dt.bfloat16
AF = mybir.ActivationFunctionType
ALU = mybir.AluOpType
AX = mybir.AxisListType

D = 2048
KC = 16
DEPTH = 4
GRID_W = 64
CH = 64
NEXP = 32
ENGS = ("sync", "scalar", "vector", "gpsimd", "tensor")
EPOCH = 6000
NDMA = 24
ARENA = 49000
FM_NOINC = True
RMS_NOINC = True


class _Rec:
    def __getattr__(self, name):
        def f(*a, **k):
            self.call = (name, a, k)
            return self
        return f


class Prog:
    def __init__(self, nc):
        self.nc = nc
        self.es = ExitStack()
        self.streams = {e: [] for e in ENGS}
        self.count = {e: 0 for e in ENGS}
        self.esems = {e: [] for e in ENGS}
        self.dsems = [self.es.enter_context(nc.semaphore(f"dq{i}")) for i in range(NDMA)]
        self.dma_n = 0
        self.dma_tok = [None] * NDMA
        self.lastw = {}
        self.readers = {}
        self.seen = {e: {} for e in ENGS}
        self.final = []
        self.nbuf = 0
        self.last_tok = {}
        self.arena = self.sbuf([128, ARENA], F32, "arena")
        self.aptr = 0
        self.banks = [self.psum([128, 512], F32, f"bank{i}") for i in range(8)]

    def sbuf(self, shape, dtype=F32, name=None):
        self.nbuf += 1
        return self.es.enter_context(self.nc.sbuf_tensor(name or f"sb{self.nbuf}", list(shape), dtype))

    def psum(self, shape, dtype=F32, name=None):
        self.nbuf += 1
        return self.es.enter_context(self.nc.psum_tensor(name or f"ps{self.nbuf}", list(shape), dtype))

    def dram(self, name, shape, dtype=F32, kind="Internal"):
        return self.nc.dram_tensor(name, list(shape), dtype, kind=kind).ap()

    def alloc(self, n, dtype=F32):
        w = n if dtype == F32 else (n + 1) // 2
        assert self.aptr + w <= ARENA, ("arena overflow", self.aptr, w)
        ap = self.arena[:, self.aptr:self.aptr + w]
        self.aptr += w
        if dtype != F32:
            ap = ap.bitcast(dtype)[:, 0:n]
        return ap

    def _esem(self, eng, epoch):
        lst = self.esems[eng]
        while len(lst) <= epoch:
            lst.append(self.es.enter_context(self.nc.semaphore(f"p_{eng}_{len(lst)}")))
        return lst[epoch]

    def _need(self, eng, tok, waits):
        if tok is None:
            return
        sem, val, key = tok
        if self.seen[eng].get(key, 0) >= val:
            return
        if key[0] == "e" and key[1] == eng and key[2] * EPOCH + val > self.count[eng]:
            return
        self.seen[eng][key] = val
        waits.append((sem, val))

    def op(self, eng, fn, reads=(), writes=(), dma=False, out_final=False, inc=True):
        rec = _Rec()
        fn(rec)
        fn = rec.call
        waits = []
        for k in reads:
            self._need(eng, self.lastw.get(k), waits)
        for k in writes:
            self._need(eng, self.lastw.get(k), waits)
            for t in self.readers.get(k, {}).values():
                self._need(eng, t, waits)
        if dma:
            slot = self.dma_n % NDMA
            val = 16 * (self.dma_n // NDMA + 1)
            self._need(eng, self.dma_tok[slot], waits)
            tok = (self.dsems[slot], val, ("d", slot))
            self.dma_tok[slot] = tok
            self.dma_n += 1
            inc = (self.dsems[slot], 16)
            if out_final:
                self.final.append(tok)
            rkey = ("d", self.dma_n)
        else:
            n = self.count[eng]
            epoch, idx = divmod(n, EPOCH)
            sem = self._esem(eng, epoch)
            tok = (sem, idx + 1, ("e", eng, epoch))
            if inc:
                self.count[eng] = n + 1
                inc = (sem, 1)
                self.last_tok[eng] = tok
            else:
                inc = None
            rkey = eng
        self.streams[eng].append((waits, fn, inc))
        for k in reads:
            self.readers.setdefault(k, {})[rkey] = tok
        for k in writes:
            self.lastw[k] = tok
            self.readers[k] = {}
        return tok

    def barrier(self):
        toks = [t for t in self.last_tok.values()] + [t for t in self.dma_tok if t is not None]
        for e in ENGS:
            waits = []
            for t in toks:
                self._need(e, t, waits)
            if waits:
                self.streams[e].append((waits, None, None))
        self.lastw = {}
        self.readers = {}

    def V(self, fn, r=(), w=()):
        return self.op("vector", fn, r, w)

    def S(self, fn, r=(), w=()):
        return self.op("scalar", fn, r, w)

    def G(self, fn, r=(), w=()):
        return self.op("gpsimd", fn, r, w)

    def T(self, fn, r=(), w=(), inc=True):
        return self.op("tensor", fn, r, w, inc=inc)

    def dma(self, out, in_, r=(), w=(), eng="sync", final=False):
        return self.op(eng, lambda e: e.dma_start(out=out, in_=in_), r, w, dma=True, out_final=final)

    def emit(self):
        nc = self.nc
        fin = [(t[0], t[1]) for t in self.final]
        streams = self.streams
        with nc.Block() as block:
            def run(engname):
                def body(e):
                    for waits, fn, inc in streams[engname]:
                        for s, v in waits:
                            e.wait_ge(s, v)
                        if fn is not None:
                            ins = getattr(e, fn[0])(*fn[1], **fn[2])
                            if inc is not None:
                                ins.then_inc(inc[0], inc[1])
                    if engname == "sync":
                        for s, v in fin:
                            e.wait_ge(s, v)
                return body
            block.sync(run("sync"))
            block.scalar(run("scalar"))
            block.vector(run("vector"))
            block.gpsimd(run("gpsimd"))
            block.tensor(run("tensor"))

    def close(self):
        self.es.close()


def _run(nc, in_maps):
    res = run_bass_kernel_spmd(nc, in_maps, core_ids=list(range(len(in_maps))))
    return res.results


def pk(v):
    return np.ascontiguousarray(np.asarray(v, np.float32).reshape(KC, 128).T)


def build_A(L, NCOL):
    nc = bass.Bass("TRN2", target_bir_lowering=False)
    cT = nc.dram_tensor("cT", [128, KC, 3], F32, kind="ExternalInput").ap()
    w = nc.dram_tensor("w", [L, D, NCOL], F32, kind="ExternalInput").ap()
    b = nc.dram_tensor("b", [L, NCOL], F32, kind="ExternalInput").ap()
    out = nc.dram_tensor("out", [L, 3, NCOL], F32, kind="ExternalOutput").ap()
    P = Prog(nc)
    s = P.alloc(KC * 3).rearrange("p (k r) -> p k r", r=3)
    P.dma(s, cT, w=["s"])
    P.S(lambda e: e.activation(out=s, in_=s, func=AF.Silu), ["s"], ["s"])
    wb = [P.alloc(KC * 512).rearrange("p (k n) -> p k n", n=512) for _ in range(2)]
    bb = [P.alloc(512) for _ in range(2)]
    ob = [P.alloc(512) for _ in range(2)]
    it = 0
    for l in range(L):
        for c0 in range(0, NCOL, 512):
            n = min(512, NCOL - c0)
            i = it % 2
            it += 1
            P.dma(wb[i][:, :, 0:n], w[l, :, c0:c0 + n].rearrange("(k p) n -> p k n", p=128), w=[("wb", i)])
            P.dma(bb[i][0:3, 0:n], b[l:l + 1, c0:c0 + n].partition_broadcast(3), w=[("bb", i)], eng="gpsimd")
            ps = P.banks[i]
            for k in range(KC):
                P.T(lambda e, k=k, i=i, n=n, ps=ps: e.matmul(ps[0:3, 0:n], lhsT=s[:, k, :], rhs=wb[i][:, k, 0:n],
                                                              start=(k == 0), stop=(k == KC - 1)),
                    ["s", ("wb", i)], [("ps", i)], inc=(k == KC - 1))
            P.V(lambda e, i=i, n=n, ps=ps: e.tensor_tensor(out=ob[i][0:3, 0:n], in0=ps[0:3, 0:n], in1=bb[i][0:3, 0:n], op=ALU.add),
                [("ps", i), ("bb", i)], [("ob", i)])
            P.dma(out[l, :, c0:c0 + n], ob[i][0:3, 0:n], r=[("ob", i)], eng="gpsimd", final=True)
    P.emit()
    P.close()
    return nc


def rms_rstd(P, xt, n, ones, msbank, sq, rstd, keys_x, key_out, tag):
    P.S(lambda e: e.activation(out=sq[:, :, 0:n], in_=xt[:, :, 0:n], func=AF.Square), keys_x, [("sq", tag)])
    for k in range(KC):
        P.T(lambda e, k=k: e.matmul(msbank[:, 0:n], lhsT=ones, rhs=sq[:, k, 0:n], start=(k == 0), stop=(k == KC - 1)),
            [("sq", tag), "consts"], [("msb", tag)], inc=(k == KC - 1) or not RMS_NOINC)
    P.S(lambda e: e.activation(out=rstd[:, 0:n], in_=msbank[:, 0:n], func=AF.Ln, scale=1.0 / D, bias=1e-6),
        [("msb", tag)], [key_out])
    P.S(lambda e: e.activation(out=rstd[:, 0:n], in_=rstd[:, 0:n], func=AF.Exp, scale=-0.5), [key_out], [key_out])


MSLOT = [(0, 1), (2, 3), (4, 4), (5, 5)]
FMA = (["GQ", "GK", "GLR", "GG"] + [f"{a}{s}" for s in range(2) for a in ("MQ", "MK", "MI", "MF", "MO")]
       + [f"{a}{j}" for j in range(3) for a in ("RR", "RK", "RV")] + ["RG01", "RG2", "W1F", "W1B", "A1F", "A1B"])
WBLK = ([("gq", 64), ("gk", 64), ("glrf", 16), ("glrb", 16), ("gg", 128)]
        + [(f"{a}{s}", n) for s in range(2) for a, n in (("mq", 64), ("mk", 64), ("mo", 128), ("mif", 64), ("mff", 64), ("mib", 64), ("mfb", 64))]
        + [(f"{a}{j}", 64) for j in range(3) for a in ("rr", "rk", "rv", "rg")]
        + [("w1f", 96), ("w1b", 96), ("a1f", 96), ("a1b", 96)])
WOFF = {}
_o = 0
for _n, _c in WBLK:
    WOFF[_n] = (_o, _c)
    _o += _c
NFMC = _o
JOBS = {"GQ": [(0, "gq", 0), (64, "gq", 1)], "GK": [(0, "gk", 0), (64, "gk", 1)],
        "GLR": [(0, "glrf", 0), (64, "glrb", 1)], "GG": [(0, "gg", 0)],
        "RG01": [(0, "rg0", 0), (64, "rg1", 0)], "RG2": [(0, "rg2", 0)],
        "W1F": [(0, "w1f", 0)], "W1B": [(0, "w1b", 1)], "A1F": [(0, "a1f", 0)], "A1B": [(0, "a1b", 1)]}
for _s in range(2):
    JOBS[f"MQ{_s}"] = [(0, f"mq{_s}", 0), (64, f"mq{_s}", 1)]
    JOBS[f"MK{_s}"] = [(0, f"mk{_s}", 0), (64, f"mk{_s}", 1)]
    JOBS[f"MI{_s}"] = [(0, f"mif{_s}", 0), (64, f"mib{_s}", 1)]
    JOBS[f"MF{_s}"] = [(0, f"mff{_s}", 0), (64, f"mfb{_s}", 1)]
    JOBS[f"MO{_s}"] = [(0, f"mo{_s}", 0)]
for _j in range(3):
    for _a in ("rr", "rk", "rv"):
        JOBS[f"{_a.upper()}{_j}"] = [(0, f"{_a}{_j}", 0), (64, f"{_a}{_j}", 1)]
NTMC = 384


def _mjobs():
    J = []

    def pair(a, b, A, B, src, keepB=True):
        c0 = WOFF[a][0]
        n = WOFF[a][1] + WOFF[b][1]
        assert WOFF[b][0] == c0 + WOFF[a][1]
        d = [(0, 64, A, 64 * src)]
        if keepB:
            d.append((64, 64, B[0], B[1]))
        J.append((c0, n if keepB else 64, src, d))
    for src in (0, 1):
        pair("gq", "gk", "GQ", ("GK", 64 * src), src)
    J.append((WOFF["glrf"][0], 16, 0, [(0, 16, "GLR", 0)]))
    J.append((WOFF["glrb"][0], 16, 1, [(0, 16, "GLR", 64)]))
    J.append((WOFF["gg"][0], 128, 0, [(0, 128, "GG", 0)]))
    for s in range(2):
        for src in (0, 1):
            pair(f"mq{s}", f"mk{s}", f"MQ{s}", (f"MK{s}", 64 * src), src)
        pair(f"mif{s}", f"mff{s}", f"MI{s}", (f"MF{s}", 0), 0)
        pair(f"mib{s}", f"mfb{s}", f"MI{s}", (f"MF{s}", 64), 1)
        J.append((WOFF[f"mo{s}"][0], 128, 0, [(0, 128, f"MO{s}", 0)]))
    for j in range(3):
        for src in (0, 1):
            pair(f"rr{j}", f"rk{j}", f"RR{j}", (f"RK{j}", 64 * src), src)
        garr, gr0 = (("RG01", 0), ("RG01", 64), ("RG2", 0))[j]
        pair(f"rv{j}", f"rg{j}", f"RV{j}", (garr, gr0), 0)
        pair(f"rv{j}", f"rg{j}", f"RV{j}", None, 1, keepB=False)
    for a, A, src in (("w1f", "W1F", 0), ("w1b", "W1B", 1), ("a1f", "A1F", 0), ("a1b", "A1B", 1)):
        J.append((WOFF[a][0], 96, src, [(0, 96, A, 0)]))
    return J


MJOBS = _mjobs()
PPN = {}
_o = 0
for _n, _c in ([("gba", 1), ("ggn", 1)]
               + [(f"{a}{s}", n) for s in range(2) for a, n in (("cwq", 9), ("cwk", 9), ("cbq", 1), ("cbk", 1), ("mbi", 1), ("mbf", 1), ("mgn", 1))]
               + [(f"{a}{j}", n) for j in range(3) for a, n in (("mur", 2), ("muk", 2), ("muv", 2), ("w0", 1), ("a0", 1), ("kk", 1), ("ka", 1), ("rk", 1), ("rgn", 1))]
               + [("mug01", 2), ("mug2", 2), ("muw1f", 2), ("muw1b", 2), ("mua1f", 2), ("mua1b", 2)]):
    PPN[_n] = (_o, _c)
    _o += _c
NPP = _o
CSTN = {}
_o = 0
for _n, _c in [("ident", 128), ("ones", 128), ("J", 128), ("mtri", 128), ("bones", 128), ("o128", 128),
               ("MS", 256), ("MI", 256), ("ML", 256), ("I2", 256), ("bm128", 256), ("bm129", 258), ("bm64", 128)]:
    CSTN[_n] = (_o, _c)
    _o += _c
NCST = _o


def make_consts():
    c = np.zeros((128, NCST), np.float32)

    def put(n, a):
        o, w = CSTN[n]
        c[:, o:o + w] = a
    idx = np.arange(128)
    put("ident", np.eye(128))
    put("ones", np.ones((128, 128)))
    put("J", np.eye(128)[::-1])
    put("mtri", ((idx[:, None] % 64) <= (idx[None, :] % 64)).astype(np.float32))
    same = (idx[:, None] // 64) == (idx[None, :] // 64)
    put("bones", same.astype(np.float32))
    put("o128", np.full((128, 128), 1.0 / 128))
    ms = (same & (idx[:, None] < idx[None, :])).astype(np.float32)
    mi = (same & (idx[:, None] <= idx[None, :])).astype(np.float32)
    put("MS", np.concatenate([ms, ms], 1))
    put("MI", np.concatenate([mi, mi], 1))
    put("ML", np.concatenate([ms.T, ms.T], 1))
    put("I2", np.concatenate([np.eye(128), np.eye(128)], 1))
    for n, dv in (("bm128", 128), ("bm129", 129), ("bm64", 64)):
        m = np.zeros((128, 2 * dv))
        m[:64, :dv] = 1
        m[64:, dv:] = 1
        put(n, m)
    return c


def build_B(CTX, SEQ):
    T = CTX + SEQ
    NT = T // 128
    NCHK = T // 64
    SEGS = [(0, CTX), (CTX, SEQ)]
    TW = 256
    NA = len(FMA)
    nc = bass.Bass("TRN2", target_bir_lowering=False)

    def din(name, shape):
        return nc.dram_tensor(name, list(shape), F32, kind="ExternalInput").ap()
    xT = din("xT", [D, T])
    modv_d = din("modv", [128, KC * 5])
    wfm = din("wfm", [D, NFMC])
    wtm = din("wtm", [D, NTMC])
    cst_d = din("cst", [128, NCST])
    pp_d = din("pp", [128, NPP])
    wa2_d = din("wa2", [128, 64])
    w2_d = din("w2", [96, 3 * 2 * 2 * 64])
    yT = nc.dram_tensor("yT", [576, T], F32, kind="ExternalOutput").ap()
    P = Prog(nc)
    FMS = P.dram("fms", [NA, 128, T])
    TMS = P.dram("tms", [2, T, NTMC])
    AI = {a: i for i, a in enumerate(FMA)}

    cst = P.sbuf([128, NCST], F32, "cst_sb")
    pp = P.sbuf([128, NPP], F32, "pp_sb")
    mv = P.sbuf([128, KC * 5], F32, "mv_sb")
    wa2 = P.sbuf([128, 64], F32, "wa2_sb")
    w2 = P.sbuf([96, 768], F32, "w2_sb")
    P.dma(cst[:], cst_d, w=["consts"])
    P.dma(pp[:], pp_d, w=["consts"])
    P.dma(mv[:], modv_d, w=["consts"])
    P.dma(wa2[:], wa2_d, w=["consts"])
    P.dma(w2[:], w2_d, w=["consts"])
    w2v = w2[:].rearrange("p (j w d c) -> p j w d c", j=3, w=2, d=2)
    mvv = mv[:].rearrange("p (k j) -> p k j", j=5)

    def C(n, r0=0, r1=128, c0=None, c1=None):
        o, w = CSTN[n]
        a = o if c0 is None else o + c0
        b = o + w if c1 is None else o + c1
        return cst[r0:r1, a:b]

    def PPc(n, j=0, w=1, r0=0, r1=128):
        o, _ = PPN[n]
        return pp[r0:r1, o + j:o + j + w]
    ident, ones, Jm = C("ident"), C("ones"), C("J")
    ones_b = cst[:, CSTN["ones"][0]:CSTN["ones"][0] + 1]

    gm = P.sbuf([128, KC * 2], F32, "gm_sb")
    gmv = gm[:].rearrange("p (k j) -> p k j", j=2)
    for j, col in enumerate((1, 3)):
        P.V(lambda e, j=j, col=col: e.tensor_scalar(out=gmv[:, :, j], in0=mvv[:, :, col], scalar1=1.0, scalar2=1.0, op0=ALU.add, op1=ALU.mult),
            ["consts"], ["gm"])
        P.V(lambda e, j=j: e.tensor_tensor(out=gmv[:, :, j], in0=gmv[:, :, j], in1=mvv[:, :, 0], op=ALU.mult), ["gm", "consts"], ["gm"])
    mark0 = P.aptr

    wsb = P.alloc(KC * NFMC, BF16).rearrange("p (k n) -> p k n", n=NFMC)
    wtsb = P.alloc(KC * NTMC, BF16).rearrange("p (k n) -> p k n", n=NTMC)
    for c0 in range(0, NFMC, 512):
        c1 = min(NFMC, c0 + 512)
        P.dma(wsb[:, :, c0:c1], wfm[:, c0:c1].rearrange("(k p) n -> p k n", p=128), w=["wsb"], eng="gpsimd")
    P.dma(wtsb[:, :, :], wtm.rearrange("(k p) n -> p k n", p=128), w=["wsb"], eng="gpsimd")
    xb = [P.alloc(KC * TW).rearrange("p (k n) -> p k n", n=TW) for _ in range(2)]
    sq = P.alloc(KC * TW).rearrange("p (k n) -> p k n", n=TW)
    rstd = P.alloc(TW)
    hT = [P.alloc(KC * TW, BF16).rearrange("p (k n) -> p k n", n=TW) for _ in range(2)]
    hR = [P.alloc(KC * TW, BF16).rearrange("p (k n) -> p k n", n=TW) for _ in range(2)]
    stg = [P.alloc(TW) for _ in range(4)]
    tstg = [P.alloc(NTMC) for _ in range(2)]
    tiles = [(s0, L, o, min(TW, L - o)) for (s0, L) in SEGS for o in range(0, L, TW)]
    cnt = 0
    tcn = 0
    for ti, (s0, L, o, n) in enumerate(tiles):
        b = ti % 2
        t0, tm = s0 + o, s0 + L - o - n
        sg = 0 if s0 == 0 else 1
        xt = xb[b]
        P.dma(xt[:, :, 0:n], xT[:, t0:t0 + n].rearrange("(k p) t -> p k t", p=128), w=[("xt", b)])
        rms_rstd(P, xt, n, ones, P.banks[0], sq, rstd, [("xt", b)], "rstd", 0)
        P.V(lambda e, xt=xt, n=n: e.tensor_tensor(out=sq[:, :, 0:n], in0=xt[:, :, 0:n],
                                                   in1=rstd[:, 0:n].unsqueeze(1).to_broadcast([128, KC, n]), op=ALU.mult),
            [("xt", b), "rstd", ("sq", 0)], [("sq", 0)])
        for k in range(KC):
            P.V(lambda e, k=k, b=b, n=n, sg=sg: e.tensor_scalar(out=hT[b][:, k, 0:n], in0=sq[:, k, 0:n], scalar1=gmv[:, k, sg:sg + 1],
                                                                 scalar2=mvv[:, k, 2 + 2 * sg:3 + 2 * sg], op0=ALU.mult, op1=ALU.add),
                [("sq", 0), "gm", "consts"], [("h", b, 0)])
        P.V(lambda e, b=b, n=n: e.tensor_copy(out=hR[b][:, :, 0:n][:, :, ::-1], in_=hT[b][:, :, 0:n]), [("h", b, 0)], [("h", b, 1)])
        hh = (hT[b], hR[b])
        for (c0, ncol, src, dests) in MJOBS:
            bi = 1 + (cnt % 4)
            si = cnt % 4
            cnt += 1
            bank = P.banks[bi]
            st = stg[si]
            for k in range(KC):
                P.T(lambda e: e.matmul(bank[0:ncol, 0:n], lhsT=wsb[:, k, c0:c0 + ncol], rhs=hh[src][:, k, 0:n], start=(k == 0), stop=(k == KC - 1)),
                    ["wsb", ("h", b, src)], [("bank", bi)], inc=(k == KC - 1) or not FM_NOINC)
            if cnt % 2 == 0:
                P.S(lambda e: e.copy(out=st[0:ncol, 0:n], in_=bank[0:ncol, 0:n]), [("bank", bi)], [("stg", si)])
            else:
                P.V(lambda e: e.tensor_copy(out=st[0:ncol, 0:n], in_=bank[0:ncol, 0:n]), [("bank", bi)], [("stg", si)])
            col = t0 if src == 0 else tm
            for (pr0, nr, arr, ar0) in dests:
                P.dma(FMS[AI[arr], ar0:ar0 + nr, col:col + n], st[pr0:pr0 + nr, 0:n], r=[("stg", si)], eng="gpsimd")
        for j in range(n // 128):
            for src in (0, 1):
                bi = 5 + (tcn % 2)
                bank = P.banks[bi]
                st = tstg[tcn % 2]
                for k in range(KC):
                    P.T(lambda e, k=k, j=j, src=src, bank=bank: e.matmul(bank[:, 0:NTMC], lhsT=hh[src][:, k, j * 128:(j + 1) * 128],
                                                                         rhs=wtsb[:, k, :], start=(k == 0), stop=(k == KC - 1)),
                        ["wsb", ("h", b, src)], [("bank", bi)], inc=(k == KC - 1))
                P.S(lambda e, st=st, bank=bank: e.copy(out=st[:, 0:NTMC], in_=bank[:, 0:NTMC]), [("bank", bi)], [("tstg", tcn % 2)])
                row0 = (t0 if src == 0 else tm) + 128 * j
                P.dma(TMS[src, row0:row0 + 128, :], st[:, 0:NTMC], r=[("tstg", tcn % 2)], eng="gpsimd")
                tcn += 1
    P.barrier()

    def v3(ap, w=64):
        return ap.rearrange("p (c k) -> p c k", k=w)

    def chunk_local(EX, OFF):
        P.G(lambda e: e.memset(OFF[:, 0:1], 0.0), [], ["OFF"])
        P.V(lambda e: e.tensor_copy(out=OFF[:, 1:NCHK], in_=v3(EX)[:, 0:NCHK - 1, 63]), ["EX"], ["OFF"])
        P.V(lambda e: e.tensor_tensor(out=v3(EX), in0=v3(EX), in1=OFF[:, 0:NCHK].unsqueeze(2).to_broadcast([128, NCHK, 64]),
                                      op=ALU.subtract), ["EX", "OFF"], ["EX"])

    def headnorm(src, n, R, center, eps, gain, gate, bonus, Y, SQ, RS, bankm, tag):
        on = ones[0:R, 0:R]
        P.S(lambda e: e.copy(out=Y[0:R, 0:n], in_=src), [tag + "src"], [tag + "Y"])
        if center:
            P.T(lambda e: e.matmul(bankm[0:R, 0:n], lhsT=on, rhs=Y[0:R, 0:n], start=True, stop=True), [tag + "Y", "consts"], [tag + "bm"])
            P.V(lambda e: e.scalar_tensor_tensor(out=Y[0:R, 0:n], in0=bankm[0:R, 0:n], scalar=-1.0 / R, in1=Y[0:R, 0:n],
                                                 op0=ALU.mult, op1=ALU.add), [tag + "bm", tag + "Y"], [tag + "Y"])
        P.S(lambda e: e.activation(out=SQ[0:R, 0:n], in_=Y[0:R, 0:n], func=AF.Square), [tag + "Y"], [tag + "SQ"])
        P.T(lambda e: e.matmul(bankm[0:R, 0:n], lhsT=on, rhs=SQ[0:R, 0:n], start=True, stop=True), [tag + "SQ", "consts"], [tag + "bm"])
        P.S(lambda e: e.activation(out=RS[0:R, 0:n], in_=bankm[0:R, 0:n], func=AF.Ln, scale=1.0 / R, bias=eps), [tag + "bm"], [tag + "RS"])
        P.S(lambda e: e.activation(out=RS[0:R, 0:n], in_=RS[0:R, 0:n], func=AF.Exp, scale=-0.5), [tag + "RS"], [tag + "RS"])
        P.V(lambda e: e.tensor_tensor(out=Y[0:R, 0:n], in0=Y[0:R, 0:n], in1=RS[0:R, 0:n], op=ALU.mult), [tag + "Y", tag + "RS"], [tag + "Y"])
        if bonus is None:
            P.V(lambda e: e.scalar_tensor_tensor(out=Y[0:R, 0:n], in0=Y[0:R, 0:n], scalar=gain, in1=gate, op0=ALU.mult, op1=ALU.mult),
                [tag + "Y", tag + "gate", "consts"], [tag + "Y"])
        else:
            P.V(lambda e: e.scalar_tensor_tensor(out=Y[0:R, 0:n], in0=Y[0:R, 0:n], scalar=gain, in1=bonus, op0=ALU.mult, op1=ALU.add),
                [tag + "Y", tag + "bonus", "consts"], [tag + "Y"])
            P.V(lambda e: e.tensor_tensor(out=Y[0:R, 0:n], in0=Y[0:R, 0:n], in1=gate, op=ALU.mult), [tag + "Y", tag + "gate"], [tag + "Y"])

    def groups():
        for (s0, L) in SEGS:
            for g0 in range(0, L, 512):
                yield s0, L, g0, min(512, L - g0)

    PADW = 65
    for s in range(2):
        for arr, cwn, cbn in ((f"MQ{s}", f"cwq{s}", f"cbq{s}"), (f"MK{s}", f"cwk{s}", f"cbk{s}")):
            P.aptr = mark0
            oc, ol = PADW, PADW + CTX + PADW
            X0 = P.alloc(T + 3 * PADW)
            XA = P.alloc(SEQ + 2 * PADW)
            XB = P.alloc(SEQ + 2 * PADW)
            ACC = P.alloc(T)
            P.G(lambda e: e.memset(X0, 0.0), [], ["X0"])
            P.G(lambda e: e.memset(XA, 0.0), [], ["XA"])
            P.G(lambda e: e.memset(XB, 0.0), [], ["XB"])
            P.dma(X0[:, oc:oc + CTX], FMS[AI[arr], :, 0:CTX], w=["X0"])
            P.dma(X0[:, ol:ol + SEQ], FMS[AI[arr], :, CTX:T], w=["X0"])
            P.S(lambda e: e.copy(out=XA[:, PADW:PADW + SEQ], in_=X0[:, ol:ol + SEQ]), ["X0"], ["XA"])
            P.G(lambda e: e.tensor_copy(out=XB[:, PADW:PADW + SEQ], in_=X0[:, ol:ol + SEQ]), ["X0"], ["XB"])
            P.S(lambda e: e.memzero(v3(XA[:, PADW:PADW + SEQ])[:, :, 63]) if False else e.activation(
                out=v3(XA[:, PADW:PADW + SEQ])[:, :, 63], in_=v3(XA[:, PADW:PADW + SEQ])[:, :, 63], func=AF.Copy, scale=0.0), ["XA"], ["XA"])
            P.G(lambda e: e.memset(v3(XB[:, PADW:PADW + SEQ])[:, :, 0], 0.0), ["XB"], ["XB"])
            cw = lambda i, j: PPc(cwn, 3 * i + j)
            cb = PPc(cbn)
            P.V(lambda e: e.tensor_scalar(out=ACC[:, 0:CTX], in0=X0[:, oc:oc + CTX], scalar1=cw(1, 1), scalar2=cb, op0=ALU.mult, op1=ALU.add),
                ["X0", "consts"], ["ACC"])
            for j in (0, 2):
                P.V(lambda e, j=j: e.scalar_tensor_tensor(out=ACC[:, 0:CTX], in0=X0[:, oc + j - 1:oc + j - 1 + CTX], scalar=cw(1, j),
                                                          in1=ACC[:, 0:CTX], op0=ALU.mult, op1=ALU.add), ["X0", "ACC", "consts"], ["ACC"])
            P.V(lambda e: e.tensor_scalar(out=ACC[:, CTX:T], in0=X0[:, ol:ol + SEQ], scalar1=cw(1, 1), scalar2=cb, op0=ALU.mult, op1=ALU.add),
                ["X0", "consts"], ["ACC"])
            for i in range(3):
                for j in range(3):
                    if i == 1 and j == 1:
                        continue
                    off = 64 * (i - 1) + (j - 1)
                    if j == 1:
                        srcv = X0[:, ol + off:ol + off + SEQ]
                    elif j == 0:
                        srcv = XA[:, PADW + off:PADW + off + SEQ]
                    else:
                        srcv = XB[:, PADW + off:PADW + off + SEQ]
                    P.V(lambda e, srcv=srcv, i=i, j=j: e.scalar_tensor_tensor(out=ACC[:, CTX:T], in0=srcv, scalar=cw(i, j), in1=ACC[:, CTX:T],
                                                                              op0=ALU.mult, op1=ALU.add), ["X0", "XA", "XB", "ACC", "consts"], ["ACC"])
            P.S(lambda e: e.activation(out=ACC, in_=ACC, func=AF.Silu), ["ACC"], ["ACC"])
            P.dma(FMS[AI[arr], :, :], ACC, r=["ACC"], eng="gpsimd")
            P.barrier()

    def glalike(kind, s):
        P.aptr = mark0
        dv = 128 if kind == "g" else 129
        QS, KS, LA, EX = P.alloc(T), P.alloc(T), P.alloc(T), P.alloc(T)
        IG = P.alloc(T) if kind == "m" else None
        V2f = P.alloc(NT * 2 * dv)
        V2 = V2f.rearrange("p (i d v) -> p i d v", d=2, v=dv)
        Of = P.alloc(NT * 256)
        O = Of.rearrange("p (i d v) -> p i d v", d=2, v=128)
        OFF, EB = P.alloc(NCHK), P.alloc(NCHK)
        Sb, tmpS = P.alloc(2 * dv), P.alloc(2 * dv)
        nb, rec = P.alloc(1), P.alloc(2)
        att = [P.alloc(128) for _ in range(2)]
        kws = [P.alloc(128) for _ in range(2)]
        Yb, SQb, RSb = P.alloc(512), P.alloc(512), P.alloc(512)
        bm = C("bm128") if kind == "g" else C("bm129")
        if kind == "g":
            P.dma(QS, FMS[AI["GQ"]], w=["QS"])
            P.dma(KS, FMS[AI["GK"]], w=["KS"])
            P.dma(EX, FMS[AI["GLR"]], w=["EX"])
            P.V(lambda e: e.tensor_scalar(out=nb, in0=PPc("gba"), scalar1=-1.0, scalar2=0.0, op0=ALU.mult, op1=ALU.add), ["consts"], ["nb"])
            for t0 in range(0, T, 512):
                n = min(512, T - t0)
                bk = P.banks[(t0 // 512) % 2]
                for d in range(2):
                    P.T(lambda e, d=d, t0=t0, n=n, bk=bk: e.matmul(bk[64 * d:64 * d + 64, 0:n], lhsT=wa2[64 * d:64 * d + 16, :],
                                                                  rhs=EX[64 * d:64 * d + 16, t0:t0 + n], start=True, stop=True),
                        ["EX", "consts"], [("bk", (t0 // 512) % 2)])
                P.S(lambda e, t0=t0, n=n, bk=bk: e.activation(out=LA[:, t0:t0 + n], in_=bk[:, 0:n], func=AF.Exp, scale=-1.0, bias=nb),
                    [("bk", (t0 // 512) % 2), "nb"], ["LA"])
            sc = 1.0 / 16.0
            gname, gfn, gain, row0, center = "GG", AF.Silu, PPc("ggn"), 0, False
        else:
            P.dma(QS, FMS[AI[f"MQ{s}"]], w=["QS"])
            P.dma(KS, FMS[AI[f"MK{s}"]], w=["KS"])
            P.dma(LA, FMS[AI[f"MF{s}"]], w=["LA"])
            P.dma(IG, FMS[AI[f"MI{s}"]], w=["IG"])
            P.V(lambda e: e.tensor_scalar(out=nb, in0=PPc(f"mbf{s}"), scalar1=-1.0, scalar2=0.0, op0=ALU.mult, op1=ALU.add), ["consts"], ["nb"])
            P.S(lambda e: e.activation(out=LA, in_=LA, func=AF.Exp, scale=-1.0, bias=nb), ["LA", "nb"], ["LA"])
            sc = 1.0
            gname, gfn, gain, row0, center = f"MO{s}", AF.Sigmoid, PPc(f"mgn{s}"), 128 + 128 * s, True
        P.S(lambda e: e.activation(out=LA, in_=LA, func=AF.Ln, bias=1.0), ["LA"], ["LA"])
        P.V(lambda e: e.tensor_tensor_scan(out=EX, data0=ones_b.to_broadcast([128, T]), data1=LA, initial=0.0, op0=ALU.mult, op1=ALU.add),
            ["LA", "consts", "EX"], ["EX"])
        chunk_local(EX, OFF)
        P.S(lambda e: e.activation(out=LA, in_=EX, func=AF.Exp, scale=-sc), ["EX"], ["LA"])
        P.V(lambda e: e.tensor_copy(out=EB[:, 0:NCHK], in_=v3(LA)[:, :, 63]), ["LA"], ["EB"])
        P.V(lambda e: e.scalar_tensor_tensor(out=QS, in0=QS, scalar=0.125, in1=LA, op0=ALU.mult, op1=ALU.mult), ["QS", "LA"], ["QS"])
        if kind == "g":
            P.S(lambda e: e.activation(out=EX, in_=EX, func=AF.Exp, scale=sc), ["EX"], ["EX"])
        else:
            P.V(lambda e: e.scalar_tensor_tensor(out=EX, in0=EX, scalar=sc, in1=IG, op0=ALU.mult, op1=ALU.add), ["EX", "IG"], ["EX"])
            P.S(lambda e: e.activation(out=EX, in_=EX, func=AF.Exp, bias=PPc(f"mbi{s}")), ["EX", "consts"], ["EX"])
        P.V(lambda e: e.tensor_tensor(out=KS, in0=KS, in1=EX, op=ALU.mult), ["KS", "EX"], ["KS"])
        tcol = {"g": 0, "m": 128 + 128 * s}[kind]
        for d in range(2):
            P.dma(V2[:, :, d, 0:128], TMS[d, :, tcol:tcol + 128].rearrange("(i p) c -> p i c", p=128), w=["V2"])
        if kind == "m":
            P.G(lambda e: e.memset(V2[:, :, :, 128:129], 1.0), ["V2"], ["V2"])
        P.G(lambda e: e.memset(Sb, 0.0), [], ["S"])
        ATTa = LA.rearrange("p (i c) -> p i c", c=128)
        KWa = EX.rearrange("p (i c) -> p i c", c=128)
        P.barrier()
        for c in range(NCHK):
            i, h = divmod(c, 2)
            pb = 64 * h
            cs = slice(c * 64, c * 64 + 64)
            z = c % 2
            ab, kb = P.banks[z], P.banks[4 + z]
            for d in range(2):
                P.T(lambda e: e.matmul(ab[pb:pb + 64, d * 64:(d + 1) * 64], lhsT=KS[64 * d:64 * d + 64, cs],
                                       rhs=QS[64 * d:64 * d + 64, cs], start=True, stop=True), ["KS", "QS"], [("ab", z)])
            P.V(lambda e: e.tensor_tensor(out=ATTa[pb:pb + 64, i, :], in0=ab[pb:pb + 64, 0:128], in1=C("mtri", pb, pb + 64), op=ALU.mult),
                [("ab", z), "consts"], [("att", c)])
            for d in range(2):
                P.T(lambda e: e.matmul(kb[pb:pb + 64, d * 64:(d + 1) * 64], lhsT=KS[64 * d:64 * d + 64, cs],
                                       rhs=ident[64 * d:64 * d + 64, 64 * d:64 * d + 64], start=True, stop=True), ["KS", "consts"], [("kb", z)])
            P.S(lambda e: e.copy(out=KWa[pb:pb + 64, i, :], in_=kb[pb:pb + 64, 0:128]), [("kb", z)], [("kwa", c)])
        for c in range(NCHK):
            i, h = divmod(c, 2)
            pb = 64 * h
            cs = slice(c * 64, c * 64 + 64)
            z = c % 2
            ob, sbk = P.banks[2 + z], P.banks[6 + z]
            P.T(lambda e: e.matmul(sbk[:, 0:2 * dv], lhsT=KWa[pb:pb + 64, i, :], rhs=V2[pb:pb + 64, i, :, :].rearrange("p d v -> p (d v)"),
                                   start=True, stop=True), [("kwa", c), "V2"], [("sbk", z)])
            P.T(lambda e: e.matmul(ob[pb:pb + 64, 0:2 * dv], lhsT=QS[:, cs], rhs=Sb[:, 0:2 * dv], start=True, stop=False),
                ["QS", "S"], [("ob", z)])
            for d in range(2):
                P.T(lambda e: e.matmul(ob[pb:pb + 64, d * dv:(d + 1) * dv], lhsT=ATTa[pb:pb + 64, i, d * 64:(d + 1) * 64],
                                       rhs=V2[pb:pb + 64, i, d, :], start=False, stop=(d == 1)), [("att", c), "V2"], [("ob", z)])
            if kind == "g":
                P.S(lambda e: e.copy(out=O[pb:pb + 64, i, :, :], in_=ob[pb:pb + 64, 0:256].rearrange("p (d v) -> p d v", d=2)),
                    [("ob", z)], ["O"])
            else:
                P.S(lambda e: e.activation(out=rec[pb:pb + 64, 0:2], in_=ob[pb:pb + 64, 0:258].rearrange("p (d v) -> p d v", d=2)[:, :, 128],
                                           func=AF.Abs), [("ob", z)], ["rec"])
                P.V(lambda e: e.tensor_scalar(out=rec[pb:pb + 64, 0:2], in0=rec[pb:pb + 64, 0:2], scalar1=1.0, scalar2=1.0, op0=ALU.max, op1=ALU.mult),
                    ["rec"], ["rec"])
                P.V(lambda e: e.reciprocal(out=rec[pb:pb + 64, 0:2], in_=rec[pb:pb + 64, 0:2]), ["rec"], ["rec"])
                for d in range(2):
                    P.V(lambda e: e.tensor_scalar_mul(out=O[pb:pb + 64, i, d, :], in0=ob[pb:pb + 64, d * dv:d * dv + 128],
                                                      scalar1=rec[pb:pb + 64, d:d + 1]), [("ob", z), "rec"], ["O"])
            P.V(lambda e: e.tensor_tensor(out=tmpS, in0=Sb, in1=sbk[:, 0:2 * dv], op=ALU.add), ["S", ("sbk", z)], ["tmpS"])
            P.V(lambda e: e.scalar_tensor_tensor(out=Sb, in0=tmpS, scalar=EB[:, c:c + 1], in1=bm, op0=ALU.mult, op1=ALU.mult),
                ["tmpS", "EB", "consts"], ["S"])
        GT = QS
        P.dma(GT, FMS[AI[gname]], w=["QS", "gate"])
        P.S(lambda e: e.activation(out=GT, in_=GT, func=gfn), ["gate"], ["pgate"])
        for gi, (s0, L, g0, n) in enumerate(groups()):
            yb = P.banks[gi % 2]
            for jj in range(n // 128):
                i = (s0 + g0) // 128 + jj
                im = s0 // 128 + (L // 128 - 1 - (i - s0 // 128))
                P.T(lambda e, yb=yb, jj=jj, i=i: e.matmul(yb[:, jj * 128:(jj + 1) * 128], lhsT=O[:, i, 0, :], rhs=ident, start=True, stop=False),
                    ["O", "consts"], ["psrc"])
                P.T(lambda e, yb=yb, jj=jj, im=im: e.matmul(yb[:, jj * 128:(jj + 1) * 128], lhsT=O[:, im, 1, :], rhs=Jm, start=False, stop=True),
                    ["O", "consts"], ["psrc"])
            t0 = s0 + g0
            headnorm(yb[:, 0:n], n, 128, center, 1e-6, gain, GT[:, t0:t0 + n], None, Yb, SQb, RSb, P.banks[2 + gi % 2], "p")
            P.dma(yT[row0:row0 + 128, t0:t0 + n], Yb[:, 0:n], r=["pY"], eng="gpsimd", final=True)
        P.barrier()

    glalike("g", 0)
    glalike("m", 0)
    glalike("m", 1)

    def shift(X, Y, rows, mun):
        cp, cn = PPc(mun, 0, 1, 0, rows), PPc(mun, 1, 1, 0, rows)
        c0 = P.alloc(1)
        P.V(lambda e: e.tensor_tensor(out=c0[0:rows], in0=cp, in1=cn, op=ALU.add), ["consts"], ["c0"])
        P.V(lambda e: e.tensor_scalar(out=c0[0:rows], in0=c0[0:rows], scalar1=-1.0, scalar2=1.0, op0=ALU.mult, op1=ALU.add), ["c0"], ["c0"])
        kx, ky = ("sh", id(X)), ("sh", id(Y))
        P.V(lambda e: e.tensor_scalar_mul(out=Y[0:rows], in0=X[0:rows], scalar1=c0[0:rows]), [kx, "c0"], [ky])
        for (s0, L) in SEGS:
            P.V(lambda e, s0=s0, L=L: e.scalar_tensor_tensor(out=Y[0:rows, s0 + 1:s0 + L], in0=X[0:rows, s0:s0 + L - 1], scalar=cp,
                                                             in1=Y[0:rows, s0 + 1:s0 + L], op0=ALU.mult, op1=ALU.add), [kx, ky, "consts"], [ky])
            P.V(lambda e, s0=s0, L=L: e.scalar_tensor_tensor(out=Y[0:rows, s0:s0 + L - 1], in0=X[0:rows, s0 + 1:s0 + L], scalar=cn,
                                                             in1=Y[0:rows, s0:s0 + L - 1], op0=ALU.mult, op1=ALU.add), [kx, ky, "consts"], [ky])
        return kx, ky

    for arr, mun, rows, fn in (("W1F", "muw1f", 96, AF.Tanh), ("W1B", "muw1b", 96, AF.Tanh), ("A1F", "mua1f", 96, None),
                               ("A1B", "mua1b", 96, None), ("RG01", "mug01", 128, AF.Sigmoid), ("RG2", "mug2", 64, AF.Sigmoid)):
        P.aptr = mark0
        X, Y = P.alloc(T), P.alloc(T)
        kx, ky = ("sh", id(X)), ("sh", id(Y))
        P.dma(X[0:rows], FMS[AI[arr], 0:rows, :], w=[kx])
        shift(X, Y, rows, mun)
        if fn is not None:
            P.S(lambda e, fn=fn: e.activation(out=Y[0:rows], in_=Y[0:rows], func=fn), [ky], [ky])
        P.dma(FMS[AI[arr], 0:rows, :], Y[0:rows], r=[ky], eng="gpsimd")
        P.barrier()

    def rwkv_head(j):
        P.aptr = mark0
        U = [P.alloc(T) for _ in range(9)]
        K_ = lambda i: ("U", i)
        Of = U[2]
        O = Of.rearrange("p (i c) -> p i c", c=128)
        OFF, EW = P.alloc(NCHK), P.alloc(NCHK)
        omka = P.alloc(1)
        SR = P.alloc(5120)
        lrt = [SR[:, z_ * 2048:(z_ + 1) * 2048].rearrange("p (a n) -> p a n", a=4) for z_ in range(2)]
        bst = [SR[:, 4096 + z_ * 512:4096 + (z_ + 1) * 512] for z_ in range(2)]
        for i_, a in enumerate(("RR", "RK", "RV")):
            P.dma(U[i_], FMS[AI[f"{a}{j}"]], w=[("sh", id(U[i_]))])
        shift(U[0], U[3], 128, f"mur{j}")
        shift(U[1], U[4], 128, f"muk{j}")
        shift(U[2], U[0], 128, f"muv{j}")
        R, Kk, Vv = U[3], U[4], U[0]
        kR, kK, kV = ("sh", id(U[3])), ("sh", id(U[4])), ("sh", id(U[0]))
        P.V(lambda e: e.scalar_tensor_tensor(out=U[1], in0=R, scalar=PPc(f"rk{j}"), in1=Kk, op0=ALU.mult, op1=ALU.mult),
            [kR, kK, ("sh", id(U[1])), "consts"], [K_(1)])
        for ti, t0 in enumerate(range(0, T, 512)):
            n = min(512, T - t0)
            z = ti % 2
            bk = P.banks[z]
            P.T(lambda e, bk=bk, t0=t0, n=n: e.matmul(bk[:, 0:n], lhsT=C("bones"), rhs=U[1][:, t0:t0 + n], start=True, stop=True),
                [K_(1), "consts"], [("bk", z)])
            P.V(lambda e, bk=bk, t0=t0, n=n, z=z: e.tensor_tensor(out=bst[z][:, 0:n], in0=bk[:, 0:n], in1=Vv[:, t0:t0 + n], op=ALU.mult),
                [("bk", z), kV], [("bst", z)])
            P.dma(FMS[AI[f"RR{j}"], :, t0:t0 + n], bst[z][:, 0:n], r=[("bst", z)], eng="gpsimd")
        for ti, t0 in enumerate(range(0, T, 512)):
            n = min(512, T - t0)
            z = ti % 2
            for ai_, a in enumerate(("W1F", "W1B", "A1F", "A1B")):
                P.dma(lrt[z][0:96, ai_, 0:n], FMS[AI[a], 0:96, t0:t0 + n], w=[("lrt", z)])
            for w_, (dst, bn, srcs) in enumerate(((U[6], f"w0{j}", (0, 1)), (U[5], f"a0{j}", (2, 3)))):
                bk = P.banks[2 + 2 * w_ + z]
                for d in range(2):
                    P.T(lambda e, bk=bk, d=d, w_=w_, srcs=srcs, z=z, n=n: e.matmul(bk[64 * d:64 * d + 64, 0:n], lhsT=w2v[:, j, w_, d, :],
                                                                                  rhs=lrt[z][0:96, srcs[d], 0:n], start=True, stop=True),
                        [("lrt", z), "consts"], [("bk2", w_, z)])
                P.S(lambda e, bk=bk, dst=dst, bn=bn, t0=t0, n=n: e.activation(out=dst[:, t0:t0 + n], in_=bk[:, 0:n], func=AF.Sigmoid, bias=PPc(bn)),
                    [("bk2", w_, z), "consts"], [K_(5 + (1 - w_))])
        P.V(lambda e: e.tensor_scalar_mul(out=U[1], in0=Kk, scalar1=PPc(f"kk{j}")), [kK, K_(1), "consts"], [K_(1)])
        P.S(lambda e: e.activation(out=U[8], in_=U[1], func=AF.Square), [K_(1)], [K_(8)])
        for ti, t0 in enumerate(range(0, T, 512)):
            n = min(512, T - t0)
            z = ti % 2
            bk = P.banks[z]
            P.T(lambda e, bk=bk, t0=t0, n=n: e.matmul(bk[:, 0:n], lhsT=C("bones"), rhs=U[8][:, t0:t0 + n], start=True, stop=True),
                [K_(8), "consts"], [("bk", z)])
            P.S(lambda e, bk=bk, t0=t0, n=n: e.activation(out=U[7][:, t0:t0 + n], in_=bk[:, 0:n], func=AF.Ln, bias=1e-24), [("bk", z)], [K_(7)])
        P.S(lambda e: e.activation(out=U[7], in_=U[7], func=AF.Exp, scale=-0.5), [K_(7)], [K_(7)])
        P.V(lambda e: e.tensor_tensor(out=U[1], in0=U[1], in1=U[7], op=ALU.mult), [K_(1), K_(7)], [K_(1)])
        P.V(lambda e: e.tensor_scalar(out=omka, in0=PPc(f"ka{j}"), scalar1=-1.0, scalar2=1.0, op0=ALU.mult, op1=ALU.add), ["consts"], ["omka"])
        P.V(lambda e: e.tensor_scalar(out=U[7], in0=U[5], scalar1=PPc(f"ka{j}"), scalar2=omka, op0=ALU.mult, op1=ALU.add),
            [K_(5), K_(7), "omka", "consts"], [K_(7)])
        P.V(lambda e: e.tensor_tensor(out=Kk, in0=Kk, in1=U[7], op=ALU.mult), [kK, K_(7), K_(1)], [kK])
        P.V(lambda e: e.tensor_scalar(out=U[6], in0=U[6], scalar1=-float(np.exp(-0.5)), scalar2=0.0, op0=ALU.mult, op1=ALU.add), [K_(6)], [K_(6)])
        P.V(lambda e: e.tensor_tensor_scan(out=U[7], data0=ones_b.to_broadcast([128, T]), data1=U[6], initial=0.0, op0=ALU.mult, op1=ALU.add),
            [K_(6), K_(7), kK, "consts"], ["EX"])
        chunk_local(U[7], OFF)
        P.S(lambda e: e.activation(out=U[8], in_=U[7], func=AF.Exp), ["EX", K_(8)], [K_(8)])
        P.V(lambda e: e.tensor_tensor(out=R, in0=R, in1=U[8], op=ALU.mult), [kR, K_(8), K_(1)], [kR])
        P.S(lambda e: e.activation(out=U[6], in_=U[6], func=AF.Exp, scale=-1.0), [K_(6), "EX"], [K_(6)])
        P.V(lambda e: e.tensor_tensor(out=U[6], in0=U[6], in1=U[8], op=ALU.mult), [K_(6), K_(8)], [K_(6)])
        P.V(lambda e: e.scalar_tensor_tensor(out=U[6], in0=U[6], scalar=-1.0, in1=U[1], op0=ALU.mult, op1=ALU.mult), [K_(6), K_(1)], [K_(6)])
        P.S(lambda e: e.activation(out=U[8], in_=U[7], func=AF.Exp, scale=-1.0), ["EX", K_(8), kR, K_(6)], [K_(8)])
        P.V(lambda e: e.tensor_tensor(out=U[1], in0=U[1], in1=U[5], op=ALU.mult), [K_(1), K_(5), K_(6)], [K_(1)])
        P.V(lambda e: e.tensor_tensor(out=U[1], in0=U[1], in1=U[8], op=ALU.mult), [K_(1), K_(8)], [K_(1)])
        P.V(lambda e: e.tensor_tensor(out=Kk, in0=Kk, in1=U[8], op=ALU.mult), [kK, K_(8)], [kK])
        P.S(lambda e: e.activation(out=EW[:, 0:NCHK], in_=v3(U[7])[:, :, 63], func=AF.Exp), ["EX"], ["EW"])
        P.V(lambda e: e.tensor_tensor(out=v3(U[5]), in0=v3(U[1]), in1=EW[:, 0:NCHK].unsqueeze(2).to_broadcast([128, NCHK, 64]), op=ALU.mult),
            [K_(1), K_(5), "EW"], [K_(5)])
        P.V(lambda e: e.tensor_tensor(out=v3(U[7]), in0=v3(Kk), in1=EW[:, 0:NCHK].unsqueeze(2).to_broadcast([128, NCHK, 64]), op=ALU.mult),
            [kK, "EX", "EW", K_(8)], ["KW"])
        AH, BH, KH, RH, BW, KW = U[6], U[1], Kk, R, U[5], U[7]
        kAH, kBH, kKH, kRH, kBW, kKW = K_(6), K_(1), kK, kR, K_(5), "KW"
        P.barrier()
        pool = [[U[8], 0, T], [SR, 0, 5120]]

        def carve(n):
            for pl in pool:
                if pl[1] + n <= pl[2]:
                    a_ = pl[0][:, pl[1]:pl[1] + n]
                    pl[1] += n
                    return a_
            return P.alloc(n)
        prod = {nm: [carve(256) for _ in range(2)] for nm in ("AAK", "ARK", "NT", "ARB", "NN")}
        tmb = {nm: [carve(128) for _ in range(2)] for nm in ("V", "BW", "KW")}
        Yi = [carve(256) for _ in range(2)]
        Zi = [carve(256) for _ in range(2)]
        Qb = [carve(256) for _ in range(2)]
        rhs_sb, u_sb = carve(128), carve(128)
        Sb, tmpS = carve(128), carve(128)
        Yb, SQb, RSb, Gt, Bt = carve(512), carve(512), carve(512), carve(512), carve(512)
        P.G(lambda e: e.memset(Sb, 0.0), [], ["S"])
        pc = 0
        for i in range(NT):
            ts = slice(128 * i, 128 * i + 128)
            z = i % 2
            specs = (("AAK", KH, AH, kKH, kAH, "MS"), ("ARK", KH, RH, kKH, kRH, "MI"), ("NT", BH, AH, kBH, kAH, "MS"),
                     ("ARB", BH, RH, kBH, kRH, "MI"), ("NN", AH, BH, kAH, kBH, "ML"))
            for nm, Lh, Rh, kl, kr, mk in specs:
                bi = pc % 2
                pc += 1
                bk = P.banks[bi]
                for d in range(2):
                    P.T(lambda e, bk=bk, d=d, Lh=Lh, Rh=Rh, ts=ts: e.matmul(bk[:, d * 128:(d + 1) * 128], lhsT=Lh[64 * d:64 * d + 64, ts],
                                                                           rhs=Rh[64 * d:64 * d + 64, ts], start=True, stop=True), [kl, kr], [("pb", bi)])
                P.V(lambda e, bk=bk, nm=nm, mk=mk, z=z: e.tensor_tensor(out=prod[nm][z], in0=bk[:, 0:256], in1=C(mk), op=ALU.mult),
                    [("pb", bi), "consts"], [(nm, z)])
            for nm, Xs, kx_ in (("V", Vv, kV), ("BW", BW, kBW), ("KW", KW, kKW)):
                bi = pc % 2
                pc += 1
                bk = P.banks[bi]
                P.T(lambda e, bk=bk, Xs=Xs, ts=ts: e.transpose(bk[:, 0:128], Xs[:, ts], ident), [kx_, "consts"], [("pb", bi)])
                P.S(lambda e, bk=bk, nm=nm, z=z: e.copy(out=tmb[nm][z], in_=bk[:, 0:128]), [("pb", bi)], [("tm" + nm, z)])
            Yc, Zc, Q = prod["NT"][z], prod["NN"][z], Qb[z]
            kY, kZ = ("NT", z), ("NN", z)
            P.V(lambda e, Q=Q, Yc=Yc: e.tensor_tensor(out=Q, in0=Yc, in1=C("I2"), op=ALU.add), [kY, "consts"], [("Q", z)])
            for it in range(5):
                Yn, Zn = Yi[it % 2], Zi[it % 2]
                for d in range(2):
                    ds_ = slice(d * 128, (d + 1) * 128)
                    P.T(lambda e, ds_=ds_, Yc=Yc, Zc=Zc: e.matmul(P.banks[2][:, ds_], lhsT=Zc[:, ds_], rhs=Yc[:, ds_], start=True, stop=True),
                        [kY, kZ], ["bY"])
                    P.T(lambda e, ds_=ds_, Yc=Yc, Zc=Zc: e.matmul(P.banks[3][:, ds_], lhsT=Yc[:, ds_], rhs=Zc[:, ds_], start=True, stop=True),
                        [kY, kZ], ["bZ"])
                P.S(lambda e, Yn=Yn: e.copy(out=Yn, in_=P.banks[2][:, 0:256]), ["bY"], [("Yi", it % 2)])
                P.V(lambda e, Zn=Zn: e.tensor_copy(out=Zn, in_=P.banks[3][:, 0:256]), ["bZ"], [("Zi", it % 2)])
                Yc, Zc, kY, kZ = Yn, Zn, ("Yi", it % 2), ("Zi", it % 2)
                for d in range(2):
                    ds_ = slice(d * 128, (d + 1) * 128)
                    P.T(lambda e, ds_=ds_, Zc=Zc, Q=Q: e.matmul(P.banks[4][:, ds_], lhsT=Zc[:, ds_], rhs=Q[:, ds_], start=True, stop=True),
                        [kZ, ("Q", z)], ["bQ"])
                P.V(lambda e, Q=Q: e.tensor_tensor(out=Q, in0=Q, in1=P.banks[4][:, 0:256], op=ALU.add), ["bQ", ("Q", z)], [("Q", z)])
            AAK, ARK, ARB = prod["AAK"][z], prod["ARK"][z], prod["ARB"][z]
            Vt, BWt, KWt = tmb["V"][z], tmb["BW"][z], tmb["KW"][z]
            for h in range(2):
                c = 2 * i + h
                pb = 64 * h
                cs = slice(c * 64, c * 64 + 64)
                bR, bU = P.banks[5][pb:pb + 64, 0:128], P.banks[5][pb:pb + 64, 128:256]
                bY2, bS = P.banks[6][pb:pb + 64, 0:128], P.banks[7][:, 0:128]

                def blk(M_, d, pb=pb):
                    return M_[pb:pb + 64, d * 128 + pb:d * 128 + pb + 64]
                P.T(lambda e, bR=bR, cs=cs: e.matmul(bR, lhsT=AH[:, cs], rhs=Sb, start=True, stop=False), [kAH, "S"], ["bR"])
                for d in range(2):
                    P.T(lambda e, d=d, bR=bR, pb=pb: e.matmul(bR[:, d * 64:(d + 1) * 64], lhsT=blk(AAK, d), rhs=Vt[pb:pb + 64, d * 64:(d + 1) * 64],
                                                              start=False, stop=(d == 1)), [("AAK", z), ("tmV", z)], ["bR"])
                P.S(lambda e, bR=bR, pb=pb: e.copy(out=rhs_sb[pb:pb + 64, :], in_=bR), ["bR"], ["rhs_sb"])
                for d in range(2):
                    P.T(lambda e, d=d, bU=bU, pb=pb: e.matmul(bU[:, d * 64:(d + 1) * 64], lhsT=blk(Q, d), rhs=rhs_sb[pb:pb + 64, d * 64:(d + 1) * 64],
                                                              start=True, stop=True), [("Q", z), "rhs_sb"], ["bU"])
                P.V(lambda e, bU=bU, pb=pb: e.tensor_copy(out=u_sb[pb:pb + 64, :], in_=bU), ["bU"], ["u_sb"])
                P.T(lambda e, bY2=bY2, cs=cs: e.matmul(bY2, lhsT=RH[:, cs], rhs=Sb, start=True, stop=False), [kRH, "S"], ["bY2"])
                for d in range(2):
                    P.T(lambda e, d=d, bY2=bY2, pb=pb: e.matmul(bY2[:, d * 64:(d + 1) * 64], lhsT=blk(ARB, d), rhs=u_sb[pb:pb + 64, d * 64:(d + 1) * 64],
                                                                start=False, stop=False), [("ARB", z), "u_sb"], ["bY2"])
                    P.T(lambda e, d=d, bY2=bY2, pb=pb: e.matmul(bY2[:, d * 64:(d + 1) * 64], lhsT=blk(ARK, d), rhs=Vt[pb:pb + 64, d * 64:(d + 1) * 64],
                                                                start=False, stop=(d == 1)), [("ARK", z), ("tmV", z)], ["bY2"])
                P.S(lambda e, bY2=bY2, pb=pb, i=i: e.copy(out=O[pb:pb + 64, i, :], in_=bY2), ["bY2", K_(2), ("sh", id(U[2]))], ["O"])
                P.T(lambda e, bS=bS, pb=pb: e.matmul(bS, lhsT=BWt[pb:pb + 64, :], rhs=u_sb[pb:pb + 64, :], start=True, stop=False),
                    [("tmBW", z), "u_sb"], ["bS"])
                P.T(lambda e, bS=bS, pb=pb: e.matmul(bS, lhsT=KWt[pb:pb + 64, :], rhs=Vt[pb:pb + 64, :], start=False, stop=True),
                    [("tmKW", z), ("tmV", z)], ["bS"])
                P.V(lambda e, bS=bS, c=c: e.scalar_tensor_tensor(out=tmpS, in0=Sb, scalar=EW[:, c:c + 1], in1=bS, op0=ALU.mult, op1=ALU.add),
                    ["S", "EW", "bS"], ["tmpS"])
                P.V(lambda e: e.tensor_tensor(out=Sb, in0=tmpS, in1=C("bm64"), op=ALU.mult), ["tmpS", "consts"], ["S"])
        garr, gr0 = (("RG01", 0), ("RG01", 64), ("RG2", 0))[j]
        for gi, (s0, L, g0, n) in enumerate(groups()):
            yb = P.banks[gi % 2]
            t0 = s0 + g0
            for jj in range(n // 128):
                i = t0 // 128 + jj
                im = s0 // 128 + (L // 128 - 1 - (i - s0 // 128))
                P.T(lambda e, yb=yb, jj=jj, i=i: e.matmul(yb[0:64, jj * 128:(jj + 1) * 128], lhsT=O[:, i, 0:64], rhs=ident, start=True, stop=False),
                    ["O", "consts"], ["rsrc"])
                P.T(lambda e, yb=yb, jj=jj, im=im: e.matmul(yb[0:64, jj * 128:(jj + 1) * 128], lhsT=O[:, im, 64:128], rhs=Jm, start=False, stop=True),
                    ["O", "consts"], ["rsrc"])
            P.dma(Gt[0:64, 0:n], FMS[AI[garr], gr0:gr0 + 64, t0:t0 + n], w=["rgate"])
            P.dma(Bt[0:64, 0:n], FMS[AI[f"RR{j}"], 0:64, t0:t0 + n], w=["rbonus"])
            headnorm(yb[0:64, 0:n], n, 64, True, 64e-5, PPc(f"rgn{j}", 0, 1, 0, 64), Gt[0:64, 0:n], Bt[0:64, 0:n], Yb, SQb, RSb, P.banks[2 + gi % 2], "r")
            P.dma(yT[384 + 64 * j:448 + 64 * j, t0:t0 + n], Yb[0:64, 0:n], r=["rY"], eng="gpsimd", final=True)
        P.barrier()

    for j in range(3):
        rwkv_head(j)
    P.emit()
    P.close()
    return nc


GLA_BASE, ML_BASE, RW_BASE = 0, 1568, 3896


def _wcols(g):
    ar = np.arange
    cols = {"gq": g * 64 + ar(64), "gk": 256 + g * 64 + ar(64), "glrf": 1536 + ar(16), "glrb": 1552 + ar(16),
            "gg": 1024 + g * 128 + ar(128), "gv": 512 + g * 128 + ar(128)}
    for s, h in enumerate(MSLOT[g]):
        cols[f"mq{s}"] = ML_BASE + h * 64 + ar(64)
        cols[f"mk{s}"] = ML_BASE + 384 + h * 64 + ar(64)
        cols[f"mv{s}"] = ML_BASE + 768 + h * 128 + ar(128)
        cols[f"mo{s}"] = ML_BASE + 1536 + h * 128 + ar(128)
        for nm, d, w in (("mif", 0, 0), ("mff", 0, 1), ("mib", 1, 0), ("mfb", 1, 1)):
            cols[f"{nm}{s}"] = np.full(64, ML_BASE + 2304 + d * 12 + w * 6 + h)
    for j in range(3):
        hh = 3 * g + j
        for k_, nm in enumerate(("rr", "rk", "rv", "rg")):
            cols[f"{nm}{j}"] = RW_BASE + 768 * k_ + hh * 64 + ar(64)
    for k_, nm in enumerate(("w1f", "w1b", "a1f", "a1b")):
        cols[nm] = RW_BASE + 3072 + 96 * k_ + ar(96)
    return cols


def prep_B(l, b, g, inp, mod, xc, xl):
    f32 = np.float32
    cols = _wcols(g)
    w_in = inp["w_in"][l]
    wfm = np.concatenate([w_in[:, cols[n]] for n, _ in WBLK], axis=1)
    wtm = np.concatenate([w_in[:, cols[n]] for n in ("gv", "mv0", "mv1")], axis=1)
    xT = np.ascontiguousarray(np.concatenate([xc[b], xl[b]], 0).T)
    sh1, sc1 = mod[l][:, 0:D], mod[l][:, D:2 * D]
    modv = np.stack([pk(inp["g_norm1"][l]), pk(sc1[2]), pk(sh1[2]), pk(sc1[b]), pk(sh1[b])], axis=2).reshape(128, KC * 5)
    pp = np.zeros((128, NPP), f32)

    def put(n, a, r0=0):
        o, w = PPN[n]
        a = np.asarray(a, f32).reshape(-1, w)
        pp[r0:r0 + a.shape[0], o:o + w] = a
    put("gba", inp["gla_b_a"][l][0, g * 64:(g + 1) * 64])
    put("gba", inp["gla_b_a"][l][1, g * 64:(g + 1) * 64], 64)
    put("ggn", inp["gla_g_norm"][l][g * 128:(g + 1) * 128])
    cwt = inp["ml_conv_w"][l].reshape(9, 768)
    for s, h in enumerate(MSLOT[g]):
        for nm, c0 in (("q", h * 64), ("k", 384 + h * 64)):
            wv = cwt[:, c0:c0 + 64].T
            put(f"cw{nm}{s}", wv)
            put(f"cw{nm}{s}", wv[:, ::-1], 64)
            put(f"cb{nm}{s}", inp["ml_conv_b"][l][c0:c0 + 64])
            put(f"cb{nm}{s}", inp["ml_conv_b"][l][c0:c0 + 64], 64)
        for d in range(2):
            put(f"mbi{s}", np.full(64, inp["ml_gate_b"][l][d, 0, h]), 64 * d)
            put(f"mbf{s}", np.full(64, inp["ml_gate_b"][l][d, 1, h]), 64 * d)
        put(f"mgn{s}", inp["ml_g_norm"][l][h * 128:(h + 1) * 128])
    mu = inp["rw_mu"][l]

    def mupair(c, swap):
        m = np.stack([mu[0][c], mu[1][c]], 1)
        return m[:, ::-1] if swap else m
    for j in range(3):
        hh = 3 * g + j
        hc = hh * 64 + np.arange(64)
        for nm, off in (("mur", 0), ("muk", 768), ("muv", 1536)):
            put(f"{nm}{j}", mupair(off + hc, False))
            put(f"{nm}{j}", mupair(off + hc, True), 64)
        for d in range(2):
            put(f"w0{j}", inp["rw_w0"][l][d, hc], 64 * d)
            put(f"a0{j}", inp["rw_a0"][l][d, hc], 64 * d)
            put(f"kk{j}", inp["rw_k_k"][l][hc], 64 * d)
            put(f"ka{j}", inp["rw_k_a"][l][hc], 64 * d)
            put(f"rk{j}", inp["rw_r_k"][l][hc], 64 * d)
        put(f"rgn{j}", inp["rw_g_norm"][l][hc])
    put("mug01", mupair(2304 + (3 * g) * 64 + np.arange(64), False))
    put("mug01", mupair(2304 + (3 * g + 1) * 64 + np.arange(64), False), 64)
    put("mug2", mupair(2304 + (3 * g + 2) * 64 + np.arange(64), False))
    put("muw1f", mupair(3072 + np.arange(96), False))
    put("muw1b", mupair(3168 + np.arange(96), True))
    put("mua1f", mupair(3264 + np.arange(96), False))
    put("mua1b", mupair(3360 + np.arange(96), True))
    wa2 = np.zeros((128, 64), f32)
    wa2[0:16] = inp["gla_w_a2"][l][0][:, g * 64:(g + 1) * 64]
    wa2[64:80] = inp["gla_w_a2"][l][1][:, g * 64:(g + 1) * 64]
    w2 = np.zeros((96, 3, 2, 2, 64), f32)
    for j in range(3):
        hc = (3 * g + j) * 64 + np.arange(64)
        for d in range(2):
            w2[:, j, 0, d] = inp["rw_w2"][l][d][:, hc]
            w2[:, j, 1, d] = inp["rw_a2"][l][d][:, hc]
    return {"xT": xT, "modv": np.ascontiguousarray(modv, f32), "wfm": np.ascontiguousarray(wfm), "wtm": np.ascontiguousarray(wtm),
            "cst": make_consts(), "pp": pp, "wa2": wa2, "w2": w2.reshape(96, 768)}


def assemble_yT(res, B, T):
    yT = np.zeros((B, D, T), np.float32)
    for b in range(B):
        for g in range(4):
            r = res[b * 4 + g]["yT"]
            yT[b, g * 128:(g + 1) * 128] = r[0:128]
            for s, h in enumerate(MSLOT[g]):
                yT[b, 512 + h * 128:512 + (h + 1) * 128] = r[128 + 128 * s:256 + 128 * s]
            for j in range(3):
                hh = 3 * g + j
                yT[b, 1280 + hh * 64:1280 + (hh + 1) * 64] = r[384 + 64 * j:448 + 64 * j]
    return yT


def build_C1(NCc, NLl):
    NTK = NCc + NLl
    TW = 256
    BIG = 1.0e30
    nc = bass.Bass("TRN2", target_bir_lowering=False)

    def din(name, shape, dt=F32):
        return nc.dram_tensor(name, list(shape), dt, kind="ExternalInput").ap()
    yT, xT = din("yT", [D, NTK]), din("xT", [D, NTK])
    wo_d, modv_d, wr_d, br_d, cst_d = din("wo", [D, D]), din("modv", [128, KC * 7]), din("wr", [D, 36]), din("br", [128, 36]), din("cst", [128, NCST])
    x1T = nc.dram_tensor("x1T", [D, NTK], F32, kind="ExternalOutput").ap()
    h2T = nc.dram_tensor("h2T", [D, NTK], BF16, kind="ExternalOutput").ap()
    gates = nc.dram_tensor("gates", [NTK, 32], F32, kind="ExternalOutput").ap()
    P = Prog(nc)
    cst = P.sbuf([128, NCST], F32, "cst_sb")
    mv = P.sbuf([128, KC * 7], F32, "mv_sb")
    wr = P.sbuf([128, KC * 36], F32, "wr_sb")
    br = P.sbuf([128, 36], F32, "br_sb")
    gm = P.sbuf([128, KC * 2], F32, "gm_sb")
    P.dma(cst[:], cst_d, w=["consts"])
    P.dma(mv[:], modv_d, w=["consts"])
    P.dma(br[:], br_d, w=["consts"])
    wrv = wr[:].rearrange("p (k n) -> p k n", n=36)
    P.dma(wrv, wr_d.rearrange("(k p) n -> p k n", p=128), w=["consts"])
    mvv = mv[:].rearrange("p (k j) -> p k j", j=7)
    gmv = gm[:].rearrange("p (k j) -> p k j", j=2)
    ones = cst[:, CSTN["ones"][0]:CSTN["ones"][0] + 128]
    for j, col in enumerate((2, 5)):
        P.V(lambda e: e.tensor_scalar(out=gmv[:, :, j], in0=mvv[:, :, col], scalar1=1.0, scalar2=1.0, op0=ALU.add, op1=ALU.mult), ["consts"], ["gm"])
        P.V(lambda e: e.tensor_tensor(out=gmv[:, :, j], in0=gmv[:, :, j], in1=mvv[:, :, 0], op=ALU.mult), ["gm", "consts"], ["gm"])
    wo = P.alloc(KC * D, BF16).rearrange("p (k n) -> p k n", n=D)
    for c0 in range(0, D, 512):
        P.dma(wo[:, :, c0:c0 + 512], wo_d[:, c0:c0 + 512].rearrange("(k p) n -> p k n", p=128), w=["wo"], eng="gpsimd")
    yb = [P.alloc(KC * TW, BF16).rearrange("p (k n) -> p k n", n=TW) for _ in range(2)]
    xb = [P.alloc(KC * TW).rearrange("p (k n) -> p k n", n=TW) for _ in range(2)]
    sq = P.alloc(KC * TW).rearrange("p (k n) -> p k n", n=TW)
    hb = P.alloc(KC * TW, BF16).rearrange("p (k n) -> p k n", n=TW)
    rstd = P.alloc(TW)
    Lg, LM, I1, I2_, G1 = P.alloc(36), P.alloc(32), P.alloc(32), P.alloc(32), P.alloc(32)
    sm = P.alloc(16)
    tiles = [(s0, o, min(TW, L - o)) for (s0, L) in ((0, NCc), (NCc, NLl)) for o in range(0, L, TW)]
    for ti, (s0, o, n) in enumerate(tiles):
        b = ti % 2
        t0 = s0 + o
        sg = 0 if s0 == 0 else 1
        yt, xt = yb[b], xb[b]
        P.dma(yt[:, :, 0:n], yT[:, t0:t0 + n].rearrange("(k p) t -> p k t", p=128), w=[("yt", b)], eng="gpsimd")
        P.dma(xt[:, :, 0:n], xT[:, t0:t0 + n].rearrange("(k p) t -> p k t", p=128), w=[("xt", b)])
        for fo in range(KC):
            bi = fo % 4
            bank = P.banks[1 + bi]
            for k in range(KC):
                P.T(lambda e: e.matmul(bank[:, 0:n], lhsT=wo[:, k, fo * 128:(fo + 1) * 128], rhs=yt[:, k, 0:n], start=(k == 0), stop=(k == KC - 1)),
                    ["wo", ("yt", b)], [("bank", bi)], inc=(k == KC - 1))
            P.V(lambda e: e.scalar_tensor_tensor(out=xt[:, fo, 0:n], in0=bank[:, 0:n], scalar=mvv[:, fo, 1 + 3 * sg:2 + 3 * sg], in1=xt[:, fo, 0:n],
                                                 op0=ALU.mult, op1=ALU.add), [("bank", bi), ("xt", b), "consts"], [("xt", b)])
        P.dma(x1T[:, t0:t0 + n].rearrange("(k p) t -> p k t", p=128), xt[:, :, 0:n], r=[("xt", b)], eng="gpsimd", final=True)
        rms_rstd(P, xt, n, ones, P.banks[0], sq, rstd, [("xt", b)], "rstd", 0)
        P.V(lambda e: e.tensor_tensor(out=sq[:, :, 0:n], in0=xt[:, :, 0:n], in1=rstd[:, 0:n].unsqueeze(1).to_broadcast([128, KC, n]), op=ALU.mult),
            [("xt", b), "rstd", ("sq", 0)], [("sq", 0)])
        for k in range(KC):
            P.V(lambda e: e.tensor_scalar(out=sq[:, k, 0:n], in0=sq[:, k, 0:n], scalar1=gmv[:, k, sg:sg + 1],
                                          scalar2=mvv[:, k, 3 + 3 * sg:4 + 3 * sg], op0=ALU.mult, op1=ALU.add), [("sq", 0), "gm", "consts"], [("sq", 0)])
        P.G(lambda e: e.tensor_copy(out=hb[:, :, 0:n], in_=sq[:, :, 0:n]), [("sq", 0)], ["hb"])
        P.dma(h2T[:, t0:t0 + n].rearrange("(k p) t -> p k t", p=128), hb[:, :, 0:n], r=["hb"], eng="gpsimd", final=True)
        for m0 in range(0, n, 128):
            m = min(128, n - m0)
            bank = P.banks[5]
            for k in range(KC):
                P.T(lambda e: e.matmul(bank[0:m, 0:36], lhsT=sq[:, k, m0:m0 + m], rhs=wrv[:, k, :], start=(k == 0), stop=(k == KC - 1)),
                    [("sq", 0), "consts"], ["rb"], inc=(k == KC - 1))
            A = lambda t_, c0=0, c1=None: t_[0:m, c0:(c1 if c1 is not None else t_.shape[1])]
            P.V(lambda e: e.tensor_tensor(out=A(Lg), in0=bank[0:m, 0:36], in1=br[0:m, :], op=ALU.add), ["rb", "consts"], ["Lg"])
            P.V(lambda e: e.reduce_max(out=sm[0:m, 0:1], in_=Lg[0:m, 0:4], axis=AX.X), ["Lg"], ["sm"])
            P.V(lambda e: e.tensor_scalar(out=sm[0:m, 1:2], in0=sm[0:m, 0:1], scalar1=-1.0, scalar2=0.0, op0=ALU.mult, op1=ALU.add), ["sm"], ["sm"])
            P.S(lambda e: e.activation(out=sm[0:m, 8:12], in_=Lg[0:m, 0:4], func=AF.Exp, bias=sm[0:m, 1:2], accum_out=sm[0:m, 2:3]), ["Lg", "sm"], ["sm"])
            P.V(lambda e: e.reciprocal(out=sm[0:m, 3:4], in_=sm[0:m, 2:3]), ["sm"], ["sm"])
            P.V(lambda e: e.tensor_scalar(out=sm[0:m, 12:16], in0=Lg[0:m, 0:4], scalar1=sm[0:m, 0:1], scalar2=BIG, op0=ALU.is_ge, op1=ALU.mult),
                ["Lg", "sm"], ["sm"])
            P.V(lambda e: e.tensor_scalar(out=sm[0:m, 12:16], in0=sm[0:m, 12:16], scalar1=-BIG, scalar2=1.0, op0=ALU.add, op1=ALU.mult), ["sm"], ["sm"])
            P.V(lambda e: e.tensor_tensor(out=LM[0:m, :].rearrange("p (g x) -> p g x", g=4), in0=Lg[0:m, 4:36].rearrange("p (g x) -> p g x", g=4),
                                          in1=sm[0:m, 12:16].unsqueeze(2).to_broadcast([m, 4, 8]), op=ALU.add), ["Lg", "sm"], ["LM"])
            P.V(lambda e: e.reduce_max(out=sm[0:m, 4:5], in_=LM[0:m, :], axis=AX.X), ["LM"], ["sm"])
            P.V(lambda e: e.tensor_scalar(out=I1[0:m, :], in0=LM[0:m, :], scalar1=sm[0:m, 4:5], scalar2=1.0, op0=ALU.is_ge, op1=ALU.mult), ["LM", "sm"], ["I1"])
            P.V(lambda e: e.scalar_tensor_tensor(out=LM[0:m, :], in0=I1[0:m, :], scalar=-BIG, in1=LM[0:m, :], op0=ALU.mult, op1=ALU.add), ["I1", "LM"], ["LM"])
            P.V(lambda e: e.reduce_max(out=sm[0:m, 5:6], in_=LM[0:m, :], axis=AX.X), ["LM"], ["sm"])
            P.V(lambda e: e.tensor_scalar(out=I2_[0:m, :], in0=LM[0:m, :], scalar1=sm[0:m, 5:6], scalar2=1.0, op0=ALU.is_ge, op1=ALU.mult), ["LM", "sm"], ["I2"])
            P.V(lambda e: e.tensor_tensor(out=sm[0:m, 6:7], in0=sm[0:m, 4:5], in1=sm[0:m, 5:6], op=ALU.subtract), ["sm"], ["sm"])
            P.S(lambda e: e.activation(out=sm[0:m, 6:7], in_=sm[0:m, 6:7], func=AF.Sigmoid), ["sm"], ["sm"])
            P.V(lambda e: e.tensor_tensor(out=sm[0:m, 6:7], in0=sm[0:m, 6:7], in1=sm[0:m, 3:4], op=ALU.mult), ["sm"], ["sm"])
            P.V(lambda e: e.tensor_tensor(out=sm[0:m, 7:8], in0=sm[0:m, 3:4], in1=sm[0:m, 6:7], op=ALU.subtract), ["sm"], ["sm"])
            P.V(lambda e: e.tensor_scalar_mul(out=G1[0:m, :], in0=I1[0:m, :], scalar1=sm[0:m, 6:7]), ["I1", "sm"], ["G1"])
            P.V(lambda e: e.scalar_tensor_tensor(out=G1[0:m, :], in0=I2_[0:m, :], scalar=sm[0:m, 7:8], in1=G1[0:m, :], op0=ALU.mult, op1=ALU.add),
                ["I2", "sm", "G1"], ["G1"])
            P.dma(gates[t0 + m0:t0 + m0 + m, :], G1[0:m, :], r=["G1"], eng="gpsimd", final=True)
    P.emit()
    P.close()
    return nc


def build_C2(TT, DE):
    NK = DE // 128
    TW = 512
    nc = bass.Bass("TRN2", target_bir_lowering=False)
    h2T = nc.dram_tensor("h2T", [D, TT], BF16, kind="ExternalInput").ap()
    gT = nc.dram_tensor("gT", [4, TT], F32, kind="ExternalInput").ap()
    wg_d = nc.dram_tensor("wg", [4, D, DE], F32, kind="ExternalInput").ap()
    wu_d = nc.dram_tensor("wu", [4, D, DE], F32, kind="ExternalInput").ap()
    wd_d = nc.dram_tensor("wd", [4, DE, D], F32, kind="ExternalInput").ap()
    fT = nc.dram_tensor("fT", [D, TT], F32, kind="ExternalOutput").ap()
    P = Prog(nc)
    wg = [P.alloc(KC * DE, BF16).rearrange("p (k n) -> p k n", n=DE) for _ in range(2)]
    wu = [P.alloc(KC * DE, BF16).rearrange("p (k n) -> p k n", n=DE) for _ in range(2)]
    wd = [P.alloc(NK * D, BF16).rearrange("p (k n) -> p k n", n=D) for _ in range(2)]
    hb = [P.alloc(KC * TW, BF16).rearrange("p (k n) -> p k n", n=TW) for _ in range(2)]
    Ab = [[[P.alloc(TW, BF16) for _ in range(NK)] for _ in range(2)] for _ in range(2)]
    gb = [[P.alloc(TW) for _ in range(2)] for _ in range(2)]
    sgb = [P.alloc(TW) for _ in range(2)]
    ost = [P.alloc(TW) for _ in range(3)]
    prv = [P.alloc(TW) for _ in range(2)]
    tiles = [(t0, min(TW, TT - t0)) for t0 in range(0, TT, TW)]
    for p in range(2):
        for e_ in range(2):
            ex = 2 * p + e_
            P.dma(wg[e_][:, :, :], wg_d[ex].rearrange("(k p) n -> p k n", p=128), w=[("W", e_)], eng="gpsimd")
            P.dma(wu[e_][:, :, :], wu_d[ex].rearrange("(k p) n -> p k n", p=128), w=[("W", e_)], eng="gpsimd")
            for c0 in range(0, D, 512):
                P.dma(wd[e_][:, :, c0:c0 + 512], wd_d[ex, :, c0:c0 + 512].rearrange("(k p) n -> p k n", p=128), w=[("W", e_)], eng="gpsimd")
        gc = 0
        for ti, (t0, n) in enumerate(tiles):
            b = ti % 2
            h = hb[b]
            P.dma(h[:, :, 0:n], h2T[:, t0:t0 + n].rearrange("(k p) t -> p k t", p=128), w=[("h", b)])
            for e_ in range(2):
                P.dma(gb[b][e_][:, 0:n], gT[2 * p + e_:2 * p + e_ + 1, t0:t0 + n].partition_broadcast(128), w=[("g", b, e_)])
            for e_ in range(2):
                for kc in range(NK):
                    z = gc % 2
                    gc += 1
                    bG, bU = P.banks[z], P.banks[2 + z]
                    for k in range(KC):
                        P.T(lambda e: e.matmul(bG[:, 0:n], lhsT=wg[e_][:, k, kc * 128:(kc + 1) * 128], rhs=h[:, k, 0:n], start=(k == 0), stop=(k == KC - 1)),
                            [("W", e_), ("h", b)], [("bG", z)], inc=(k == KC - 1))
                    for k in range(KC):
                        P.T(lambda e: e.matmul(bU[:, 0:n], lhsT=wu[e_][:, k, kc * 128:(kc + 1) * 128], rhs=h[:, k, 0:n], start=(k == 0), stop=(k == KC - 1)),
                            [("W", e_), ("h", b)], [("bU", z)], inc=(k == KC - 1))
                    P.S(lambda e: e.activation(out=sgb[z][:, 0:n], in_=bG[:, 0:n], func=AF.Silu), [("bG", z)], [("sg", z)])
                    P.V(lambda e: e.tensor_tensor(out=sgb[z][:, 0:n], in0=sgb[z][:, 0:n], in1=bU[:, 0:n], op=ALU.mult), [("sg", z), ("bU", z)], [("sg", z)])
                    P.G(lambda e: e.tensor_tensor(out=Ab[b][e_][kc][:, 0:n], in0=sgb[z][:, 0:n], in1=gb[b][e_][:, 0:n], op=ALU.mult),
                        [("sg", z), ("g", b, e_)], [("A", b)])
            for fo in range(KC):
                z = fo % 2
                bF = P.banks[4 + z]
                o_ = ost[fo % 3]
                first = True
                for e_ in range(2):
                    for kc in range(NK):
                        last = (e_ == 1 and kc == NK - 1)
                        P.T(lambda e: e.matmul(bF[:, 0:n], lhsT=wd[e_][:, kc, fo * 128:(fo + 1) * 128], rhs=Ab[b][e_][kc][:, 0:n], start=first, stop=last),
                            [("W", e_), ("A", b)], [("bF", z)], inc=last)
                        first = False
                if p == 0:
                    P.S(lambda e: e.copy(out=o_[:, 0:n], in_=bF[:, 0:n]), [("bF", z)], [("ost", fo % 3)])
                else:
                    pv = prv[fo % 2]
                    P.dma(pv[:, 0:n], fT[fo * 128:(fo + 1) * 128, t0:t0 + n], w=[("prv", fo % 2)])
                    P.V(lambda e: e.tensor_tensor(out=o_[:, 0:n], in0=bF[:, 0:n], in1=pv[:, 0:n], op=ALU.add), [("bF", z), ("prv", fo % 2)], [("ost", fo % 3)])
                P.dma(fT[fo * 128:(fo + 1) * 128, t0:t0 + n], o_[:, 0:n], r=[("ost", fo % 3)], eng="gpsimd", final=(p == 1))
        P.barrier()
    P.emit()
    P.close()
    return nc


def build_C3(NCc, NLl, NP):
    NTK = NCc + NLl
    TW = 256
    nc = bass.Bass("TRN2", target_bir_lowering=False)
    fTp = nc.dram_tensor("fTp", [NP, D, NTK], F32, kind="ExternalInput").ap()
    x1T = nc.dram_tensor("x1T", [D, NTK], F32, kind="ExternalInput").ap()
    modv_d = nc.dram_tensor("modv", [128, KC * 3], F32, kind="ExternalInput").ap()
    cst_d = nc.dram_tensor("cst", [128, NCST], F32, kind="ExternalInput").ap()
    x2T = nc.dram_tensor("x2T", [D, NTK], F32, kind="ExternalOutput").ap()
    onT = nc.dram_tensor("onT", [D, NTK], F32, kind="ExternalOutput").ap()
    P = Prog(nc)
    cst = P.sbuf([128, NCST], F32, "cst_sb")
    mv = P.sbuf([128, KC * 3], F32, "mv_sb")
    P.dma(cst[:], cst_d, w=["consts"])
    P.dma(mv[:], modv_d, w=["consts"])
    mvv = mv[:].rearrange("p (k j) -> p k j", j=3)
    ones = cst[:, CSTN["ones"][0]:CSTN["ones"][0] + 128]
    pb_ = [P.alloc(KC * TW).rearrange("p (k n) -> p k n", n=TW) for _ in range(3)]
    acc = [P.alloc(KC * TW).rearrange("p (k n) -> p k n", n=TW) for _ in range(2)]
    xb = [P.alloc(KC * TW).rearrange("p (k n) -> p k n", n=TW) for _ in range(2)]
    sq = P.alloc(KC * TW).rearrange("p (k n) -> p k n", n=TW)
    rstd = P.alloc(TW)
    tiles = [(s0, o, min(TW, L - o)) for (s0, L) in ((0, NCc), (NCc, NLl)) for o in range(0, L, TW)]
    pc = 0
    for ti, (s0, o, n) in enumerate(tiles):
        b = ti % 2
        t0 = s0 + o
        sg = 0 if s0 == 0 else 1
        a_, xt = acc[b], xb[b]
        P.dma(xt[:, :, 0:n], x1T[:, t0:t0 + n].rearrange("(k p) t -> p k t", p=128), w=[("xt", b)])
        P.dma(a_[:, :, 0:n], fTp[0, :, t0:t0 + n].rearrange("(k p) t -> p k t", p=128), w=[("acc", b)])
        for c in range(1, NP):
            z = pc % 3
            pc += 1
            P.dma(pb_[z][:, :, 0:n], fTp[c, :, t0:t0 + n].rearrange("(k p) t -> p k t", p=128), w=[("pb", z)])
            P.V(lambda e: e.tensor_tensor(out=a_[:, :, 0:n], in0=a_[:, :, 0:n], in1=pb_[z][:, :, 0:n], op=ALU.add), [("acc", b), ("pb", z)], [("acc", b)])
        P.V(lambda e: e.tensor_tensor(out=a_[:, :, 0:n], in0=a_[:, :, 0:n], in1=mvv[:, :, sg:sg + 1].to_broadcast([128, KC, n]), op=ALU.mult),
            [("acc", b), "consts"], [("acc", b)])
        P.V(lambda e: e.tensor_tensor(out=a_[:, :, 0:n], in0=a_[:, :, 0:n], in1=xt[:, :, 0:n], op=ALU.add), [("acc", b), ("xt", b)], [("acc", b)])
        P.dma(x2T[:, t0:t0 + n].rearrange("(k p) t -> p k t", p=128), a_[:, :, 0:n], r=[("acc", b)], eng="gpsimd", final=True)
        rms_rstd(P, a_, n, ones, P.banks[0], sq, rstd, [("acc", b)], "rstd", 0)
        P.V(lambda e: e.tensor_tensor(out=sq[:, :, 0:n], in0=a_[:, :, 0:n], in1=rstd[:, 0:n].unsqueeze(1).to_broadcast([128, KC, n]), op=ALU.mult),
            [("acc", b), "rstd", ("sq", 0)], [("sq", 0)])
        P.V(lambda e: e.tensor_tensor(out=sq[:, :, 0:n], in0=sq[:, :, 0:n], in1=mvv[:, :, 2:3].to_broadcast([128, KC, n]), op=ALU.mult),
            [("sq", 0), "consts"], [("sq", 0)])
        P.dma(onT[:, t0:t0 + n].rearrange("(k p) t -> p k t", p=128), sq[:, :, 0:n], r=[("sq", 0)], eng="gpsimd", final=True)
    P.emit()
    P.close()
    return nc


_NC_CACHE = {}


def _get(key, fn):
    if key not in _NC_CACHE:
        _NC_CACHE[key] = fn()
    return _NC_CACHE[key]


def kernel(x, c, ctx, c_ctx, w_ada, b_ada, g_norm1, g_norm2, w_in, gla_w_a2, gla_b_a,
           gla_g_norm, ml_conv_w, ml_conv_b, ml_gate_b, ml_g_norm, rw_mu, rw_w2, rw_w0,
           rw_a2, rw_a0, rw_k_k, rw_k_a, rw_r_k, rw_g_norm, w_out, moe_w_rg, moe_b_rg,
           moe_w_re, moe_b_re, moe_w_gate, moe_w_up, moe_w_down, g_final):
    f32 = np.float32
    inp = dict(w_in=w_in, g_norm1=g_norm1, gla_w_a2=gla_w_a2, gla_b_a=gla_b_a, gla_g_norm=gla_g_norm, ml_conv_w=ml_conv_w,
               ml_conv_b=ml_conv_b, ml_gate_b=ml_gate_b, ml_g_norm=ml_g_norm, rw_mu=rw_mu, rw_w2=rw_w2, rw_w0=rw_w0, rw_a2=rw_a2,
               rw_a0=rw_a0, rw_k_k=rw_k_k, rw_k_a=rw_k_a, rw_r_k=rw_r_k, rw_g_norm=rw_g_norm)
    inp = {k: np.asarray(v, f32) for k, v in inp.items()}
    x, ctx = np.asarray(x, f32), np.asarray(ctx, f32)
    B, SEQ, _ = x.shape
    CTX = ctx.shape[1]
    L = w_ada.shape[0]
    DE = moe_w_gate.shape[-1]
    T = CTX + SEQ
    TT = B * T
    NCORE = 8
    NCc, NLl = CTX // 4, SEQ // 4
    NTK = NCc + NLl
    assert B == 2 and CTX % 512 == 0 or True
    cstv = make_consts()
    NCOL = 6 * D // NCORE
    ncA = _get(("A", L, NCOL), lambda: build_A(L, NCOL))
    cv = np.concatenate([np.asarray(c, f32), np.asarray(c_ctx, f32)[None]], 0)
    cT = np.ascontiguousarray(cv.T.reshape(KC, 128, 3).transpose(1, 0, 2))
    w_ada, b_ada = np.asarray(w_ada, f32), np.asarray(b_ada, f32)
    resA = _run(ncA, [{"cT": cT, "w": np.ascontiguousarray(w_ada[:, :, i * NCOL:(i + 1) * NCOL]),
                       "b": np.ascontiguousarray(b_ada[:, i * NCOL:(i + 1) * NCOL])} for i in range(NCORE)])
    modall = np.concatenate([r["out"] for r in resA], axis=2)
    mod = [modall[l] for l in range(L)]
    ncB = _get(("B", CTX, SEQ), lambda: build_B(CTX, SEQ))
    ncC1 = _get(("C1", NCc, NLl), lambda: build_C1(NCc, NLl))
    ncC2 = _get(("C2", TT, DE), lambda: build_C2(TT, DE))
    ncC3 = _get(("C3", NCc, NLl), lambda: build_C3(NCc, NLl, NCORE))
    xc, xl = ctx.copy(), x.copy()
    tok_idx = [np.concatenate([q * NCc + np.arange(NCc), CTX + q * NLl + np.arange(NLl)]) for q in range(4)]
    out = None
    for l in range(L):
        m = mod[l]
        sh1, sc1, gt1, sh2, sc2, gt2 = [m[:, i * D:(i + 1) * D] for i in range(6)]
        resB = _run(ncB, [prep_B(l, b, g, inp, mod, xc, xl) for b in range(B) for g in range(4)])
        yT = assemble_yT(resB, B, T)
        XT = [np.ascontiguousarray(np.concatenate([xc[b], xl[b]], 0).T) for b in range(B)]
        wr = np.ascontiguousarray(np.concatenate([np.asarray(moe_w_rg[l], f32), np.asarray(moe_w_re[l], f32)], 1))
        br = np.ascontiguousarray(np.broadcast_to(np.concatenate([np.asarray(moe_b_rg[l], f32), np.asarray(moe_b_re[l], f32)])[None], (128, 36)))
        wo = np.ascontiguousarray(np.asarray(w_out[l], f32))
        g2 = pk(np.asarray(g_norm2[l], f32))
        mapsC1 = []
        for b in range(B):
            for q in range(4):
                modv = np.stack([g2, pk(gt1[2]), pk(sc2[2]), pk(sh2[2]), pk(gt1[b]), pk(sc2[b]), pk(sh2[b])], axis=2).reshape(128, KC * 7)
                mapsC1.append({"yT": np.ascontiguousarray(yT[b][:, tok_idx[q]]), "xT": np.ascontiguousarray(XT[b][:, tok_idx[q]]),
                               "wo": wo, "modv": np.ascontiguousarray(modv), "wr": wr, "br": br, "cst": cstv})
        resC1 = _run(ncC1, mapsC1)
        h2all = np.ascontiguousarray(np.concatenate([r["h2T"] for r in resC1], axis=1))
        gall = np.concatenate([r["gates"] for r in resC1], axis=0)
        mapsC2 = [{"h2T": h2all, "gT": np.ascontiguousarray(gall[:, 4 * i:4 * i + 4].T),
                   "wg": np.ascontiguousarray(np.asarray(moe_w_gate[l][4 * i:4 * i + 4], f32)),
                   "wu": np.ascontiguousarray(np.asarray(moe_w_up[l][4 * i:4 * i + 4], f32)),
                   "wd": np.ascontiguousarray(np.asarray(moe_w_down[l][4 * i:4 * i + 4], f32))} for i in range(NCORE)]
        resC2 = _run(ncC2, mapsC2)
        gf = pk(np.asarray(g_final, f32))
        mapsC3 = []
        for b in range(B):
            for q in range(4):
                ci = b * 4 + q
                fTp = np.ascontiguousarray(np.stack([resC2[i]["fT"][:, ci * NTK:(ci + 1) * NTK] for i in range(NCORE)], 0))
                modv = np.stack([pk(gt2[2]), pk(gt2[b]), gf], axis=2).reshape(128, KC * 3)
                mapsC3.append({"fTp": fTp, "x1T": resC1[ci]["x1T"], "modv": np.ascontiguousarray(modv), "cst": cstv})
        resC3 = _run(ncC3, mapsC3)
        for b in range(B):
            X = XT[b]
            for q in range(4):
                X[:, tok_idx[q]] = resC3[b * 4 + q]["x2T"]
            xc[b] = X[:, :CTX].T
            xl[b] = X[:, CTX:].T
        if l == L - 1:
            out = np.zeros((B, SEQ, D), f32)
            for b in range(B):
                for q in range(4):
                    out[b, q * NLl:(q + 1) * NLl] = resC3[b * 4 + q]["onT"][:, NCc:].T
    return out
```
